# Optimizing a Trainium2 kernel written in Bass

```python
import math
import jax
import jax.numpy as jnp
from jax import lax
import numpy as np

D_MODEL = 2048
BATCH = 4
SEQ = 2048
DEPTH = 2
DEC_BATCH = 128
DEC_SEQ = 8
PAST_LEN = 16384
PAGE_SIZE = 128

MIX = D_MODEL
GROUP = MIX // 4
RW_HD = 64
RW_H = GROUP // RW_HD
RW_W_LORA = 64
RW_A_LORA = 64
RW_G_LORA = 128
RW_LN_EPS = 64e-5
RW_IN = 3 * GROUP + RW_W_LORA + RW_A_LORA + RW_G_LORA
MB_HD = 64
MB_H = GROUP // MB_HD
MB_N = 128
MB_G = 2
MB_CONV = 4
MB_CHUNK = 128
MB_CONV_DIM = GROUP + 2 * MB_G * MB_N
MB_IN = GROUP + MB_CONV_DIM + MB_H
GLA_H = 4
GLA_DK = GROUP // 2 // GLA_H
GLA_DV = GROUP // GLA_H
GLA_LORA = 16
GLA_TAU = 16.0
GLA_CHUNK = 16
GLA_QK = GLA_H * GLA_DK
GLA_IN = 2 * GLA_QK + GROUP + GLA_LORA + GROUP
RET_H = 4
RET_HD = GROUP // RET_H
RET_CHUNK = 128
ROPE_BASE = 10000.0
RET_IN = 4 * GROUP
N_IN = RW_IN + MB_IN + GLA_IN + RET_IN
N_MEM = 256
XA_H = 4
XA_HD = D_MODEL // XA_H
D_FF = -(-8 * D_MODEL // (3 * 256)) * 256
DN_ALPHA = (2 * DEPTH) ** 0.25
DN_BETA = (8 * DEPTH) ** -0.25

RW_SPLITS = (GROUP, 2 * GROUP, 3 * GROUP, 3 * GROUP + RW_W_LORA, 3 * GROUP + RW_W_LORA + RW_A_LORA)
MB_SPLITS = (GROUP, GROUP + MB_CONV_DIM)
GLA_SPLITS = (GLA_QK, 2 * GLA_QK, 2 * GLA_QK + GROUP, 2 * GLA_QK + GROUP + GLA_LORA)
IN_SPLITS = (RW_IN, RW_IN + MB_IN, RW_IN + MB_IN + GLA_IN)

kernel_name = 'hybrid_rwkv7_ssd_gla_retnet_decoder_step'


def _layer_norm(x, g, b, eps=1e-5):
    xf = x.astype(jnp.float32)
    xc = xf - jnp.mean(xf, -1, keepdims=True)
    var = jnp.mean(xc * xc, -1, keepdims=True)
    return (xc * lax.rsqrt(var + eps)).astype(x.dtype) * g + b


def _head_layer_norm(x, g, b, eps):
    xc = x - jnp.mean(x, -1, keepdims=True)
    return xc * lax.rsqrt(jnp.mean(xc * xc, -1, keepdims=True) + eps) * g + b


def _rms_norm(x, g, eps=1e-6):
    return x * lax.rsqrt(jnp.mean(x * x, -1, keepdims=True) + eps) * g


def _causal_mask(c):
    return jnp.tril(jnp.ones((c, c), dtype=bool))


def _chunked_scalar_decay(q, k, v, log_a, h0, chunk):
    bsz, t, h, dk = q.shape
    dv = v.shape[-1]
    c = math.gcd(t, chunk)
    n = t // c
    q = q.reshape(bsz, n, c, h, dk)
    k = k.reshape(bsz, n, c, h, dk)
    v = v.reshape(bsz, n, c, h, dv)
    cum = jnp.cumsum(log_a.reshape(bsz, n, c, h), axis=2)
    mask = _causal_mask(c)[None, None, :, :, None]
    seg = cum[:, :, :, None, :] - cum[:, :, None, :, :]
    decay = jnp.exp(jnp.where(mask, seg, -jnp.inf))
    scores = jnp.einsum('bnihk,bnjhk->bnijh', q, k) * decay
    y_intra = jnp.einsum('bnijh,bnjhv->bnihv', scores, v)
    dec_end = jnp.exp(cum[:, :, -1:, :] - cum)
    chunk_state = jnp.einsum('bnjhk,bnjh,bnjhv->bnhkv', k, dec_end, v)
    chunk_decay = jnp.exp(cum[:, :, -1, :])

    def step(hs, inp):
        cs, cd = inp
        return hs * cd[:, :, None, None] + cs, hs

    h_final, h_in = lax.scan(step, h0, (chunk_state.swapaxes(0, 1), chunk_decay.swapaxes(0, 1)))
    h_in = h_in.swapaxes(0, 1)
    y_inter = jnp.einsum('bnihk,bnhkv->bnihv', q * jnp.exp(cum)[..., None], h_in)
    return (y_intra + y_inter).reshape(bsz, t, h, dv), h_final


def _chunked_vector_decay(q, k, v, log_a, h0, chunk):
    bsz, t, h, dk = q.shape
    dv = v.shape[-1]
    c = math.gcd(t, chunk)
    n = t // c
    q = q.reshape(bsz, n, c, h, dk)
    k = k.reshape(bsz, n, c, h, dk)
    v = v.reshape(bsz, n, c, h, dv)
    cum = jnp.cumsum(log_a.reshape(bsz, n, c, h, dk), axis=2)
    mask = _causal_mask(c)[None, None, :, :, None, None]
    seg = cum[:, :, :, None] - cum[:, :, None, :]
    decay = jnp.exp(jnp.where(mask, seg, -jnp.inf))
    scores = jnp.einsum('bnihk,bnjhk,bnijhk->bnijh', q, k, decay)
    y_intra = jnp.einsum('bnijh,bnjhv->bnihv', scores, v)
    dec_end = jnp.exp(cum[:, :, -1:] - cum)
    chunk_state = jnp.einsum('bnjhk,bnjhv->bnhkv', k * dec_end, v)
    chunk_decay = jnp.exp(cum[:, :, -1])

    def step(hs, inp):
        cs, cd = inp
        return hs * cd[..., None] + cs, hs

    h_final, h_in = lax.scan(step, h0, (chunk_state.swapaxes(0, 1), chunk_decay.swapaxes(0, 1)))
    h_in = h_in.swapaxes(0, 1)
    y_inter = jnp.einsum('bnihk,bnhkv->bnihv', q * jnp.exp(cum), h_in)
    return (y_intra + y_inter).reshape(bsz, t, h, dv), h_final


def _rotary(x, pos):
    half = x.shape[-1] // 2
    inv = 1.0 / (ROPE_BASE ** jnp.linspace(0.0, 1.0, half, dtype=jnp.float32))
    ang = pos[:, None] * inv[None, :]
    cos = jnp.cos(ang)[None, :, None, :]
    sin = jnp.sin(ang)[None, :, None, :]
    x1 = x[..., 0::2]
    x2 = x[..., 1::2]
    return jnp.stack([x1 * cos - x2 * sin, x1 * sin + x2 * cos], axis=-1).reshape(x.shape)


def _rwkv7(p, shift_prev, state, lp):
    bsz, t = p.shape[:2]
    f32 = jnp.float32
    prev = jnp.concatenate([shift_prev[:, None].astype(p.dtype), p[:, :-1]], axis=1)
    xs = p + (prev - p) * lp['rw_mu']
    r, k, v, wd, ad, gd = jnp.split(xs, RW_SPLITS, axis=-1)
    w = -jax.nn.softplus(-(lp['rw_w0'] + jnp.tanh(wd) @ lp['rw_w_up'])) - 0.5
    decay = jnp.exp(-jnp.exp(w.astype(f32)))
    a = jax.nn.sigmoid((lp['rw_a0'] + ad @ lp['rw_a_up']).astype(f32))
    g = jax.nn.sigmoid(gd) @ lp['rw_g_up']
    heads = lambda z: z.reshape(bsz, t, RW_H, RW_HD).astype(f32)
    r, k, v, a, decay = heads(r), heads(k), heads(v), heads(a), heads(decay)
    kk = k * lp['rw_k_k'].reshape(RW_H, RW_HD).astype(f32)
    kk = kk * lax.rsqrt(jnp.maximum(jnp.sum(kk * kk, -1, keepdims=True), 1e-24))
    k = k * (1.0 + (a - 1.0) * lp['rw_k_a'].reshape(RW_H, RW_HD).astype(f32))

    def step(s, inp):
        r_t, d_t, k_t, v_t, kk_t, a_t = inp
        sa = jnp.einsum('bhvk,bhk->bhv', s, kk_t)
        s = (s * d_t[:, :, None, :] - sa[..., None] * (kk_t * a_t)[:, :, None, :]
             + v_t[..., None] * k_t[:, :, None, :])
        return s, jnp.einsum('bhvk,bhk->bhv', s, r_t)

    seq = tuple(z.swapaxes(0, 1) for z in (r, decay, k, v, kk, a))
    s_new, o = lax.scan(step, state.astype(f32), seq)
    o = o.swapaxes(0, 1)
    o = _head_layer_norm(o, lp['rw_ln_g'].reshape(RW_H, RW_HD), lp['rw_ln_b'].reshape(RW_H, RW_HD), RW_LN_EPS)
    o = o + jnp.sum(r * k * lp['rw_r_k'].astype(f32), -1, keepdims=True) * v
    o = o.reshape(bsz, t, GROUP) * g.astype(f32)
    return o.astype(p.dtype), p[:, -1], s_new.astype(state.dtype)


def _mamba2(p, conv_prev, ssm_state, lp):
    bsz, t = p.shape[:2]
    f32 = jnp.float32
    z, xbc, dt = jnp.split(p, MB_SPLITS, axis=-1)
    xpad = jnp.concatenate([conv_prev.astype(p.dtype), xbc], axis=1)
    cw = lp['mb_conv_w']
    conv = lp['mb_conv_b'] + sum(xpad[:, i:i + t] * cw[i] for i in range(MB_CONV))
    new_conv = xpad[:, t:]
    xbc = jax.nn.silu(conv.astype(f32))
    xh, bm, cm = jnp.split(xbc, (GROUP, GROUP + MB_G * MB_N), axis=-1)
    xh = xh.reshape(bsz, t, MB_H, MB_HD)
    rep = MB_H // MB_G
    bm = jnp.repeat(bm.reshape(bsz, t, MB_G, MB_N), rep, axis=2)
    cm = jnp.repeat(cm.reshape(bsz, t, MB_G, MB_N), rep, axis=2)
    dt = jax.nn.softplus((dt + lp['mb_dt_bias']).astype(f32))
    a_neg = -jnp.exp(lp['mb_a_log'].astype(f32))
    y, s_new = _chunked_scalar_decay(cm, bm, xh * dt[..., None], dt * a_neg,
                                     ssm_state.astype(f32), MB_CHUNK)
    y = y + lp['mb_d'].astype(f32)[:, None] * xh
    y = y.reshape(bsz, t, GROUP) * jax.nn.silu(z.astype(f32))
    y = _rms_norm(y.reshape(bsz, t, MB_G, GROUP // MB_G),
                  lp['mb_norm_g'].reshape(MB_G, GROUP // MB_G), 1e-5).reshape(bsz, t, GROUP)
    return y.astype(p.dtype), new_conv, s_new.astype(ssm_state.dtype)


def _gla(p, state, lp):
    bsz, t = p.shape[:2]
    f32 = jnp.float32
    q, k, v, gkd, g = jnp.split(p, GLA_SPLITS, axis=-1)
    log_a = jax.nn.log_sigmoid((gkd @ lp['gla_gk_up'] + lp['gla_gk_b']).astype(f32)) / GLA_TAU
    hk = lambda z: z.reshape(bsz, t, GLA_H, GLA_DK).astype(f32)
    q = hk(q) * GLA_DK ** -0.5
    v = v.reshape(bsz, t, GLA_H, GLA_DV).astype(f32)
    o, s_new = _chunked_vector_decay(q, hk(k), v, hk(log_a), state.astype(f32), GLA_CHUNK)
    o = _rms_norm(o, lp['gla_norm_g'].reshape(GLA_H, GLA_DV), 1e-5)
    o = o.reshape(bsz, t, GROUP) * jax.nn.silu(g.astype(f32))
    return o.astype(p.dtype), s_new.astype(state.dtype)


def _retention(p, pos0, state, lp):
    bsz, t = p.shape[:2]
    f32 = jnp.float32
    q, k, v, g = jnp.split(p, 4, axis=-1)
    heads = lambda z: z.reshape(bsz, t, RET_H, RET_HD).astype(f32)
    pos = pos0 + jnp.arange(t, dtype=f32)
    q = _rotary(heads(q), pos)
    k = _rotary(heads(k), pos) * RET_HD ** -0.5
    log_gamma = jnp.log1p(-jnp.exp2(-5.0 - jnp.arange(RET_H, dtype=f32)))
    log_a = jnp.broadcast_to(log_gamma, (bsz, t, RET_H))
    o, s_new = _chunked_scalar_decay(q, k, heads(v), log_a, state.astype(f32), RET_CHUNK)
    o = _rms_norm(o, lp['ret_norm_g'].reshape(RET_H, RET_HD), 1e-5)
    o = o.reshape(bsz, t, GROUP) * jax.nn.silu(g.astype(f32))
    return o.astype(p.dtype), s_new.astype(state.dtype)


def _mem_kv(mem, wk, wv):
    bsz = mem.shape[0]
    k = (mem @ wk).reshape(bsz, N_MEM, XA_H, XA_HD)
    v = (mem @ wv).reshape(bsz, N_MEM, XA_H, XA_HD)
    return k, v


def _cross_attn(x, mem_k, mem_v, wq, wo):
    bsz, t = x.shape[:2]
    q = (x @ wq).reshape(bsz, t, XA_H, XA_HD)
    s = jnp.einsum('bthd,bmhd->bhtm', q, mem_k).astype(jnp.float32) * XA_HD ** -0.5
    pr = jax.nn.softmax(s, axis=-1).astype(x.dtype)
    o = jnp.einsum('bhtm,bmhd->bthd', pr, mem_v).reshape(bsz, t, D_MODEL)
    return o @ wo


def _layer(x, pos0, rw_shift, rw_state, mb_conv, mb_state, gla_state, ret_state, mem_k, mem_v, lp):
    p = x @ lp['w_in']
    p_rw, p_mb, p_gla, p_ret = jnp.split(p, IN_SPLITS, axis=-1)
    o_rw, rw_shift, rw_state = _rwkv7(p_rw, rw_shift, rw_state, lp)
    o_mb, mb_conv, mb_state = _mamba2(p_mb, mb_conv, mb_state, lp)
    o_gla, gla_state = _gla(p_gla, gla_state, lp)
    o_ret, ret_state = _retention(p_ret, pos0, ret_state, lp)
    mix = jnp.concatenate([o_rw, o_mb, o_gla, o_ret], axis=-1) @ lp['w_out']
    x = _layer_norm(DN_ALPHA * x + mix, lp['ln1_g'], lp['ln1_b'])
    x = _layer_norm(DN_ALPHA * x + _cross_attn(x, mem_k, mem_v, lp['xa_wq'], lp['xa_wo']),
                    lp['ln2_g'], lp['ln2_b'])
    h = jax.nn.silu(x @ lp['ffn_w_gate']) * (x @ lp['ffn_w_up'])
    x = _layer_norm(DN_ALPHA * x + h @ lp['ffn_w_down'], lp['ln3_g'], lp['ln3_b'])
    return x, rw_shift, rw_state, mb_conv, mb_state, gla_state, ret_state


def setup_inputs(seed: int = 0) -> dict:
    key = jax.random.key(seed)
    ks = iter(jax.random.split(key, 64))
    f32 = jnp.float32
    nrm = lambda shape, scale: jax.random.normal(next(ks), shape, f32) * scale
    gain = lambda shape: 1.0 + nrm(shape, 0.02)
    uni = lambda shape, lo, hi: jax.random.uniform(next(ks), shape, f32, minval=lo, maxval=hi)
    L = DEPTH
    mb_dt = jnp.exp(uni((L, MB_H), math.log(1e-3), math.log(1e-1)))
    return {
        'x_prompt': nrm((BATCH, SEQ, D_MODEL), 1.0),
        'x_sample': nrm((DEC_BATCH, DEC_SEQ, D_MODEL), 1.0),
        'state_rwkv_shift': nrm((L, DEC_BATCH, RW_IN), 1.0),
        'state_rwkv_wkv': nrm((L, DEC_BATCH, RW_H, RW_HD, RW_HD), 0.3),
        'state_mamba_conv': nrm((L, DEC_BATCH, MB_CONV - 1, MB_CONV_DIM), 1.0),
        'state_mamba_ssm': nrm((L, DEC_BATCH, MB_H, MB_N, MB_HD), 0.3),
        'state_gla': nrm((L, DEC_BATCH, GLA_H, GLA_DK, GLA_DV), 0.3),
        'state_ret': nrm((L, DEC_BATCH, RET_H, RET_HD, RET_HD), 0.3),
        'cache_mem_k': nrm((L, DEC_BATCH, N_MEM, XA_H, XA_HD), 1.0),
        'cache_mem_v': nrm((L, DEC_BATCH, N_MEM, XA_H, XA_HD), 1.0),
        'mem_prompt': nrm((BATCH, N_MEM, D_MODEL), 1.0),
        'w_in': nrm((L, D_MODEL, N_IN), D_MODEL ** -0.5),
        'w_out': nrm((L, MIX, D_MODEL), MIX ** -0.5 * DN_BETA),
        'ln1_g': gain((L, D_MODEL)),
        'ln1_b': nrm((L, D_MODEL), 0.02),
        'rw_mu': uni((L, RW_IN), 0.0, 1.0),
        'rw_w0': uni((L, GROUP), -6.0, -1.0),
        'rw_w_up': nrm((L, RW_W_LORA, GROUP), 0.1),
        'rw_a0': nrm((L, GROUP), 0.1),
        'rw_a_up': nrm((L, RW_A_LORA, GROUP), 0.1),
        'rw_g_up': nrm((L, RW_G_LORA, GROUP), RW_G_LORA ** -0.5),
        'rw_k_k': 0.85 + nrm((L, GROUP), 0.02),
        'rw_k_a': gain((L, GROUP)),
        'rw_r_k': nrm((L, RW_H, RW_HD), 0.1),
        'rw_ln_g': gain((L, GROUP)),
        'rw_ln_b': nrm((L, GROUP), 0.02),
        'mb_conv_w': nrm((L, MB_CONV, MB_CONV_DIM), MB_CONV ** -0.5),
        'mb_conv_b': nrm((L, MB_CONV_DIM), 0.02),
        'mb_dt_bias': mb_dt + jnp.log(-jnp.expm1(-mb_dt)),
        'mb_a_log': jnp.log(uni((L, MB_H), 1.0, 16.0)),
        'mb_d': gain((L, MB_H)),
        'mb_norm_g': gain((L, GROUP)),
        'gla_gk_up': nrm((L, GLA_LORA, GLA_QK), GLA_LORA ** -0.5),
        'gla_gk_b': nrm((L, GLA_QK), 0.02),
        'gla_norm_g': gain((L, GROUP)),
        'ret_norm_g': gain((L, GROUP)),
        'ln2_g': gain((L, D_MODEL)),
        'ln2_b': nrm((L, D_MODEL), 0.02),
        'xa_wq': nrm((L, D_MODEL, D_MODEL), D_MODEL ** -0.5),
        'xa_wk': nrm((L, D_MODEL, D_MODEL), D_MODEL ** -0.5),
        'xa_wv': nrm((L, D_MODEL, D_MODEL), D_MODEL ** -0.5),
        'xa_wo': nrm((L, D_MODEL, D_MODEL), D_MODEL ** -0.5 * DN_BETA),
        'ln3_g': gain((L, D_MODEL)),
        'ln3_b': nrm((L, D_MODEL), 0.02),
        'ffn_w_gate': nrm((L, D_MODEL, D_FF), D_MODEL ** -0.5),
        'ffn_w_up': nrm((L, D_MODEL, D_FF), D_MODEL ** -0.5),
        'ffn_w_down': nrm((L, D_FF, D_MODEL), D_FF ** -0.5 * DN_BETA),
    }


def reference(x_prompt, x_sample, state_rwkv_shift, state_rwkv_wkv, state_mamba_conv, state_mamba_ssm,
              state_gla, state_ret, cache_mem_k, cache_mem_v, mem_prompt,
              w_in, w_out, ln1_g, ln1_b, rw_mu, rw_w0, rw_w_up, rw_a0, rw_a_up, rw_g_up, rw_k_k, rw_k_a,
              rw_r_k, rw_ln_g, rw_ln_b, mb_conv_w, mb_conv_b, mb_dt_bias, mb_a_log, mb_d, mb_norm_g,
              gla_gk_up, gla_gk_b, gla_norm_g, ret_norm_g, ln2_g, ln2_b, xa_wq, xa_wk, xa_wv, xa_wo,
              ln3_g, ln3_b, ffn_w_gate, ffn_w_up, ffn_w_down):
    bp = x_prompt.shape[0]
    dt_p = x_prompt.dtype
    yp = x_prompt
    ys = x_sample
    p_sh, p_wkv, p_cv, p_ssm, p_gl, p_rt, p_mk, p_mv = [], [], [], [], [], [], [], []
    s_sh, s_wkv, s_cv, s_ssm, s_gl, s_rt = [], [], [], [], [], []
    for i in range(DEPTH):
        lp = {
            'w_in': w_in[i], 'w_out': w_out[i], 'ln1_g': ln1_g[i], 'ln1_b': ln1_b[i],
            'rw_mu': rw_mu[i], 'rw_w0': rw_w0[i], 'rw_w_up': rw_w_up[i], 'rw_a0': rw_a0[i],
            'rw_a_up': rw_a_up[i], 'rw_g_up': rw_g_up[i], 'rw_k_k': rw_k_k[i], 'rw_k_a': rw_k_a[i],
            'rw_r_k': rw_r_k[i], 'rw_ln_g': rw_ln_g[i], 'rw_ln_b': rw_ln_b[i],
            'mb_conv_w': mb_conv_w[i], 'mb_conv_b': mb_conv_b[i], 'mb_dt_bias': mb_dt_bias[i],
            'mb_a_log': mb_a_log[i], 'mb_d': mb_d[i], 'mb_norm_g': mb_norm_g[i],
            'gla_gk_up': gla_gk_up[i], 'gla_gk_b': gla_gk_b[i], 'gla_norm_g': gla_norm_g[i],
            'ret_norm_g': ret_norm_g[i], 'ln2_g': ln2_g[i], 'ln2_b': ln2_b[i],
            'xa_wq': xa_wq[i], 'xa_wo': xa_wo[i], 'ln3_g': ln3_g[i], 'ln3_b': ln3_b[i],
            'ffn_w_gate': ffn_w_gate[i], 'ffn_w_up': ffn_w_up[i], 'ffn_w_down': ffn_w_down[i],
        }
        mk, mv = _mem_kv(mem_prompt, xa_wk[i], xa_wv[i])
        yp, sh, wkv, cv, ssm, gl, rt = _layer(
            yp, 0,
            jnp.zeros((bp, RW_IN), dt_p), jnp.zeros((bp, RW_H, RW_HD, RW_HD), dt_p),
            jnp.zeros((bp, MB_CONV - 1, MB_CONV_DIM), dt_p), jnp.zeros((bp, MB_H, MB_N, MB_HD), dt_p),
            jnp.zeros((bp, GLA_H, GLA_DK, GLA_DV), dt_p), jnp.zeros((bp, RET_H, RET_HD, RET_HD), dt_p),
            mk, mv, lp)
        p_sh.append(sh); p_wkv.append(wkv); p_cv.append(cv); p_ssm.append(ssm)
        p_gl.append(gl); p_rt.append(rt); p_mk.append(mk); p_mv.append(mv)
        ys, sh, wkv, cv, ssm, gl, rt = _layer(
            ys, PAST_LEN, state_rwkv_shift[i], state_rwkv_wkv[i], state_mamba_conv[i],
            state_mamba_ssm[i], state_gla[i], state_ret[i], cache_mem_k[i], cache_mem_v[i], lp)
        s_sh.append(sh); s_wkv.append(wkv); s_cv.append(cv); s_ssm.append(ssm)
        s_gl.append(gl); s_rt.append(rt)
    p_rwkv_shift = jnp.stack(p_sh)
    p_rwkv_wkv = jnp.stack(p_wkv)
    p_mamba_conv = jnp.stack(p_cv)
    p_mamba_ssm = jnp.stack(p_ssm)
    p_gla = jnp.stack(p_gl)
    p_ret = jnp.stack(p_rt)
    p_mem_k = jnp.stack(p_mk)
    p_mem_v = jnp.stack(p_mv)
    s_rwkv_shift = jnp.stack(s_sh)
    s_rwkv_wkv = jnp.stack(s_wkv)
    s_mamba_conv = jnp.stack(s_cv)
    s_mamba_ssm = jnp.stack(s_ssm)
    s_gla = jnp.stack(s_gl)
    s_ret = jnp.stack(s_rt)
    return (yp, ys, p_rwkv_shift, p_rwkv_wkv, p_mamba_conv, p_mamba_ssm, p_gla, p_ret, p_mem_k, p_mem_v,
            s_rwkv_shift, s_rwkv_wkv, s_mamba_conv, s_mamba_ssm, s_gla, s_ret)
```

```python
import os
import math
import numpy as np
from contextlib import ExitStack, contextmanager
import concourse.bass as bass
import concourse.mybir as mybir
from concourse.bass_utils import run_bass_kernel_spmd

F32 = mybir.dt.float32
BF16 = mybir.dt.bfloat16
AF = mybir.ActivationFunctionType
ALU = mybir.AluOpType
AX = mybir.AxisListType

D = 2048
SEQ = 2048
DEPTH = 2
NB_S = 16
TS = 8
NT = 17
TOK = NT * 128
KC = D // 128
GROUP = 512
RW_IN = 1792
MB_IN = 1544
GLA_IN = 1552
RET_IN = 2048
N_IN = 6936
C_RW = 0
C_MB = RW_IN
C_GLA = RW_IN + MB_IN
C_RET = C_GLA + GLA_IN
D_FF = 5632
FC = D_FF // 128
N_MEM = 256
DN_ALPHA = (2 * DEPTH) ** 0.25
PAST_LEN = 16384
RW_LN_EPS = 64e-5
LB = 32
NBLK = SEQ // LB + NB_S
TBLK = [(i * 512, 512) for i in range(4)] + [(2048, 128)]
PF_RW = 0
PF_XBC = 1792
PF_GQ = 2816
PF_GK = 3072
PF_GKD = 3328
PF_ROWS = 3344

STAGE = int(os.environ.get("KSTAGE", "99"))
NL = int(os.environ.get("KLAYERS", "2"))
KATT = int(os.environ.get("KATT", "9"))


class Dep:
    __slots__ = ("writer", "readers")

    def __init__(self):
        self.writer = None
        self.readers = {}


class Ctx:
    COMPUTE = ("pe", "act", "dve", "pool")

    def __init__(self, nc, stack):
        self.nc = nc
        self.eng = {"pe": nc.tensor, "act": nc.scalar, "dve": nc.vector, "pool": nc.gpsimd, "sp": nc.sync}
        self.sems = {}
        self.cnt = {}
        for e in self.COMPUTE:
            self.sems[e] = stack.enter_context(nc.semaphore("c_" + e))
            self.cnt[e] = 0
        self.ring = {}
        self.ring_i = {}
        for e, n in (("sp", 48), ("pool", 40), ("act", 8)):
            self.ring[e] = [stack.enter_context(nc.semaphore("d_%s_%d" % (e, i))) for i in range(n)]
            self.ring_i[e] = 0
        self.waited = {}
        self.semobj = {}
        for e in self.COMPUTE:
            self.semobj[("c", e)] = self.sems[e]
        for e in self.ring:
            for i, s in enumerate(self.ring[e]):
                self.semobj[("d", e, i)] = s
        self.n_inst = 0
        self.n_wait = 0

    def wait(self, eng, tok, force=False):
        key, val = tok
        if key[0] == "c" and key[1] == eng and not force:
            if eng == "pe":
                return
        w = self.waited.get((eng, key), 0)
        if w >= val:
            return
        self.eng[eng].wait_ge(self.semobj[key], val)
        self.waited[(eng, key)] = val
        self.n_wait += 1

    def _deps(self, eng, reads, writes, force=False):
        toks = {}

        def add(t):
            if t is None:
                return
            k, v = t
            if toks.get(k, 0) < v:
                toks[k] = v
        for d in reads:
            add(d.writer)
        for d in writes:
            add(d.writer)
            for k, v in d.readers.items():
                add((k, v))
        for k, v in toks.items():
            self.wait(eng, (k, v), force)

    def _update(self, tok, reads, writes):
        k, v = tok
        for d in reads:
            if d.readers.get(k, 0) < v:
                d.readers[k] = v
        for d in writes:
            d.writer = tok
            d.readers = {}

    def op(self, eng, fn, reads=(), writes=()):
        self._deps(eng, reads, writes)
        ins = fn(self.eng[eng])
        self.cnt[eng] += 1
        ins.then_inc(self.sems[eng], 1)
        tok = (("c", eng), self.cnt[eng])
        self._update(tok, reads, writes)
        self.n_inst += 1
        return tok

    def dma(self, issuer, out, in_, reads=(), writes=(), **kw):
        i = self.ring_i[issuer]
        n = len(self.ring[issuer])
        slot = i % n
        rnd = i // n
        key = ("d", issuer, slot)
        if rnd > 0:
            self.wait(issuer, (key, 16 * rnd))
        self._deps(issuer, reads, writes, force=True)
        ins = self.eng[issuer].dma_start(out=out, in_=in_, **kw)
        ins.then_inc(self.ring[issuer][slot], 16)
        self.ring_i[issuer] = i + 1
        tok = (key, 16 * (rnd + 1))
        self._update(tok, reads, writes)
        self.n_inst += 1
        return tok

    def barrier(self, engines=("pe", "act", "dve", "pool", "sp")):
        toks = []
        for e in self.COMPUTE:
            if self.cnt[e] > 0:
                toks.append((("c", e), self.cnt[e]))
        for e in self.ring:
            i = self.ring_i[e]
            n = len(self.ring[e])
            for slot in range(n):
                if i > slot:
                    rnd = (i - 1 - slot) // n
                    toks.append((("d", e, slot), 16 * (rnd + 1)))
        for e in engines:
            for t in toks:
                self.wait(e, t, force=True)


def _bf(x):
    return x


def build_consts():
    c = {}
    i = np.arange(128)
    c["ident"] = np.eye(128, dtype=np.float32)
    c["ones"] = np.ones((128, 128), np.float32)
    le = (i[:, None] <= i[None, :]).astype(np.float32)
    gt = (i[:, None] > i[None, :]).astype(np.float32)
    c["le_P"] = le
    c["gt_P"] = gt
    c["causal_P"] = le.copy()
    same = (i[:, None] // TS == i[None, :] // TS).astype(np.float32)
    c["le_S"] = le * same
    c["gt_S"] = gt * same
    c["causal_S"] = le * same
    c["seqmask"] = (i[:, None] // TS == np.arange(NB_S)[None, :]).astype(np.float32)
    mr = (np.arange(128)[None, :] // TS == np.arange(NB_S)[:, None]).astype(np.float32)
    maskrow = np.broadcast_to(mr.reshape(1, NB_S * 128), (128, NB_S * 128)).astype(np.float32).copy()
    bo = np.zeros((128, 128), np.float32)
    bo[:64, :64] = 1
    bo[64:, 64:] = 1
    c["blockones"] = bo
    hs = np.zeros((128, 2), np.float32)
    hs[:64, 0] = 1
    hs[64:, 1] = 1
    c["halfsel"] = hs
    lg = np.log1p(-np.exp2(-5.0 - np.arange(4, dtype=np.float64)))
    dP = np.zeros((128, 4, 128), np.float64)
    dS = np.zeros((128, 4, 128), np.float64)
    for h in range(4):
        diff = (i[None, :] - i[:, None]).astype(np.float64)
        dP[:, h, :] = np.where(diff >= 0, np.exp(lg[h] * diff), 0.0)
        dS[:, h, :] = np.where((diff >= 0) & (same > 0), np.exp(lg[h] * diff), 0.0)
    c["retdec_P"] = dP.reshape(128, 512).astype(np.float32)
    c["retdec_S"] = dS.reshape(128, 512).astype(np.float32)
    c["ret_expcum_P"] = np.exp(lg[None, :] * (i[:, None] + 1)).astype(np.float32)
    c["ret_decend_P"] = np.exp(lg[None, :] * (127 - i[:, None])).astype(np.float32)
    c["ret_expcum_S"] = np.exp(lg[None, :] * ((i[:, None] % TS) + 1)).astype(np.float32)
    c["ret_decend_S"] = np.exp(lg[None, :] * (TS - 1 - (i[:, None] % TS))).astype(np.float32)
    cdP = np.exp(lg * 128)
    cdS = np.exp(lg * TS)
    c["ret_cd_P"] = np.broadcast_to(np.repeat(cdP, 128)[None, :], (128, 512)).astype(np.float32).copy()
    c["ret_cd_S"] = np.broadcast_to(np.repeat(cdS, 128)[None, :], (128, 512)).astype(np.float32).copy()
    bm = np.zeros((128, 256), np.float32)
    for r in range(8):
        cc = r // 2
        bm[r, cc * 64:(cc + 1) * 64] = 1
        bm[32 + r, cc * 64:(cc + 1) * 64] = 1
    c["rw_blockmask"] = bm
    hm = np.zeros((128, 2), np.float32)
    hm[:64, 0] = 1
    hm[64:, 1] = 1
    c["rw_halfmask"] = hm
    offs = {}
    o = 0
    for k, v in c.items():
        offs[k] = (o, v.shape[1])
        o += v.shape[1]
    pack = np.concatenate([c[k] for k in c], axis=1).astype(np.float32)
    t = np.arange(TOK)
    reset = np.ones(TOK, np.float32)
    notlast = np.ones(TOK, np.float32)
    reset[:SEQ][t[:SEQ] % LB == 0] = 0
    notlast[:SEQ][t[:SEQ] % LB == LB - 1] = 0
    ts_ = t[SEQ:] - SEQ
    reset[SEQ:][ts_ % TS == 0] = 0
    notlast[SEQ:][ts_ % TS == TS - 1] = 0
    tokmask = np.stack([reset, notlast], 0).astype(np.float32)
    half = 64
    inv = (1.0 / (10000.0 ** np.linspace(0.0, 1.0, half, dtype=np.float32))).astype(np.float32)
    pos = np.concatenate([np.arange(SEQ, dtype=np.float32),
                          np.tile(PAST_LEN + np.arange(TS, dtype=np.float32), NB_S)])
    ang = (pos[:, None] * inv[None, :]).astype(np.float32)
    cs = np.cos(ang).astype(np.float32)
    sn = np.sin(ang).astype(np.float32)
    sc = np.float32(128 ** -0.5)
    rot = np.concatenate([cs, sn, cs * sc, sn * sc], axis=1).reshape(NT, 128, 256).astype(np.float32)
    return pack, offs, tokmask, rot, maskrow


_CONSTS = build_consts()

WEIGHT_NAMES = ["w_in", "w_out", "ln1_g", "ln1_b", "rw_mu", "rw_w0", "rw_w_up", "rw_a0", "rw_a_up", "rw_g_up",
                "rw_k_k", "rw_k_a", "rw_r_k", "rw_ln_g", "rw_ln_b", "mb_conv_w", "mb_conv_b", "mb_dt_bias",
                "mb_a_log", "mb_d", "mb_norm_g", "gla_gk_up", "gla_gk_b", "gla_norm_g", "ret_norm_g",
                "ln2_g", "ln2_b", "xa_wq", "xa_wk", "xa_wv", "xa_wo", "ln3_g", "ln3_b",
                "ffn_w_gate", "ffn_w_up", "ffn_w_down"]

IN_SHAPES = {
    "xp": [SEQ, D], "xs": [128, D], "memp": [N_MEM, D],
    "st_shift": [DEPTH, NB_S, RW_IN], "st_wkv": [DEPTH, NB_S, 8, 64, 64], "st_conv": [DEPTH, NB_S, 3, 1024],
    "st_ssm": [DEPTH, NB_S, 8, 128, 64], "st_gla": [DEPTH, NB_S, 4, 64, 128], "st_ret": [DEPTH, NB_S, 4, 128, 128],
    "ck": [DEPTH, NB_S, N_MEM, D], "cv": [DEPTH, NB_S, N_MEM, D],
    "w_in": [DEPTH, D, N_IN], "w_out": [DEPTH, D, D], "ln1_g": [DEPTH, D], "ln1_b": [DEPTH, D],
    "rw_mu": [DEPTH, RW_IN], "rw_w0": [DEPTH, 512], "rw_w_up": [DEPTH, 64, 512], "rw_a0": [DEPTH, 512],
    "rw_a_up": [DEPTH, 64, 512], "rw_g_up": [DEPTH, 128, 512], "rw_k_k": [DEPTH, 512], "rw_k_a": [DEPTH, 512],
    "rw_r_k": [DEPTH, 512], "rw_ln_g": [DEPTH, 512], "rw_ln_b": [DEPTH, 512],
    "mb_conv_w": [DEPTH, 4, 1024], "mb_conv_b": [DEPTH, 1024], "mb_dt_bias": [DEPTH, 8], "mb_a_log": [DEPTH, 8],
    "mb_d": [DEPTH, 8], "mb_norm_g": [DEPTH, 512], "gla_gk_up": [DEPTH, 16, 256], "gla_gk_b": [DEPTH, 256],
    "gla_norm_g": [DEPTH, 512], "ret_norm_g": [DEPTH, 512], "ln2_g": [DEPTH, D], "ln2_b": [DEPTH, D],
    "xa_wq": [DEPTH, D, D], "xa_wk": [DEPTH, D, D], "xa_wv": [DEPTH, D, D], "xa_wo": [DEPTH, D, D],
    "ln3_g": [DEPTH, D], "ln3_b": [DEPTH, D], "ffn_w_gate": [DEPTH, D, D_FF], "ffn_w_up": [DEPTH, D, D_FF],
    "ffn_w_down": [DEPTH, D_FF, D],
    "cpack": list(_CONSTS[0].shape), "tokmask": [2, TOK], "rot": [NT, 128, 256], "maskrow": [128, NB_S * 128],
}
OUT_SHAPES = {
    "yp": [SEQ, D], "ys": [128, D],
    "p_shift": [DEPTH, RW_IN], "p_wkv": [DEPTH, 8, 64, 64], "p_conv": [DEPTH, 3, 1024], "p_ssm": [DEPTH, 8, 128, 64],
    "p_gla": [DEPTH, 4, 64, 128], "p_ret": [DEPTH, 4, 128, 128], "p_mk": [DEPTH, N_MEM, D], "p_mv": [DEPTH, N_MEM, D],
    "s_shift": [DEPTH, NB_S, RW_IN], "s_wkv": [DEPTH, NB_S, 8, 64, 64], "s_conv": [DEPTH, NB_S, 3, 1024],
    "s_ssm": [DEPTH, NB_S, 8, 128, 64], "s_gla": [DEPTH, NB_S, 4, 64, 128], "s_ret": [DEPTH, NB_S, 4, 128, 128],
}


def in_shapes():
    out = {}
    for k, v in IN_SHAPES.items():
        if STAGE < 7 and (k.startswith("xa_") or k in ("ck", "cv", "memp", "ln2_g", "ln2_b")):
            continue
        if STAGE < 8 and (k.startswith("ffn_") or k in ("ln3_g", "ln3_b")):
            continue
        if STAGE < 6 and k in ("w_out", "ln1_g", "ln1_b"):
            continue
        v = list(v)
        if v[0] == DEPTH and k not in ("tokmask",) and len(v) >= 2 and k in WEIGHT_NAMES + ["st_shift", "st_wkv", "st_conv", "st_ssm", "st_gla", "st_ret", "ck", "cv"]:
            v[0] = NL
        out[k] = v
    return out


class Rot:
    def __init__(self, items):
        self.items = items
        self.i = 0

    def next(self):
        it = self.items[self.i % len(self.items)]
        self.i += 1
        return it


class Prog:
    def __init__(self):
        nc = bass.Bass("TRN2", target_bir_lowering=False)
        self.nc = nc
        self.I = {k: nc.dram_tensor(k, list(v), F32, kind="ExternalInput").ap() for k, v in in_shapes().items()}
        self.O = {k: nc.dram_tensor(k, list(v), F32, kind="ExternalOutput").ap() for k, v in OUT_SHAPES.items()}
        self.uid = 0
        self.out_deps = []

    def scr(self, name, shape, dt=F32):
        return self.nc.dram_tensor("scr_" + name, list(shape), dt).ap()

    def sbt(self, name, shape, dt=F32):
        self.uid += 1
        return self.scopes[-1].enter_context(self.nc.sbuf_tensor("%s_%d" % (name, self.uid), list(shape), dt))

    def rot(self, name, n, shape, dt=F32):
        return Rot([(self.sbt(name, shape, dt), Dep()) for _ in range(n)])

    @contextmanager
    def scope(self):
        st = ExitStack()
        self.scopes.append(st)
        try:
            yield
        finally:
            self.c.barrier()
            self.scopes.pop()
            st.close()

    def pb(self):
        i = self.ps_i % 8
        self.ps_i += 1
        return i

    def cst(self, name):
        o, n = _CONSTS[1][name]
        return self.cpk[:, o:o + n]

    def odep(self):
        d = Dep()
        self.out_deps.append(d)
        return d

    def build(self):
        nc = self.nc
        with ExitStack() as st:
            self.c = c = Ctx(nc, st)
            self.scopes = [st]
            st.enter_context(nc.Block())
            self.ps = [st.enter_context(nc.psum_tensor("psb%d" % i, [128, 512], F32)) for i in range(8)]
            self.dps = [Dep() for _ in range(8)]
            self.ps_i = 0
            self.actT = self.sbt("actT", [128, KC, TOK], BF16)
            self.dact = [[Dep() for _ in range(4)] for _ in range(NT)]
            ncst = _CONSTS[0].shape[1]
            self.cpk = self.sbt("cpk", [128, ncst], F32)
            self.dcst = Dep()
            c.dma("sp", self.cpk[:], self.I["cpack"], writes=[self.dcst])
            self.ident_bf = self.sbt("identbf", [128, 128], BF16)
            o, n = _CONSTS[1]["ident"]
            c.dma("pool", self.ident_bf[:], self.I["cpack"][:, o:o + n], writes=[self.dcst])
            self.maskrow = self.sbt("maskrow", [128, NB_S, 128], BF16)
            c.dma("pool", self.maskrow[:], self.I["maskrow"].rearrange("p (b i) -> p b i", b=NB_S), writes=[self.dcst])
            self.PT = self.scr("PT", [TOK, N_IN])
            self.dPT = [Dep() for _ in range(NT)]
            self.PF = self.scr("PF", [PF_ROWS, TOK])
            self.dPF = Dep()
            self.OS = self.scr("OS", [TOK, D])
            self.dOS = [Dep() for _ in range(NT)]
            self.MIX = self.scr("MIX", [TOK, D])
            self.dMIX = [Dep() for _ in range(NT)]
            self.XRES = self.scr("XRES", [TOK, D])
            self.dXRES = [Dep() for _ in range(NT)]
            self.HT = self.scr("HT", [FC, 128, TOK], BF16)
            self.dHT = Dep()
            self.ORW = self.scr("ORW", [TOK, 512], BF16)
            self.dORW = Dep()
            self.KAPT = self.scr("KAPT", [4, 128, TOK + 8], BF16)
            self.RHT = self.scr("RHT", [4, 128, TOK], BF16)
            self.BH = self.scr("BH", [TOK, 512], BF16)
            self.KH = self.scr("KH", [TOK, 512], BF16)
            self.VV = self.scr("VV", [TOK, 512], F32)
            self.dRWS = Dep()
            self.epsT = self.sbt("epsT", [128, 8], F32)
            c.op("dve", lambda e: e.memset(self.epsT[:, 0:1], 1e-5), writes=[self.dcst])
            c.op("dve", lambda e: e.memset(self.epsT[:, 1:2], RW_LN_EPS), writes=[self.dcst])
            c.op("dve", lambda e: e.memset(self.epsT[:, 2:3], 0.0), writes=[self.dcst])
            c.op("dve", lambda e: e.memset(self.epsT[:, 3:4], 1.0), writes=[self.dcst])
            self.epst = {1e-5: self.epsT[:, 0:1], float(RW_LN_EPS): self.epsT[:, 1:2], 0.0: self.epsT[:, 2:3], 1.0: self.epsT[:, 3:4]}
            c.barrier()
            self.res_from_input = True
            self.load_x0()
            for l in range(NL):
                if STAGE < 1:
                    break
                self.inproj(l)
                if STAGE < 2:
                    break
                self.mix_ret(l)
                if STAGE < 3:
                    break
                self.mix_gla(l)
                if STAGE < 4:
                    break
                self.mix_ssd(l)
                if STAGE < 5:
                    break
                self.mix_rwkv(l)
                if STAGE < 6:
                    break
                self.out_ln1(l)
                if STAGE < 7:
                    break
                self.attn(l)
                if STAGE < 8:
                    break
                self.ffn(l)
            for d in self.out_deps:
                if d.writer is not None:
                    c.wait("sp", d.writer, force=True)
            c.barrier(engines=("sp",))
        return nc

    def tile_rows(self, tt):
        return slice(tt * 128, (tt + 1) * 128)

    def resid_src(self, l, tt):
        if l == 0:
            if tt < 16:
                return self.I["xp"][tt * 128:(tt + 1) * 128, :], None
            return self.I["xs"], None
        return self.XRES[tt * 128:(tt + 1) * 128, :], self.dXRES[tt]

    def to_actT(self, xh, dxh, tt):
        c = self.c
        for g in range(2):
            pi = self.pb()
            psb = self.ps[pi][:].bitcast(BF16)
            for j in range(8):
                kc = g * 8 + j
                c.op("pe", lambda e, j=j, kc=kc: e.transpose(psb[:, j * 128:(j + 1) * 128], xh[:, kc * 128:(kc + 1) * 128], self.ident_bf[:]),
                     reads=[dxh, self.dcst], writes=[self.dps[pi]])
            dst = self.actT[:, g * 8:(g + 1) * 8, tt * 128:(tt + 1) * 128]
            src = psb.rearrange("p (j t) -> p j t", j=8)
            eng = "act" if g == 0 else "dve"
            if eng == "act":
                c.op("act", lambda e: e.copy(out=dst, in_=src), reads=[self.dps[pi]], writes=[self.dact[tt][2 * g], self.dact[tt][2 * g + 1]])
            else:
                c.op("dve", lambda e: e.tensor_copy(out=dst, in_=src), reads=[self.dps[pi]], writes=[self.dact[tt][2 * g], self.dact[tt][2 * g + 1]])

    def load_x0(self):
        c = self.c
        with self.scope():
            xb = self.rot("x0", 3, [128, D], BF16)
            for tt in range(NT):
                src, _ = self.resid_src(0, tt)
                buf, d = xb.next()
                c.dma("pool", buf[:], src, writes=[d])
                self.to_actT(buf, d, tt)

    def load_w(self, pool, w_ap, n0, ncols, kcn):
        buf, dep = pool.next()
        src = w_ap[:, n0:n0 + ncols].rearrange("(kc p) n -> p kc n", p=128)
        self.c.dma("pool", buf[:, 0:kcn, 0:ncols], src, writes=[dep])
        return buf, dep

    def evac(self, pi, rows, cols, dst, ddst, toggle):
        c = self.c
        src = self.ps[pi][0:rows, 0:cols]
        if toggle % 2 == 0:
            c.op("act", lambda e: e.copy(out=dst, in_=src), reads=[self.dps[pi]], writes=[ddst])
        else:
            c.op("dve", lambda e: e.tensor_copy(out=dst, in_=src), reads=[self.dps[pi]], writes=[ddst])

    def proj_T(self, w_ap, blocks, kcn, tiles_fn, lhs_fn, lhs_deps_fn, consume, wpool):
        c = self.c
        nxt = self.load_w(wpool, w_ap, blocks[0][0], blocks[0][1], kcn)
        for bi, (n0, ncols) in enumerate(blocks):
            wb, wd = nxt
            if bi + 1 < len(blocks):
                nxt = self.load_w(wpool, w_ap, blocks[bi + 1][0], blocks[bi + 1][1], kcn)
            for tt in tiles_fn(bi):
                pi = self.pb()
                ld = lhs_deps_fn(tt)
                for kc in range(kcn):
                    c.op("pe", lambda e, kc=kc, pi=pi, tt=tt: e.matmul(self.ps[pi][:, 0:ncols], lhs_fn(tt, kc), wb[:, kc, 0:ncols],
                                                                     start=(kc == 0), stop=(kc == kcn - 1)),
                         reads=[wd] + ld, writes=[self.dps[pi]])
                consume(bi, n0, ncols, tt, pi)

    def proj_F(self, w_ap, blocks, kcn, consume, wpool, rhs_fn=None, rhs_deps_fn=None, tblk=TBLK):
        c = self.c
        if rhs_fn is None:
            rhs_fn = lambda kc, t0, tw: self.actT[:, kc, t0:t0 + tw]
            rhs_deps_fn = lambda t0, tw: [d for tt in range(t0 // 128, (t0 + tw) // 128) for d in self.dact[tt]]
        nxt = self.load_w(wpool, w_ap, blocks[0][0], blocks[0][1], kcn)
        for bi, (n0, ncols) in enumerate(blocks):
            wb, wd = nxt
            if bi + 1 < len(blocks):
                nxt = self.load_w(wpool, w_ap, blocks[bi + 1][0], blocks[bi + 1][1], kcn)
            for j in range((ncols + 127) // 128):
                cw = min(128, ncols - 128 * j)
                for (t0, tw) in tblk:
                    pi = self.pb()
                    rd = rhs_deps_fn(t0, tw)
                    for kc in range(kcn):
                        c.op("pe", lambda e, kc=kc, pi=pi, j=j, cw=cw, t0=t0, tw=tw: e.matmul(
                            self.ps[pi][0:cw, 0:tw], wb[:, kc, 128 * j:128 * j + cw], rhs_fn(kc, t0, tw),
                            start=(kc == 0), stop=(kc == kcn - 1)), reads=[wd] + rd, writes=[self.dps[pi]])
                    consume(n0 + 128 * j, cw, t0, tw, pi)

    def act_lhs(self, tt, kc):
        return self.actT[:, kc, tt * 128:(tt + 1) * 128]

    def act_deps(self, tt):
        return list(self.dact[tt])

    def inproj(self, l):
        c = self.c
        w = self.I["w_in"][l]
        with self.scope():
            wpool = self.rot("win", 3, [128, KC, 512], BF16)
            stg = self.rot("stg", 4, [128, 512], F32)
            self.tog = 0
            def blocks_of(c0, n):
                out = []
                o = 0
                while o < n:
                    out.append((c0 + o, min(512, n - o)))
                    o += 512
                return out
            allt = list(range(NT))
            last2 = [15, 16]
            tb = []
            for b in blocks_of(C_RW, RW_IN):
                tb.append((b, last2))
            for b in blocks_of(C_MB, 512):
                tb.append((b, allt))
            for b in blocks_of(C_MB + 512, 1024):
                tb.append((b, last2))
            tb.append(((C_MB + 1536, 8), allt))
            for b in blocks_of(C_GLA + 256, 256):
                tb.append((b, allt))
            for b in blocks_of(C_GLA + 512, 512):
                tb.append((b, allt))
            for b in blocks_of(C_GLA + 1040, 512):
                tb.append((b, allt))
            for b in blocks_of(C_RET, RET_IN):
                tb.append((b, allt))
            blocks = [x[0] for x in tb]

            def consume_T(bi, n0, ncols, tt, pi):
                buf, d = stg.next()
                self.evac(pi, 128, ncols, buf[:, 0:ncols], d, self.tog)
                self.tog += 1
                c.dma("sp", self.PT[tt * 128:(tt + 1) * 128, n0:n0 + ncols], buf[:, 0:ncols], reads=[d], writes=[self.dPT[tt]])
            self.proj_T(w, blocks, KC, lambda bi: tb[bi][1], self.act_lhs, self.act_deps, consume_T, wpool)
            fsegs = [(C_RW, RW_IN, PF_RW), (C_MB + 512, 1024, PF_XBC), (C_GLA, 256, PF_GQ), (C_GLA + 256, 256, PF_GK),
                     (C_GLA + 1024, 16, PF_GKD)]
            for (c0, n, r0) in fsegs:
                def consume_F(col0, cw, t0, tw, pi, c0=c0, r0=r0):
                    buf, d = stg.next()
                    self.evac(pi, cw, tw, buf[0:cw, 0:tw], d, self.tog)
                    self.tog += 1
                    rr = r0 + (col0 - c0)
                    c.dma("sp", self.PF[rr:rr + cw, t0:t0 + tw], buf[0:cw, 0:tw], reads=[d], writes=[self.dPF])
                self.proj_F(w, blocks_of(c0, n), KC, consume_F, wpool)
            d1 = self.odep()
            c.dma("sp", self.O["p_shift"][l:l + 1, :], self.PT[2047:2048, C_RW:C_RW + RW_IN], reads=[self.dPT[15]], writes=[d1])
            d2 = self.odep()
            c.dma("sp", self.O["p_conv"][l], self.PT[2045:2048, C_MB + 512:C_MB + 1536], reads=[self.dPT[15]], writes=[d2])
            d3 = self.odep()
            src = self.PT[2048:2176, C_RW:C_RW + RW_IN].rearrange("(b t) n -> b t n", t=TS)[:, TS - 1, :]
            c.dma("sp", self.O["s_shift"][l], src, reads=[self.dPT[16]], writes=[d3])
            d4 = self.odep()
            src = self.PT[2048:2176, C_MB + 512:C_MB + 1536].rearrange("(b t) n -> b t n", t=TS)[:, TS - 3:TS, :]
            c.dma("sp", self.O["s_conv"][l], src, reads=[self.dPT[16]], writes=[d4])

    def bcast_row(self, name, row_ap, n):
        t = self.sbt(name, [128, n], F32)
        d = Dep()
        self.c.dma("sp", t[:], row_ap.partition_broadcast(128), writes=[d])
        return t, d

    def col_load(self, name, vec_ap, nchunks):
        t = self.sbt(name, [128, nchunks], F32)
        d = Dep()
        with self.nc.allow_non_contiguous_dma(reason="small per-feature vector"):
            self.c.dma("sp", t[:], vec_ap.rearrange("(c p) -> p c", p=128), writes=[d])
        return t, d

    def V(self, eng, fn, reads, writes):
        return self.c.op(eng, fn, reads=reads, writes=writes)

    def rms_gate(self, y, dy, ngroups, gsz, gn, dgn, gate_ap, dgate, eps, out_dram, dout, tmp, dtmp, small, dsmall):
        c = self.c
        y3 = y[:].rearrange("p (g v) -> p g v", g=ngroups)
        c.op("dve", lambda e: e.tensor_tensor(out=tmp[:], in0=y[:], in1=y[:], op=ALU.mult), reads=[dy], writes=[dtmp])
        c.op("dve", lambda e: e.tensor_reduce(out=small[:, 0:ngroups], in_=tmp[:].rearrange("p (g v) -> p g v", g=ngroups), axis=AX.X, op=ALU.add),
             reads=[dtmp], writes=[dsmall])
        c.op("act", lambda e: e.activation(out=small[:, 8:8 + ngroups], in_=small[:, 0:ngroups], func=AF.Sqrt, scale=1.0 / gsz, bias=self.epsb(eps)),
             reads=[dsmall, self.dcst], writes=[dsmall])
        c.op("dve", lambda e: e.reciprocal(out=small[:, 16:16 + ngroups], in_=small[:, 8:8 + ngroups]), reads=[dsmall], writes=[dsmall])
        rb = small[:, 16:16 + ngroups].unsqueeze(2).broadcast_to([128, ngroups, gsz])
        c.op("dve", lambda e: e.tensor_tensor(out=y3, in0=y3, in1=rb, op=ALU.mult), reads=[dy, dsmall], writes=[dy])
        c.op("pool", lambda e: e.tensor_tensor(out=y[:], in0=y[:], in1=gn[:], op=ALU.mult), reads=[dy, dgn], writes=[dy])
        c.op("act", lambda e: e.activation(out=tmp[:], in_=gate_ap, func=AF.Silu), reads=[dgate, dtmp], writes=[dtmp])
        c.op("dve", lambda e: e.tensor_tensor(out=y[:], in0=y[:], in1=tmp[:], op=ALU.mult), reads=[dy, dtmp], writes=[dy])
        c.dma("sp", out_dram, y[:], reads=[dy], writes=[dout])

    def epsb(self, eps):
        key = float(eps)
        if key not in self.epst:
            raise KeyError(key)
        return self.epst[key]

    def mix_ret(self, l):
        c = self.c
        with self.scope():
            gn, dgn = self.bcast_row("retgn", self.I["ret_norm_g"][l:l + 1, :], 512)
            S = self.sbt("retS", [128, 512], F32)
            Sbf = self.sbt("retSbf", [128, 512], BF16)
            dS = Dep()
            dSbf = Dep()
            c.op("dve", lambda e: e.memset(S[:], 0.0), writes=[dS])
            c.op("dve", lambda e: e.memset(Sbf[:], 0.0), writes=[dSbf])
            S0 = self.sbt("retS0", [128, NB_S, 512], F32)
            S0bf = self.sbt("retS0bf", [128, NB_S, 512], BF16)
            dS0 = Dep()
            dS0bf = Dep()
            c.dma("sp", S0[:].rearrange("d b (h v) -> d b h v", h=4), self.I["st_ret"][l].rearrange("b h d v -> d b h v"), writes=[dS0])
            c.op("act", lambda e: e.copy(out=S0bf[:], in_=S0[:]), reads=[dS0], writes=[dS0bf])
            inb = self.rot("retin", 2, [128, 2048], F32)
            rtb = self.rot("retrot", 2, [128, 256], F32)
            t1 = self.sbt("rt1", [128, 256], F32); t2 = self.sbt("rt2", [128, 256], F32)
            t3 = self.sbt("rt3", [128, 256], F32); t4 = self.sbt("rt4", [128, 256], F32)
            dt1 = Dep(); dt2 = Dep(); dt3 = Dep(); dt4 = Dep()
            qr = self.sbt("qr", [128, 512], BF16); kr = self.sbt("kr", [128, 512], BF16)
            dqr = Dep(); dkr = Dep()
            qT = self.sbt("qT", [128, 512], BF16); kT = self.sbt("kT", [128, 512], BF16)
            dqT = Dep(); dkT = Dep()
            W = self.sbt("retW", [128, 512], BF16); dW = Dep()
            vbf = self.sbt("retvbf", [128, 512], BF16); dvbf = Dep()
            kd = self.sbt("retkd", [128, 512], BF16); dkd = Dep()
            kdb = self.rot("retkdb", 2, [128, 512], BF16)
            qmall = self.sbt("retqmall", [128, NB_S, 512], BF16); dqmall = Dep()
            y = self.sbt("rety", [128, 512], F32); dy = Dep()
            tmp = self.sbt("rettmp", [128, 512], F32); dtmp = Dep()
            small = self.sbt("retsmall", [128, 32], F32); dsmall = Dep()
            stg = self.rot("retstg", 2, [128, 512], F32)
            for tt in range(NT):
                smp = tt == 16
                sfx = "_S" if smp else "_P"
                buf, dbuf = inb.next()
                c.dma("sp", buf[:], self.PT[tt * 128:(tt + 1) * 128, C_RET:C_RET + 2048], reads=[self.dPT[tt]], writes=[dbuf])
                rt, drt = rtb.next()
                c.dma("sp", rt[:], self.I["rot"][tt], writes=[drt])
                for (eng, x0, co, so, out, dout, ta, dta, tb_, dtb) in (("dve", 0, 0, 64, qr, dqr, t1, dt1, t2, dt2),
                                                                         ("pool", 512, 128, 192, kr, dkr, t3, dt3, t4, dt4)):
                    x4 = buf[:, x0:x0 + 512].rearrange("p (h i two) -> p h i two", h=4, two=2)
                    xe = x4[:, :, :, 0]
                    xo = x4[:, :, :, 1]
                    cs = rt[:, co:co + 64].unsqueeze(1).broadcast_to([128, 4, 64])
                    sn = rt[:, so:so + 64].unsqueeze(1).broadcast_to([128, 4, 64])
                    o4 = out[:].rearrange("p (h i two) -> p h i two", h=4, two=2)
                    a3 = ta[:].rearrange("p (h i) -> p h i", h=4)
                    b3 = tb_[:].rearrange("p (h i) -> p h i", h=4)
                    c.op(eng, lambda e, a3=a3, xe=xe, cs=cs: e.tensor_tensor(out=a3, in0=xe, in1=cs, op=ALU.mult), reads=[dbuf, drt], writes=[dta])
                    c.op(eng, lambda e, b3=b3, xo=xo, sn=sn: e.tensor_tensor(out=b3, in0=xo, in1=sn, op=ALU.mult), reads=[dbuf, drt], writes=[dtb])
                    c.op(eng, lambda e, o4=o4, a3=a3, b3=b3: e.tensor_tensor(out=o4[:, :, :, 0], in0=a3, in1=b3, op=ALU.subtract), reads=[dta, dtb], writes=[dout])
                    c.op(eng, lambda e, a3=a3, xe=xe, sn=sn: e.tensor_tensor(out=a3, in0=xe, in1=sn, op=ALU.mult), reads=[dbuf, drt, dout], writes=[dta])
                    c.op(eng, lambda e, b3=b3, xo=xo, cs=cs: e.tensor_tensor(out=b3, in0=xo, in1=cs, op=ALU.mult), reads=[dbuf, drt, dout], writes=[dtb])
                    c.op(eng, lambda e, o4=o4, a3=a3, b3=b3: e.tensor_tensor(out=o4[:, :, :, 1], in0=a3, in1=b3, op=ALU.add), reads=[dta, dtb], writes=[dout])
                for (src, dsrc, dst, ddst, eng) in ((qr, dqr, qT, dqT, "act"), (kr, dkr, kT, dkT, "dve")):
                    pi = self.pb()
                    psb = self.ps[pi][:].bitcast(BF16)
                    for h in range(4):
                        c.op("pe", lambda e, h=h, psb=psb, src=src: e.transpose(psb[:, h * 128:(h + 1) * 128], src[:, h * 128:(h + 1) * 128], self.ident_bf[:]),
                             reads=[dsrc, self.dcst], writes=[self.dps[pi]])
                    if eng == "act":
                        c.op("act", lambda e, psb=psb, dst=dst: e.copy(out=dst[:], in_=psb[:, 0:512]), reads=[self.dps[pi]], writes=[ddst])
                    else:
                        c.op("dve", lambda e, psb=psb, dst=dst: e.tensor_copy(out=dst[:], in_=psb[:, 0:512]), reads=[self.dps[pi]], writes=[ddst])
                p1 = self.pb()
                for h in range(4):
                    c.op("pe", lambda e, h=h: e.matmul(self.ps[p1][:, h * 128:(h + 1) * 128], kT[:, h * 128:(h + 1) * 128], qT[:, h * 128:(h + 1) * 128], start=True, stop=True),
                         reads=[dkT, dqT], writes=[self.dps[p1]])
                c.op("dve", lambda e: e.tensor_tensor(out=W[:], in0=self.ps[p1][:, 0:512], in1=self.cst("retdec" + sfx), op=ALU.mult),
                     reads=[self.dps[p1], self.dcst], writes=[dW])
                c.op("act", lambda e: e.copy(out=vbf[:], in_=buf[:, 1024:1536]), reads=[dbuf], writes=[dvbf])
                p2 = self.pb()
                for h in range(4):
                    c.op("pe", lambda e, h=h: e.matmul(self.ps[p2][:, h * 128:(h + 1) * 128], W[:, h * 128:(h + 1) * 128], vbf[:, h * 128:(h + 1) * 128], start=True, stop=True),
                         reads=[dW, dvbf], writes=[self.dps[p2]])
                p3 = self.pb()
                if not smp:
                    for h in range(4):
                        c.op("pe", lambda e, h=h: e.matmul(self.ps[p3][:, h * 128:(h + 1) * 128], qT[:, h * 128:(h + 1) * 128], Sbf[:, h * 128:(h + 1) * 128], start=True, stop=True),
                             reads=[dqT, dSbf], writes=[self.dps[p3]])
                else:
                    for b in range(NB_S):
                        mrb = self.maskrow[:, b, :].unsqueeze(1).broadcast_to([128, 4, 128])
                        c.op("pool", lambda e, b=b, mrb=mrb: e.tensor_tensor(out=qmall[:, b, :].rearrange("p (h i) -> p h i", h=4), in0=qT[:].rearrange("p (h i) -> p h i", h=4), in1=mrb, op=ALU.mult),
                             reads=[dqT, self.dcst], writes=[dqmall])
                    for h in range(4):
                        for b in range(NB_S):
                            c.op("pe", lambda e, h=h, b=b: e.matmul(self.ps[p3][:, h * 128:(h + 1) * 128], qmall[:, b, h * 128:(h + 1) * 128], S0bf[:, b, h * 128:(h + 1) * 128],
                                                                      start=(b == 0), stop=(b == NB_S - 1), skip_group_check=True),
                                 reads=[dqmall, dS0bf], writes=[self.dps[p3]])
                ecb = self.cst("ret_expcum" + sfx).unsqueeze(2).broadcast_to([128, 4, 128])
                c.op("dve", lambda e: e.tensor_tensor(out=y[:].rearrange("p (h v) -> p h v", h=4), in0=self.ps[p3][:, 0:512].rearrange("p (h v) -> p h v", h=4), in1=ecb, op=ALU.mult),
                     reads=[self.dps[p3], self.dcst, self.dOS[tt]], writes=[dy])
                c.op("dve", lambda e: e.tensor_tensor(out=y[:], in0=y[:], in1=self.ps[p2][:, 0:512], op=ALU.add), reads=[dy, self.dps[p2]], writes=[dy])
                self.rms_gate(y, dy, 4, 128, gn, dgn, buf[:, 1536:2048], dbuf, 1e-5, self.OS[tt * 128:(tt + 1) * 128, 1536:2048], self.dOS[tt], tmp, dtmp, small, dsmall)
                deb = self.cst("ret_decend" + sfx).unsqueeze(2).broadcast_to([128, 4, 128])
                c.op("pool", lambda e: e.tensor_tensor(out=kd[:].rearrange("p (h d) -> p h d", h=4), in0=kr[:].rearrange("p (h d) -> p h d", h=4), in1=deb, op=ALU.mult),
                     reads=[dkr, self.dcst], writes=[dkd])
                if not smp:
                    p4 = self.pb()
                    for h in range(4):
                        c.op("pe", lambda e, h=h: e.matmul(self.ps[p4][:, h * 128:(h + 1) * 128], kd[:, h * 128:(h + 1) * 128], vbf[:, h * 128:(h + 1) * 128], start=True, stop=True),
                             reads=[dkd, dvbf], writes=[self.dps[p4]])
                    c.op("pool", lambda e: e.tensor_tensor(out=S[:], in0=S[:], in1=self.cst("ret_cd_P"), op=ALU.mult), reads=[dS, self.dcst], writes=[dS])
                    c.op("dve", lambda e: e.tensor_tensor(out=S[:], in0=S[:], in1=self.ps[p4][:, 0:512], op=ALU.add), reads=[dS, self.dps[p4]], writes=[dS])
                    c.op("act", lambda e: e.copy(out=Sbf[:], in_=S[:]), reads=[dS], writes=[dSbf])
                    if tt == 15:
                        do = self.odep()
                        c.dma("sp", self.O["p_ret"][l].rearrange("h d v -> d h v"), S[:].rearrange("d (h v) -> d h v", h=4), reads=[dS], writes=[do])
                else:
                    for b in range(NB_S):
                        kb, dkb = kdb.next()
                        c.op("pool", lambda e, kb=kb, b=b: e.tensor_scalar(out=kb[:], in0=kd[:], scalar1=self.cst("seqmask")[:, b:b + 1], scalar2=None, op0=ALU.mult),
                             reads=[dkd, self.dcst], writes=[dkb])
                        p4 = self.pb()
                        for h in range(4):
                            c.op("pe", lambda e, h=h, kb=kb, p4=p4: e.matmul(self.ps[p4][:, h * 128:(h + 1) * 128], kb[:, h * 128:(h + 1) * 128], vbf[:, h * 128:(h + 1) * 128], start=True, stop=True),
                                 reads=[dkb, dvbf], writes=[self.dps[p4]])
                        sb_, dsb = stg.next()
                        c.op("pool", lambda e, sb_=sb_, b=b: e.tensor_tensor(out=sb_[:], in0=S0[:, b, :], in1=self.cst("ret_cd_S"), op=ALU.mult), reads=[dS0, self.dcst], writes=[dsb])
                        c.op("dve", lambda e, sb_=sb_, p4=p4: e.tensor_tensor(out=sb_[:], in0=sb_[:], in1=self.ps[p4][:, 0:512], op=ALU.add), reads=[dsb, self.dps[p4]], writes=[dsb])
                        do = self.odep()
                        c.dma("sp", self.O["s_ret"][l, b].rearrange("h d v -> d h v"), sb_[:].rearrange("d (h v) -> d h v", h=4), reads=[dsb], writes=[do])


_PROG_CACHE = {}


def _get_prog():
    if "p" not in _PROG_CACHE:
        p = Prog()
        p.build()
        _PROG_CACHE["p"] = p
    return _PROG_CACHE["p"]


def kernel(**inp):
    f = lambda a: np.ascontiguousarray(np.asarray(a, dtype=np.float32))
    prog = _get_prog()
    pack, offs, tokmask, rot, maskrow = _CONSTS
    shared = {k: f(inp[k]) for k in WEIGHT_NAMES}
    shared["rw_r_k"] = shared["rw_r_k"].reshape(DEPTH, 512)
    shared["cpack"] = pack
    shared["tokmask"] = tokmask
    shared["rot"] = rot
    shared["maskrow"] = maskrow
    xp = f(inp["x_prompt"]); xs = f(inp["x_sample"]); memp = f(inp["mem_prompt"])
    st = {"st_shift": f(inp["state_rwkv_shift"]), "st_wkv": f(inp["state_rwkv_wkv"]), "st_conv": f(inp["state_mamba_conv"]),
          "st_ssm": f(inp["state_mamba_ssm"]), "st_gla": f(inp["state_gla"]), "st_ret": f(inp["state_ret"]),
          "ck": f(inp["cache_mem_k"]), "cv": f(inp["cache_mem_v"])}
    in_maps = []
    for cid in range(8):
        b = cid % 4
        m = dict(shared)
        m["xp"] = xp[b]
        m["xs"] = np.ascontiguousarray(xs[cid * NB_S:(cid + 1) * NB_S].reshape(128, D))
        m["memp"] = memp[b]
        for k, v in st.items():
            sl = np.ascontiguousarray(v[:, cid * NB_S:(cid + 1) * NB_S])
            if k in ("ck", "cv"):
                sl = sl.reshape(DEPTH, NB_S, N_MEM, D)
            m[k] = sl
        ish = in_shapes()
        m = {k: (np.ascontiguousarray(v[:NL]) if (k in ish and ish[k][0] == NL and v.shape[0] == DEPTH and NL != DEPTH) else v) for k, v in m.items() if k in ish}
        for k in m:
            assert list(m[k].shape) == ish[k], (k, m[k].shape, ish[k])
        in_maps.append(m)
    res = run_bass_kernel_spmd(prog.nc, in_maps, core_ids=list(range(8)))
    R = res.results
    B = 4
    yp = np.stack([R[b]["yp"] for b in range(B)], 0)
    ys = np.concatenate([R[c]["ys"].reshape(NB_S, TS, D) for c in range(8)], 0)

    def pstack(k):
        return np.stack([R[b][k] for b in range(B)], 1)

    def sstack(k):
        return np.concatenate([R[c][k] for c in range(8)], 1)
    p_mk = pstack("p_mk").reshape(DEPTH, B, N_MEM, 4, 512)
    p_mv = pstack("p_mv").reshape(DEPTH, B, N_MEM, 4, 512)
    outs = (yp, ys, pstack("p_shift"), pstack("p_wkv"), pstack("p_conv"), pstack("p_ssm"), pstack("p_gla"), pstack("p_ret"),
            p_mk, p_mv, sstack("s_shift"), sstack("s_wkv"), sstack("s_conv"), sstack("s_ssm"), sstack("s_gla"), sstack("s_ret"))
    outs = tuple(np.ascontiguousarray(o.astype(np.float32)) for o in outs)
    if os.environ.get("KDUMP"):
        for i, o in enumerate(outs):
            np.save(os.environ["KDUMP"] + "_%d.npy" % i, o)
    return outs


def _gla_phase(self, l):
    c = self.c
    with self.scope():
        gn, dgn = self.bcast_row("glagn", self.I["gla_norm_g"][l:l + 1, :], 512)
        gb, dgb = self.bcast_row("glagb", self.I["gla_gk_b"][l:l + 1, :], 256)
        gkup = self.sbt("gkup", [16, 256], BF16); dgkup = Dep()
        c.dma("pool", gkup[:], self.I["gla_gk_up"][l], writes=[dgkup])
        St = self.sbt("glaS", [128, 256], F32); dSt = Dep()
        Stbf = self.sbt("glaSbf", [128, 256], BF16); dStbf = Dep()
        c.op("dve", lambda e: e.memset(St[:], 0.0), writes=[dSt])
        c.op("dve", lambda e: e.memset(Stbf[:], 0.0), writes=[dStbf])
        S0 = self.sbt("glaS0", [128, NB_S, 256], F32); dS0 = Dep()
        S0bf = self.sbt("glaS0bf", [128, NB_S, 256], BF16); dS0bf = Dep()
        c.dma("sp", S0[:].rearrange("p b (c v) -> p b c v", c=2), self.I["st_gla"][l].rearrange("b (c h2) k v -> (h2 k) b c v", h2=2), writes=[dS0])
        c.op("act", lambda e: e.copy(out=S0bf[:], in_=S0[:]), reads=[dS0], writes=[dS0bf])
        qkb = self.rot("glaqk", 2, [128, 4, 128], F32)
        tmb = self.rot("glatm", 2, [128, 1280], F32)
        gkd = self.rot("glagkd", 2, [16, 128], BF16)
        xg = self.sbt("glaxg", [128, 256], F32); dxg = Dep()
        sp = self.sbt("glasp", [128, 256], F32); dsp = Dep()
        E3 = self.sbt("glaE3", [128, 256], F32); dE3 = Dep()
        E1T = self.sbt("glaE1T", [128, 2, 128], F32); dE1T = Dep()
        E2T = self.sbt("glaE2T", [128, 2, 128], F32); dE2T = Dep()
        khat = self.sbt("glakhat", [128, 256], BF16); dkhat = Dep()
        qhT = self.sbt("glaqhT", [128, 2, 128], BF16); dqhT = Dep()
        khT = self.sbt("glakhT", [128, 2, 128], BF16); dkhT = Dep()
        W = self.sbt("glaW", [128, 512], BF16); dW = Dep()
        vbf = self.sbt("glavbf", [128, 512], BF16); dvbf = Dep()
        y = self.sbt("glay", [128, 512], F32); dy = Dep()
        tmp = self.sbt("glatmp", [128, 512], F32); dtmp = Dep()
        small = self.sbt("glasmall", [128, 32], F32); dsmall = Dep()
        qmall = self.sbt("glaqmall", [128, NB_S, 2, 128], BF16); dqmall = Dep()
        kbb = self.rot("glakb", 2, [128, 256], BF16)
        stg = self.rot("glastg", 2, [128, 256], F32)
        for tt in range(NT):
            smp = tt == 16
            sfx = "_S" if smp else "_P"
            t0 = tt * 128
            qk, dqk = qkb.next()
            c.dma("sp", qk[:, 0:2, :], self.PF[PF_GQ:PF_GQ + 256, t0:t0 + 128].rearrange("(c p) t -> p c t", p=128), reads=[self.dPF], writes=[dqk])
            c.dma("sp", qk[:, 2:4, :], self.PF[PF_GK:PF_GK + 256, t0:t0 + 128].rearrange("(c p) t -> p c t", p=128), reads=[self.dPF], writes=[dqk])
            tm, dtm = tmb.next()
            c.dma("sp", tm[:, 0:768], self.PT[t0:t0 + 128, C_GLA + 256:C_GLA + 1024], reads=[self.dPT[tt]], writes=[dtm])
            c.dma("sp", tm[:, 768:1280], self.PT[t0:t0 + 128, C_GLA + 1040:C_GLA + 1552], reads=[self.dPT[tt]], writes=[dtm])
            gk, dgk = gkd.next()
            c.dma("pool", gk[:], self.PF[PF_GKD:PF_GKD + 16, t0:t0 + 128], reads=[self.dPF], writes=[dgk])
            p0 = self.pb()
            c.op("pe", lambda e: e.matmul(self.ps[p0][:, 0:256], gk[:], gkup[:], start=True, stop=True), reads=[dgk, dgkup], writes=[self.dps[p0]])
            c.op("dve", lambda e: e.tensor_tensor(out=xg[:], in0=self.ps[p0][:, 0:256], in1=gb[:], op=ALU.add), reads=[self.dps[p0], dgb], writes=[dxg])
            c.op("act", lambda e: e.activation(out=xg[:], in_=xg[:], func=AF.Exp, scale=-1.0), reads=[dxg], writes=[dxg])
            c.op("act", lambda e: e.activation(out=sp[:], in_=xg[:], func=AF.Ln, bias=self.epsb(1.0), scale=1.0), reads=[dxg, self.dcst], writes=[dsp])
            p1 = self.pb()
            c.op("pe", lambda e: e.matmul(self.ps[p1][:, 0:256], self.cst("le" + sfx), sp[:], start=True, stop=True), reads=[dsp, self.dcst], writes=[self.dps[p1]])
            for cc in range(2):
                c.op("pe", lambda e, cc=cc: e.matmul(self.ps[p1][:, 256 + cc * 128:256 + (cc + 1) * 128], sp[:, cc * 128:(cc + 1) * 128], self.cst("le" + sfx), start=True, stop=True),
                     reads=[dsp, self.dcst], writes=[self.dps[p1]])
            c.op("act", lambda e: e.activation(out=E3[:], in_=self.ps[p1][:, 0:256], func=AF.Exp, scale=1.0 / 16.0), reads=[self.dps[p1]], writes=[dE3])
            c.op("act", lambda e: e.activation(out=E1T[:].rearrange("p c t -> p (c t)"), in_=self.ps[p1][:, 256:512], func=AF.Exp, scale=-1.0 / 16.0), reads=[self.dps[p1]], writes=[dE1T])
            c.op("act", lambda e: e.activation(out=E2T[:].rearrange("p c t -> p (c t)"), in_=self.ps[p1][:, 256:512], func=AF.Exp, scale=1.0 / 16.0), reads=[self.dps[p1]], writes=[dE2T])
            c.op("dve", lambda e: e.tensor_tensor(out=khat[:], in0=tm[:, 0:256], in1=E3[:], op=ALU.mult), reads=[dtm, dE3], writes=[dkhat])
            c.op("dve", lambda e: e.scalar_tensor_tensor(out=qhT[:].rearrange("p c t -> p (c t)"), in0=qk[:, 0:2, :].rearrange("p c t -> p (c t)"), scalar=0.125,
                                                        in1=E1T[:].rearrange("p c t -> p (c t)"), op0=ALU.mult, op1=ALU.mult), reads=[dqk, dE1T], writes=[dqhT])
            c.op("pool", lambda e: e.tensor_tensor(out=khT[:], in0=qk[:, 2:4, :], in1=E2T[:], op=ALU.mult), reads=[dqk, dE2T], writes=[dkhT])
            c.op("act", lambda e: e.copy(out=vbf[:], in_=tm[:, 256:768]), reads=[dtm], writes=[dvbf])
            p2 = [self.pb(), self.pb()]
            for h2 in range(2):
                for cc in range(2):
                    c.op("pe", lambda e, cc=cc, h2=h2: e.matmul(self.ps[p2[h2]][:, cc * 128:(cc + 1) * 128], khT[h2 * 64:(h2 + 1) * 64, cc, :], qhT[h2 * 64:(h2 + 1) * 64, cc, :], start=True, stop=True),
                         reads=[dkhT, dqhT], writes=[self.dps[p2[h2]]])
            cau = self.cst("causal" + sfx).unsqueeze(1).broadcast_to([128, 2, 128])
            for h2 in range(2):
                c.op("dve", lambda e, h2=h2: e.tensor_tensor(out=W[:, h2 * 256:(h2 + 1) * 256].rearrange("p (c i) -> p c i", c=2), in0=self.ps[p2[h2]][:, 0:256].rearrange("p (c i) -> p c i", c=2), in1=cau, op=ALU.mult),
                     reads=[self.dps[p2[h2]], self.dcst], writes=[dW])
            if smp:
                for b in range(NB_S):
                    mrb = self.maskrow[:, b, :].unsqueeze(1).broadcast_to([128, 2, 128])
                    c.op("pool", lambda e, b=b, mrb=mrb: e.tensor_tensor(out=qmall[:, b, :, :], in0=qhT[:], in1=mrb, op=ALU.mult), reads=[dqhT, self.dcst], writes=[dqmall])
            p3 = [self.pb(), self.pb()]
            for h2 in range(2):
                for cc in range(2):
                    h = 2 * cc + h2
                    c.op("pe", lambda e, h=h, cc=cc, h2=h2: e.matmul(self.ps[p3[h2]][:, cc * 128:(cc + 1) * 128], W[:, h2 * 256 + cc * 128:h2 * 256 + (cc + 1) * 128], vbf[:, h * 128:(h + 1) * 128],
                                                                 start=True, stop=False, skip_group_check=True), reads=[dW, dvbf], writes=[self.dps[p3[h2]]])
                    if not smp:
                        c.op("pe", lambda e, cc=cc, h2=h2: e.matmul(self.ps[p3[h2]][:, cc * 128:(cc + 1) * 128], qhT[h2 * 64:(h2 + 1) * 64, cc, :], Stbf[h2 * 64:(h2 + 1) * 64, cc * 128:(cc + 1) * 128],
                                                                start=False, stop=True, skip_group_check=True), reads=[dqhT, dStbf], writes=[self.dps[p3[h2]]])
                    else:
                        for b in range(NB_S):
                            c.op("pe", lambda e, cc=cc, h2=h2, b=b: e.matmul(self.ps[p3[h2]][:, cc * 128:(cc + 1) * 128], qmall[h2 * 64:(h2 + 1) * 64, b, cc, :],
                                                                         S0bf[h2 * 64:(h2 + 1) * 64, b, cc * 128:(cc + 1) * 128],
                                                                         start=False, stop=(b == NB_S - 1), skip_group_check=True),
                                 reads=[dqmall, dS0bf], writes=[self.dps[p3[h2]]])
            y4 = y[:].rearrange("p (c h v) -> p c h v", c=2, h=2)
            for h2 in range(2):
                c.op("act", lambda e, h2=h2: e.copy(out=y4[:, :, h2, :], in_=self.ps[p3[h2]][:, 0:256].rearrange("p (c v) -> p c v", c=2)), reads=[self.dps[p3[h2]], self.dOS[tt]], writes=[dy])
            self.rms_gate(y, dy, 4, 128, gn, dgn, tm[:, 768:1280], dtm, 1e-5, self.OS[t0:t0 + 128, 1024:1536], self.dOS[tt], tmp, dtmp, small, dsmall)
            if not smp:
                p4 = self.pb()
                for h in range(4):
                    cc, h2 = h // 2, h % 2
                    c.op("pe", lambda e, h=h, cc=cc, h2=h2: e.matmul(self.ps[p4][h2 * 64:(h2 + 1) * 64, cc * 128:(cc + 1) * 128], khat[:, h * 64:(h + 1) * 64], vbf[:, h * 128:(h + 1) * 128],
                                                                 start=True, stop=True, skip_group_check=True), reads=[dkhat, dvbf], writes=[self.dps[p4]])
                c.op("dve", lambda e: e.tensor_tensor(out=St[:], in0=St[:], in1=self.ps[p4][:, 0:256], op=ALU.add), reads=[dSt, self.dps[p4]], writes=[dSt])
                eb = E1T[:, :, 127:128].broadcast_to([128, 2, 128])
                c.op("dve", lambda e: e.tensor_tensor(out=St[:].rearrange("p (c v) -> p c v", c=2), in0=St[:].rearrange("p (c v) -> p c v", c=2), in1=eb, op=ALU.mult),
                     reads=[dSt, dE1T], writes=[dSt])
                c.op("act", lambda e: e.copy(out=Stbf[:], in_=St[:]), reads=[dSt], writes=[dStbf])
                if tt == 15:
                    do = self.odep()
                    c.dma("sp", self.O["p_gla"][l].rearrange("(c h2) k v -> (h2 k) c v", h2=2), St[:].rearrange("p (c v) -> p c v", c=2), reads=[dSt], writes=[do])
            else:
                for b in range(NB_S):
                    kb, dkb = kbb.next()
                    c.op("pool", lambda e, kb=kb, b=b: e.tensor_scalar(out=kb[:], in0=khat[:], scalar1=self.cst("seqmask")[:, b:b + 1], scalar2=None, op0=ALU.mult),
                         reads=[dkhat, self.dcst], writes=[dkb])
                    p4 = self.pb()
                    for h in range(4):
                        cc, h2 = h // 2, h % 2
                        c.op("pe", lambda e, h=h, cc=cc, h2=h2, kb=kb, p4=p4: e.matmul(self.ps[p4][h2 * 64:(h2 + 1) * 64, cc * 128:(cc + 1) * 128], kb[:, h * 64:(h + 1) * 64], vbf[:, h * 128:(h + 1) * 128],
                                                                                  start=True, stop=True, skip_group_check=True), reads=[dkb, dvbf], writes=[self.dps[p4]])
                    sb_, dsb = stg.next()
                    c.op("dve", lambda e, sb_=sb_, b=b, p4=p4: e.tensor_tensor(out=sb_[:], in0=S0[:, b, :], in1=self.ps[p4][:, 0:256], op=ALU.add), reads=[dS0, self.dps[p4]], writes=[dsb])
                    col = TS * b + TS - 1
                    eb = E1T[:, :, col:col + 1].broadcast_to([128, 2, 128])
                    c.op("dve", lambda e, sb_=sb_, eb=eb: e.tensor_tensor(out=sb_[:].rearrange("p (c v) -> p c v", c=2), in0=sb_[:].rearrange("p (c v) -> p c v", c=2), in1=eb, op=ALU.mult),
                         reads=[dsb, dE1T], writes=[dsb])
                    do = self.odep()
                    c.dma("sp", self.O["s_gla"][l, b].rearrange("(c h2) k v -> (h2 k) c v", h2=2), sb_[:].rearrange("p (c v) -> p c v", c=2), reads=[dsb], writes=[do])


Prog.mix_gla = _gla_phase


def _ssd_phase(self, l):
    c = self.c
    with self.scope():
        gn, dgn = self.bcast_row("mbgn", self.I["mb_norm_g"][l:l + 1, :], 512)
        dtb, ddtb = self.bcast_row("mbdtb", self.I["mb_dt_bias"][l:l + 1, :], 8)
        aneg, daneg = self.bcast_row("mbaneg", self.I["mb_a_log"][l:l + 1, :], 8)
        c.op("act", lambda e: e.activation(out=aneg[:], in_=aneg[:], func=AF.Exp), reads=[daneg], writes=[daneg])
        c.op("dve", lambda e: e.tensor_scalar(out=aneg[:], in0=aneg[:], scalar1=-1.0, scalar2=None, op0=ALU.mult), reads=[daneg], writes=[daneg])
        Dt, dDt = self.bcast_row("mbD", self.I["mb_d"][l:l + 1, :], 8)
        cw = self.sbt("mbcw", [128, 4, 8], F32); dcw = Dep()
        with self.nc.allow_non_contiguous_dma(reason="tiny conv weights"):
            for wi in range(4):
                c.dma("sp", cw[:, wi, :], self.I["mb_conv_w"][l, wi].rearrange("(c p) -> p c", p=128), writes=[dcw])
        cb, dcb = self.col_load("mbcb", self.I["mb_conv_b"][l], 8)
        hT = self.sbt("mbhT", [48, 1024], F32); dhT = Dep()
        c.dma("sp", hT[:], self.I["st_conv"][l].rearrange("b r n -> (b r) n"), writes=[dhT])
        hist = self.sbt("mbhist", [128, 8, 48], F32); dhist = Dep()
        ph = self.pb()
        for j in range(8):
            c.op("pe", lambda e, j=j: e.transpose(self.ps[ph][:, j * 48:(j + 1) * 48], hT[:, j * 128:(j + 1) * 128], self.cst("ident")[0:48, 0:48]),
                 reads=[dhT, self.dcst], writes=[self.dps[ph]])
        c.op("dve", lambda e: e.tensor_copy(out=hist[:].rearrange("p c r -> p (c r)"), in_=self.ps[ph][:, 0:384]), reads=[self.dps[ph]], writes=[dhist])
        XH = self.sbt("mbXH", [128, 4, TOK], F32); dXH = Dep()
        BT = self.sbt("mbBT", [128, 2, TOK], BF16); dBT = Dep()
        CT = self.sbt("mbCT", [128, 2, TOK], BF16); dCT = Dep()
        with self.scope():
            xbp = self.rot("mbxb", 2, [128, 3 + SEQ], F32)
            xsp = self.rot("mbxs", 2, [128, NB_S, 3 + TS], F32)
            cv = self.sbt("mbcv", [128, TOK], F32); dcv = Dep()
            for j in range(8):
                xb, dxb = xbp.next()
                xs, dxs = xsp.next()
                r0 = PF_XBC + j * 128
                c.op("pool", lambda e, xb=xb: e.memset(xb[:, 0:3], 0.0), writes=[dxb])
                c.dma("sp", xb[:, 3:3 + SEQ], self.PF[r0:r0 + 128, 0:SEQ], reads=[self.dPF], writes=[dxb])
                c.dma("sp", xs[:, :, 3:3 + TS], self.PF[r0:r0 + 128, SEQ:TOK].rearrange("p (b t) -> p b t", t=TS), reads=[self.dPF], writes=[dxs])
                c.op("pool", lambda e, xs=xs, j=j: e.tensor_copy(out=xs[:, :, 0:3], in_=hist[:, j, :].rearrange("p (b r) -> p b r", r=3)), reads=[dhist, dxs], writes=[dxs])
                cvp = cv[:, 0:SEQ]
                cvs = cv[:, SEQ:TOK].rearrange("p (b t) -> p b t", t=TS)
                c.op("dve", lambda e, xb=xb, j=j: e.tensor_scalar(out=cvp, in0=xb[:, 3:3 + SEQ], scalar1=cw[:, 3, j:j + 1], scalar2=cb[:, j:j + 1], op0=ALU.mult, op1=ALU.add),
                     reads=[dxb, dcw, dcb], writes=[dcv])
                c.op("pool", lambda e, xs=xs, j=j: e.tensor_scalar(out=cvs, in0=xs[:, :, 3:3 + TS], scalar1=cw[:, 3, j:j + 1], scalar2=cb[:, j:j + 1], op0=ALU.mult, op1=ALU.add),
                     reads=[dxs, dcw, dcb], writes=[dcv])
                for i in range(3):
                    c.op("dve", lambda e, xb=xb, j=j, i=i: e.scalar_tensor_tensor(out=cvp, in0=xb[:, i:i + SEQ], scalar=cw[:, i, j:j + 1], in1=cvp, op0=ALU.mult, op1=ALU.add),
                         reads=[dxb, dcw, dcv], writes=[dcv])
                    c.op("dve", lambda e, xs=xs, j=j, i=i: e.scalar_tensor_tensor(out=cvs, in0=xs[:, :, i:i + TS], scalar=cw[:, i, j:j + 1], in1=cvs, op0=ALU.mult, op1=ALU.add),
                         reads=[dxs, dcw, dcv], writes=[dcv])
                if j < 4:
                    c.op("act", lambda e, j=j: e.activation(out=XH[:, j, :], in_=cv[:], func=AF.Silu), reads=[dcv], writes=[dXH])
                elif j < 6:
                    c.op("act", lambda e, j=j: e.activation(out=BT[:, j - 4, :], in_=cv[:], func=AF.Silu), reads=[dcv], writes=[dBT])
                else:
                    c.op("act", lambda e, j=j: e.activation(out=CT[:, j - 6, :], in_=cv[:], func=AF.Silu), reads=[dcv], writes=[dCT])
        H = self.sbt("mbH", [128, 512], F32); dH = Dep()
        Hbf = self.sbt("mbHbf", [128, 512], BF16); dHbf = Dep()
        c.op("dve", lambda e: e.memset(H[:], 0.0), writes=[dH])
        c.op("dve", lambda e: e.memset(Hbf[:], 0.0), writes=[dHbf])
        S0bf = self.sbt("mbS0bf", [128, NB_S, 512], BF16); dS0bf = Dep()
        for bq in range(4):
            c.dma("pool", S0bf[:, bq * 4:(bq + 1) * 4, :].rearrange("n b (h d) -> n b h d", h=8), self.I["st_ssm"][l, bq * 4:(bq + 1) * 4].rearrange("b h n d -> n b h d"), writes=[dS0bf])
        s0p = self.rot("mbs0", 2, [128, 512], F32)
        tmb = self.rot("mbtm", 2, [128, 520], F32)
        xh = self.sbt("mbxh", [128, 512], F32); dxh = Dep()
        Bt = self.sbt("mbBt", [128, 256], BF16); dBt = Dep()
        la = self.sbt("mbla", [128, 8], F32); dla = Dep()
        dtt = self.sbt("mbdt", [128, 8], F32); ddt = Dep()
        lam = self.sbt("mblam", [128, NB_S, 8], F32); dlam = Dep()
        e3 = self.sbt("mbe3", [128, 24], F32); de3 = Dep()
        cdS = self.sbt("mbcdS", [128, NB_S, 8], F32); dcdS = Dep()
        Ah = self.rot("mbAh", 2, [128, 128], F32)
        Edec = self.sbt("mbEdec", [128, 1024], F32); dEdec = Dep()
        msc = self.sbt("mbmsc", [128, 256], F32); dmsc = Dep()
        W = self.sbt("mbW", [128, 1024], BF16); dW = Dep()
        xdt = self.sbt("mbxdt", [128, 512], BF16); dxdt = Dep()
        xdd = self.sbt("mbxdd", [128, 512], BF16); dxdd = Dep()
        xddb = self.rot("mbxddb", 2, [128, 512], BF16)
        ctmall = self.sbt("mbctmall", [128, NB_S, 2, 128], BF16); dctmall = Dep()
        y = self.sbt("mby", [128, 512], F32); dy = Dep()
        tmp = self.sbt("mbtmp", [128, 512], F32); dtmp = Dep()
        small = self.sbt("mbsmall", [128, 32], F32); dsmall = Dep()
        for tt in range(NT):
            smp = tt == 16
            sfx = "_S" if smp else "_P"
            t0 = tt * 128
            tm, dtm = tmb.next()
            c.dma("sp", tm[:, 0:512], self.PT[t0:t0 + 128, C_MB:C_MB + 512], reads=[self.dPT[tt]], writes=[dtm])
            c.dma("sp", tm[:, 512:520], self.PT[t0:t0 + 128, C_MB + 1536:C_MB + 1544], reads=[self.dPT[tt]], writes=[dtm])
            p0 = self.pb()
            for j in range(4):
                c.op("pe", lambda e, j=j: e.transpose(self.ps[p0][:, j * 128:(j + 1) * 128], XH[:, j, t0:t0 + 128], self.cst("ident")), reads=[dXH, self.dcst], writes=[self.dps[p0]])
            c.op("act", lambda e: e.copy(out=xh[:], in_=self.ps[p0][:, 0:512]), reads=[self.dps[p0]], writes=[dxh])
            p1 = self.pb()
            psb = self.ps[p1][:].bitcast(BF16)
            for g in range(2):
                c.op("pe", lambda e, g=g: e.transpose(psb[:, g * 128:(g + 1) * 128], BT[:, g, t0:t0 + 128], self.ident_bf[:]), reads=[dBT, self.dcst], writes=[self.dps[p1]])
            c.op("dve", lambda e: e.tensor_copy(out=Bt[:], in_=psb[:, 0:256]), reads=[self.dps[p1]], writes=[dBt])
            c.op("dve", lambda e: e.tensor_tensor(out=dtt[:], in0=tm[:, 512:520], in1=dtb[:], op=ALU.add), reads=[dtm, ddtb], writes=[ddt])
            c.op("act", lambda e: e.activation(out=dtt[:], in_=dtt[:], func=AF.Exp), reads=[ddt], writes=[ddt])
            c.op("act", lambda e: e.activation(out=dtt[:], in_=dtt[:], func=AF.Ln, bias=self.epsb(1.0), scale=1.0), reads=[ddt, self.dcst], writes=[ddt])
            c.op("dve", lambda e: e.tensor_tensor(out=la[:], in0=dtt[:], in1=aneg[:], op=ALU.mult), reads=[ddt, daneg], writes=[dla])
            p2 = self.pb()
            c.op("pe", lambda e: e.matmul(self.ps[p2][:, 0:8], self.cst("le" + sfx), la[:], start=True, stop=True), reads=[dla, self.dcst], writes=[self.dps[p2]])
            c.op("pe", lambda e: e.matmul(self.ps[p2][:, 8:16], self.cst("gt" + sfx), la[:], start=True, stop=True), reads=[dla, self.dcst], writes=[self.dps[p2]])
            c.op("pe", lambda e: e.matmul(self.ps[p2][:, 16:24], self.cst("ones"), la[:], start=True, stop=True), reads=[dla, self.dcst], writes=[self.dps[p2]])
            if smp:
                c.op("dve", lambda e: e.tensor_tensor(out=lam[:], in0=la[:].unsqueeze(1).broadcast_to([128, NB_S, 8]), in1=self.cst("seqmask").unsqueeze(2).broadcast_to([128, NB_S, 8]), op=ALU.mult),
                     reads=[dla, self.dcst], writes=[dlam])
                c.op("pe", lambda e: e.matmul(self.ps[p2][:, 128:256], self.cst("ones"), lam[:].rearrange("p b h -> p (b h)"), start=True, stop=True), reads=[dlam, self.dcst], writes=[self.dps[p2]])
                c.op("act", lambda e: e.activation(out=cdS[:].rearrange("p b h -> p (b h)"), in_=self.ps[p2][:, 128:256], func=AF.Exp), reads=[self.dps[p2]], writes=[dcdS])
            c.op("act", lambda e: e.activation(out=e3[:], in_=self.ps[p2][:, 0:24], func=AF.Exp), reads=[self.dps[p2]], writes=[de3])
            for hg in range(2):
                p3 = self.pb()
                for hh in range(4):
                    h = hg * 4 + hh
                    ah, dah = Ah.next()
                    c.op("dve", lambda e, ah=ah, h=h: e.tensor_scalar(out=ah[:], in0=self.cst("gt" + sfx), scalar1=la[:, h:h + 1], scalar2=None, op0=ALU.mult), reads=[dla, self.dcst], writes=[dah])
                    c.op("pe", lambda e, ah=ah, hh=hh, p3=p3: e.matmul(self.ps[p3][:, hh * 128:(hh + 1) * 128], ah[:], self.cst("le" + sfx), start=True, stop=True), reads=[dah, self.dcst], writes=[self.dps[p3]])
                c.op("act", lambda e, hg=hg, p3=p3: e.activation(out=Edec[:, hg * 512:(hg + 1) * 512], in_=self.ps[p3][:, 0:512], func=AF.Exp), reads=[self.dps[p3]], writes=[dEdec])
            p4 = self.pb()
            for g in range(2):
                c.op("pe", lambda e, g=g: e.matmul(self.ps[p4][:, g * 128:(g + 1) * 128], BT[:, g, t0:t0 + 128], CT[:, g, t0:t0 + 128], start=True, stop=True), reads=[dBT, dCT], writes=[self.dps[p4]])
            cau = self.cst("causal" + sfx).unsqueeze(1).broadcast_to([128, 2, 128])
            c.op("dve", lambda e: e.tensor_tensor(out=msc[:].rearrange("p (g i) -> p g i", g=2), in0=self.ps[p4][:, 0:256].rearrange("p (g i) -> p g i", g=2), in1=cau, op=ALU.mult),
                 reads=[self.dps[p4], self.dcst], writes=[dmsc])
            for g in range(2):
                mb_ = msc[:, g * 128:(g + 1) * 128].unsqueeze(1).broadcast_to([128, 4, 128])
                eng = "dve" if g == 0 else "pool"
                c.op(eng, lambda e, g=g, mb_=mb_: e.tensor_tensor(out=W[:, g * 512:(g + 1) * 512].rearrange("p (h i) -> p h i", h=4), in0=Edec[:, g * 512:(g + 1) * 512].rearrange("p (h i) -> p h i", h=4), in1=mb_, op=ALU.mult),
                     reads=[dEdec, dmsc], writes=[dW])
            dtbc = dtt[:].unsqueeze(2).broadcast_to([128, 8, 64])
            c.op("dve", lambda e: e.tensor_tensor(out=xdt[:].rearrange("p (h d) -> p h d", h=8), in0=xh[:].rearrange("p (h d) -> p h d", h=8), in1=dtbc, op=ALU.mult), reads=[dxh, ddt], writes=[dxdt])
            debc = e3[:, 8:16].unsqueeze(2).broadcast_to([128, 8, 64])
            c.op("pool", lambda e: e.tensor_tensor(out=xdd[:].rearrange("p (h d) -> p h d", h=8), in0=xdt[:].rearrange("p (h d) -> p h d", h=8), in1=debc, op=ALU.mult), reads=[dxdt, de3], writes=[dxdd])
            p5 = self.pb()
            for h in range(8):
                c.op("pe", lambda e, h=h: e.matmul(self.ps[p5][:, h * 64:(h + 1) * 64], W[:, h * 128:(h + 1) * 128], xdt[:, h * 64:(h + 1) * 64], start=True, stop=True), reads=[dW, dxdt], writes=[self.dps[p5]])
            p6 = self.pb()
            if not smp:
                for g in range(2):
                    c.op("pe", lambda e, g=g: e.matmul(self.ps[p6][:, g * 256:(g + 1) * 256], CT[:, g, t0:t0 + 128], Hbf[:, g * 256:(g + 1) * 256], start=True, stop=True), reads=[dCT, dHbf], writes=[self.dps[p6]])
            else:
                for b in range(NB_S):
                    mrb = self.maskrow[:, b, :].unsqueeze(1).broadcast_to([128, 2, 128])
                    c.op("pool", lambda e, b=b, mrb=mrb: e.tensor_tensor(out=ctmall[:, b, :, :], in0=CT[:, :, t0:t0 + 128], in1=mrb, op=ALU.mult), reads=[dCT, self.dcst], writes=[dctmall])
                for g in range(2):
                    for b in range(NB_S):
                        c.op("pe", lambda e, g=g, b=b: e.matmul(self.ps[p6][:, g * 256:(g + 1) * 256], ctmall[:, b, g, :], S0bf[:, b, g * 256:(g + 1) * 256], start=(b == 0), stop=(b == NB_S - 1), skip_group_check=True),
                             reads=[dctmall, dS0bf], writes=[self.dps[p6]])
            ecb = e3[:, 0:8].unsqueeze(2).broadcast_to([128, 8, 64])
            c.op("dve", lambda e: e.tensor_tensor(out=y[:].rearrange("p (h d) -> p h d", h=8), in0=self.ps[p6][:, 0:512].rearrange("p (h d) -> p h d", h=8), in1=ecb, op=ALU.mult),
                 reads=[self.dps[p6], de3, self.dOS[tt]], writes=[dy])
            c.op("dve", lambda e: e.tensor_tensor(out=y[:], in0=y[:], in1=self.ps[p5][:, 0:512], op=ALU.add), reads=[dy, self.dps[p5]], writes=[dy])
            Dbc = Dt[:].unsqueeze(2).broadcast_to([128, 8, 64])
            c.op("pool", lambda e: e.tensor_tensor(out=tmp[:].rearrange("p (h d) -> p h d", h=8), in0=xh[:].rearrange("p (h d) -> p h d", h=8), in1=Dbc, op=ALU.mult), reads=[dxh, dDt], writes=[dtmp])
            c.op("dve", lambda e: e.tensor_tensor(out=y[:], in0=y[:], in1=tmp[:], op=ALU.add), reads=[dy, dtmp], writes=[dy])
            c.op("act", lambda e: e.activation(out=tmp[:], in_=tm[:, 0:512], func=AF.Silu), reads=[dtm, dtmp], writes=[dtmp])
            c.op("dve", lambda e: e.tensor_tensor(out=y[:], in0=y[:], in1=tmp[:], op=ALU.mult), reads=[dy, dtmp], writes=[dy])
            c.op("dve", lambda e: e.tensor_tensor(out=tmp[:], in0=y[:], in1=y[:], op=ALU.mult), reads=[dy], writes=[dtmp])
            c.op("dve", lambda e: e.tensor_reduce(out=small[:, 0:2], in_=tmp[:].rearrange("p (g v) -> p g v", g=2), axis=AX.X, op=ALU.add), reads=[dtmp], writes=[dsmall])
            c.op("act", lambda e: e.activation(out=small[:, 8:10], in_=small[:, 0:2], func=AF.Sqrt, scale=1.0 / 256, bias=self.epsb(1e-5)), reads=[dsmall, self.dcst], writes=[dsmall])
            c.op("dve", lambda e: e.reciprocal(out=small[:, 16:18], in_=small[:, 8:10]), reads=[dsmall], writes=[dsmall])
            rb = small[:, 16:18].unsqueeze(2).broadcast_to([128, 2, 256])
            c.op("dve", lambda e: e.tensor_tensor(out=y[:].rearrange("p (g v) -> p g v", g=2), in0=y[:].rearrange("p (g v) -> p g v", g=2), in1=rb, op=ALU.mult), reads=[dy, dsmall], writes=[dy])
            c.op("pool", lambda e: e.tensor_tensor(out=y[:], in0=y[:], in1=gn[:], op=ALU.mult), reads=[dy, dgn], writes=[dy])
            c.dma("sp", self.OS[t0:t0 + 128, 512:1024], y[:], reads=[dy], writes=[self.dOS[tt]])
            if not smp:
                p7 = self.pb()
                for g in range(2):
                    c.op("pe", lambda e, g=g: e.matmul(self.ps[p7][:, g * 256:(g + 1) * 256], Bt[:, g * 128:(g + 1) * 128], xdd[:, g * 256:(g + 1) * 256], start=True, stop=True), reads=[dBt, dxdd], writes=[self.dps[p7]])
                cdb = e3[:, 16:24].unsqueeze(2).broadcast_to([128, 8, 64])
                c.op("pool", lambda e: e.tensor_tensor(out=H[:].rearrange("p (h d) -> p h d", h=8), in0=H[:].rearrange("p (h d) -> p h d", h=8), in1=cdb, op=ALU.mult), reads=[dH, de3], writes=[dH])
                c.op("dve", lambda e: e.tensor_tensor(out=H[:], in0=H[:], in1=self.ps[p7][:, 0:512], op=ALU.add), reads=[dH, self.dps[p7]], writes=[dH])
                c.op("act", lambda e: e.copy(out=Hbf[:], in_=H[:]), reads=[dH], writes=[dHbf])
                if tt == 15:
                    do = self.odep()
                    c.dma("sp", self.O["p_ssm"][l].rearrange("h n d -> n h d"), H[:].rearrange("n (h d) -> n h d", h=8), reads=[dH], writes=[do])
            else:
                for b in range(NB_S):
                    xb_, dxb_ = xddb.next()
                    c.op("pool", lambda e, xb_=xb_, b=b: e.tensor_scalar(out=xb_[:], in0=xdd[:], scalar1=self.cst("seqmask")[:, b:b + 1], scalar2=None, op0=ALU.mult), reads=[dxdd, self.dcst], writes=[dxb_])
                    p7 = self.pb()
                    for g in range(2):
                        c.op("pe", lambda e, g=g, xb_=xb_, p7=p7: e.matmul(self.ps[p7][:, g * 256:(g + 1) * 256], Bt[:, g * 128:(g + 1) * 128], xb_[:, g * 256:(g + 1) * 256], start=True, stop=True), reads=[dBt, dxb_], writes=[self.dps[p7]])
                    s0, ds0 = s0p.next()
                    c.dma("sp", s0[:].rearrange("n (h d) -> n h d", h=8), self.I["st_ssm"][l, b].rearrange("h n d -> n h d"), writes=[ds0])
                    cdb = cdS[:, b, :].unsqueeze(2).broadcast_to([128, 8, 64])
                    c.op("pool", lambda e, s0=s0, cdb=cdb: e.tensor_tensor(out=s0[:].rearrange("p (h d) -> p h d", h=8), in0=s0[:].rearrange("p (h d) -> p h d", h=8), in1=cdb, op=ALU.mult), reads=[ds0, dcdS], writes=[ds0])
                    c.op("dve", lambda e, s0=s0, p7=p7: e.tensor_tensor(out=s0[:], in0=s0[:], in1=self.ps[p7][:, 0:512], op=ALU.add), reads=[ds0, self.dps[p7]], writes=[ds0])
                    do = self.odep()
                    c.dma("sp", self.O["s_ssm"][l, b].rearrange("h n d -> n h d"), s0[:].rearrange("n (h d) -> n h d", h=8), reads=[ds0], writes=[do])


Prog.mix_ssd = _ssd_phase


SDEC = 0.6065306597126334


def _rwkv_phase(self, l):
    c = self.c
    with self.scope():
        mu, dmu = self.col_load("rwmu", self.I["rw_mu"][l], 14)
        w0, dw0 = self.col_load("rww0", self.I["rw_w0"][l], 4)
        a0, da0 = self.col_load("rwa0", self.I["rw_a0"][l], 4)
        kk_, dkk_ = self.col_load("rwkk", self.I["rw_k_k"][l], 4)
        ka, dka = self.col_load("rwka", self.I["rw_k_a"][l], 4)
        rk_, drk_ = self.col_load("rwrk", self.I["rw_r_k"][l], 4)
        omka = self.sbt("rwomka", [128, 4], F32); domka = Dep()
        c.op("dve", lambda e: e.tensor_scalar(out=omka[:], in0=ka[:], scalar1=-1.0, scalar2=1.0, op0=ALU.mult, op1=ALU.add), reads=[dka], writes=[domka])
        lora = self.sbt("rwlora", [128, 512], BF16); dlora = Dep()
        c.dma("pool", lora[0:64, :], self.I["rw_w_up"][l], writes=[dlora])
        c.dma("pool", lora[64:128, :], self.I["rw_a_up"][l], writes=[dlora])
        gup = self.sbt("rwgup", [128, 512], BF16); dgup = Dep()
        c.dma("pool", gup[:], self.I["rw_g_up"][l], writes=[dgup])
        lng, dlng = self.bcast_row("rwlng", self.I["rw_ln_g"][l:l + 1, :], 512)
        lnb, dlnb = self.bcast_row("rwlnb", self.I["rw_ln_b"][l:l + 1, :], 512)
        shT = self.sbt("rwshT", [128, 14, 16], F32); dshT = Dep()
        with self.scope():
            shraw = self.sbt("rwshraw", [16, RW_IN], F32); dshraw = Dep()
            c.dma("sp", shraw[:], self.I["st_shift"][l], writes=[dshraw])
            ph = self.pb()
            for j in range(14):
                c.op("pe", lambda e, j=j: e.transpose(self.ps[ph][:, j * 16:(j + 1) * 16], shraw[:, j * 128:(j + 1) * 128], self.cst("ident")[0:16, 0:16]), reads=[dshraw, self.dcst], writes=[self.dps[ph]])
            c.op("dve", lambda e: e.tensor_copy(out=shT[:].rearrange("p j b -> p (j b)"), in_=self.ps[ph][:, 0:224]), reads=[self.dps[ph]], writes=[dshT])
        EB = self.sbt("rwEB", [128, 4, NBLK], F32); dEB = Dep()
        RK = self.sbt("rwRK", [128, NT, 8], F32); dRK = Dep()
        sgd = self.sbt("rwsgd", [128, TOK], BF16); dsgd = Dep()
        dscr = self.dRWS
        with self.scope():
            reset = self.sbt("rwreset", [128, TOK], F32); dreset = Dep()
            c.dma("sp", reset[:], self.I["tokmask"][0:1, :].partition_broadcast(128), writes=[dreset])
            X = self.sbt("rwX", [128, 1 + TOK], F32); dX = Dep()
            bufs = {n: (self.sbt("rw" + n, [128, TOK], F32), Dep()) for n in "ABCDEF"}
            twd = self.sbt("rwtwd", [128, TOK], BF16); dtwd = Dep()
            adb = self.sbt("rwadb", [128, TOK], BF16); dadb = Dep()
            bT = self.sbt("rwbT", [128, TOK], BF16); dbT = Dep()
            kT = self.sbt("rwkT", [128, TOK], BF16); dkT = Dep()
            stb = self.rot("rwstb", 2, [128, TOK], BF16)
            tst = self.rot("rwtst", 3, [128, 128], BF16)
            tsf = self.rot("rwtsf", 2, [128, 128], F32)
            c.op("pool", lambda e: e.memset(X[:, 0:1], 0.0), writes=[dX])

            def xs_chunk(j, dst, ddst):
                r0 = PF_RW + j * 128
                c.dma("sp", X[:, 1:1 + TOK], self.PF[r0:r0 + 128, :], reads=[self.dPF], writes=[dX])
                c.op("dve", lambda e: e.tensor_tensor(out=dst[:], in0=X[:, 0:TOK], in1=X[:, 1:1 + TOK], op=ALU.subtract), reads=[dX], writes=[ddst])
                c.op("dve", lambda e: e.scalar_tensor_tensor(out=dst[:], in0=dst[:], scalar=mu[:, j:j + 1], in1=X[:, 1:1 + TOK], op0=ALU.mult, op1=ALU.add), reads=[dX, dmu, ddst], writes=[ddst])
                d0 = dst[:, SEQ:TOK].rearrange("p (b t) -> p b t", t=TS)[:, :, 0]
                p0 = X[:, 1 + SEQ:1 + TOK].rearrange("p (b t) -> p b t", t=TS)[:, :, 0]
                c.op("dve", lambda e: e.tensor_tensor(out=d0, in0=shT[:, j, :], in1=p0, op=ALU.subtract), reads=[dX, dshT, ddst], writes=[ddst])
                c.op("dve", lambda e: e.scalar_tensor_tensor(out=d0, in0=d0, scalar=mu[:, j:j + 1], in1=p0, op0=ALU.mult, op1=ALU.add), reads=[dX, dmu, ddst], writes=[ddst])

            A, dA = bufs["A"]; B, dB = bufs["B"]; C, dC = bufs["C"]; Dd, dDd = bufs["D"]; E, dE = bufs["E"]; Fb, dFb = bufs["F"]
            xs_chunk(12, A, dA)
            c.op("act", lambda e: e.activation(out=twd[0:64, :], in_=A[0:64, :], func=AF.Tanh), reads=[dA], writes=[dtwd])
            c.op("act", lambda e: e.copy(out=adb[64:128, :], in_=A[64:128, :]), reads=[dA], writes=[dadb])
            xs_chunk(13, A, dA)
            c.op("act", lambda e: e.activation(out=sgd[:], in_=A[:], func=AF.Sigmoid), reads=[dA], writes=[dsgd])
            for cc in range(4):
                xs_chunk(8 + cc, A, dA)
                for tt in range(NT):
                    pi = self.pb()
                    c.op("pe", lambda e, tt=tt, pi=pi: e.transpose(self.ps[pi][:, 0:128], A[:, tt * 128:(tt + 1) * 128], self.cst("ident")), reads=[dA, self.dcst], writes=[self.dps[pi]])
                    sf, dsf = tsf.next()
                    self.evac(pi, 128, 128, sf[:], dsf, tt)
                    c.dma("sp", self.VV[tt * 128:(tt + 1) * 128, cc * 128:(cc + 1) * 128], sf[:], reads=[dsf], writes=[dscr])
                for (t0, tw) in TBLK:
                    pi = self.pb()
                    c.op("pe", lambda e, pi=pi, t0=t0, tw=tw: e.matmul(self.ps[pi][:, 0:tw], lora[0:64, cc * 128:(cc + 1) * 128], twd[0:64, t0:t0 + tw], start=True, stop=True), reads=[dlora, dtwd], writes=[self.dps[pi]])
                    c.op("act", lambda e, pi=pi, t0=t0, tw=tw: e.activation(out=B[:, t0:t0 + tw], in_=self.ps[pi][:, 0:tw], func=AF.Sigmoid, bias=w0[:, cc:cc + 1], scale=1.0), reads=[self.dps[pi], dw0], writes=[dB])
                    pi = self.pb()
                    c.op("pe", lambda e, pi=pi, t0=t0, tw=tw: e.matmul(self.ps[pi][:, 0:tw], lora[64:128, cc * 128:(cc + 1) * 128], adb[64:128, t0:t0 + tw], start=True, stop=True), reads=[dlora, dadb], writes=[self.dps[pi]])
                    c.op("act", lambda e, pi=pi, t0=t0, tw=tw: e.activation(out=C[:, t0:t0 + tw], in_=self.ps[pi][:, 0:tw], func=AF.Sigmoid, bias=a0[:, cc:cc + 1], scale=1.0), reads=[self.dps[pi], da0], writes=[dC])
                c.op("dve", lambda e: e.tensor_tensor_scan(out=Dd[:], data0=reset[:], data1=B[:], initial=0.0, op0=ALU.mult, op1=ALU.add), reads=[dreset, dB], writes=[dDd])
                xs_chunk(4 + cc, A, dA)
                c.op("dve", lambda e: e.tensor_scalar(out=E[:], in0=A[:], scalar1=kk_[:, cc:cc + 1], scalar2=None, op0=ALU.mult), reads=[dA, dkk_], writes=[dE])
                c.op("pool", lambda e: e.tensor_tensor(out=Fb[:], in0=E[:], in1=E[:], op=ALU.mult), reads=[dE], writes=[dFb])
                for (t0, tw) in TBLK:
                    pi = self.pb()
                    c.op("pe", lambda e, pi=pi, t0=t0, tw=tw: e.matmul(self.ps[pi][:, 0:tw], self.cst("blockones"), Fb[:, t0:t0 + tw], start=True, stop=True), reads=[dFb, self.dcst], writes=[self.dps[pi]])
                    c.op("dve", lambda e, pi=pi, t0=t0, tw=tw: e.tensor_scalar(out=Fb[:, t0:t0 + tw], in0=self.ps[pi][:, 0:tw], scalar1=1e-24, scalar2=None, op0=ALU.max), reads=[self.dps[pi], dFb], writes=[dFb])
                c.op("act", lambda e: e.activation(out=Fb[:], in_=Fb[:], func=AF.Sqrt), reads=[dFb], writes=[dFb])
                c.op("dve", lambda e: e.reciprocal(out=Fb[:], in_=Fb[:]), reads=[dFb], writes=[dFb])
                c.op("dve", lambda e: e.tensor_tensor(out=E[:], in0=E[:], in1=Fb[:], op=ALU.mult), reads=[dE, dFb], writes=[dE])
                c.op("dve", lambda e: e.tensor_scalar(out=Fb[:], in0=C[:], scalar1=ka[:, cc:cc + 1], scalar2=omka[:, cc:cc + 1], op0=ALU.mult, op1=ALU.add), reads=[dC, dka, domka, dFb], writes=[dFb])
                c.op("pool", lambda e: e.tensor_tensor(out=Fb[:], in0=Fb[:], in1=A[:], op=ALU.mult), reads=[dFb, dA], writes=[dFb])
                c.op("dve", lambda e: e.tensor_tensor(out=C[:], in0=C[:], in1=E[:], op=ALU.mult), reads=[dC, dE], writes=[dC])
                c.op("dve", lambda e: e.tensor_tensor(out=A[:], in0=Dd[:], in1=B[:], op=ALU.subtract), reads=[dDd, dB, dA], writes=[dA])
                c.op("act", lambda e: e.activation(out=A[:], in_=A[:], func=AF.Exp, scale=-SDEC), reads=[dA], writes=[dA])
                st1, dst1 = stb.next()
                c.op("dve", lambda e, st1=st1: e.tensor_tensor(out=st1[:], in0=E[:], in1=A[:], op=ALU.mult), reads=[dE, dA], writes=[dst1])
                c.dma("sp", self.KAPT[cc][:, 0:TOK], st1[:], reads=[dst1], writes=[dscr])
                c.op("act", lambda e: e.activation(out=A[:], in_=Dd[:], func=AF.Exp, scale=SDEC), reads=[dDd, dA], writes=[dA])
                c.op("dve", lambda e: e.scalar_tensor_tensor(out=bT[:], in0=C[:], scalar=-1.0, in1=A[:], op0=ALU.mult, op1=ALU.mult), reads=[dC, dA], writes=[dbT])
                c.op("pool", lambda e: e.tensor_tensor(out=kT[:], in0=Fb[:], in1=A[:], op=ALU.mult), reads=[dFb, dA], writes=[dkT])
                for tt in range(NT):
                    for (srcT, dsrcT, dstS) in ((bT, dbT, self.BH), (kT, dkT, self.KH)):
                        pi = self.pb()
                        psb = self.ps[pi][:].bitcast(BF16)
                        c.op("pe", lambda e, tt=tt, psb=psb, srcT=srcT: e.transpose(psb[:, 0:128], srcT[:, tt * 128:(tt + 1) * 128], self.ident_bf[:]), reads=[dsrcT, self.dcst], writes=[self.dps[pi]])
                        sb_, dsb = tst.next()
                        c.op("act" if tt % 2 == 0 else "dve", (lambda e, sb_=sb_, psb=psb: e.copy(out=sb_[:], in_=psb[:, 0:128])) if tt % 2 == 0 else (lambda e, sb_=sb_, psb=psb: e.tensor_copy(out=sb_[:], in_=psb[:, 0:128])),
                             reads=[self.dps[pi]], writes=[dsb])
                        c.dma("sp", dstS[tt * 128:(tt + 1) * 128, cc * 128:(cc + 1) * 128], sb_[:], reads=[dsb], writes=[dscr])
                xs_chunk(cc, E, dE)
                c.op("dve", lambda e: e.scalar_tensor_tensor(out=C[:], in0=E[:], scalar=rk_[:, cc:cc + 1], in1=Fb[:], op0=ALU.mult, op1=ALU.mult), reads=[dE, drk_, dFb, dC], writes=[dC])
                for tt in range(NT):
                    pi = self.pb()
                    c.op("pe", lambda e, tt=tt, pi=pi: e.matmul(self.ps[pi][:, 0:2], C[:, tt * 128:(tt + 1) * 128], self.cst("halfsel"), start=True, stop=True), reads=[dC, self.dcst], writes=[self.dps[pi]])
                    c.op("dve", lambda e, tt=tt, pi=pi: e.tensor_copy(out=RK[:, tt, 2 * cc:2 * cc + 2], in_=self.ps[pi][:, 0:2]), reads=[self.dps[pi]], writes=[dRK])
                c.op("act", lambda e: e.activation(out=A[:], in_=Dd[:], func=AF.Exp, scale=-SDEC), reads=[dDd, dA], writes=[dA])
                c.op("dve", lambda e: e.tensor_copy(out=EB[:, cc, 0:SEQ // LB], in_=A[:, 0:SEQ].rearrange("p (n t) -> p n t", t=LB)[:, :, LB - 1]), reads=[dA], writes=[dEB])
                c.op("dve", lambda e: e.tensor_copy(out=EB[:, cc, SEQ // LB:NBLK], in_=A[:, SEQ:TOK].rearrange("p (n t) -> p n t", t=TS)[:, :, TS - 1]), reads=[dA], writes=[dEB])
                st2, dst2 = stb.next()
                c.op("dve", lambda e, st2=st2: e.tensor_tensor(out=st2[:], in0=E[:], in1=A[:], op=ALU.mult), reads=[dE, dA], writes=[dst2])
                c.op("pool", lambda e, st2=st2: e.tensor_copy(out=st2[:, 0:SEQ].rearrange("p (n t) -> p n t", t=LB)[:, :, LB - 1], in_=E[:, 0:SEQ].rearrange("p (n t) -> p n t", t=LB)[:, :, LB - 1]), reads=[dE, dst2], writes=[dst2])
                c.op("pool", lambda e, st2=st2: e.tensor_copy(out=st2[:, SEQ:TOK].rearrange("p (n t) -> p n t", t=TS)[:, :, TS - 1], in_=E[:, SEQ:TOK].rearrange("p (n t) -> p n t", t=TS)[:, :, TS - 1]), reads=[dE, dst2], writes=[dst2])
                c.dma("sp", self.RHT[cc], st2[:], reads=[dst2], writes=[dscr])
        self.rwkv_scan(l, EB, dEB)
        self.rwkv_post(l, RK, dRK, sgd, dsgd, gup, dgup, lng, dlng, lnb, dlnb)


Prog.mix_rwkv = _rwkv_phase


def _rwkv_scan(self, l, EB, dEB):
    c = self.c
    dscr = self.dRWS
    NR = 32
    with self.scope():
        self.ps_i = 0
        old_pb = self.pb

        def pb4():
            i = self.ps_i % 4
            self.ps_i += 1
            return i
        self.pb = pb4
        ACC = [4, 5]
        P1 = [6, 7]
        zt = self.sbt("rwz", [128, 8], BF16); dzt = Dep()
        c.op("dve", lambda e: e.memset(zt[:], 0.0), writes=[dzt])
        for cc in range(4):
            c.dma("sp", self.KAPT[cc][:, TOK:TOK + 8], zt[:], reads=[dzt], writes=[dscr])
        Sbf = self.sbt("rwSbf", [128, 256], BF16)
        dSbf = [Dep(), Dep()]
        L1p = self.rot("rwL1", 2, [128, 129, 48], BF16)
        L2p = self.rot("rwL2", 1, [72, 128, 128], BF16)
        Rp = [self.rot("rwRa", 2, [72, NR, 128], BF16), self.rot("rwRb", 2, [72, NR, 128], BF16)]
        for (t_, d_) in L1p.items + L2p.items + Rp[0].items + Rp[1].items:
            c.op("pool", lambda e, t_=t_: e.memset(t_[:], 0.0), writes=[d_])
        kapb = self.rot("rwkap", 2, [128, 4, 129], BF16)
        rhb = self.rot("rwrh", 2, [128, 4, 128], BF16)
        s0raw = self.rot("rws0raw", 2, [64, 512], F32)
        s0st = self.rot("rws0st", 2, [128, 256], F32)
        sfin = self.rot("rwsfin", 2, [128, 256], F32)
        sout = self.rot("rwsout", 2, [64, 512], F32)
        bm = self.cst("rw_blockmask")
        hm = self.cst("rw_halfmask")

        def build_tile(tt):
            t0 = tt * 128
            kap, dkap = kapb.next()
            rh, drh = rhb.next()
            for cc in range(4):
                c.dma("sp", kap[:, cc, :], self.KAPT[cc][:, t0:t0 + 129], reads=[dscr], writes=[dkap])
                c.dma("sp", rh[:, cc, :], self.RHT[cc][:, t0:t0 + 128], reads=[dscr], writes=[drh])
            L1, dL1 = L1p.next()
            hmb = hm.unsqueeze(1).unsqueeze(1).broadcast_to([128, 129, 4, 2])
            c.op("pool", lambda e: e.tensor_tensor(out=L1[:, :, 0:8].rearrange("p t (c h) -> p t c h", c=4), in0=kap[:].rearrange("p c t -> p t c").unsqueeze(3).broadcast_to([128, 129, 4, 2]),
                                                   in1=hmb, op=ALU.mult), reads=[dkap, self.dcst], writes=[dL1])
            hmb2 = hm.unsqueeze(1).unsqueeze(1).broadcast_to([128, 128, 4, 2])
            c.op("pool", lambda e: e.tensor_tensor(out=L1[:, 1:129, 32:40].rearrange("p t (c h) -> p t c h", c=4), in0=rh[:].rearrange("p c t -> p t c").unsqueeze(3).broadcast_to([128, 128, 4, 2]),
                                                   in1=hmb2, op=ALU.mult), reads=[drh, self.dcst], writes=[dL1])
            L2, dL2 = L2p.next()
            for h2 in range(2):
                srcb = self.BH[t0:t0 + 128, :].rearrange("t (c h k) -> c h t k", c=4, h=2)[:, h2]
                c.dma("sp", L2[h2:8:2, :, h2 * 64:(h2 + 1) * 64], srcb, reads=[dscr], writes=[dL2])
                srck = self.KH[t0:t0 + 128, :].rearrange("t (c h k) -> c h t k", c=4, h=2)[:, h2]
                c.dma("sp", L2[64 + h2:72:2, :, h2 * 64:(h2 + 1) * 64], srck, reads=[dscr], writes=[dL2])
            return L1, dL1, L2, dL2

        def fill_R(X, tok0, n):
            R, dR = Rp[X].next()
            for cl in range(2):
                cc = 2 * X + cl
                src = self.VV[tok0:tok0 + n, cc * 128:(cc + 1) * 128].rearrange("t (h v) -> h t v", h=2)
                c.dma("pool", R[64 + 2 * cc:64 + 2 * cc + 2, 0:n, cl * 64:(cl + 1) * 64], src, reads=[dscr], writes=[dR])
            return R, dR

        def extract_o(X, R, dR, tok_first, s_first, n):
            for cl in range(2):
                cc = 2 * X + cl
                dst = self.ORW[tok_first:tok_first + n, cc * 128:(cc + 1) * 128].rearrange("t (h v) -> h t v", h=2)
                c.dma("sp", dst, R[32 + 2 * cc:32 + 2 * cc + 2, s_first:s_first + n, cl * 64:(cl + 1) * 64], reads=[dR], writes=[self.dORW])

        def mask_op(X, R, dR, slot):
            c.op("dve", lambda e: e.tensor_tensor(out=R[0:40, slot, :], in0=self.ps[P1[X]][0:40, 0:128], in1=bm[0:40, X * 128:(X + 1) * 128], op=ALU.mult),
                 reads=[self.dps[P1[X]], self.dcst], writes=[dR])

        def stage1(X, L1, dL1, entry):
            c.op("pe", lambda e: e.matmul(self.ps[P1[X]][0:40, 0:128], L1[:, entry, 0:40], Sbf[:, X * 128:(X + 1) * 128], start=True, stop=True),
                 reads=[dL1, dSbf[X]], writes=[self.dps[P1[X]]])

        def run_chain(t_first, n, first_start, blk_of):
            Rcur = [None, None]
            for idx in range(n + 1):
                t = t_first + idx
                slot = idx % NR
                q = idx // NR
                for X in range(2):
                    if slot == 0:
                        if Rcur[X] is not None:
                            pb_ = t_first + (q - 1) * NR
                            s0_ = 1 if q - 1 == 0 else 0
                            extract_o(X, Rcur[X][0], Rcur[X][1], pb_ + s0_ - 1, s0_, NR - s0_)
                        if idx < n:
                            Rcur[X] = fill_R(X, t, min(NR, n - idx))
                        else:
                            Rcur[X] = Rp[X].next()
                    R, dR = Rcur[X]
                    mask_op(X, R, dR, slot)
                    if idx == n:
                        base = t_first + q * NR
                        s0_ = 1 if q == 0 else 0
                        if slot + 1 - s0_ > 0:
                            extract_o(X, R, dR, base + s0_ - 1, s0_, slot + 1 - s0_)
                if idx == n:
                    break
                tl = t % 128
                if tl == 0 and self._cur_tile != t // 128:
                    self._cur_L = build_tile(t // 128)
                    self._cur_tile = t // 128
                L1, dL1, L2, dL2 = self._cur_L
                bend = blk_of(t)
                for X in range(2):
                    R, dR = Rcur[X]
                    acc = self.ps[ACC[X]]
                    c.op("pe", lambda e, acc=acc, R=R: e.matmul(acc[:, 0:128], L2[0:72, tl, :], R[0:72, slot, :], start=(first_start and idx == 0), stop=True, skip_group_check=True),
                         reads=[dL2, dR], writes=[self.dps[ACC[X]]])
                for X in range(2):
                    acc = self.ps[ACC[X]]
                    if bend is not None:
                        eb = EB[:, 2 * X:2 * X + 2, bend:bend + 1].broadcast_to([128, 2, 64])
                        c.op("dve", lambda e, acc=acc, eb=eb: e.tensor_tensor(out=acc[:, 0:128].rearrange("p (c v) -> p c v", c=2), in0=acc[:, 0:128].rearrange("p (c v) -> p c v", c=2), in1=eb, op=ALU.mult),
                             reads=[self.dps[ACC[X]], dEB], writes=[self.dps[ACC[X]]])
                    c.op("act", lambda e, acc=acc, X=X: e.copy(out=Sbf[:, X * 128:(X + 1) * 128], in_=acc[:, 0:128]), reads=[self.dps[ACC[X]]], writes=[dSbf[X]])
                for X in range(2):
                    stage1(X, L1, dL1, tl + 1)

        self._cur_L = None
        self._cur_tile = -1
        c.op("dve", lambda e: e.memset(Sbf[:], 0.0), writes=dSbf)
        self._cur_L = build_tile(0)
        self._cur_tile = 0
        for X in range(2):
            stage1(X, self._cur_L[0], self._cur_L[1], 0)
        run_chain(0, SEQ, True, lambda t: (t // LB) if (t % LB == LB - 1) else None)
        self._rw_state_out(ACC, sfin, sout, self.O["p_wkv"][l])
        for b in range(NB_S):
            raw, draw = s0raw.next()
            c.dma("sp", raw[:].rearrange("v (h k) -> v h k", h=8), self.I["st_wkv"][l, b].rearrange("h v k -> v h k"), writes=[draw])
            pi = self.pb()
            for cc in range(4):
                c.op("pe", lambda e, cc=cc, pi=pi: e.transpose(self.ps[pi][:, cc * 64:(cc + 1) * 64], raw[:, cc * 128:(cc + 1) * 128], self.cst("ident")[0:64, 0:64]), reads=[draw, self.dcst], writes=[self.dps[pi]])
            s0, ds0 = s0st.next()
            c.op("dve", lambda e, s0=s0, pi=pi: e.tensor_copy(out=s0[:], in_=self.ps[pi][:, 0:256]), reads=[self.dps[pi]], writes=[ds0])
            for X in range(2):
                c.op("pe", lambda e, s0=s0, X=X: e.matmul(self.ps[ACC[X]][:, 0:128], self.cst("ident"), s0[:, X * 128:(X + 1) * 128], start=True, stop=True, skip_group_check=True),
                     reads=[ds0, self.dcst], writes=[self.dps[ACC[X]]])
                c.op("act", lambda e, s0=s0, X=X: e.copy(out=Sbf[:, X * 128:(X + 1) * 128], in_=s0[:, X * 128:(X + 1) * 128]), reads=[ds0], writes=[dSbf[X]])
            t0 = SEQ + b * TS
            if self._cur_tile != 16:
                self._cur_L = build_tile(16)
                self._cur_tile = 16
            for X in range(2):
                stage1(X, self._cur_L[0], self._cur_L[1], (t0 % 128))
            run_chain(t0, TS, False, lambda t: (SEQ // LB + (t - SEQ) // TS) if ((t - SEQ) % TS == TS - 1) else None)
            self._rw_state_out(ACC, sfin, sout, self.O["s_wkv"][l, b])
        self.pb = old_pb


def _rw_state_out(self, ACC, sfin, sout, out_ap):
    c = self.c
    sf, dsf = sfin.next()
    for X in range(2):
        c.op("dve", lambda e, X=X: e.tensor_copy(out=sf[:, X * 128:(X + 1) * 128], in_=self.ps[ACC[X]][:, 0:128]), reads=[self.dps[ACC[X]]], writes=[dsf])
    pi = self.pb()
    for cc in range(4):
        c.op("pe", lambda e, cc=cc: e.transpose(self.ps[pi][0:64, cc * 128:(cc + 1) * 128], sf[:, cc * 64:(cc + 1) * 64], self.cst("ident")), reads=[dsf, self.dcst], writes=[self.dps[pi]])
    so, dso = sout.next()
    c.op("act", lambda e: e.copy(out=so[:], in_=self.ps[pi][0:64, 0:512]), reads=[self.dps[pi]], writes=[dso])
    do = self.odep()
    c.dma("sp", out_ap.rearrange("h v k -> v h k"), so[:].rearrange("v (h k) -> v h k", h=8), reads=[dso], writes=[do])


def _rwkv_post(self, l, RK, dRK, sgd, dsgd, gup, dgup, lng, dlng, lnb, dlnb):
    c = self.c
    with self.scope():
        ob = self.rot("rwo", 2, [128, 512], BF16)
        vb = self.rot("rwv", 2, [128, 512], F32)
        on = self.sbt("rwon", [128, 512], F32); don = Dep()
        tmp = self.sbt("rwtmp", [128, 512], F32); dtmp = Dep()
        sm = self.sbt("rwsm", [128, 48], F32); dsm = Dep()
        for tt in range(NT):
            t0 = tt * 128
            o, do_ = ob.next()
            c.dma("sp", o[:], self.ORW[t0:t0 + 128, :], reads=[self.dORW], writes=[do_])
            v, dv = vb.next()
            c.dma("sp", v[:], self.VV[t0:t0 + 128, :], reads=[self.dRWS], writes=[dv])
            o3 = o[:].rearrange("p (h v) -> p h v", h=8)
            c.op("dve", lambda e: e.tensor_reduce(out=sm[:, 0:8], in_=o3, axis=AX.X, op=ALU.add), reads=[do_], writes=[dsm])
            c.op("dve", lambda e: e.tensor_tensor(out=tmp[:], in0=o[:], in1=o[:], op=ALU.mult), reads=[do_], writes=[dtmp])
            c.op("dve", lambda e: e.tensor_reduce(out=sm[:, 8:16], in_=tmp[:].rearrange("p (h v) -> p h v", h=8), axis=AX.X, op=ALU.add), reads=[dtmp], writes=[dsm])
            c.op("dve", lambda e: e.tensor_scalar(out=sm[:, 0:16], in0=sm[:, 0:16], scalar1=1.0 / 64, scalar2=None, op0=ALU.mult), reads=[dsm], writes=[dsm])
            c.op("dve", lambda e: e.tensor_tensor(out=sm[:, 16:24], in0=sm[:, 0:8], in1=sm[:, 0:8], op=ALU.mult), reads=[dsm], writes=[dsm])
            c.op("dve", lambda e: e.tensor_tensor(out=sm[:, 24:32], in0=sm[:, 8:16], in1=sm[:, 16:24], op=ALU.subtract), reads=[dsm], writes=[dsm])
            c.op("act", lambda e: e.activation(out=sm[:, 32:40], in_=sm[:, 24:32], func=AF.Sqrt, bias=self.epsb(float(RW_LN_EPS)), scale=1.0), reads=[dsm, self.dcst], writes=[dsm])
            c.op("dve", lambda e: e.reciprocal(out=sm[:, 40:48], in_=sm[:, 32:40]), reads=[dsm], writes=[dsm])
            on3 = on[:].rearrange("p (h v) -> p h v", h=8)
            c.op("dve", lambda e: e.tensor_tensor(out=on3, in0=o3, in1=sm[:, 0:8].unsqueeze(2).broadcast_to([128, 8, 64]), op=ALU.subtract), reads=[do_, dsm, self.dOS[tt]], writes=[don])
            c.op("dve", lambda e: e.tensor_tensor(out=on3, in0=on3, in1=sm[:, 40:48].unsqueeze(2).broadcast_to([128, 8, 64]), op=ALU.mult), reads=[don, dsm], writes=[don])
            c.op("pool", lambda e: e.tensor_tensor(out=on[:], in0=on[:], in1=lng[:], op=ALU.mult), reads=[don, dlng], writes=[don])
            c.op("pool", lambda e: e.tensor_tensor(out=on[:], in0=on[:], in1=lnb[:], op=ALU.add), reads=[don, dlnb], writes=[don])
            c.op("dve", lambda e: e.tensor_tensor(out=tmp[:].rearrange("p (h v) -> p h v", h=8), in0=v[:].rearrange("p (h v) -> p h v", h=8), in1=RK[:, tt, :].unsqueeze(2).broadcast_to([128, 8, 64]), op=ALU.mult),
                 reads=[dv, dRK, dtmp], writes=[dtmp])
            c.op("dve", lambda e: e.tensor_tensor(out=on[:], in0=on[:], in1=tmp[:], op=ALU.add), reads=[don, dtmp], writes=[don])
            pi = self.pb()
            c.op("pe", lambda e, pi=pi: e.matmul(self.ps[pi][:, 0:512], sgd[:, t0:t0 + 128], gup[:], start=True, stop=True), reads=[dsgd, dgup], writes=[self.dps[pi]])
            c.op("dve", lambda e, pi=pi: e.tensor_tensor(out=on[:], in0=on[:], in1=self.ps[pi][:, 0:512], op=ALU.mult), reads=[don, self.dps[pi]], writes=[don])
            c.dma("sp", self.OS[t0:t0 + 128, 0:512], on[:], reads=[don], writes=[self.dOS[tt]])


Prog.rwkv_scan = _rwkv_scan
Prog._rw_state_out = _rw_state_out
Prog.rwkv_post = _rwkv_post


def _ln_phase(self, l, gname, bname, final):
    c = self.c
    with self.scope():
        gt, dgt = self.bcast_row("lng", self.I[gname][l:l + 1, :], D)
        bt, dbt = self.bcast_row("lnb", self.I[bname][l:l + 1, :], D)
        xa_p = self.rot("lnxa", 2, [128, D], F32)
        xb_p = self.rot("lnxb", 2, [128, D], F32)
        xh_p = self.rot("lnxh", 2, [128, D], BF16)
        st = self.sbt("lnst", [128, 32], F32); dst_ = Dep()
        for tt in range(NT):
            rows = slice(tt * 128, (tt + 1) * 128)
            xa, dxa = xa_p.next()
            xb, dxb = xb_p.next()
            if self.res_from_input:
                src = self.I["xp"][rows, :] if tt < 16 else self.I["xs"]
                c.dma("sp", xa[:], src, writes=[dxa])
            else:
                c.dma("sp", xa[:], self.XRES[rows, :], reads=[self.dXRES[tt]], writes=[dxa])
            c.dma("sp", xb[:], self.MIX[rows, :], reads=[self.dMIX[tt]], writes=[dxb])
            c.op("dve", lambda e: e.scalar_tensor_tensor(out=xa[:], in0=xa[:], scalar=float(DN_ALPHA), in1=xb[:], op0=ALU.mult, op1=ALU.add), reads=[dxa, dxb], writes=[dxa])
            for i in range(4):
                c.op("dve", lambda e, i=i: e.bn_stats(out=st[:, i * 6:(i + 1) * 6], in_=xa[:, i * 512:(i + 1) * 512]), reads=[dxa], writes=[dst_])
            c.op("dve", lambda e: e.bn_aggr(out=st[:, 24:26], in_=st[:, 0:24]), reads=[dst_], writes=[dst_])
            c.op("act", lambda e: e.activation(out=st[:, 26:27], in_=st[:, 25:26], func=AF.Sqrt, bias=self.epsb(1e-5), scale=1.0), reads=[dst_, self.dcst], writes=[dst_])
            c.op("dve", lambda e: e.reciprocal(out=st[:, 27:28], in_=st[:, 26:27]), reads=[dst_], writes=[dst_])
            c.op("dve", lambda e: e.tensor_scalar(out=st[:, 28:29], in0=st[:, 24:25], scalar1=st[:, 27:28], scalar2=-1.0, op0=ALU.mult, op1=ALU.mult), reads=[dst_], writes=[dst_])
            c.op("act", lambda e: e.activation(out=xb[:], in_=xa[:], func=AF.Identity, scale=st[:, 27:28], bias=st[:, 28:29]), reads=[dxa, dst_, dxb], writes=[dxb])
            c.op("pool", lambda e: e.tensor_tensor(out=xb[:], in0=xb[:], in1=gt[:], op=ALU.mult), reads=[dxb, dgt], writes=[dxb])
            c.op("pool", lambda e: e.tensor_tensor(out=xb[:], in0=xb[:], in1=bt[:], op=ALU.add), reads=[dxb, dbt], writes=[dxb])
            if final:
                do = self.odep()
                dst = self.O["yp"][rows, :] if tt < 16 else self.O["ys"]
                c.dma("sp", dst, xb[:], reads=[dxb], writes=[do])
            else:
                c.dma("sp", self.XRES[rows, :], xb[:], reads=[dxb], writes=[self.dXRES[tt]])
                xh, dxh = xh_p.next()
                c.op("act", lambda e: e.copy(out=xh[:], in_=xb[:]), reads=[dxb], writes=[dxh])
                self.to_actT(xh, dxh, tt)
    self.res_from_input = False


def _proj_to_mix(self, w_ap):
    c = self.c
    with self.scope():
        wpool = self.rot("wpm", 3, [128, KC, 512], BF16)
        stg = self.rot("stgm", 4, [128, 512], F32)
        self.tog = 0

        def consume(bi, n0, ncols, tt, pi):
            buf, d = stg.next()
            self.evac(pi, 128, ncols, buf[:, 0:ncols], d, self.tog)
            self.tog += 1
            c.dma("sp", self.MIX[tt * 128:(tt + 1) * 128, n0:n0 + ncols], buf[:, 0:ncols], reads=[d], writes=[self.dMIX[tt]])
        blocks = [(i * 512, 512) for i in range(4)]
        self.proj_T(w_ap, blocks, KC, lambda bi: list(range(NT)), self.act_lhs, self.act_deps, consume, wpool)


def _out_ln1(self, l):
    c = self.c
    with self.scope():
        xb = self.rot("osb", 3, [128, D], BF16)
        for tt in range(NT):
            buf, d = xb.next()
            c.dma("pool", buf[:], self.OS[tt * 128:(tt + 1) * 128, :], reads=[self.dOS[tt]], writes=[d])
            self.to_actT(buf, d, tt)
    self.proj_to_mix(self.I["w_out"][l])
    self.ln_phase(l, "ln1_g", "ln1_b", False)


def _attn(self, l):
    c = self.c
    scale = 512 ** -0.5
    with self.scope():
        KT = self.sbt("xaKT", [128, KC, N_MEM], BF16); dKT = Dep()
        Vb = self.sbt("xaVb", [128, 2, D], BF16); dVb = Dep()
        with self.scope():
            mem = self.sbt("xamem", [128, 2, D], BF16); dmem = Dep()
            c.dma("pool", mem[:], self.I["memp"].rearrange("(c p) d -> p c d", p=128), writes=[dmem])
            memT = self.sbt("xamemT", [128, KC, N_MEM], BF16); dmemT = Dep()
            for g in range(4):
                pi = self.pb()
                psb = self.ps[pi][:].bitcast(BF16)
                for j in range(4):
                    kc = g * 4 + j
                    for mc in range(2):
                        c.op("pe", lambda e, j=j, mc=mc, kc=kc, psb=psb: e.transpose(psb[:, (j * 2 + mc) * 128:(j * 2 + mc + 1) * 128], mem[:, mc, kc * 128:(kc + 1) * 128], self.ident_bf[:]),
                             reads=[dmem, self.dcst], writes=[self.dps[pi]])
                c.op("dve" if g % 2 else "act", (lambda e, g=g, psb=psb: e.tensor_copy(out=memT[:, g * 4:(g + 1) * 4, :].rearrange("p k m -> p (k m)"), in_=psb[:, 0:1024])) if g % 2 else
                     (lambda e, g=g, psb=psb: e.copy(out=memT[:, g * 4:(g + 1) * 4, :].rearrange("p k m -> p (k m)"), in_=psb[:, 0:1024])), reads=[self.dps[pi]], writes=[dmemT])
            wpool = self.rot("xawkv", 3, [128, KC, 512], BF16)
            stg = self.rot("xastg", 4, [128, 512], F32)
            for (wname, oname) in (("xa_wk", "p_mk"), ("xa_wv", "p_mv")):
                w = self.I[wname][l]
                for nb in range(4):
                    wb, wd = self.load_w(wpool, w, nb * 512, 512, KC)
                    for mt in range(2):
                        pi = self.pb()
                        for kc in range(KC):
                            c.op("pe", lambda e, kc=kc, pi=pi, mt=mt, wb=wb: e.matmul(self.ps[pi][:, 0:512], memT[:, kc, mt * 128:(mt + 1) * 128], wb[:, kc, :], start=(kc == 0), stop=(kc == KC - 1)),
                                 reads=[dmemT, wd], writes=[self.dps[pi]])
                        buf, d = stg.next()
                        c.op("act", lambda e, buf=buf, pi=pi: e.copy(out=buf[:], in_=self.ps[pi][:, 0:512]), reads=[self.dps[pi]], writes=[d])
                        do = self.odep()
                        c.dma("sp", self.O[oname][l, mt * 128:(mt + 1) * 128, nb * 512:(nb + 1) * 512], buf[:], reads=[d], writes=[do])
                        if wname == "xa_wv":
                            c.op("dve", lambda e, buf=buf, mt=mt, nb=nb: e.tensor_copy(out=Vb[:, mt, nb * 512:(nb + 1) * 512], in_=buf[:]), reads=[d], writes=[dVb])
                    if wname == "xa_wk":
                        for j in range(4):
                            pi = self.pb()
                            for kc in range(KC):
                                c.op("pe", lambda e, kc=kc, pi=pi, j=j, wb=wb: e.matmul(self.ps[pi][:, 0:N_MEM], wb[:, kc, j * 128:(j + 1) * 128], memT[:, kc, :], start=(kc == 0), stop=(kc == KC - 1)),
                                     reads=[dmemT, wd], writes=[self.dps[pi]])
                            c.op("dve", lambda e, pi=pi, j=j, nb=nb: e.tensor_copy(out=KT[:, nb * 4 + j, :], in_=self.ps[pi][:, 0:N_MEM]), reads=[self.dps[pi]], writes=[dKT])
        if KATT < 2:
            return
        qT = self.sbt("xaqT", [128, KC, TOK], BF16)
        dqT = [[Dep() for _ in range(4)] for _ in range(NT)]
        with self.scope():
            wpool = self.rot("xawq", 2, [128, KC, 512], BF16)
            self.tog = 0

            def consume_q(col0, cw, t0, tw, pi):
                ch = col0 // 128
                deps = [dqT[tt][ch // 4] for tt in range(t0 // 128, (t0 + tw) // 128)]
                dst = qT[:, ch, t0:t0 + tw]
                src = self.ps[pi][:, 0:tw]
                if self.tog % 2 == 0:
                    c.op("act", lambda e: e.copy(out=dst, in_=src), reads=[self.dps[pi]], writes=deps)
                else:
                    c.op("dve", lambda e: e.tensor_copy(out=dst, in_=src), reads=[self.dps[pi]], writes=deps)
                self.tog += 1
            self.proj_F(self.I["xa_wq"][l], [(i * 512, 512) for i in range(4)], KC, consume_q, wpool)
        if KATT < 3:
            return
        with self.scope():
            ef = self.rot("xae", 2, [128, N_MEM], F32)
            scp = self.rot("xasc", 2, [128, N_MEM], F32)
            prp = self.rot("xapr", 2, [128, N_MEM], BF16)
            prTp = self.rot("xaprT", 2, [128, 2, 128], BF16)
            sm = self.sbt("xasm", [128, 16], F32); dsm = Dep()

            def softmax(pi, smc):
                sc_, dsc_ = scp.next()
                c.op("act", lambda e: e.copy(out=sc_[:], in_=self.ps[pi][:, 0:N_MEM]), reads=[self.dps[pi]], writes=[dsc_])
                c.op("dve", lambda e: e.tensor_reduce(out=sm[:, smc:smc + 1], in_=sc_[:], axis=AX.X, op=ALU.max), reads=[dsc_], writes=[dsm])
                c.op("dve", lambda e: e.tensor_scalar(out=sm[:, smc + 1:smc + 2], in0=sm[:, smc:smc + 1], scalar1=-scale, scalar2=None, op0=ALU.mult), reads=[dsm], writes=[dsm])
                e_, de_ = ef.next()
                c.op("act", lambda e: e.activation(out=e_[:], in_=sc_[:], func=AF.Exp, scale=scale, bias=sm[:, smc + 1:smc + 2]),
                     reads=[dsc_, dsm], writes=[de_])
                c.op("dve", lambda e: e.tensor_reduce(out=sm[:, smc + 2:smc + 3], in_=e_[:], axis=AX.X, op=ALU.add), reads=[de_], writes=[dsm])
                c.op("dve", lambda e: e.reciprocal(out=sm[:, smc + 3:smc + 4], in_=sm[:, smc + 2:smc + 3]), reads=[dsm], writes=[dsm])
                pr, dpr = prp.next()
                c.op("dve", lambda e: e.tensor_scalar(out=pr[:], in0=e_[:], scalar1=sm[:, smc + 3:smc + 4], scalar2=None, op0=ALU.mult), reads=[de_, dsm], writes=[dpr])
                return pr, dpr

            def transpose_pr(pr, dpr, prT, dprT):
                pi = self.pb()
                psb = self.ps[pi][:].bitcast(BF16)
                for mc in range(2):
                    c.op("pe", lambda e, mc=mc: e.transpose(psb[:, mc * 128:(mc + 1) * 128], pr[:, mc * 128:(mc + 1) * 128], self.ident_bf[:]), reads=[dpr, self.dcst], writes=[self.dps[pi]])
                c.op("act", lambda e: e.copy(out=prT[:].rearrange("p c t -> p (c t)"), in_=psb[:, 0:256]), reads=[self.dps[pi]], writes=[dprT])

            for tt in range(16):
                t0 = tt * 128
                for h in range(4):
                    pi = self.pb()
                    for dc in range(4):
                        c.op("pe", lambda e, dc=dc, pi=pi: e.matmul(self.ps[pi][:, 0:N_MEM], qT[:, 4 * h + dc, t0:t0 + 128], KT[:, 4 * h + dc, :], start=(dc == 0), stop=(dc == 3)),
                             reads=[dqT[tt][h], dKT], writes=[self.dps[pi]])
                    pr, dpr = softmax(pi, (h % 2) * 4)
                    prT, dprT = prTp.next()
                    transpose_pr(pr, dpr, prT, dprT)
                    p2 = self.pb()
                    for dc in range(4):
                        for mc in range(2):
                            c.op("pe", lambda e, dc=dc, mc=mc, p2=p2: e.matmul(self.ps[p2][:, dc * 128:(dc + 1) * 128], Vb[:, mc, (4 * h + dc) * 128:(4 * h + dc + 1) * 128], prT[:, mc, :], start=(mc == 0), stop=(mc == 1)),
                                 reads=[dVb, dprT], writes=[self.dps[p2]])
                    c.op("dve", lambda e, p2=p2: e.tensor_copy(out=self.actT[:, 4 * h:4 * h + 4, t0:t0 + 128], in_=self.ps[p2][:, 0:512].rearrange("p (k t) -> p k t", k=4)),
                         reads=[self.dps[p2]], writes=[self.dact[tt][h]])
            if KATT < 4:
                return
            t0 = 16 * 128
            old_pb = self.pb
            self.ps_i = 0

            def pb4():
                i = self.ps_i % 4
                self.ps_i += 1
                return i
            self.pb = pb4
            SB = [4, 5, 6, 7]
            kvb = self.rot("xakv", 1, [128, 2, D], BF16)
            KTb = self.rot("xaKTb", 1, [128, KC, N_MEM], BF16)
            qmb = self.rot("xaqm", 1, [128, KC, 128], BF16)
            prS = self.sbt("xaprS", [128, 4, N_MEM], BF16); dprS = Dep()
            prTS = self.sbt("xaprTS", [128, 4, 2, 128], BF16); dprTS = Dep()
            for b in range(NB_S):
                kb, dkb = kvb.next()
                c.dma("pool", kb[:], self.I["ck"][l, b].rearrange("(c p) d -> p c d", p=128), writes=[dkb])
                ktb, dktb = KTb.next()
                for g in range(4):
                    pi = self.pb()
                    psb = self.ps[pi][:].bitcast(BF16)
                    for j in range(4):
                        kc = g * 4 + j
                        for mc in range(2):
                            c.op("pe", lambda e, j=j, mc=mc, kc=kc, psb=psb: e.transpose(psb[:, (j * 2 + mc) * 128:(j * 2 + mc + 1) * 128], kb[:, mc, kc * 128:(kc + 1) * 128], self.ident_bf[:]),
                                 reads=[dkb, self.dcst], writes=[self.dps[pi]])
                    c.op("dve" if g % 2 else "act", (lambda e, g=g, psb=psb: e.tensor_copy(out=ktb[:, g * 4:(g + 1) * 4, :].rearrange("p k m -> p (k m)"), in_=psb[:, 0:1024])) if g % 2 else
                         (lambda e, g=g, psb=psb: e.copy(out=ktb[:, g * 4:(g + 1) * 4, :].rearrange("p k m -> p (k m)"), in_=psb[:, 0:1024])), reads=[self.dps[pi]], writes=[dktb])
                qm, dqm = qmb.next()
                mrb = self.maskrow[:, b, :].unsqueeze(1).broadcast_to([128, KC, 128])
                c.op("pool", lambda e: e.tensor_tensor(out=qm[:], in0=qT[:, :, t0:t0 + 128], in1=mrb, op=ALU.mult), reads=dqT[16] + [self.dcst], writes=[dqm])
                for h in range(4):
                    for dc in range(4):
                        c.op("pe", lambda e, h=h, dc=dc: e.matmul(self.ps[SB[h]][:, 0:N_MEM], qm[:, 4 * h + dc, :], ktb[:, 4 * h + dc, :], start=(b == 0 and dc == 0), stop=(b == NB_S - 1 and dc == 3), skip_group_check=True),
                             reads=[dqm, dktb], writes=[self.dps[SB[h]]])
            for h in range(4):
                pr, dpr = softmax(SB[h], (h % 2) * 4)
                c.op("pool", lambda e, h=h, pr=pr: e.tensor_copy(out=prS[:, h, :], in_=pr[:]), reads=[dpr], writes=[dprS])
            for h in range(4):
                pi = self.pb()
                psb = self.ps[pi][:].bitcast(BF16)
                for mc in range(2):
                    c.op("pe", lambda e, mc=mc, h=h: e.transpose(psb[:, mc * 128:(mc + 1) * 128], prS[:, h, mc * 128:(mc + 1) * 128], self.ident_bf[:]), reads=[dprS, self.dcst], writes=[self.dps[pi]])
                c.op("act", lambda e, h=h: e.copy(out=prTS[:, h, :, :].rearrange("p c t -> p (c t)"), in_=psb[:, 0:256]), reads=[self.dps[pi]], writes=[dprTS])
            zrow = self.sbt("xazrow", [1, 512], BF16); dzrow = Dep()
            c.op("dve", lambda e: e.memset(zrow[:], 0.0), writes=[dzrow])
            for h in range(4):
                c.op("pe", lambda e, h=h: e.matmul(self.ps[SB[h]][:, 0:512], zrow[0:1, 0:128], zrow[0:1, 0:512], start=True, stop=False, skip_group_check=True), reads=[dzrow], writes=[self.dps[SB[h]]])
            prm = self.rot("xaprm", 2, [128, 4, 2, 128], BF16)
            for b in range(NB_S):
                vb_, dvb_ = kvb.next()
                c.dma("pool", vb_[:], self.I["cv"][l, b].rearrange("(c p) d -> p c d", p=128), writes=[dvb_])
                pm, dpm = prm.next()
                mrb = self.maskrow[:, b, :].unsqueeze(1).broadcast_to([128, 8, 128])
                c.op("pool", lambda e: e.tensor_tensor(out=pm[:].rearrange("p h c t -> p (h c) t"), in0=prTS[:].rearrange("p h c t -> p (h c) t"), in1=mrb, op=ALU.mult), reads=[dprTS, self.dcst], writes=[dpm])
                for ch in range(KC):
                    h = ch // 4
                    for mc in range(2):
                        c.op("pe", lambda e, ch=ch, mc=mc, h=h: e.matmul(self.ps[SB[h]][:, (ch % 4) * 128:(ch % 4 + 1) * 128], vb_[:, mc, ch * 128:(ch + 1) * 128], pm[:, h, mc, :],
                                                                   start=False, stop=(b == NB_S - 1 and mc == 1), skip_group_check=True), reads=[dvb_, dpm], writes=[self.dps[SB[h]]])
            for h in range(4):
                c.op("dve" if h % 2 else "act", (lambda e, h=h: e.tensor_copy(out=self.actT[:, 4 * h:4 * h + 4, t0:t0 + 128], in_=self.ps[SB[h]][:, 0:512].rearrange("p (k t) -> p k t", k=4))) if h % 2 else
                     (lambda e, h=h: e.copy(out=self.actT[:, 4 * h:4 * h + 4, t0:t0 + 128], in_=self.ps[SB[h]][:, 0:512].rearrange("p (k t) -> p k t", k=4))),
                     reads=[self.dps[SB[h]]], writes=[self.dact[16][h]])
            self.pb = old_pb
    if KATT < 5:
        return
    self.proj_to_mix(self.I["xa_wo"][l])
    if KATT < 6:
        return
    self.ln_phase(l, "ln2_g", "ln2_b", False)


def _ffn(self, l):
    c = self.c
    with self.scope():
        wpool = self.rot("ffw", 4, [128, KC, 512], BF16)
        sgp = self.rot("ffsg", 2, [128, 512], F32)
        hbp = self.rot("ffhb", 3, [128, 512], BF16)
        wg = self.I["ffn_w_gate"][l]
        wu = self.I["ffn_w_up"][l]
        nxt = (self.load_w(wpool, wg, 0, 512, KC), self.load_w(wpool, wu, 0, 512, KC))
        for nb in range(D_FF // 512):
            (gb, gd), (ub, ud) = nxt
            if nb + 1 < D_FF // 512:
                nxt = (self.load_w(wpool, wg, (nb + 1) * 512, 512, KC), self.load_w(wpool, wu, (nb + 1) * 512, 512, KC))
            for j in range(4):
                for (t0, tw) in TBLK:
                    rd = [d for tt in range(t0 // 128, (t0 + tw) // 128) for d in self.dact[tt]]
                    pg = self.pb()
                    for kc in range(KC):
                        c.op("pe", lambda e, kc=kc, pg=pg: e.matmul(self.ps[pg][:, 0:tw], gb[:, kc, j * 128:(j + 1) * 128], self.actT[:, kc, t0:t0 + tw], start=(kc == 0), stop=(kc == KC - 1)),
                             reads=[gd] + rd, writes=[self.dps[pg]])
                    pu = self.pb()
                    for kc in range(KC):
                        c.op("pe", lambda e, kc=kc, pu=pu: e.matmul(self.ps[pu][:, 0:tw], ub[:, kc, j * 128:(j + 1) * 128], self.actT[:, kc, t0:t0 + tw], start=(kc == 0), stop=(kc == KC - 1)),
                             reads=[ud] + rd, writes=[self.dps[pu]])
                    sg, dsg = sgp.next()
                    c.op("act", lambda e: e.activation(out=sg[:, 0:tw], in_=self.ps[pg][:, 0:tw], func=AF.Silu), reads=[self.dps[pg]], writes=[dsg])
                    hb, dhb = hbp.next()
                    c.op("dve", lambda e: e.tensor_tensor(out=hb[:, 0:tw], in0=sg[:, 0:tw], in1=self.ps[pu][:, 0:tw], op=ALU.mult), reads=[dsg, self.dps[pu]], writes=[dhb])
                    c.dma("sp", self.HT[nb * 4 + j][:, t0:t0 + tw], hb[:, 0:tw], reads=[dhb], writes=[self.dHT])
    with self.scope():
        wpool = self.rot("ffwd", 2, [128, FC, 256], BF16)
        hTp = self.rot("ffhT", 2, [128, FC, 256], BF16)
        stg = self.rot("ffstg", 4, [128, 256], F32)
        wd_ = self.I["ffn_w_down"][l]
        groups = [(i * 256, 256) for i in range(8)] + [(2048, 128)]
        self.tog = 0
        nxt = self.load_w(wpool, wd_, 0, 256, FC)
        for nb in range(D // 256):
            wb, wdp = nxt
            if nb + 1 < D // 256:
                nxt = self.load_w(wpool, wd_, (nb + 1) * 256, 256, FC)
            for (g0, gw) in groups:
                hT, dhT = hTp.next()
                c.dma("sp", hT[:, :, 0:gw], self.HT[:, :, g0:g0 + gw].rearrange("c p t -> p c t"), reads=[self.dHT], writes=[dhT])
                for tl in range(gw // 128):
                    tt = g0 // 128 + tl
                    pi = self.pb()
                    for kc in range(FC):
                        c.op("pe", lambda e, kc=kc, pi=pi, tl=tl: e.matmul(self.ps[pi][:, 0:256], hT[:, kc, tl * 128:(tl + 1) * 128], wb[:, kc, :], start=(kc == 0), stop=(kc == FC - 1)),
                             reads=[dhT, wdp], writes=[self.dps[pi]])
                    buf, d = stg.next()
                    self.evac(pi, 128, 256, buf[:], d, self.tog)
                    self.tog += 1
                    c.dma("sp", self.MIX[tt * 128:(tt + 1) * 128, nb * 256:(nb + 1) * 256], buf[:], reads=[d], writes=[self.dMIX[tt]])
    self.ln_phase(l, "ln3_g", "ln3_b", l == DEPTH - 1)


Prog.ln_phase = _ln_phase
Prog.proj_to_mix = _proj_to_mix
Prog.out_ln1 = _out_ln1
Prog.attn = _attn
Prog.ffn = _ffn
```

```python
import os
import math
import numpy as np
from contextlib import ExitStack, contextmanager
import concourse.bass as bass
import concourse.mybir as mybir
from concourse.bass_utils import run_bass_kernel_spmd

F32 = mybir.dt.float32
BF16 = mybir.dt.bfloat16
AF = mybir.ActivationFunctionType
ALU = mybir.AluOpType
AX = mybir.AxisListType

D = 2048
SEQ = 2048
DEPTH = 2
NB_S = 16
TS = 8
NT = 17
TOK = NT * 128
KC = D // 128
GROUP = 512
RW_IN = 1792
MB_IN = 1544
GLA_IN = 1552
RET_IN = 2048
N_IN = 6936
C_RW = 0
C_MB = RW_IN
C_GLA = RW_IN + MB_IN
C_RET = C_GLA + GLA_IN
D_FF = 5632
FC = D_FF // 128
N_MEM = 256
DN_ALPHA = (2 * DEPTH) ** 0.25
PAST_LEN = 16384
RW_LN_EPS = 64e-5
LB = 32
NBLK = SEQ // LB + NB_S
TBLK = [(i * 512, 512) for i in range(4)] + [(2048, 128)]
PF_RW = 0
PF_XBC = 1792
PF_GQ = 2816
PF_GK = 3072
PF_GKD = 3328
PF_ROWS = 3344

STAGE = int(os.environ.get("KSTAGE", "99"))
NL = int(os.environ.get("KLAYERS", "2"))
KATT = int(os.environ.get("KATT", "9"))


class Dep:
    __slots__ = ("writer", "readers")

    def __init__(self):
        self.writer = None
        self.readers = {}


class Ctx:
    COMPUTE = ("pe", "act", "dve", "pool")

    def __init__(self, nc, stack):
        self.nc = nc
        self.eng = {"pe": nc.tensor, "act": nc.scalar, "dve": nc.vector, "pool": nc.gpsimd, "sp": nc.sync}
        self.sems = {}
        self.cnt = {}
        for e in self.COMPUTE:
            self.sems[e] = stack.enter_context(nc.semaphore("c_" + e))
            self.cnt[e] = 0
        self.ring = {}
        self.ring_i = {}
        for e, n in (("sp", 48), ("pool", 40), ("act", 8)):
            self.ring[e] = [stack.enter_context(nc.semaphore("d_%s_%d" % (e, i))) for i in range(n)]
            self.ring_i[e] = 0
        self.waited = {}
        self.semobj = {}
        for e in self.COMPUTE:
            self.semobj[("c", e)] = self.sems[e]
        for e in self.ring:
            for i, s in enumerate(self.ring[e]):
                self.semobj[("d", e, i)] = s
        self.n_inst = 0
        self.n_wait = 0

    def wait(self, eng, tok, force=False):
        key, val = tok
        if key[0] == "c" and key[1] == eng and not force:
            if eng == "pe":
                return
        w = self.waited.get((eng, key), 0)
        if w >= val:
            return
        self.eng[eng].wait_ge(self.semobj[key], val)
        self.waited[(eng, key)] = val
        self.n_wait += 1

    def _deps(self, eng, reads, writes, force=False):
        toks = {}

        def add(t):
            if t is None:
                return
            k, v = t
            if toks.get(k, 0) < v:
                toks[k] = v
        for d in reads:
            add(d.writer)
        for d in writes:
            add(d.writer)
            for k, v in d.readers.items():
                add((k, v))
        for k, v in toks.items():
            self.wait(eng, (k, v), force)

    def _update(self, tok, reads, writes):
        k, v = tok
        for d in reads:
            if d.readers.get(k, 0) < v:
                d.readers[k] = v
        for d in writes:
            d.writer = tok
            d.readers = {}

    def op(self, eng, fn, reads=(), writes=()):
        self._deps(eng, reads, writes)
        ins = fn(self.eng[eng])
        self.cnt[eng] += 1
        ins.then_inc(self.sems[eng], 1)
        tok = (("c", eng), self.cnt[eng])
        self._update(tok, reads, writes)
        self.n_inst += 1
        return tok

    def dma(self, issuer, out, in_, reads=(), writes=(), **kw):
        i = self.ring_i[issuer]
        n = len(self.ring[issuer])
        slot = i % n
        rnd = i // n
        key = ("d", issuer, slot)
        if rnd > 0:
            self.wait(issuer, (key, 16 * rnd))
        self._deps(issuer, reads, writes, force=True)
        ins = self.eng[issuer].dma_start(out=out, in_=in_, **kw)
        ins.then_inc(self.ring[issuer][slot], 16)
        self.ring_i[issuer] = i + 1
        tok = (key, 16 * (rnd + 1))
        self._update(tok, reads, writes)
        self.n_inst += 1
        return tok

    def barrier(self, engines=("pe", "act", "dve", "pool", "sp")):
        toks = []
        for e in self.COMPUTE:
            if self.cnt[e] > 0:
                toks.append((("c", e), self.cnt[e]))
        for e in self.ring:
            i = self.ring_i[e]
            n = len(self.ring[e])
            for slot in range(n):
                if i > slot:
                    rnd = (i - 1 - slot) // n
                    toks.append((("d", e, slot), 16 * (rnd + 1)))
        for e in engines:
            for t in toks:
                self.wait(e, t, force=True)


def _bf(x):
    return x


def build_consts():
    c = {}
    i = np.arange(128)
    c["ident"] = np.eye(128, dtype=np.float32)
    c["ones"] = np.ones((128, 128), np.float32)
    le = (i[:, None] <= i[None, :]).astype(np.float32)
    gt = (i[:, None] > i[None, :]).astype(np.float32)
    c["le_P"] = le
    c["gt_P"] = gt
    c["causal_P"] = le.copy()
    same = (i[:, None] // TS == i[None, :] // TS).astype(np.float32)
    c["le_S"] = le * same
    c["gt_S"] = gt * same
    c["causal_S"] = le * same
    c["seqmask"] = (i[:, None] // TS == np.arange(NB_S)[None, :]).astype(np.float32)
    mr = (np.arange(128)[None, :] // TS == np.arange(NB_S)[:, None]).astype(np.float32)
    maskrow = np.broadcast_to(mr.reshape(1, NB_S * 128), (128, NB_S * 128)).astype(np.float32).copy()
    bo = np.zeros((128, 128), np.float32)
    bo[:64, :64] = 1
    bo[64:, 64:] = 1
    c["blockones"] = bo
    hs = np.zeros((128, 2), np.float32)
    hs[:64, 0] = 1
    hs[64:, 1] = 1
    c["halfsel"] = hs
    lg = np.log1p(-np.exp2(-5.0 - np.arange(4, dtype=np.float64)))
    dP = np.zeros((128, 4, 128), np.float64)
    dS = np.zeros((128, 4, 128), np.float64)
    for h in range(4):
        diff = (i[None, :] - i[:, None]).astype(np.float64)
        dP[:, h, :] = np.where(diff >= 0, np.exp(lg[h] * diff), 0.0)
        dS[:, h, :] = np.where((diff >= 0) & (same > 0), np.exp(lg[h] * diff), 0.0)
    c["retdec_P"] = dP.reshape(128, 512).astype(np.float32)
    c["retdec_S"] = dS.reshape(128, 512).astype(np.float32)
    c["ret_expcum_P"] = np.exp(lg[None, :] * (i[:, None] + 1)).astype(np.float32)
    c["ret_decend_P"] = np.exp(lg[None, :] * (127 - i[:, None])).astype(np.float32)
    c["ret_expcum_S"] = np.exp(lg[None, :] * ((i[:, None] % TS) + 1)).astype(np.float32)
    c["ret_decend_S"] = np.exp(lg[None, :] * (TS - 1 - (i[:, None] % TS))).astype(np.float32)
    cdP = np.exp(lg * 128)
    cdS = np.exp(lg * TS)
    c["ret_cd_P"] = np.broadcast_to(np.repeat(cdP, 128)[None, :], (128, 512)).astype(np.float32).copy()
    c["ret_cd_S"] = np.broadcast_to(np.repeat(cdS, 128)[None, :], (128, 512)).astype(np.float32).copy()
    bm = np.zeros((128, 256), np.float32)
    for r in range(8):
        cc = r // 2
        bm[r, cc * 64:(cc + 1) * 64] = 1
        bm[32 + r, cc * 64:(cc + 1) * 64] = 1
    c["rw_blockmask"] = bm
    hm = np.zeros((128, 2), np.float32)
    hm[:64, 0] = 1
    hm[64:, 1] = 1
    c["rw_halfmask"] = hm
    offs = {}
    o = 0
    for k, v in c.items():
        offs[k] = (o, v.shape[1])
        o += v.shape[1]
    pack = np.concatenate([c[k] for k in c], axis=1).astype(np.float32)
    t = np.arange(TOK)
    reset = np.ones(TOK, np.float32)
    notlast = np.ones(TOK, np.float32)
    reset[:SEQ][t[:SEQ] % LB == 0] = 0
    notlast[:SEQ][t[:SEQ] % LB == LB - 1] = 0
    ts_ = t[SEQ:] - SEQ
    reset[SEQ:][ts_ % TS == 0] = 0
    notlast[SEQ:][ts_ % TS == TS - 1] = 0
    tokmask = np.stack([reset, notlast], 0).astype(np.float32)
    half = 64
    inv = (1.0 / (10000.0 ** np.linspace(0.0, 1.0, half, dtype=np.float32))).astype(np.float32)
    pos = np.concatenate([np.arange(SEQ, dtype=np.float32),
                          np.tile(PAST_LEN + np.arange(TS, dtype=np.float32), NB_S)])
    ang = (pos[:, None] * inv[None, :]).astype(np.float32)
    cs = np.cos(ang).astype(np.float32)
    sn = np.sin(ang).astype(np.float32)
    sc = np.float32(128 ** -0.5)
    rot = np.concatenate([cs, sn, cs * sc, sn * sc], axis=1).reshape(NT, 128, 256).astype(np.float32)
    return pack, offs, tokmask, rot, maskrow


_CONSTS = build_consts()

WEIGHT_NAMES = ["w_in", "w_out", "ln1_g", "ln1_b", "rw_mu", "rw_w0", "rw_w_up", "rw_a0", "rw_a_up", "rw_g_up",
                "rw_k_k", "rw_k_a", "rw_r_k", "rw_ln_g", "rw_ln_b", "mb_conv_w", "mb_conv_b", "mb_dt_bias",
                "mb_a_log", "mb_d", "mb_norm_g", "gla_gk_up", "gla_gk_b", "gla_norm_g", "ret_norm_g",
                "ln2_g", "ln2_b", "xa_wq", "xa_wk", "xa_wv", "xa_wo", "ln3_g", "ln3_b",
                "ffn_w_gate", "ffn_w_up", "ffn_w_down"]

IN_SHAPES = {
    "xp": [SEQ, D], "xs": [128, D], "memp": [N_MEM, D],
    "st_shift": [DEPTH, NB_S, RW_IN], "st_wkv": [DEPTH, NB_S, 8, 64, 64], "st_conv": [DEPTH, NB_S, 3, 1024],
    "st_ssm": [DEPTH, NB_S, 8, 128, 64], "st_gla": [DEPTH, NB_S, 4, 64, 128], "st_ret": [DEPTH, NB_S, 4, 128, 128],
    "ck": [DEPTH, NB_S, N_MEM, D], "cv": [DEPTH, NB_S, N_MEM, D],
    "w_in": [DEPTH, D, N_IN], "w_out": [DEPTH, D, D], "ln1_g": [DEPTH, D], "ln1_b": [DEPTH, D],
    "rw_mu": [DEPTH, RW_IN], "rw_w0": [DEPTH, 512], "rw_w_up": [DEPTH, 64, 512], "rw_a0": [DEPTH, 512],
    "rw_a_up": [DEPTH, 64, 512], "rw_g_up": [DEPTH, 128, 512], "rw_k_k": [DEPTH, 512], "rw_k_a": [DEPTH, 512],
    "rw_r_k": [DEPTH, 512], "rw_ln_g": [DEPTH, 512], "rw_ln_b": [DEPTH, 512],
    "mb_conv_w": [DEPTH, 4, 1024], "mb_conv_b": [DEPTH, 1024], "mb_dt_bias": [DEPTH, 8], "mb_a_log": [DEPTH, 8],
    "mb_d": [DEPTH, 8], "mb_norm_g": [DEPTH, 512], "gla_gk_up": [DEPTH, 16, 256], "gla_gk_b": [DEPTH, 256],
    "gla_norm_g": [DEPTH, 512], "ret_norm_g": [DEPTH, 512], "ln2_g": [DEPTH, D], "ln2_b": [DEPTH, D],
    "xa_wq": [DEPTH, D, D], "xa_wk": [DEPTH, D, D], "xa_wv": [DEPTH, D, D], "xa_wo": [DEPTH, D, D],
    "ln3_g": [DEPTH, D], "ln3_b": [DEPTH, D], "ffn_w_gate": [DEPTH, D, D_FF], "ffn_w_up": [DEPTH, D, D_FF],
    "ffn_w_down": [DEPTH, D_FF, D],
    "cpack": list(_CONSTS[0].shape), "tokmask": [2, TOK], "rot": [NT, 128, 256], "maskrow": [128, NB_S * 128],
}
OUT_SHAPES = {
    "yp": [SEQ, D], "ys": [128, D],
    "p_shift": [DEPTH, RW_IN], "p_wkv": [DEPTH, 8, 64, 64], "p_conv": [DEPTH, 3, 1024], "p_ssm": [DEPTH, 8, 128, 64],
    "p_gla": [DEPTH, 4, 64, 128], "p_ret": [DEPTH, 4, 128, 128], "p_mk": [DEPTH, N_MEM, D], "p_mv": [DEPTH, N_MEM, D],
    "s_shift": [DEPTH, NB_S, RW_IN], "s_wkv": [DEPTH, NB_S, 8, 64, 64], "s_conv": [DEPTH, NB_S, 3, 1024],
    "s_ssm": [DEPTH, NB_S, 8, 128, 64], "s_gla": [DEPTH, NB_S, 4, 64, 128], "s_ret": [DEPTH, NB_S, 4, 128, 128],
}


def in_shapes():
    out = {}
    for k, v in IN_SHAPES.items():
        if STAGE < 7 and (k.startswith("xa_") or k in ("ck", "cv", "memp", "ln2_g", "ln2_b")):
            continue
        if STAGE < 8 and (k.startswith("ffn_") or k in ("ln3_g", "ln3_b")):
            continue
        if STAGE < 6 and k in ("w_out", "ln1_g", "ln1_b"):
            continue
        v = list(v)
        if v[0] == DEPTH and k not in ("tokmask",) and len(v) >= 2 and k in WEIGHT_NAMES + ["st_shift", "st_wkv", "st_conv", "st_ssm", "st_gla", "st_ret", "ck", "cv"]:
            v[0] = NL
        out[k] = v
    return out


class Rot:
    def __init__(self, items):
        self.items = items
        self.i = 0

    def next(self):
        it = self.items[self.i % len(self.items)]
        self.i += 1
        return it


class Prog:
    def __init__(self):
        nc = bass.Bass("TRN2", target_bir_lowering=False)
        self.nc = nc
        self.I = {k: nc.dram_tensor(k, list(v), F32, kind="ExternalInput").ap() for k, v in in_shapes().items()}
        self.O = {k: nc.dram_tensor(k, list(v), F32, kind="ExternalOutput").ap() for k, v in OUT_SHAPES.items()}
        self.uid = 0
        self.out_deps = []

    def scr(self, name, shape, dt=F32):
        return self.nc.dram_tensor("scr_" + name, list(shape), dt).ap()

    def sbt(self, name, shape, dt=F32):
        self.uid += 1
        return self.scopes[-1].enter_context(self.nc.sbuf_tensor("%s_%d" % (name, self.uid), list(shape), dt))

    def rot(self, name, n, shape, dt=F32):
        return Rot([(self.sbt(name, shape, dt), Dep()) for _ in range(n)])

    @contextmanager
    def scope(self):
        st = ExitStack()
        self.scopes.append(st)
        try:
            yield
        finally:
            self.c.barrier()
            self.scopes.pop()
            st.close()

    def pb(self):
        i = self.ps_i % 8
        self.ps_i += 1
        return i

    def cst(self, name):
        o, n = _CONSTS[1][name]
        return self.cpk[:, o:o + n]

    def odep(self):
        d = Dep()
        self.out_deps.append(d)
        return d

    def build(self):
        nc = self.nc
        with ExitStack() as st:
            self.c = c = Ctx(nc, st)
            self.scopes = [st]
            st.enter_context(nc.Block())
            self.ps = [st.enter_context(nc.psum_tensor("psb%d" % i, [128, 512], F32)) for i in range(8)]
            self.dps = [Dep() for _ in range(8)]
            self.ps_i = 0
            self.actT = self.sbt("actT", [128, KC, TOK], BF16)
            self.dact = [[Dep() for _ in range(4)] for _ in range(NT)]
            ncst = _CONSTS[0].shape[1]
            self.cpk = self.sbt("cpk", [128, ncst], F32)
            self.dcst = Dep()
            c.dma("sp", self.cpk[:], self.I["cpack"], writes=[self.dcst])
            self.ident_bf = self.sbt("identbf", [128, 128], BF16)
            o, n = _CONSTS[1]["ident"]
            c.dma("pool", self.ident_bf[:], self.I["cpack"][:, o:o + n], writes=[self.dcst])
            self.maskrow = self.sbt("maskrow", [128, NB_S, 128], BF16)
            c.dma("pool", self.maskrow[:], self.I["maskrow"].rearrange("p (b i) -> p b i", b=NB_S), writes=[self.dcst])
            self.PT = self.scr("PT", [TOK, N_IN])
            self.dPT = [Dep() for _ in range(NT)]
            self.PF = self.scr("PF", [PF_ROWS, TOK])
            self.dPF = Dep()
            self.OS = self.scr("OS", [TOK, D])
            self.dOS = [Dep() for _ in range(NT)]
            self.MIX = self.scr("MIX", [TOK, D])
            self.dMIX = [Dep() for _ in range(NT)]
            self.XRES = self.scr("XRES", [TOK, D])
            self.dXRES = [Dep() for _ in range(NT)]
            self.HT = self.scr("HT", [FC, 128, TOK], BF16)
            self.dHT = Dep()
            self.ORW = self.scr("ORW", [TOK, 512], BF16)
            self.dORW = Dep()
            self.KAPT = self.scr("KAPT", [4, 128, TOK + 8], BF16)
            self.RHT = self.scr("RHT", [4, 128, TOK], BF16)
            self.BH = self.scr("BH", [TOK, 512], BF16)
            self.KH = self.scr("KH", [TOK, 512], BF16)
            self.VV = self.scr("VV", [TOK, 512], F32)
            self.dRWS = Dep()
            self.epsT = self.sbt("epsT", [128, 8], F32)
            c.op("dve", lambda e: e.memset(self.epsT[:, 0:1], 1e-5), writes=[self.dcst])
            c.op("dve", lambda e: e.memset(self.epsT[:, 1:2], RW_LN_EPS), writes=[self.dcst])
            c.op("dve", lambda e: e.memset(self.epsT[:, 2:3], 0.0), writes=[self.dcst])
            c.op("dve", lambda e: e.memset(self.epsT[:, 3:4], 1.0), writes=[self.dcst])
            self.epst = {1e-5: self.epsT[:, 0:1], float(RW_LN_EPS): self.epsT[:, 1:2], 0.0: self.epsT[:, 2:3], 1.0: self.epsT[:, 3:4]}
            c.barrier()
            self.res_from_input = True
            self.load_x0()
            for l in range(NL):
                if STAGE < 1:
                    break
                self.inproj(l)
                if STAGE < 2:
                    break
                self.mix_ret(l)
                if STAGE < 3:
                    break
                self.mix_gla(l)
                if STAGE < 4:
                    break
                self.mix_ssd(l)
                if STAGE < 5:
                    break
                self.mix_rwkv(l)
                if STAGE < 6:
                    break
                self.out_ln1(l)
                if STAGE < 7:
                    break
                self.attn(l)
                if STAGE < 8:
                    break
                self.ffn(l)
            for d in self.out_deps:
                if d.writer is not None:
                    c.wait("sp", d.writer, force=True)
            c.barrier(engines=("sp",))
        return nc

    def tile_rows(self, tt):
        return slice(tt * 128, (tt + 1) * 128)

    def resid_src(self, l, tt):
        if l == 0:
            if tt < 16:
                return self.I["xp"][tt * 128:(tt + 1) * 128, :], None
            return self.I["xs"], None
        return self.XRES[tt * 128:(tt + 1) * 128, :], self.dXRES[tt]

    def to_actT(self, xh, dxh, tt):
        c = self.c
        for g in range(2):
            pi = self.pb()
            psb = self.ps[pi][:].bitcast(BF16)
            for j in range(8):
                kc = g * 8 + j
                c.op("pe", lambda e, j=j, kc=kc: e.transpose(psb[:, j * 128:(j + 1) * 128], xh[:, kc * 128:(kc + 1) * 128], self.ident_bf[:]),
                     reads=[dxh, self.dcst], writes=[self.dps[pi]])
            dst = self.actT[:, g * 8:(g + 1) * 8, tt * 128:(tt + 1) * 128]
            src = psb.rearrange("p (j t) -> p j t", j=8)
            eng = "act" if g == 0 else "dve"
            if eng == "act":
                c.op("act", lambda e: e.copy(out=dst, in_=src), reads=[self.dps[pi]], writes=[self.dact[tt][2 * g], self.dact[tt][2 * g + 1]])
            else:
                c.op("dve", lambda e: e.tensor_copy(out=dst, in_=src), reads=[self.dps[pi]], writes=[self.dact[tt][2 * g], self.dact[tt][2 * g + 1]])

    def load_x0(self):
        c = self.c
        with self.scope():
            xb = self.rot("x0", 3, [128, D], BF16)
            for tt in range(NT):
                src, _ = self.resid_src(0, tt)
                buf, d = xb.next()
                c.dma("pool", buf[:], src, writes=[d])
                self.to_actT(buf, d, tt)

    def load_w(self, pool, w_ap, n0, ncols, kcn):
        buf, dep = pool.next()
        src = w_ap[:, n0:n0 + ncols].rearrange("(kc p) n -> p kc n", p=128)
        self.c.dma("pool", buf[:, 0:kcn, 0:ncols], src, writes=[dep])
        return buf, dep

    def evac(self, pi, rows, cols, dst, ddst, toggle):
        c = self.c
        src = self.ps[pi][0:rows, 0:cols]
        if toggle % 2 == 0:
            c.op("act", lambda e: e.copy(out=dst, in_=src), reads=[self.dps[pi]], writes=[ddst])
        else:
            c.op("dve", lambda e: e.tensor_copy(out=dst, in_=src), reads=[self.dps[pi]], writes=[ddst])

    def proj_T(self, w_ap, blocks, kcn, tiles_fn, lhs_fn, lhs_deps_fn, consume, wpool):
        c = self.c
        nxt = self.load_w(wpool, w_ap, blocks[0][0], blocks[0][1], kcn)
        for bi, (n0, ncols) in enumerate(blocks):
            wb, wd = nxt
            if bi + 1 < len(blocks):
                nxt = self.load_w(wpool, w_ap, blocks[bi + 1][0], blocks[bi + 1][1], kcn)
            for tt in tiles_fn(bi):
                pi = self.pb()
                ld = lhs_deps_fn(tt)
                for kc in range(kcn):
                    c.op("pe", lambda e, kc=kc, pi=pi, tt=tt: e.matmul(self.ps[pi][:, 0:ncols], lhs_fn(tt, kc), wb[:, kc, 0:ncols],
                                                                     start=(kc == 0), stop=(kc == kcn - 1)),
                         reads=[wd] + ld, writes=[self.dps[pi]])
                consume(bi, n0, ncols, tt, pi)

    def proj_F(self, w_ap, blocks, kcn, consume, wpool, rhs_fn=None, rhs_deps_fn=None, tblk=TBLK):
        c = self.c
        if rhs_fn is None:
            rhs_fn = lambda kc, t0, tw: self.actT[:, kc, t0:t0 + tw]
            rhs_deps_fn = lambda t0, tw: [d for tt in range(t0 // 128, (t0 + tw) // 128) for d in self.dact[tt]]
        nxt = self.load_w(wpool, w_ap, blocks[0][0], blocks[0][1], kcn)
        for bi, (n0, ncols) in enumerate(blocks):
            wb, wd = nxt
            if bi + 1 < len(blocks):
                nxt = self.load_w(wpool, w_ap, blocks[bi + 1][0], blocks[bi + 1][1], kcn)
            for j in range((ncols + 127) // 128):
                cw = min(128, ncols - 128 * j)
                for (t0, tw) in tblk:
                    pi = self.pb()
                    rd = rhs_deps_fn(t0, tw)
                    for kc in range(kcn):
                        c.op("pe", lambda e, kc=kc, pi=pi, j=j, cw=cw, t0=t0, tw=tw: e.matmul(
                            self.ps[pi][0:cw, 0:tw], wb[:, kc, 128 * j:128 * j + cw], rhs_fn(kc, t0, tw),
                            start=(kc == 0), stop=(kc == kcn - 1)), reads=[wd] + rd, writes=[self.dps[pi]])
                    consume(n0 + 128 * j, cw, t0, tw, pi)

    def act_lhs(self, tt, kc):
        return self.actT[:, kc, tt * 128:(tt + 1) * 128]

    def act_deps(self, tt):
        return list(self.dact[tt])

    def inproj(self, l):
        c = self.c
        w = self.I["w_in"][l]
        with self.scope():
            wpool = self.rot("win", 3, [128, KC, 512], BF16)
            stg = self.rot("stg", 4, [128, 512], F32)
            self.tog = 0
            def blocks_of(c0, n):
                out = []
                o = 0
                while o < n:
                    out.append((c0 + o, min(512, n - o)))
                    o += 512
                return out
            allt = list(range(NT))
            last2 = [15, 16]
            tb = []
            for b in blocks_of(C_RW, RW_IN):
                tb.append((b, last2))
            for b in blocks_of(C_MB, 512):
                tb.append((b, allt))
            for b in blocks_of(C_MB + 512, 1024):
                tb.append((b, last2))
            tb.append(((C_MB + 1536, 8), allt))
            for b in blocks_of(C_GLA + 256, 256):
                tb.append((b, allt))
            for b in blocks_of(C_GLA + 512, 512):
                tb.append((b, allt))
            for b in blocks_of(C_GLA + 1040, 512):
                tb.append((b, allt))
            for b in blocks_of(C_RET, RET_IN):
                tb.append((b, allt))
            blocks = [x[0] for x in tb]

            def consume_T(bi, n0, ncols, tt, pi):
                buf, d = stg.next()
                self.evac(pi, 128, ncols, buf[:, 0:ncols], d, self.tog)
                self.tog += 1
                c.dma("sp", self.PT[tt * 128:(tt + 1) * 128, n0:n0 + ncols], buf[:, 0:ncols], reads=[d], writes=[self.dPT[tt]])
            self.proj_T(w, blocks, KC, lambda bi: tb[bi][1], self.act_lhs, self.act_deps, consume_T, wpool)
            fsegs = [(C_RW, RW_IN, PF_RW), (C_MB + 512, 1024, PF_XBC), (C_GLA, 256, PF_GQ), (C_GLA + 256, 256, PF_GK),
                     (C_GLA + 1024, 16, PF_GKD)]
            for (c0, n, r0) in fsegs:
                def consume_F(col0, cw, t0, tw, pi, c0=c0, r0=r0):
                    buf, d = stg.next()
                    self.evac(pi, cw, tw, buf[0:cw, 0:tw], d, self.tog)
                    self.tog += 1
                    rr = r0 + (col0 - c0)
                    c.dma("sp", self.PF[rr:rr + cw, t0:t0 + tw], buf[0:cw, 0:tw], reads=[d], writes=[self.dPF])
                self.proj_F(w, blocks_of(c0, n), KC, consume_F, wpool)
            d1 = self.odep()
            c.dma("sp", self.O["p_shift"][l:l + 1, :], self.PT[2047:2048, C_RW:C_RW + RW_IN], reads=[self.dPT[15]], writes=[d1])
            d2 = self.odep()
            c.dma("sp", self.O["p_conv"][l], self.PT[2045:2048, C_MB + 512:C_MB + 1536], reads=[self.dPT[15]], writes=[d2])
            d3 = self.odep()
            src = self.PT[2048:2176, C_RW:C_RW + RW_IN].rearrange("(b t) n -> b t n", t=TS)[:, TS - 1, :]
            c.dma("sp", self.O["s_shift"][l], src, reads=[self.dPT[16]], writes=[d3])
            d4 = self.odep()
            src = self.PT[2048:2176, C_MB + 512:C_MB + 1536].rearrange("(b t) n -> b t n", t=TS)[:, TS - 3:TS, :]
            c.dma("sp", self.O["s_conv"][l], src, reads=[self.dPT[16]], writes=[d4])

    def bcast_row(self, name, row_ap, n):
        t = self.sbt(name, [128, n], F32)
        d = Dep()
        self.c.dma("sp", t[:], row_ap.partition_broadcast(128), writes=[d])
        return t, d

    def col_load(self, name, vec_ap, nchunks):
        t = self.sbt(name, [128, nchunks], F32)
        d = Dep()
        with self.nc.allow_non_contiguous_dma(reason="small per-feature vector"):
            self.c.dma("sp", t[:], vec_ap.rearrange("(c p) -> p c", p=128), writes=[d])
        return t, d

    def V(self, eng, fn, reads, writes):
        return self.c.op(eng, fn, reads=reads, writes=writes)

    def rms_gate(self, y, dy, ngroups, gsz, gn, dgn, gate_ap, dgate, eps, out_dram, dout, tmp, dtmp, small, dsmall):
        c = self.c
        y3 = y[:].rearrange("p (g v) -> p g v", g=ngroups)
        c.op("dve", lambda e: e.tensor_tensor(out=tmp[:], in0=y[:], in1=y[:], op=ALU.mult), reads=[dy], writes=[dtmp])
        c.op("dve", lambda e: e.tensor_reduce(out=small[:, 0:ngroups], in_=tmp[:].rearrange("p (g v) -> p g v", g=ngroups), axis=AX.X, op=ALU.add),
             reads=[dtmp], writes=[dsmall])
        c.op("act", lambda e: e.activation(out=small[:, 8:8 + ngroups], in_=small[:, 0:ngroups], func=AF.Sqrt, scale=1.0 / gsz, bias=self.epsb(eps)),
             reads=[dsmall, self.dcst], writes=[dsmall])
        c.op("dve", lambda e: e.reciprocal(out=small[:, 16:16 + ngroups], in_=small[:, 8:8 + ngroups]), reads=[dsmall], writes=[dsmall])
        rb = small[:, 16:16 + ngroups].unsqueeze(2).broadcast_to([128, ngroups, gsz])
        c.op("dve", lambda e: e.tensor_tensor(out=y3, in0=y3, in1=rb, op=ALU.mult), reads=[dy, dsmall], writes=[dy])
        c.op("pool", lambda e: e.tensor_tensor(out=y[:], in0=y[:], in1=gn[:], op=ALU.mult), reads=[dy, dgn], writes=[dy])
        c.op("act", lambda e: e.activation(out=tmp[:], in_=gate_ap, func=AF.Silu), reads=[dgate, dtmp], writes=[dtmp])
        c.op("dve", lambda e: e.tensor_tensor(out=y[:], in0=y[:], in1=tmp[:], op=ALU.mult), reads=[dy, dtmp], writes=[dy])
        c.dma("sp", out_dram, y[:], reads=[dy], writes=[dout])

    def epsb(self, eps):
        key = float(eps)
        if key not in self.epst:
            raise KeyError(key)
        return self.epst[key]

    def mix_ret(self, l):
        c = self.c
        with self.scope():
            gn, dgn = self.bcast_row("retgn", self.I["ret_norm_g"][l:l + 1, :], 512)
            S = self.sbt("retS", [128, 512], F32)
            Sbf = self.sbt("retSbf", [128, 512], BF16)
            dS = Dep()
            dSbf = Dep()
            c.op("dve", lambda e: e.memset(S[:], 0.0), writes=[dS])
            c.op("dve", lambda e: e.memset(Sbf[:], 0.0), writes=[dSbf])
            S0 = self.sbt("retS0", [128, NB_S, 512], F32)
            S0bf = self.sbt("retS0bf", [128, NB_S, 512], BF16)
            dS0 = Dep()
            dS0bf = Dep()
            c.dma("sp", S0[:].rearrange("d b (h v) -> d b h v", h=4), self.I["st_ret"][l].rearrange("b h d v -> d b h v"), writes=[dS0])
            c.op("act", lambda e: e.copy(out=S0bf[:], in_=S0[:]), reads=[dS0], writes=[dS0bf])
            inb = self.rot("retin", 2, [128, 2048], F32)
            rtb = self.rot("retrot", 2, [128, 256], F32)
            t1 = self.sbt("rt1", [128, 256], F32); t2 = self.sbt("rt2", [128, 256], F32)
            t3 = self.sbt("rt3", [128, 256], F32); t4 = self.sbt("rt4", [128, 256], F32)
            dt1 = Dep(); dt2 = Dep(); dt3 = Dep(); dt4 = Dep()
            qr = self.sbt("qr", [128, 512], BF16); kr = self.sbt("kr", [128, 512], BF16)
            dqr = Dep(); dkr = Dep()
            qT = self.sbt("qT", [128, 512], BF16); kT = self.sbt("kT", [128, 512], BF16)
            dqT = Dep(); dkT = Dep()
            W = self.sbt("retW", [128, 512], BF16); dW = Dep()
            vbf = self.sbt("retvbf", [128, 512], BF16); dvbf = Dep()
            kd = self.sbt("retkd", [128, 512], BF16); dkd = Dep()
            kdb = self.rot("retkdb", 2, [128, 512], BF16)
            qmall = self.sbt("retqmall", [128, NB_S, 512], BF16); dqmall = Dep()
            y = self.sbt("rety", [128, 512], F32); dy = Dep()
            tmp = self.sbt("rettmp", [128, 512], F32); dtmp = Dep()
            small = self.sbt("retsmall", [128, 32], F32); dsmall = Dep()
            stg = self.rot("retstg", 2, [128, 512], F32)
            for tt in range(NT):
                smp = tt == 16
                sfx = "_S" if smp else "_P"
                buf, dbuf = inb.next()
                c.dma("sp", buf[:], self.PT[tt * 128:(tt + 1) * 128, C_RET:C_RET + 2048], reads=[self.dPT[tt]], writes=[dbuf])
                rt, drt = rtb.next()
                c.dma("sp", rt[:], self.I["rot"][tt], writes=[drt])
                for (eng, x0, co, so, out, dout, ta, dta, tb_, dtb) in (("dve", 0, 0, 64, qr, dqr, t1, dt1, t2, dt2),
                                                                         ("pool", 512, 128, 192, kr, dkr, t3, dt3, t4, dt4)):
                    x4 = buf[:, x0:x0 + 512].rearrange("p (h i two) -> p h i two", h=4, two=2)
                    xe = x4[:, :, :, 0]
                    xo = x4[:, :, :, 1]
                    cs = rt[:, co:co + 64].unsqueeze(1).broadcast_to([128, 4, 64])
                    sn = rt[:, so:so + 64].unsqueeze(1).broadcast_to([128, 4, 64])
                    o4 = out[:].rearrange("p (h i two) -> p h i two", h=4, two=2)
                    a3 = ta[:].rearrange("p (h i) -> p h i", h=4)
                    b3 = tb_[:].rearrange("p (h i) -> p h i", h=4)
                    c.op(eng, lambda e, a3=a3, xe=xe, cs=cs: e.tensor_tensor(out=a3, in0=xe, in1=cs, op=ALU.mult), reads=[dbuf, drt], writes=[dta])
                    c.op(eng, lambda e, b3=b3, xo=xo, sn=sn: e.tensor_tensor(out=b3, in0=xo, in1=sn, op=ALU.mult), reads=[dbuf, drt], writes=[dtb])
                    c.op(eng, lambda e, o4=o4, a3=a3, b3=b3: e.tensor_tensor(out=o4[:, :, :, 0], in0=a3, in1=b3, op=ALU.subtract), reads=[dta, dtb], writes=[dout])
                    c.op(eng, lambda e, a3=a3, xe=xe, sn=sn: e.tensor_tensor(out=a3, in0=xe, in1=sn, op=ALU.mult), reads=[dbuf, drt, dout], writes=[dta])
                    c.op(eng, lambda e, b3=b3, xo=xo, cs=cs: e.tensor_tensor(out=b3, in0=xo, in1=cs, op=ALU.mult), reads=[dbuf, drt, dout], writes=[dtb])
                    c.op(eng, lambda e, o4=o4, a3=a3, b3=b3: e.tensor_tensor(out=o4[:, :, :, 1], in0=a3, in1=b3, op=ALU.add), reads=[dta, dtb], writes=[dout])
                for (src, dsrc, dst, ddst, eng) in ((qr, dqr, qT, dqT, "act"), (kr, dkr, kT, dkT, "dve")):
                    pi = self.pb()
                    psb = self.ps[pi][:].bitcast(BF16)
                    for h in range(4):
                        c.op("pe", lambda e, h=h, psb=psb, src=src: e.transpose(psb[:, h * 128:(h + 1) * 128], src[:, h * 128:(h + 1) * 128], self.ident_bf[:]),
                             reads=[dsrc, self.dcst], writes=[self.dps[pi]])
                    if eng == "act":
                        c.op("act", lambda e, psb=psb, dst=dst: e.copy(out=dst[:], in_=psb[:, 0:512]), reads=[self.dps[pi]], writes=[ddst])
                    else:
                        c.op("dve", lambda e, psb=psb, dst=dst: e.tensor_copy(out=dst[:], in_=psb[:, 0:512]), reads=[self.dps[pi]], writes=[ddst])
                p1 = self.pb()
                for h in range(4):
                    c.op("pe", lambda e, h=h: e.matmul(self.ps[p1][:, h * 128:(h + 1) * 128], kT[:, h * 128:(h + 1) * 128], qT[:, h * 128:(h + 1) * 128], start=True, stop=True),
                         reads=[dkT, dqT], writes=[self.dps[p1]])
                c.op("dve", lambda e: e.tensor_tensor(out=W[:], in0=self.ps[p1][:, 0:512], in1=self.cst("retdec" + sfx), op=ALU.mult),
                     reads=[self.dps[p1], self.dcst], writes=[dW])
                c.op("act", lambda e: e.copy(out=vbf[:], in_=buf[:, 1024:1536]), reads=[dbuf], writes=[dvbf])
                p2 = self.pb()
                for h in range(4):
                    c.op("pe", lambda e, h=h: e.matmul(self.ps[p2][:, h * 128:(h + 1) * 128], W[:, h * 128:(h + 1) * 128], vbf[:, h * 128:(h + 1) * 128], start=True, stop=True),
                         reads=[dW, dvbf], writes=[self.dps[p2]])
                p3 = self.pb()
                if not smp:
                    for h in range(4):
                        c.op("pe", lambda e, h=h: e.matmul(self.ps[p3][:, h * 128:(h + 1) * 128], qT[:, h * 128:(h + 1) * 128], Sbf[:, h * 128:(h + 1) * 128], start=True, stop=True),
                             reads=[dqT, dSbf], writes=[self.dps[p3]])
                else:
                    for b in range(NB_S):
                        mrb = self.maskrow[:, b, :].unsqueeze(1).broadcast_to([128, 4, 128])
                        c.op("pool", lambda e, b=b, mrb=mrb: e.tensor_tensor(out=qmall[:, b, :].rearrange("p (h i) -> p h i", h=4), in0=qT[:].rearrange("p (h i) -> p h i", h=4), in1=mrb, op=ALU.mult),
                             reads=[dqT, self.dcst], writes=[dqmall])
                    for h in range(4):
                        for b in range(NB_S):
                            c.op("pe", lambda e, h=h, b=b: e.matmul(self.ps[p3][:, h * 128:(h + 1) * 128], qmall[:, b, h * 128:(h + 1) * 128], S0bf[:, b, h * 128:(h + 1) * 128],
                                                                      start=(b == 0), stop=(b == NB_S - 1), skip_group_check=True),
                                 reads=[dqmall, dS0bf], writes=[self.dps[p3]])
                ecb = self.cst("ret_expcum" + sfx).unsqueeze(2).broadcast_to([128, 4, 128])
                c.op("dve", lambda e: e.tensor_tensor(out=y[:].rearrange("p (h v) -> p h v", h=4), in0=self.ps[p3][:, 0:512].rearrange("p (h v) -> p h v", h=4), in1=ecb, op=ALU.mult),
                     reads=[self.dps[p3], self.dcst, self.dOS[tt]], writes=[dy])
                c.op("dve", lambda e: e.tensor_tensor(out=y[:], in0=y[:], in1=self.ps[p2][:, 0:512], op=ALU.add), reads=[dy, self.dps[p2]], writes=[dy])
                self.rms_gate(y, dy, 4, 128, gn, dgn, buf[:, 1536:2048], dbuf, 1e-5, self.OS[tt * 128:(tt + 1) * 128, 1536:2048], self.dOS[tt], tmp, dtmp, small, dsmall)
                deb = self.cst("ret_decend" + sfx).unsqueeze(2).broadcast_to([128, 4, 128])
                c.op("pool", lambda e: e.tensor_tensor(out=kd[:].rearrange("p (h d) -> p h d", h=4), in0=kr[:].rearrange("p (h d) -> p h d", h=4), in1=deb, op=ALU.mult),
                     reads=[dkr, self.dcst], writes=[dkd])
                if not smp:
                    p4 = self.pb()
                    for h in range(4):
                        c.op("pe", lambda e, h=h: e.matmul(self.ps[p4][:, h * 128:(h + 1) * 128], kd[:, h * 128:(h + 1) * 128], vbf[:, h * 128:(h + 1) * 128], start=True, stop=True),
                             reads=[dkd, dvbf], writes=[self.dps[p4]])
                    c.op("pool", lambda e: e.tensor_tensor(out=S[:], in0=S[:], in1=self.cst("ret_cd_P"), op=ALU.mult), reads=[dS, self.dcst], writes=[dS])
                    c.op("dve", lambda e: e.tensor_tensor(out=S[:], in0=S[:], in1=self.ps[p4][:, 0:512], op=ALU.add), reads=[dS, self.dps[p4]], writes=[dS])
                    c.op("act", lambda e: e.copy(out=Sbf[:], in_=S[:]), reads=[dS], writes=[dSbf])
                    if tt == 15:
                        do = self.odep()
                        c.dma("sp", self.O["p_ret"][l].rearrange("h d v -> d h v"), S[:].rearrange("d (h v) -> d h v", h=4), reads=[dS], writes=[do])
                else:
                    for b in range(NB_S):
                        kb, dkb = kdb.next()
                        c.op("pool", lambda e, kb=kb, b=b: e.tensor_scalar(out=kb[:], in0=kd[:], scalar1=self.cst("seqmask")[:, b:b + 1], scalar2=None, op0=ALU.mult),
                             reads=[dkd, self.dcst], writes=[dkb])
                        p4 = self.pb()
                        for h in range(4):
                            c.op("pe", lambda e, h=h, kb=kb, p4=p4: e.matmul(self.ps[p4][:, h * 128:(h + 1) * 128], kb[:, h * 128:(h + 1) * 128], vbf[:, h * 128:(h + 1) * 128], start=True, stop=True),
                                 reads=[dkb, dvbf], writes=[self.dps[p4]])
                        sb_, dsb = stg.next()
                        c.op("pool", lambda e, sb_=sb_, b=b: e.tensor_tensor(out=sb_[:], in0=S0[:, b, :], in1=self.cst("ret_cd_S"), op=ALU.mult), reads=[dS0, self.dcst], writes=[dsb])
                        c.op("dve", lambda e, sb_=sb_, p4=p4: e.tensor_tensor(out=sb_[:], in0=sb_[:], in1=self.ps[p4][:, 0:512], op=ALU.add), reads=[dsb, self.dps[p4]], writes=[dsb])
                        do = self.odep()
                        c.dma("sp", self.O["s_ret"][l, b].rearrange("h d v -> d h v"), sb_[:].rearrange("d (h v) -> d h v", h=4), reads=[dsb], writes=[do])


_PROG_CACHE = {}


def _get_prog():
    if "p" not in _PROG_CACHE:
        p = Prog()
        p.build()
        _PROG_CACHE["p"] = p
    return _PROG_CACHE["p"]


def kernel(**inp):
    f = lambda a: np.ascontiguousarray(np.asarray(a, dtype=np.float32))
    prog = _get_prog()
    pack, offs, tokmask, rot, maskrow = _CONSTS
    shared = {k: f(inp[k]) for k in WEIGHT_NAMES}
    shared["rw_r_k"] = shared["rw_r_k"].reshape(DEPTH, 512)
    shared["cpack"] = pack
    shared["tokmask"] = tokmask
    shared["rot"] = rot
    shared["maskrow"] = maskrow
    xp = f(inp["x_prompt"]); xs = f(inp["x_sample"]); memp = f(inp["mem_prompt"])
    st = {"st_shift": f(inp["state_rwkv_shift"]), "st_wkv": f(inp["state_rwkv_wkv"]), "st_conv": f(inp["state_mamba_conv"]),
          "st_ssm": f(inp["state_mamba_ssm"]), "st_gla": f(inp["state_gla"]), "st_ret": f(inp["state_ret"]),
          "ck": f(inp["cache_mem_k"]), "cv": f(inp["cache_mem_v"])}
    in_maps = []
    for cid in range(8):
        b = cid % 4
        m = dict(shared)
        m["xp"] = xp[b]
        m["xs"] = np.ascontiguousarray(xs[cid * NB_S:(cid + 1) * NB_S].reshape(128, D))
        m["memp"] = memp[b]
        for k, v in st.items():
            sl = np.ascontiguousarray(v[:, cid * NB_S:(cid + 1) * NB_S])
            if k in ("ck", "cv"):
                sl = sl.reshape(DEPTH, NB_S, N_MEM, D)
            m[k] = sl
        ish = in_shapes()
        m = {k: (np.ascontiguousarray(v[:NL]) if (k in ish and ish[k][0] == NL and v.shape[0] == DEPTH and NL != DEPTH) else v) for k, v in m.items() if k in ish}
        for k in m:
            assert list(m[k].shape) == ish[k], (k, m[k].shape, ish[k])
        in_maps.append(m)
    res = run_bass_kernel_spmd(prog.nc, in_maps, core_ids=list(range(8)))
    R = res.results
    B = 4
    yp = np.stack([R[b]["yp"] for b in range(B)], 0)
    ys = np.concatenate([R[c]["ys"].reshape(NB_S, TS, D) for c in range(8)], 0)

    def pstack(k):
        return np.stack([R[b][k] for b in range(B)], 1)

    def sstack(k):
        return np.concatenate([R[c][k] for c in range(8)], 1)
    p_mk = pstack("p_mk").reshape(DEPTH, B, N_MEM, 4, 512)
    p_mv = pstack("p_mv").reshape(DEPTH, B, N_MEM, 4, 512)
    outs = (yp, ys, pstack("p_shift"), pstack("p_wkv"), pstack("p_conv"), pstack("p_ssm"), pstack("p_gla"), pstack("p_ret"),
            p_mk, p_mv, sstack("s_shift"), sstack("s_wkv"), sstack("s_conv"), sstack("s_ssm"), sstack("s_gla"), sstack("s_ret"))
    outs = tuple(np.ascontiguousarray(o.astype(np.float32)) for o in outs)
    if os.environ.get("KDUMP"):
        for i, o in enumerate(outs):
            np.save(os.environ["KDUMP"] + "_%d.npy" % i, o)
    return outs


def _gla_phase(self, l):
    c = self.c
    with self.scope():
        gn, dgn = self.bcast_row("glagn", self.I["gla_norm_g"][l:l + 1, :], 512)
        gb, dgb = self.bcast_row("glagb", self.I["gla_gk_b"][l:l + 1, :], 256)
        gkup = self.sbt("gkup", [16, 256], BF16); dgkup = Dep()
        c.dma("pool", gkup[:], self.I["gla_gk_up"][l], writes=[dgkup])
        St = self.sbt("glaS", [128, 256], F32); dSt = Dep()
        Stbf = self.sbt("glaSbf", [128, 256], BF16); dStbf = Dep()
        c.op("dve", lambda e: e.memset(St[:], 0.0), writes=[dSt])
        c.op("dve", lambda e: e.memset(Stbf[:], 0.0), writes=[dStbf])
        S0 = self.sbt("glaS0", [128, NB_S, 256], F32); dS0 = Dep()
        S0bf = self.sbt("glaS0bf", [128, NB_S, 256], BF16); dS0bf = Dep()
        c.dma("sp", S0[:].rearrange("p b (c v) -> p b c v", c=2), self.I["st_gla"][l].rearrange("b (c h2) k v -> (h2 k) b c v", h2=2), writes=[dS0])
        c.op("act", lambda e: e.copy(out=S0bf[:], in_=S0[:]), reads=[dS0], writes=[dS0bf])
        qkb = self.rot("glaqk", 2, [128, 4, 128], F32)
        tmb = self.rot("glatm", 2, [128, 1280], F32)
        gkd = self.rot("glagkd", 2, [16, 128], BF16)
        xg = self.sbt("glaxg", [128, 256], F32); dxg = Dep()
        sp = self.sbt("glasp", [128, 256], F32); dsp = Dep()
        E3 = self.sbt("glaE3", [128, 256], F32); dE3 = Dep()
        E1T = self.sbt("glaE1T", [128, 2, 128], F32); dE1T = Dep()
        E2T = self.sbt("glaE2T", [128, 2, 128], F32); dE2T = Dep()
        khat = self.sbt("glakhat", [128, 256], BF16); dkhat = Dep()
        qhT = self.sbt("glaqhT", [128, 2, 128], BF16); dqhT = Dep()
        khT = self.sbt("glakhT", [128, 2, 128], BF16); dkhT = Dep()
        W = self.sbt("glaW", [128, 512], BF16); dW = Dep()
        vbf = self.sbt("glavbf", [128, 512], BF16); dvbf = Dep()
        y = self.sbt("glay", [128, 512], F32); dy = Dep()
        tmp = self.sbt("glatmp", [128, 512], F32); dtmp = Dep()
        small = self.sbt("glasmall", [128, 32], F32); dsmall = Dep()
        qmall = self.sbt("glaqmall", [128, NB_S, 2, 128], BF16); dqmall = Dep()
        kbb = self.rot("glakb", 2, [128, 256], BF16)
        stg = self.rot("glastg", 2, [128, 256], F32)
        for tt in range(NT):
            smp = tt == 16
            sfx = "_S" if smp else "_P"
            t0 = tt * 128
            qk, dqk = qkb.next()
            c.dma("sp", qk[:, 0:2, :], self.PF[PF_GQ:PF_GQ + 256, t0:t0 + 128].rearrange("(c p) t -> p c t", p=128), reads=[self.dPF], writes=[dqk])
            c.dma("sp", qk[:, 2:4, :], self.PF[PF_GK:PF_GK + 256, t0:t0 + 128].rearrange("(c p) t -> p c t", p=128), reads=[self.dPF], writes=[dqk])
            tm, dtm = tmb.next()
            c.dma("sp", tm[:, 0:768], self.PT[t0:t0 + 128, C_GLA + 256:C_GLA + 1024], reads=[self.dPT[tt]], writes=[dtm])
            c.dma("sp", tm[:, 768:1280], self.PT[t0:t0 + 128, C_GLA + 1040:C_GLA + 1552], reads=[self.dPT[tt]], writes=[dtm])
            gk, dgk = gkd.next()
            c.dma("pool", gk[:], self.PF[PF_GKD:PF_GKD + 16, t0:t0 + 128], reads=[self.dPF], writes=[dgk])
            p0 = self.pb()
            c.op("pe", lambda e: e.matmul(self.ps[p0][:, 0:256], gk[:], gkup[:], start=True, stop=True), reads=[dgk, dgkup], writes=[self.dps[p0]])
            c.op("dve", lambda e: e.tensor_tensor(out=xg[:], in0=self.ps[p0][:, 0:256], in1=gb[:], op=ALU.add), reads=[self.dps[p0], dgb], writes=[dxg])
            c.op("act", lambda e: e.activation(out=xg[:], in_=xg[:], func=AF.Exp, scale=-1.0), reads=[dxg], writes=[dxg])
            c.op("act", lambda e: e.activation(out=sp[:], in_=xg[:], func=AF.Ln, bias=self.epsb(1.0), scale=1.0), reads=[dxg, self.dcst], writes=[dsp])
            p1 = self.pb()
            c.op("pe", lambda e: e.matmul(self.ps[p1][:, 0:256], self.cst("le" + sfx), sp[:], start=True, stop=True), reads=[dsp, self.dcst], writes=[self.dps[p1]])
            for cc in range(2):
                c.op("pe", lambda e, cc=cc: e.matmul(self.ps[p1][:, 256 + cc * 128:256 + (cc + 1) * 128], sp[:, cc * 128:(cc + 1) * 128], self.cst("le" + sfx), start=True, stop=True),
                     reads=[dsp, self.dcst], writes=[self.dps[p1]])
            c.op("act", lambda e: e.activation(out=E3[:], in_=self.ps[p1][:, 0:256], func=AF.Exp, scale=1.0 / 16.0), reads=[self.dps[p1]], writes=[dE3])
            c.op("act", lambda e: e.activation(out=E1T[:].rearrange("p c t -> p (c t)"), in_=self.ps[p1][:, 256:512], func=AF.Exp, scale=-1.0 / 16.0), reads=[self.dps[p1]], writes=[dE1T])
            c.op("act", lambda e: e.activation(out=E2T[:].rearrange("p c t -> p (c t)"), in_=self.ps[p1][:, 256:512], func=AF.Exp, scale=1.0 / 16.0), reads=[self.dps[p1]], writes=[dE2T])
            c.op("dve", lambda e: e.tensor_tensor(out=khat[:], in0=tm[:, 0:256], in1=E3[:], op=ALU.mult), reads=[dtm, dE3], writes=[dkhat])
            c.op("dve", lambda e: e.scalar_tensor_tensor(out=qhT[:].rearrange("p c t -> p (c t)"), in0=qk[:, 0:2, :].rearrange("p c t -> p (c t)"), scalar=0.125,
                                                        in1=E1T[:].rearrange("p c t -> p (c t)"), op0=ALU.mult, op1=ALU.mult), reads=[dqk, dE1T], writes=[dqhT])
            c.op("pool", lambda e: e.tensor_tensor(out=khT[:], in0=qk[:, 2:4, :], in1=E2T[:], op=ALU.mult), reads=[dqk, dE2T], writes=[dkhT])
            c.op("act", lambda e: e.copy(out=vbf[:], in_=tm[:, 256:768]), reads=[dtm], writes=[dvbf])
            p2 = [self.pb(), self.pb()]
            for h2 in range(2):
                for cc in range(2):
                    c.op("pe", lambda e, cc=cc, h2=h2: e.matmul(self.ps[p2[h2]][:, cc * 128:(cc + 1) * 128], khT[h2 * 64:(h2 + 1) * 64, cc, :], qhT[h2 * 64:(h2 + 1) * 64, cc, :], start=True, stop=True),
                         reads=[dkhT, dqhT], writes=[self.dps[p2[h2]]])
            cau = self.cst("causal" + sfx).unsqueeze(1).broadcast_to([128, 2, 128])
            for h2 in range(2):
                c.op("dve", lambda e, h2=h2: e.tensor_tensor(out=W[:, h2 * 256:(h2 + 1) * 256].rearrange("p (c i) -> p c i", c=2), in0=self.ps[p2[h2]][:, 0:256].rearrange("p (c i) -> p c i", c=2), in1=cau, op=ALU.mult),
                     reads=[self.dps[p2[h2]], self.dcst], writes=[dW])
            if smp:
                for b in range(NB_S):
                    mrb = self.maskrow[:, b, :].unsqueeze(1).broadcast_to([128, 2, 128])
                    c.op("pool", lambda e, b=b, mrb=mrb: e.tensor_tensor(out=qmall[:, b, :, :], in0=qhT[:], in1=mrb, op=ALU.mult), reads=[dqhT, self.dcst], writes=[dqmall])
            p3 = [self.pb(), self.pb()]
            for h2 in range(2):
                for cc in range(2):
                    h = 2 * cc + h2
                    c.op("pe", lambda e, h=h, cc=cc, h2=h2: e.matmul(self.ps[p3[h2]][:, cc * 128:(cc + 1) * 128], W[:, h2 * 256 + cc * 128:h2 * 256 + (cc + 1) * 128], vbf[:, h * 128:(h + 1) * 128],
                                                                 start=True, stop=False, skip_group_check=True), reads=[dW, dvbf], writes=[self.dps[p3[h2]]])
                    if not smp:
                        c.op("pe", lambda e, cc=cc, h2=h2: e.matmul(self.ps[p3[h2]][:, cc * 128:(cc + 1) * 128], qhT[h2 * 64:(h2 + 1) * 64, cc, :], Stbf[h2 * 64:(h2 + 1) * 64, cc * 128:(cc + 1) * 128],
                                                                start=False, stop=True, skip_group_check=True), reads=[dqhT, dStbf], writes=[self.dps[p3[h2]]])
                    else:
                        for b in range(NB_S):
                            c.op("pe", lambda e, cc=cc, h2=h2, b=b: e.matmul(self.ps[p3[h2]][:, cc * 128:(cc + 1) * 128], qmall[h2 * 64:(h2 + 1) * 64, b, cc, :],
                                                                         S0bf[h2 * 64:(h2 + 1) * 64, b, cc * 128:(cc + 1) * 128],
                                                                         start=False, stop=(b == NB_S - 1), skip_group_check=True),
                                 reads=[dqmall, dS0bf], writes=[self.dps[p3[h2]]])
            y4 = y[:].rearrange("p (c h v) -> p c h v", c=2, h=2)
            for h2 in range(2):
                c.op("act", lambda e, h2=h2: e.copy(out=y4[:, :, h2, :], in_=self.ps[p3[h2]][:, 0:256].rearrange("p (c v) -> p c v", c=2)), reads=[self.dps[p3[h2]], self.dOS[tt]], writes=[dy])
            self.rms_gate(y, dy, 4, 128, gn, dgn, tm[:, 768:1280], dtm, 1e-5, self.OS[t0:t0 + 128, 1024:1536], self.dOS[tt], tmp, dtmp, small, dsmall)
            if not smp:
                p4 = self.pb()
                for h in range(4):
                    cc, h2 = h // 2, h % 2
                    c.op("pe", lambda e, h=h, cc=cc, h2=h2: e.matmul(self.ps[p4][h2 * 64:(h2 + 1) * 64, cc * 128:(cc + 1) * 128], khat[:, h * 64:(h + 1) * 64], vbf[:, h * 128:(h + 1) * 128],
                                                                 start=True, stop=True, skip_group_check=True), reads=[dkhat, dvbf], writes=[self.dps[p4]])
                c.op("dve", lambda e: e.tensor_tensor(out=St[:], in0=St[:], in1=self.ps[p4][:, 0:256], op=ALU.add), reads=[dSt, self.dps[p4]], writes=[dSt])
                eb = E1T[:, :, 127:128].broadcast_to([128, 2, 128])
                c.op("dve", lambda e: e.tensor_tensor(out=St[:].rearrange("p (c v) -> p c v", c=2), in0=St[:].rearrange("p (c v) -> p c v", c=2), in1=eb, op=ALU.mult),
                     reads=[dSt, dE1T], writes=[dSt])
                c.op("act", lambda e: e.copy(out=Stbf[:], in_=St[:]), reads=[dSt], writes=[dStbf])
                if tt == 15:
                    do = self.odep()
                    c.dma("sp", self.O["p_gla"][l].rearrange("(c h2) k v -> (h2 k) c v", h2=2), St[:].rearrange("p (c v) -> p c v", c=2), reads=[dSt], writes=[do])
            else:
                for b in range(NB_S):
                    kb, dkb = kbb.next()
                    c.op("pool", lambda e, kb=kb, b=b: e.tensor_scalar(out=kb[:], in0=khat[:], scalar1=self.cst("seqmask")[:, b:b + 1], scalar2=None, op0=ALU.mult),
                         reads=[dkhat, self.dcst], writes=[dkb])
                    p4 = self.pb()
                    for h in range(4):
                        cc, h2 = h // 2, h % 2
                        c.op("pe", lambda e, h=h, cc=cc, h2=h2, kb=kb, p4=p4: e.matmul(self.ps[p4][h2 * 64:(h2 + 1) * 64, cc * 128:(cc + 1) * 128], kb[:, h * 64:(h + 1) * 64], vbf[:, h * 128:(h + 1) * 128],
                                                                                  start=True, stop=True, skip_group_check=True), reads=[dkb, dvbf], writes=[self.dps[p4]])
                    sb_, dsb = stg.next()
                    c.op("dve", lambda e, sb_=sb_, b=b, p4=p4: e.tensor_tensor(out=sb_[:], in0=S0[:, b, :], in1=self.ps[p4][:, 0:256], op=ALU.add), reads=[dS0, self.dps[p4]], writes=[dsb])
                    col = TS * b + TS - 1
                    eb = E1T[:, :, col:col + 1].broadcast_to([128, 2, 128])
                    c.op("dve", lambda e, sb_=sb_, eb=eb: e.tensor_tensor(out=sb_[:].rearrange("p (c v) -> p c v", c=2), in0=sb_[:].rearrange("p (c v) -> p c v", c=2), in1=eb, op=ALU.mult),
                         reads=[dsb, dE1T], writes=[dsb])
                    do = self.odep()
                    c.dma("sp", self.O["s_gla"][l, b].rearrange("(c h2) k v -> (h2 k) c v", h2=2), sb_[:].rearrange("p (c v) -> p c v", c=2), reads=[dsb], writes=[do])


Prog.mix_gla = _gla_phase


def _ssd_phase(self, l):
    c = self.c
    with self.scope():
        gn, dgn = self.bcast_row("mbgn", self.I["mb_norm_g"][l:l + 1, :], 512)
        dtb, ddtb = self.bcast_row("mbdtb", self.I["mb_dt_bias"][l:l + 1, :], 8)
        aneg, daneg = self.bcast_row("mbaneg", self.I["mb_a_log"][l:l + 1, :], 8)
        c.op("act", lambda e: e.activation(out=aneg[:], in_=aneg[:], func=AF.Exp), reads=[daneg], writes=[daneg])
        c.op("dve", lambda e: e.tensor_scalar(out=aneg[:], in0=aneg[:], scalar1=-1.0, scalar2=None, op0=ALU.mult), reads=[daneg], writes=[daneg])
        Dt, dDt = self.bcast_row("mbD", self.I["mb_d"][l:l + 1, :], 8)
        cw = self.sbt("mbcw", [128, 4, 8], F32); dcw = Dep()
        with self.nc.allow_non_contiguous_dma(reason="tiny conv weights"):
            for wi in range(4):
                c.dma("sp", cw[:, wi, :], self.I["mb_conv_w"][l, wi].rearrange("(c p) -> p c", p=128), writes=[dcw])
        cb, dcb = self.col_load("mbcb", self.I["mb_conv_b"][l], 8)
        hT = self.sbt("mbhT", [48, 1024], F32); dhT = Dep()
        c.dma("sp", hT[:], self.I["st_conv"][l].rearrange("b r n -> (b r) n"), writes=[dhT])
        hist = self.sbt("mbhist", [128, 8, 48], F32); dhist = Dep()
        ph = self.pb()
        for j in range(8):
            c.op("pe", lambda e, j=j: e.transpose(self.ps[ph][:, j * 48:(j + 1) * 48], hT[:, j * 128:(j + 1) * 128], self.cst("ident")[0:48, 0:48]),
                 reads=[dhT, self.dcst], writes=[self.dps[ph]])
        c.op("dve", lambda e: e.tensor_copy(out=hist[:].rearrange("p c r -> p (c r)"), in_=self.ps[ph][:, 0:384]), reads=[self.dps[ph]], writes=[dhist])
        XH = self.sbt("mbXH", [128, 4, TOK], F32); dXH = Dep()
        BT = self.sbt("mbBT", [128, 2, TOK], BF16); dBT = Dep()
        CT = self.sbt("mbCT", [128, 2, TOK], BF16); dCT = Dep()
        with self.scope():
            xbp = self.rot("mbxb", 2, [128, 3 + SEQ], F32)
            xsp = self.rot("mbxs", 2, [128, NB_S, 3 + TS], F32)
            cv = self.sbt("mbcv", [128, TOK], F32); dcv = Dep()
            for j in range(8):
                xb, dxb = xbp.next()
                xs, dxs = xsp.next()
                r0 = PF_XBC + j * 128
                c.op("pool", lambda e, xb=xb: e.memset(xb[:, 0:3], 0.0), writes=[dxb])
                c.dma("sp", xb[:, 3:3 + SEQ], self.PF[r0:r0 + 128, 0:SEQ], reads=[self.dPF], writes=[dxb])
                c.dma("sp", xs[:, :, 3:3 + TS], self.PF[r0:r0 + 128, SEQ:TOK].rearrange("p (b t) -> p b t", t=TS), reads=[self.dPF], writes=[dxs])
                c.op("pool", lambda e, xs=xs, j=j: e.tensor_copy(out=xs[:, :, 0:3], in_=hist[:, j, :].rearrange("p (b r) -> p b r", r=3)), reads=[dhist, dxs], writes=[dxs])
                cvp = cv[:, 0:SEQ]
                cvs = cv[:, SEQ:TOK].rearrange("p (b t) -> p b t", t=TS)
                c.op("dve", lambda e, xb=xb, j=j: e.tensor_scalar(out=cvp, in0=xb[:, 3:3 + SEQ], scalar1=cw[:, 3, j:j + 1], scalar2=cb[:, j:j + 1], op0=ALU.mult, op1=ALU.add),
                     reads=[dxb, dcw, dcb], writes=[dcv])
                c.op("pool", lambda e, xs=xs, j=j: e.tensor_scalar(out=cvs, in0=xs[:, :, 3:3 + TS], scalar1=cw[:, 3, j:j + 1], scalar2=cb[:, j:j + 1], op0=ALU.mult, op1=ALU.add),
                     reads=[dxs, dcw, dcb], writes=[dcv])
                for i in range(3):
                    c.op("dve", lambda e, xb=xb, j=j, i=i: e.scalar_tensor_tensor(out=cvp, in0=xb[:, i:i + SEQ], scalar=cw[:, i, j:j + 1], in1=cvp, op0=ALU.mult, op1=ALU.add),
                         reads=[dxb, dcw, dcv], writes=[dcv])
                    c.op("dve", lambda e, xs=xs, j=j, i=i: e.scalar_tensor_tensor(out=cvs, in0=xs[:, :, i:i + TS], scalar=cw[:, i, j:j + 1], in1=cvs, op0=ALU.mult, op1=ALU.add),
                         reads=[dxs, dcw, dcv], writes=[dcv])
                if j < 4:
                    c.op("act", lambda e, j=j: e.activation(out=XH[:, j, :], in_=cv[:], func=AF.Silu), reads=[dcv], writes=[dXH])
                elif j < 6:
                    c.op("act", lambda e, j=j: e.activation(out=BT[:, j - 4, :], in_=cv[:], func=AF.Silu), reads=[dcv], writes=[dBT])
                else:
                    c.op("act", lambda e, j=j: e.activation(out=CT[:, j - 6, :], in_=cv[:], func=AF.Silu), reads=[dcv], writes=[dCT])
        H = self.sbt("mbH", [128, 512], F32); dH = Dep()
        Hbf = self.sbt("mbHbf", [128, 512], BF16); dHbf = Dep()
        c.op("dve", lambda e: e.memset(H[:], 0.0), writes=[dH])
        c.op("dve", lambda e: e.memset(Hbf[:], 0.0), writes=[dHbf])
        S0bf = self.sbt("mbS0bf", [128, NB_S, 512], BF16); dS0bf = Dep()
        for bq in range(4):
            c.dma("pool", S0bf[:, bq * 4:(bq + 1) * 4, :].rearrange("n b (h d) -> n b h d", h=8), self.I["st_ssm"][l, bq * 4:(bq + 1) * 4].rearrange("b h n d -> n b h d"), writes=[dS0bf])
        s0p = self.rot("mbs0", 2, [128, 512], F32)
        tmb = self.rot("mbtm", 2, [128, 520], F32)
        xh = self.sbt("mbxh", [128, 512], F32); dxh = Dep()
        Bt = self.sbt("mbBt", [128, 256], BF16); dBt = Dep()
        la = self.sbt("mbla", [128, 8], F32); dla = Dep()
        dtt = self.sbt("mbdt", [128, 8], F32); ddt = Dep()
        lam = self.sbt("mblam", [128, NB_S, 8], F32); dlam = Dep()
        e3 = self.sbt("mbe3", [128, 24], F32); de3 = Dep()
        cdS = self.sbt("mbcdS", [128, NB_S, 8], F32); dcdS = Dep()
        Ah = self.rot("mbAh", 2, [128, 128], F32)
        Edec = self.sbt("mbEdec", [128, 1024], F32); dEdec = Dep()
        msc = self.sbt("mbmsc", [128, 256], F32); dmsc = Dep()
        W = self.sbt("mbW", [128, 1024], BF16); dW = Dep()
        xdt = self.sbt("mbxdt", [128, 512], BF16); dxdt = Dep()
        xdd = self.sbt("mbxdd", [128, 512], BF16); dxdd = Dep()
        xddb = self.rot("mbxddb", 2, [128, 512], BF16)
        ctmall = self.sbt("mbctmall", [128, NB_S, 2, 128], BF16); dctmall = Dep()
        y = self.sbt("mby", [128, 512], F32); dy = Dep()
        tmp = self.sbt("mbtmp", [128, 512], F32); dtmp = Dep()
        small = self.sbt("mbsmall", [128, 32], F32); dsmall = Dep()
        for tt in range(NT):
            smp = tt == 16
            sfx = "_S" if smp else "_P"
            t0 = tt * 128
            tm, dtm = tmb.next()
            c.dma("sp", tm[:, 0:512], self.PT[t0:t0 + 128, C_MB:C_MB + 512], reads=[self.dPT[tt]], writes=[dtm])
            c.dma("sp", tm[:, 512:520], self.PT[t0:t0 + 128, C_MB + 1536:C_MB + 1544], reads=[self.dPT[tt]], writes=[dtm])
            p0 = self.pb()
            for j in range(4):
                c.op("pe", lambda e, j=j: e.transpose(self.ps[p0][:, j * 128:(j + 1) * 128], XH[:, j, t0:t0 + 128], self.cst("ident")), reads=[dXH, self.dcst], writes=[self.dps[p0]])
            c.op("act", lambda e: e.copy(out=xh[:], in_=self.ps[p0][:, 0:512]), reads=[self.dps[p0]], writes=[dxh])
            p1 = self.pb()
            psb = self.ps[p1][:].bitcast(BF16)
            for g in range(2):
                c.op("pe", lambda e, g=g: e.transpose(psb[:, g * 128:(g + 1) * 128], BT[:, g, t0:t0 + 128], self.ident_bf[:]), reads=[dBT, self.dcst], writes=[self.dps[p1]])
            c.op("dve", lambda e: e.tensor_copy(out=Bt[:], in_=psb[:, 0:256]), reads=[self.dps[p1]], writes=[dBt])
            c.op("dve", lambda e: e.tensor_tensor(out=dtt[:], in0=tm[:, 512:520], in1=dtb[:], op=ALU.add), reads=[dtm, ddtb], writes=[ddt])
            c.op("act", lambda e: e.activation(out=dtt[:], in_=dtt[:], func=AF.Exp), reads=[ddt], writes=[ddt])
            c.op("act", lambda e: e.activation(out=dtt[:], in_=dtt[:], func=AF.Ln, bias=self.epsb(1.0), scale=1.0), reads=[ddt, self.dcst], writes=[ddt])
            c.op("dve", lambda e: e.tensor_tensor(out=la[:], in0=dtt[:], in1=aneg[:], op=ALU.mult), reads=[ddt, daneg], writes=[dla])
            p2 = self.pb()
            c.op("pe", lambda e: e.matmul(self.ps[p2][:, 0:8], self.cst("le" + sfx), la[:], start=True, stop=True), reads=[dla, self.dcst], writes=[self.dps[p2]])
            c.op("pe", lambda e: e.matmul(self.ps[p2][:, 8:16], self.cst("gt" + sfx), la[:], start=True, stop=True), reads=[dla, self.dcst], writes=[self.dps[p2]])
            c.op("pe", lambda e: e.matmul(self.ps[p2][:, 16:24], self.cst("ones"), la[:], start=True, stop=True), reads=[dla, self.dcst], writes=[self.dps[p2]])
            if smp:
                c.op("dve", lambda e: e.tensor_tensor(out=lam[:], in0=la[:].unsqueeze(1).broadcast_to([128, NB_S, 8]), in1=self.cst("seqmask").unsqueeze(2).broadcast_to([128, NB_S, 8]), op=ALU.mult),
                     reads=[dla, self.dcst], writes=[dlam])
                c.op("pe", lambda e: e.matmul(self.ps[p2][:, 128:256], self.cst("ones"), lam[:].rearrange("p b h -> p (b h)"), start=True, stop=True), reads=[dlam, self.dcst], writes=[self.dps[p2]])
                c.op("act", lambda e: e.activation(out=cdS[:].rearrange("p b h -> p (b h)"), in_=self.ps[p2][:, 128:256], func=AF.Exp), reads=[self.dps[p2]], writes=[dcdS])
            c.op("act", lambda e: e.activation(out=e3[:], in_=self.ps[p2][:, 0:24], func=AF.Exp), reads=[self.dps[p2]], writes=[de3])
            for hg in range(2):
                p3 = self.pb()
                for hh in range(4):
                    h = hg * 4 + hh
                    ah, dah = Ah.next()
                    c.op("dve", lambda e, ah=ah, h=h: e.tensor_scalar(out=ah[:], in0=self.cst("gt" + sfx), scalar1=la[:, h:h + 1], scalar2=None, op0=ALU.mult), reads=[dla, self.dcst], writes=[dah])
                    c.op("pe", lambda e, ah=ah, hh=hh, p3=p3: e.matmul(self.ps[p3][:, hh * 128:(hh + 1) * 128], ah[:], self.cst("le" + sfx), start=True, stop=True), reads=[dah, self.dcst], writes=[self.dps[p3]])
                c.op("act", lambda e, hg=hg, p3=p3: e.activation(out=Edec[:, hg * 512:(hg + 1) * 512], in_=self.ps[p3][:, 0:512], func=AF.Exp), reads=[self.dps[p3]], writes=[dEdec])
            p4 = self.pb()
            for g in range(2):
                c.op("pe", lambda e, g=g: e.matmul(self.ps[p4][:, g * 128:(g + 1) * 128], BT[:, g, t0:t0 + 128], CT[:, g, t0:t0 + 128], start=True, stop=True), reads=[dBT, dCT], writes=[self.dps[p4]])
            cau = self.cst("causal" + sfx).unsqueeze(1).broadcast_to([128, 2, 128])
            c.op("dve", lambda e: e.tensor_tensor(out=msc[:].rearrange("p (g i) -> p g i", g=2), in0=self.ps[p4][:, 0:256].rearrange("p (g i) -> p g i", g=2), in1=cau, op=ALU.mult),
                 reads=[self.dps[p4], self.dcst], writes=[dmsc])
            for g in range(2):
                mb_ = msc[:, g * 128:(g + 1) * 128].unsqueeze(1).broadcast_to([128, 4, 128])
                eng = "dve" if g == 0 else "pool"
                c.op(eng, lambda e, g=g, mb_=mb_: e.tensor_tensor(out=W[:, g * 512:(g + 1) * 512].rearrange("p (h i) -> p h i", h=4), in0=Edec[:, g * 512:(g + 1) * 512].rearrange("p (h i) -> p h i", h=4), in1=mb_, op=ALU.mult),
                     reads=[dEdec, dmsc], writes=[dW])
            dtbc = dtt[:].unsqueeze(2).broadcast_to([128, 8, 64])
            c.op("dve", lambda e: e.tensor_tensor(out=xdt[:].rearrange("p (h d) -> p h d", h=8), in0=xh[:].rearrange("p (h d) -> p h d", h=8), in1=dtbc, op=ALU.mult), reads=[dxh, ddt], writes=[dxdt])
            debc = e3[:, 8:16].unsqueeze(2).broadcast_to([128, 8, 64])
            c.op("pool", lambda e: e.tensor_tensor(out=xdd[:].rearrange("p (h d) -> p h d", h=8), in0=xdt[:].rearrange("p (h d) -> p h d", h=8), in1=debc, op=ALU.mult), reads=[dxdt, de3], writes=[dxdd])
            p5 = self.pb()
            for h in range(8):
                c.op("pe", lambda e, h=h: e.matmul(self.ps[p5][:, h * 64:(h + 1) * 64], W[:, h * 128:(h + 1) * 128], xdt[:, h * 64:(h + 1) * 64], start=True, stop=True), reads=[dW, dxdt], writes=[self.dps[p5]])
            p6 = self.pb()
            if not smp:
                for g in range(2):
                    c.op("pe", lambda e, g=g: e.matmul(self.ps[p6][:, g * 256:(g + 1) * 256], CT[:, g, t0:t0 + 128], Hbf[:, g * 256:(g + 1) * 256], start=True, stop=True), reads=[dCT, dHbf], writes=[self.dps[p6]])
            else:
                for b in range(NB_S):
                    mrb = self.maskrow[:, b, :].unsqueeze(1).broadcast_to([128, 2, 128])
                    c.op("pool", lambda e, b=b, mrb=mrb: e.tensor_tensor(out=ctmall[:, b, :, :], in0=CT[:, :, t0:t0 + 128], in1=mrb, op=ALU.mult), reads=[dCT, self.dcst], writes=[dctmall])
                for g in range(2):
                    for b in range(NB_S):
                        c.op("pe", lambda e, g=g, b=b: e.matmul(self.ps[p6][:, g * 256:(g + 1) * 256], ctmall[:, b, g, :], S0bf[:, b, g * 256:(g + 1) * 256], start=(b == 0), stop=(b == NB_S - 1), skip_group_check=True),
                             reads=[dctmall, dS0bf], writes=[self.dps[p6]])
            ecb = e3[:, 0:8].unsqueeze(2).broadcast_to([128, 8, 64])
            c.op("dve", lambda e: e.tensor_tensor(out=y[:].rearrange("p (h d) -> p h d", h=8), in0=self.ps[p6][:, 0:512].rearrange("p (h d) -> p h d", h=8), in1=ecb, op=ALU.mult),
                 reads=[self.dps[p6], de3, self.dOS[tt]], writes=[dy])
            c.op("dve", lambda e: e.tensor_tensor(out=y[:], in0=y[:], in1=self.ps[p5][:, 0:512], op=ALU.add), reads=[dy, self.dps[p5]], writes=[dy])
            Dbc = Dt[:].unsqueeze(2).broadcast_to([128, 8, 64])
            c.op("pool", lambda e: e.tensor_tensor(out=tmp[:].rearrange("p (h d) -> p h d", h=8), in0=xh[:].rearrange("p (h d) -> p h d", h=8), in1=Dbc, op=ALU.mult), reads=[dxh, dDt], writes=[dtmp])
            c.op("dve", lambda e: e.tensor_tensor(out=y[:], in0=y[:], in1=tmp[:], op=ALU.add), reads=[dy, dtmp], writes=[dy])
            c.op("act", lambda e: e.activation(out=tmp[:], in_=tm[:, 0:512], func=AF.Silu), reads=[dtm, dtmp], writes=[dtmp])
            c.op("dve", lambda e: e.tensor_tensor(out=y[:], in0=y[:], in1=tmp[:], op=ALU.mult), reads=[dy, dtmp], writes=[dy])
            c.op("dve", lambda e: e.tensor_tensor(out=tmp[:], in0=y[:], in1=y[:], op=ALU.mult), reads=[dy], writes=[dtmp])
            c.op("dve", lambda e: e.tensor_reduce(out=small[:, 0:2], in_=tmp[:].rearrange("p (g v) -> p g v", g=2), axis=AX.X, op=ALU.add), reads=[dtmp], writes=[dsmall])
            c.op("act", lambda e: e.activation(out=small[:, 8:10], in_=small[:, 0:2], func=AF.Sqrt, scale=1.0 / 256, bias=self.epsb(1e-5)), reads=[dsmall, self.dcst], writes=[dsmall])
            c.op("dve", lambda e: e.reciprocal(out=small[:, 16:18], in_=small[:, 8:10]), reads=[dsmall], writes=[dsmall])
            rb = small[:, 16:18].unsqueeze(2).broadcast_to([128, 2, 256])
            c.op("dve", lambda e: e.tensor_tensor(out=y[:].rearrange("p (g v) -> p g v", g=2), in0=y[:].rearrange("p (g v) -> p g v", g=2), in1=rb, op=ALU.mult), reads=[dy, dsmall], writes=[dy])
            c.op("pool", lambda e: e.tensor_tensor(out=y[:], in0=y[:], in1=gn[:], op=ALU.mult), reads=[dy, dgn], writes=[dy])
            c.dma("sp", self.OS[t0:t0 + 128, 512:1024], y[:], reads=[dy], writes=[self.dOS[tt]])
            if not smp:
                p7 = self.pb()
                for g in range(2):
                    c.op("pe", lambda e, g=g: e.matmul(self.ps[p7][:, g * 256:(g + 1) * 256], Bt[:, g * 128:(g + 1) * 128], xdd[:, g * 256:(g + 1) * 256], start=True, stop=True), reads=[dBt, dxdd], writes=[self.dps[p7]])
                cdb = e3[:, 16:24].unsqueeze(2).broadcast_to([128, 8, 64])
                c.op("pool", lambda e: e.tensor_tensor(out=H[:].rearrange("p (h d) -> p h d", h=8), in0=H[:].rearrange("p (h d) -> p h d", h=8), in1=cdb, op=ALU.mult), reads=[dH, de3], writes=[dH])
                c.op("dve", lambda e: e.tensor_tensor(out=H[:], in0=H[:], in1=self.ps[p7][:, 0:512], op=ALU.add), reads=[dH, self.dps[p7]], writes=[dH])
                c.op("act", lambda e: e.copy(out=Hbf[:], in_=H[:]), reads=[dH], writes=[dHbf])
                if tt == 15:
                    do = self.odep()
                    c.dma("sp", self.O["p_ssm"][l].rearrange("h n d -> n h d"), H[:].rearrange("n (h d) -> n h d", h=8), reads=[dH], writes=[do])
            else:
                for b in range(NB_S):
                    xb_, dxb_ = xddb.next()
                    c.op("pool", lambda e, xb_=xb_, b=b: e.tensor_scalar(out=xb_[:], in0=xdd[:], scalar1=self.cst("seqmask")[:, b:b + 1], scalar2=None, op0=ALU.mult), reads=[dxdd, self.dcst], writes=[dxb_])
                    p7 = self.pb()
                    for g in range(2):
                        c.op("pe", lambda e, g=g, xb_=xb_, p7=p7: e.matmul(self.ps[p7][:, g * 256:(g + 1) * 256], Bt[:, g * 128:(g + 1) * 128], xb_[:, g * 256:(g + 1) * 256], start=True, stop=True), reads=[dBt, dxb_], writes=[self.dps[p7]])
                    s0, ds0 = s0p.next()
                    c.dma("sp", s0[:].rearrange("n (h d) -> n h d", h=8), self.I["st_ssm"][l, b].rearrange("h n d -> n h d"), writes=[ds0])
                    cdb = cdS[:, b, :].unsqueeze(2).broadcast_to([128, 8, 64])
                    c.op("pool", lambda e, s0=s0, cdb=cdb: e.tensor_tensor(out=s0[:].rearrange("p (h d) -> p h d", h=8), in0=s0[:].rearrange("p (h d) -> p h d", h=8), in1=cdb, op=ALU.mult), reads=[ds0, dcdS], writes=[ds0])
                    c.op("dve", lambda e, s0=s0, p7=p7: e.tensor_tensor(out=s0[:], in0=s0[:], in1=self.ps[p7][:, 0:512], op=ALU.add), reads=[ds0, self.dps[p7]], writes=[ds0])
                    do = self.odep()
                    c.dma("sp", self.O["s_ssm"][l, b].rearrange("h n d -> n h d"), s0[:].rearrange("n (h d) -> n h d", h=8), reads=[ds0], writes=[do])


Prog.mix_ssd = _ssd_phase


SDEC = 0.6065306597126334


def _rwkv_phase(self, l):
    c = self.c
    with self.scope():
        mu, dmu = self.col_load("rwmu", self.I["rw_mu"][l], 14)
        w0, dw0 = self.col_load("rww0", self.I["rw_w0"][l], 4)
        a0, da0 = self.col_load("rwa0", self.I["rw_a0"][l], 4)
        kk_, dkk_ = self.col_load("rwkk", self.I["rw_k_k"][l], 4)
        ka, dka = self.col_load("rwka", self.I["rw_k_a"][l], 4)
        rk_, drk_ = self.col_load("rwrk", self.I["rw_r_k"][l], 4)
        omka = self.sbt("rwomka", [128, 4], F32); domka = Dep()
        c.op("dve", lambda e: e.tensor_scalar(out=omka[:], in0=ka[:], scalar1=-1.0, scalar2=1.0, op0=ALU.mult, op1=ALU.add), reads=[dka], writes=[domka])
        lora = self.sbt("rwlora", [128, 512], BF16); dlora = Dep()
        c.dma("pool", lora[0:64, :], self.I["rw_w_up"][l], writes=[dlora])
        c.dma("pool", lora[64:128, :], self.I["rw_a_up"][l], writes=[dlora])
        gup = self.sbt("rwgup", [128, 512], BF16); dgup = Dep()
        c.dma("pool", gup[:], self.I["rw_g_up"][l], writes=[dgup])
        lng, dlng = self.bcast_row("rwlng", self.I["rw_ln_g"][l:l + 1, :], 512)
        lnb, dlnb = self.bcast_row("rwlnb", self.I["rw_ln_b"][l:l + 1, :], 512)
        shT = self.sbt("rwshT", [128, 14, 16], F32); dshT = Dep()
        with self.scope():
            shraw = self.sbt("rwshraw", [16, RW_IN], F32); dshraw = Dep()
            c.dma("sp", shraw[:], self.I["st_shift"][l], writes=[dshraw])
            ph = self.pb()
            for j in range(14):
                c.op("pe", lambda e, j=j: e.transpose(self.ps[ph][:, j * 16:(j + 1) * 16], shraw[:, j * 128:(j + 1) * 128], self.cst("ident")[0:16, 0:16]), reads=[dshraw, self.dcst], writes=[self.dps[ph]])
            c.op("dve", lambda e: e.tensor_copy(out=shT[:].rearrange("p j b -> p (j b)"), in_=self.ps[ph][:, 0:224]), reads=[self.dps[ph]], writes=[dshT])
        EB = self.sbt("rwEB", [128, 4, NBLK], F32); dEB = Dep()
        RK = self.sbt("rwRK", [128, NT, 8], F32); dRK = Dep()
        sgd = self.sbt("rwsgd", [128, TOK], BF16); dsgd = Dep()
        dscr = self.dRWS
        with self.scope():
            reset = self.sbt("rwreset", [128, TOK], F32); dreset = Dep()
            c.dma("sp", reset[:], self.I["tokmask"][0:1, :].partition_broadcast(128), writes=[dreset])
            X = self.sbt("rwX", [128, 1 + TOK], F32); dX = Dep()
            bufs = {n: (self.sbt("rw" + n, [128, TOK], F32), Dep()) for n in "ABCDEF"}
            twd = self.sbt("rwtwd", [128, TOK], BF16); dtwd = Dep()
            adb = self.sbt("rwadb", [128, TOK], BF16); dadb = Dep()
            bT = self.sbt("rwbT", [128, TOK], BF16); dbT = Dep()
            kT = self.sbt("rwkT", [128, TOK], BF16); dkT = Dep()
            stb = self.rot("rwstb", 2, [128, TOK], BF16)
            tst = self.rot("rwtst", 3, [128, 128], BF16)
            tsf = self.rot("rwtsf", 2, [128, 128], F32)
            c.op("pool", lambda e: e.memset(X[:, 0:1], 0.0), writes=[dX])

            def xs_chunk(j, dst, ddst):
                r0 = PF_RW + j * 128
                c.dma("sp", X[:, 1:1 + TOK], self.PF[r0:r0 + 128, :], reads=[self.dPF], writes=[dX])
                c.op("dve", lambda e: e.tensor_tensor(out=dst[:], in0=X[:, 0:TOK], in1=X[:, 1:1 + TOK], op=ALU.subtract), reads=[dX], writes=[ddst])
                c.op("dve", lambda e: e.scalar_tensor_tensor(out=dst[:], in0=dst[:], scalar=mu[:, j:j + 1], in1=X[:, 1:1 + TOK], op0=ALU.mult, op1=ALU.add), reads=[dX, dmu, ddst], writes=[ddst])
                d0 = dst[:, SEQ:TOK].rearrange("p (b t) -> p b t", t=TS)[:, :, 0]
                p0 = X[:, 1 + SEQ:1 + TOK].rearrange("p (b t) -> p b t", t=TS)[:, :, 0]
                c.op("dve", lambda e: e.tensor_tensor(out=d0, in0=shT[:, j, :], in1=p0, op=ALU.subtract), reads=[dX, dshT, ddst], writes=[ddst])
                c.op("dve", lambda e: e.scalar_tensor_tensor(out=d0, in0=d0, scalar=mu[:, j:j + 1], in1=p0, op0=ALU.mult, op1=ALU.add), reads=[dX, dmu, ddst], writes=[ddst])

            A, dA = bufs["A"]; B, dB = bufs["B"]; C, dC = bufs["C"]; Dd, dDd = bufs["D"]; E, dE = bufs["E"]; Fb, dFb = bufs["F"]
            xs_chunk(12, A, dA)
            c.op("act", lambda e: e.activation(out=twd[0:64, :], in_=A[0:64, :], func=AF.Tanh), reads=[dA], writes=[dtwd])
            c.op("act", lambda e: e.copy(out=adb[64:128, :], in_=A[64:128, :]), reads=[dA], writes=[dadb])
            xs_chunk(13, A, dA)
            c.op("act", lambda e: e.activation(out=sgd[:], in_=A[:], func=AF.Sigmoid), reads=[dA], writes=[dsgd])
            for cc in range(4):
                xs_chunk(8 + cc, A, dA)
                for tt in range(NT):
                    pi = self.pb()
                    c.op("pe", lambda e, tt=tt, pi=pi: e.transpose(self.ps[pi][:, 0:128], A[:, tt * 128:(tt + 1) * 128], self.cst("ident")), reads=[dA, self.dcst], writes=[self.dps[pi]])
                    sf, dsf = tsf.next()
                    self.evac(pi, 128, 128, sf[:], dsf, tt)
                    c.dma("sp", self.VV[tt * 128:(tt + 1) * 128, cc * 128:(cc + 1) * 128], sf[:], reads=[dsf], writes=[dscr])
                for (t0, tw) in TBLK:
                    pi = self.pb()
                    c.op("pe", lambda e, pi=pi, t0=t0, tw=tw: e.matmul(self.ps[pi][:, 0:tw], lora[0:64, cc * 128:(cc + 1) * 128], twd[0:64, t0:t0 + tw], start=True, stop=True), reads=[dlora, dtwd], writes=[self.dps[pi]])
                    c.op("act", lambda e, pi=pi, t0=t0, tw=tw: e.activation(out=B[:, t0:t0 + tw], in_=self.ps[pi][:, 0:tw], func=AF.Sigmoid, bias=w0[:, cc:cc + 1], scale=1.0), reads=[self.dps[pi], dw0], writes=[dB])
                    pi = self.pb()
                    c.op("pe", lambda e, pi=pi, t0=t0, tw=tw: e.matmul(self.ps[pi][:, 0:tw], lora[64:128, cc * 128:(cc + 1) * 128], adb[64:128, t0:t0 + tw], start=True, stop=True), reads=[dlora, dadb], writes=[self.dps[pi]])
                    c.op("act", lambda e, pi=pi, t0=t0, tw=tw: e.activation(out=C[:, t0:t0 + tw], in_=self.ps[pi][:, 0:tw], func=AF.Sigmoid, bias=a0[:, cc:cc + 1], scale=1.0), reads=[self.dps[pi], da0], writes=[dC])
                c.op("dve", lambda e: e.tensor_tensor_scan(out=Dd[:], data0=reset[:], data1=B[:], initial=0.0, op0=ALU.mult, op1=ALU.add), reads=[dreset, dB], writes=[dDd])
                xs_chunk(4 + cc, A, dA)
                c.op("dve", lambda e: e.tensor_scalar(out=E[:], in0=A[:], scalar1=kk_[:, cc:cc + 1], scalar2=None, op0=ALU.mult), reads=[dA, dkk_], writes=[dE])
                c.op("pool", lambda e: e.tensor_tensor(out=Fb[:], in0=E[:], in1=E[:], op=ALU.mult), reads=[dE], writes=[dFb])
                for (t0, tw) in TBLK:
                    pi = self.pb()
                    c.op("pe", lambda e, pi=pi, t0=t0, tw=tw: e.matmul(self.ps[pi][:, 0:tw], self.cst("blockones"), Fb[:, t0:t0 + tw], start=True, stop=True), reads=[dFb, self.dcst], writes=[self.dps[pi]])
                    c.op("dve", lambda e, pi=pi, t0=t0, tw=tw: e.tensor_scalar(out=Fb[:, t0:t0 + tw], in0=self.ps[pi][:, 0:tw], scalar1=1e-24, scalar2=None, op0=ALU.max), reads=[self.dps[pi], dFb], writes=[dFb])
                c.op("act", lambda e: e.activation(out=Fb[:], in_=Fb[:], func=AF.Sqrt), reads=[dFb], writes=[dFb])
                c.op("dve", lambda e: e.reciprocal(out=Fb[:], in_=Fb[:]), reads=[dFb], writes=[dFb])
                c.op("dve", lambda e: e.tensor_tensor(out=E[:], in0=E[:], in1=Fb[:], op=ALU.mult), reads=[dE, dFb], writes=[dE])
                c.op("dve", lambda e: e.tensor_scalar(out=Fb[:], in0=C[:], scalar1=ka[:, cc:cc + 1], scalar2=omka[:, cc:cc + 1], op0=ALU.mult, op1=ALU.add), reads=[dC, dka, domka, dFb], writes=[dFb])
                c.op("pool", lambda e: e.tensor_tensor(out=Fb[:], in0=Fb[:], in1=A[:], op=ALU.mult), reads=[dFb, dA], writes=[dFb])
                c.op("dve", lambda e: e.tensor_tensor(out=C[:], in0=C[:], in1=E[:], op=ALU.mult), reads=[dC, dE], writes=[dC])
                c.op("dve", lambda e: e.tensor_tensor(out=A[:], in0=Dd[:], in1=B[:], op=ALU.subtract), reads=[dDd, dB, dA], writes=[dA])
                c.op("act", lambda e: e.activation(out=A[:], in_=A[:], func=AF.Exp, scale=-SDEC), reads=[dA], writes=[dA])
                st1, dst1 = stb.next()
                c.op("dve", lambda e, st1=st1: e.tensor_tensor(out=st1[:], in0=E[:], in1=A[:], op=ALU.mult), reads=[dE, dA], writes=[dst1])
                c.dma("sp", self.KAPT[cc][:, 0:TOK], st1[:], reads=[dst1], writes=[dscr])
                c.op("act", lambda e: e.activation(out=A[:], in_=Dd[:], func=AF.Exp, scale=SDEC), reads=[dDd, dA], writes=[dA])
                c.op("dve", lambda e: e.scalar_tensor_tensor(out=bT[:], in0=C[:], scalar=-1.0, in1=A[:], op0=ALU.mult, op1=ALU.mult), reads=[dC, dA], writes=[dbT])
                c.op("pool", lambda e: e.tensor_tensor(out=kT[:], in0=Fb[:], in1=A[:], op=ALU.mult), reads=[dFb, dA], writes=[dkT])
                for tt in range(NT):
                    for (srcT, dsrcT, dstS) in ((bT, dbT, self.BH), (kT, dkT, self.KH)):
                        pi = self.pb()
                        psb = self.ps[pi][:].bitcast(BF16)
                        c.op("pe", lambda e, tt=tt, psb=psb, srcT=srcT: e.transpose(psb[:, 0:128], srcT[:, tt * 128:(tt + 1) * 128], self.ident_bf[:]), reads=[dsrcT, self.dcst], writes=[self.dps[pi]])
                        sb_, dsb = tst.next()
                        c.op("act" if tt % 2 == 0 else "dve", (lambda e, sb_=sb_, psb=psb: e.copy(out=sb_[:], in_=psb[:, 0:128])) if tt % 2 == 0 else (lambda e, sb_=sb_, psb=psb: e.tensor_copy(out=sb_[:], in_=psb[:, 0:128])),
                             reads=[self.dps[pi]], writes=[dsb])
                        c.dma("sp", dstS[tt * 128:(tt + 1) * 128, cc * 128:(cc + 1) * 128], sb_[:], reads=[dsb], writes=[dscr])
                xs_chunk(cc, E, dE)
                c.op("dve", lambda e: e.scalar_tensor_tensor(out=C[:], in0=E[:], scalar=rk_[:, cc:cc + 1], in1=Fb[:], op0=ALU.mult, op1=ALU.mult), reads=[dE, drk_, dFb, dC], writes=[dC])
                for tt in range(NT):
                    pi = self.pb()
                    c.op("pe", lambda e, tt=tt, pi=pi: e.matmul(self.ps[pi][:, 0:2], C[:, tt * 128:(tt + 1) * 128], self.cst("halfsel"), start=True, stop=True), reads=[dC, self.dcst], writes=[self.dps[pi]])
                    c.op("dve", lambda e, tt=tt, pi=pi: e.tensor_copy(out=RK[:, tt, 2 * cc:2 * cc + 2], in_=self.ps[pi][:, 0:2]), reads=[self.dps[pi]], writes=[dRK])
                c.op("act", lambda e: e.activation(out=A[:], in_=Dd[:], func=AF.Exp, scale=-SDEC), reads=[dDd, dA], writes=[dA])
                c.op("dve", lambda e: e.tensor_copy(out=EB[:, cc, 0:SEQ // LB], in_=A[:, 0:SEQ].rearrange("p (n t) -> p n t", t=LB)[:, :, LB - 1]), reads=[dA], writes=[dEB])
                c.op("dve", lambda e: e.tensor_copy(out=EB[:, cc, SEQ // LB:NBLK], in_=A[:, SEQ:TOK].rearrange("p (n t) -> p n t", t=TS)[:, :, TS - 1]), reads=[dA], writes=[dEB])
                st2, dst2 = stb.next()
                c.op("dve", lambda e, st2=st2: e.tensor_tensor(out=st2[:], in0=E[:], in1=A[:], op=ALU.mult), reads=[dE, dA], writes=[dst2])
                c.op("pool", lambda e, st2=st2: e.tensor_copy(out=st2[:, 0:SEQ].rearrange("p (n t) -> p n t", t=LB)[:, :, LB - 1], in_=E[:, 0:SEQ].rearrange("p (n t) -> p n t", t=LB)[:, :, LB - 1]), reads=[dE, dst2], writes=[dst2])
                c.op("pool", lambda e, st2=st2: e.tensor_copy(out=st2[:, SEQ:TOK].rearrange("p (n t) -> p n t", t=TS)[:, :, TS - 1], in_=E[:, SEQ:TOK].rearrange("p (n t) -> p n t", t=TS)[:, :, TS - 1]), reads=[dE, dst2], writes=[dst2])
                c.dma("sp", self.RHT[cc], st2[:], reads=[dst2], writes=[dscr])
        self.rwkv_scan(l, EB, dEB)
        self.rwkv_post(l, RK, dRK, sgd, dsgd, gup, dgup, lng, dlng, lnb, dlnb)


Prog.mix_rwkv = _rwkv_phase


def _rwkv_scan(self, l, EB, dEB):
    c = self.c
    dscr = self.dRWS
    NR = 32
    with self.scope():
        self.ps_i = 0
        old_pb = self.pb

        def pb4():
            i = self.ps_i % 4
            self.ps_i += 1
            return i
        self.pb = pb4
        ACC = [4, 5]
        P1 = [6, 7]
        zt = self.sbt("rwz", [128, 8], BF16); dzt = Dep()
        c.op("dve", lambda e: e.memset(zt[:], 0.0), writes=[dzt])
        for cc in range(4):
            c.dma("sp", self.KAPT[cc][:, TOK:TOK + 8], zt[:], reads=[dzt], writes=[dscr])
        Sbf = self.sbt("rwSbf", [128, 256], BF16)
        dSbf = [Dep(), Dep()]
        L1p = self.rot("rwL1", 2, [128, 129, 48], BF16)
        L2p = self.rot("rwL2", 1, [72, 128, 128], BF16)
        Rp = [self.rot("rwRa", 2, [72, NR, 128], BF16), self.rot("rwRb", 2, [72, NR, 128], BF16)]
        for (t_, d_) in L1p.items + L2p.items + Rp[0].items + Rp[1].items:
            c.op("pool", lambda e, t_=t_: e.memset(t_[:], 0.0), writes=[d_])
        kapb = self.rot("rwkap", 2, [128, 4, 129], BF16)
        rhb = self.rot("rwrh", 2, [128, 4, 128], BF16)
        s0raw = self.rot("rws0raw", 2, [64, 512], F32)
        s0st = self.rot("rws0st", 2, [128, 256], F32)
        sfin = self.rot("rwsfin", 2, [128, 256], F32)
        sout = self.rot("rwsout", 2, [64, 512], F32)
        bm = self.cst("rw_blockmask")
        hm = self.cst("rw_halfmask")

        def build_tile(tt):
            t0 = tt * 128
            kap, dkap = kapb.next()
            rh, drh = rhb.next()
            for cc in range(4):
                c.dma("sp", kap[:, cc, :], self.KAPT[cc][:, t0:t0 + 129], reads=[dscr], writes=[dkap])
                c.dma("sp", rh[:, cc, :], self.RHT[cc][:, t0:t0 + 128], reads=[dscr], writes=[drh])
            L1, dL1 = L1p.next()
            hmb = hm.unsqueeze(1).unsqueeze(1).broadcast_to([128, 129, 4, 2])
            c.op("pool", lambda e: e.tensor_tensor(out=L1[:, :, 0:8].rearrange("p t (c h) -> p t c h", c=4), in0=kap[:].rearrange("p c t -> p t c").unsqueeze(3).broadcast_to([128, 129, 4, 2]),
                                                   in1=hmb, op=ALU.mult), reads=[dkap, self.dcst], writes=[dL1])
            hmb2 = hm.unsqueeze(1).unsqueeze(1).broadcast_to([128, 128, 4, 2])
            c.op("pool", lambda e: e.tensor_tensor(out=L1[:, 1:129, 32:40].rearrange("p t (c h) -> p t c h", c=4), in0=rh[:].rearrange("p c t -> p t c").unsqueeze(3).broadcast_to([128, 128, 4, 2]),
                                                   in1=hmb2, op=ALU.mult), reads=[drh, self.dcst], writes=[dL1])
            L2, dL2 = L2p.next()
            for h2 in range(2):
                srcb = self.BH[t0:t0 + 128, :].rearrange("t (c h k) -> c h t k", c=4, h=2)[:, h2]
                c.dma("sp", L2[h2:8:2, :, h2 * 64:(h2 + 1) * 64], srcb, reads=[dscr], writes=[dL2])
                srck = self.KH[t0:t0 + 128, :].rearrange("t (c h k) -> c h t k", c=4, h=2)[:, h2]
                c.dma("sp", L2[64 + h2:72:2, :, h2 * 64:(h2 + 1) * 64], srck, reads=[dscr], writes=[dL2])
            return L1, dL1, L2, dL2

        def fill_R(X, tok0, n):
            R, dR = Rp[X].next()
            for cl in range(2):
                cc = 2 * X + cl
                src = self.VV[tok0:tok0 + n, cc * 128:(cc + 1) * 128].rearrange("t (h v) -> h t v", h=2)
                c.dma("pool", R[64 + 2 * cc:64 + 2 * cc + 2, 0:n, cl * 64:(cl + 1) * 64], src, reads=[dscr], writes=[dR])
            return R, dR

        def extract_o(X, R, dR, tok_first, s_first, n):
            for cl in range(2):
                cc = 2 * X + cl
                dst = self.ORW[tok_first:tok_first + n, cc * 128:(cc + 1) * 128].rearrange("t (h v) -> h t v", h=2)
                c.dma("sp", dst, R[32 + 2 * cc:32 + 2 * cc + 2, s_first:s_first + n, cl * 64:(cl + 1) * 64], reads=[dR], writes=[self.dORW])

        def mask_op(X, R, dR, slot):
            c.op("dve", lambda e: e.tensor_tensor(out=R[0:40, slot, :], in0=self.ps[P1[X]][0:40, 0:128], in1=bm[0:40, X * 128:(X + 1) * 128], op=ALU.mult),
                 reads=[self.dps[P1[X]], self.dcst], writes=[dR])

        def stage1(X, L1, dL1, entry):
            c.op("pe", lambda e: e.matmul(self.ps[P1[X]][0:40, 0:128], L1[:, entry, 0:40], Sbf[:, X * 128:(X + 1) * 128], start=True, stop=True),
                 reads=[dL1, dSbf[X]], writes=[self.dps[P1[X]]])

        def run_chain(t_first, n, first_start, blk_of):
            Rcur = [None, None]

            def ring(X, idx):
                t = t_first + idx
                slot = idx % NR
                q = idx // NR
                if slot == 0:
                    if Rcur[X] is not None:
                        pb_ = t_first + (q - 1) * NR
                        s0_ = 1 if q - 1 == 0 else 0
                        extract_o(X, Rcur[X][0], Rcur[X][1], pb_ + s0_ - 1, s0_, NR - s0_)
                    if idx < n:
                        Rcur[X] = fill_R(X, t, min(NR, n - idx))
                    else:
                        Rcur[X] = Rp[X].next()
                R, dR = Rcur[X]
                mask_op(X, R, dR, slot)
                if idx == n:
                    base = t_first + q * NR
                    s0_ = 1 if q == 0 else 0
                    if slot + 1 - s0_ > 0:
                        extract_o(X, R, dR, base + s0_ - 1, s0_, slot + 1 - s0_)

            def s2(X, idx, L2, dL2):
                t = t_first + idx
                R, dR = Rcur[X]
                acc = self.ps[ACC[X]]
                c.op("pe", lambda e: e.matmul(acc[:, 0:128], L2[0:72, t % 128, :], R[0:72, idx % NR, :], start=(first_start and idx == 0), stop=True, skip_group_check=True),
                     reads=[dL2, dR], writes=[self.dps[ACC[X]]])

            def cp(X, idx):
                t = t_first + idx
                acc = self.ps[ACC[X]]
                bend = blk_of(t)
                if bend is not None:
                    eb = EB[:, 2 * X:2 * X + 2, bend:bend + 1].broadcast_to([128, 2, 64])
                    c.op("dve", lambda e: e.tensor_tensor(out=acc[:, 0:128].rearrange("p (c v) -> p c v", c=2), in0=acc[:, 0:128].rearrange("p (c v) -> p c v", c=2), in1=eb, op=ALU.mult),
                         reads=[self.dps[ACC[X]], dEB], writes=[self.dps[ACC[X]]])
                c.op("act", lambda e: e.copy(out=Sbf[:, X * 128:(X + 1) * 128], in_=acc[:, 0:128]), reads=[self.dps[ACC[X]]], writes=[dSbf[X]])

            Lprev = None
            for idx in range(n):
                t = t_first + idx
                tl = t % 128
                if tl == 0 and self._cur_tile != t // 128:
                    self._cur_L = build_tile(t // 128)
                    self._cur_tile = t // 128
                L1, dL1, L2, dL2 = self._cur_L
                ring(0, idx)
                s2(0, idx, L2, dL2)
                if idx > 0:
                    pL1, pdL1, ptl = Lprev
                    stage1(1, pL1, pdL1, ptl + 1)
                cp(0, idx)
                stage1(0, L1, dL1, tl + 1)
                ring(1, idx)
                s2(1, idx, L2, dL2)
                cp(1, idx)
                Lprev = (L1, dL1, tl)
            pL1, pdL1, ptl = Lprev
            stage1(1, pL1, pdL1, ptl + 1)
            ring(0, n)
            ring(1, n)

        self._cur_L = None
        self._cur_tile = -1
        c.op("dve", lambda e: e.memset(Sbf[:], 0.0), writes=dSbf)
        self._cur_L = build_tile(0)
        self._cur_tile = 0
        for X in range(2):
            stage1(X, self._cur_L[0], self._cur_L[1], 0)
        run_chain(0, SEQ, True, lambda t: (t // LB) if (t % LB == LB - 1) else None)
        self._rw_state_out(ACC, sfin, sout, self.O["p_wkv"][l])
        for b in range(NB_S):
            raw, draw = s0raw.next()
            c.dma("sp", raw[:].rearrange("v (h k) -> v h k", h=8), self.I["st_wkv"][l, b].rearrange("h v k -> v h k"), writes=[draw])
            pi = self.pb()
            for cc in range(4):
                c.op("pe", lambda e, cc=cc, pi=pi: e.transpose(self.ps[pi][:, cc * 64:(cc + 1) * 64], raw[:, cc * 128:(cc + 1) * 128], self.cst("ident")[0:64, 0:64]), reads=[draw, self.dcst], writes=[self.dps[pi]])
            s0, ds0 = s0st.next()
            c.op("dve", lambda e, s0=s0, pi=pi: e.tensor_copy(out=s0[:], in_=self.ps[pi][:, 0:256]), reads=[self.dps[pi]], writes=[ds0])
            for X in range(2):
                c.op("pe", lambda e, s0=s0, X=X: e.matmul(self.ps[ACC[X]][:, 0:128], self.cst("ident"), s0[:, X * 128:(X + 1) * 128], start=True, stop=True, skip_group_check=True),
                     reads=[ds0, self.dcst], writes=[self.dps[ACC[X]]])
                c.op("act", lambda e, s0=s0, X=X: e.copy(out=Sbf[:, X * 128:(X + 1) * 128], in_=s0[:, X * 128:(X + 1) * 128]), reads=[ds0], writes=[dSbf[X]])
            t0 = SEQ + b * TS
            if self._cur_tile != 16:
                self._cur_L = build_tile(16)
                self._cur_tile = 16
            for X in range(2):
                stage1(X, self._cur_L[0], self._cur_L[1], (t0 % 128))
            run_chain(t0, TS, False, lambda t: (SEQ // LB + (t - SEQ) // TS) if ((t - SEQ) % TS == TS - 1) else None)
            self._rw_state_out(ACC, sfin, sout, self.O["s_wkv"][l, b])
        self.pb = old_pb


def _rw_state_out(self, ACC, sfin, sout, out_ap):
    c = self.c
    sf, dsf = sfin.next()
    for X in range(2):
        c.op("dve", lambda e, X=X: e.tensor_copy(out=sf[:, X * 128:(X + 1) * 128], in_=self.ps[ACC[X]][:, 0:128]), reads=[self.dps[ACC[X]]], writes=[dsf])
    pi = self.pb()
    for cc in range(4):
        c.op("pe", lambda e, cc=cc: e.transpose(self.ps[pi][0:64, cc * 128:(cc + 1) * 128], sf[:, cc * 64:(cc + 1) * 64], self.cst("ident")), reads=[dsf, self.dcst], writes=[self.dps[pi]])
    so, dso = sout.next()
    c.op("act", lambda e: e.copy(out=so[:], in_=self.ps[pi][0:64, 0:512]), reads=[self.dps[pi]], writes=[dso])
    do = self.odep()
    c.dma("sp", out_ap.rearrange("h v k -> v h k"), so[:].rearrange("v (h k) -> v h k", h=8), reads=[dso], writes=[do])


def _rwkv_post(self, l, RK, dRK, sgd, dsgd, gup, dgup, lng, dlng, lnb, dlnb):
    c = self.c
    with self.scope():
        ob = self.rot("rwo", 2, [128, 512], BF16)
        vb = self.rot("rwv", 2, [128, 512], F32)
        on = self.sbt("rwon", [128, 512], F32); don = Dep()
        tmp = self.sbt("rwtmp", [128, 512], F32); dtmp = Dep()
        sm = self.sbt("rwsm", [128, 48], F32); dsm = Dep()
        for tt in range(NT):
            t0 = tt * 128
            o, do_ = ob.next()
            c.dma("sp", o[:], self.ORW[t0:t0 + 128, :], reads=[self.dORW], writes=[do_])
            v, dv = vb.next()
            c.dma("sp", v[:], self.VV[t0:t0 + 128, :], reads=[self.dRWS], writes=[dv])
            o3 = o[:].rearrange("p (h v) -> p h v", h=8)
            c.op("dve", lambda e: e.tensor_reduce(out=sm[:, 0:8], in_=o3, axis=AX.X, op=ALU.add), reads=[do_], writes=[dsm])
            c.op("dve", lambda e: e.tensor_tensor(out=tmp[:], in0=o[:], in1=o[:], op=ALU.mult), reads=[do_], writes=[dtmp])
            c.op("dve", lambda e: e.tensor_reduce(out=sm[:, 8:16], in_=tmp[:].rearrange("p (h v) -> p h v", h=8), axis=AX.X, op=ALU.add), reads=[dtmp], writes=[dsm])
            c.op("dve", lambda e: e.tensor_scalar(out=sm[:, 0:16], in0=sm[:, 0:16], scalar1=1.0 / 64, scalar2=None, op0=ALU.mult), reads=[dsm], writes=[dsm])
            c.op("dve", lambda e: e.tensor_tensor(out=sm[:, 16:24], in0=sm[:, 0:8], in1=sm[:, 0:8], op=ALU.mult), reads=[dsm], writes=[dsm])
            c.op("dve", lambda e: e.tensor_tensor(out=sm[:, 24:32], in0=sm[:, 8:16], in1=sm[:, 16:24], op=ALU.subtract), reads=[dsm], writes=[dsm])
            c.op("act", lambda e: e.activation(out=sm[:, 32:40], in_=sm[:, 24:32], func=AF.Sqrt, bias=self.epsb(float(RW_LN_EPS)), scale=1.0), reads=[dsm, self.dcst], writes=[dsm])
            c.op("dve", lambda e: e.reciprocal(out=sm[:, 40:48], in_=sm[:, 32:40]), reads=[dsm], writes=[dsm])
            on3 = on[:].rearrange("p (h v) -> p h v", h=8)
            c.op("dve", lambda e: e.tensor_tensor(out=on3, in0=o3, in1=sm[:, 0:8].unsqueeze(2).broadcast_to([128, 8, 64]), op=ALU.subtract), reads=[do_, dsm, self.dOS[tt]], writes=[don])
            c.op("dve", lambda e: e.tensor_tensor(out=on3, in0=on3, in1=sm[:, 40:48].unsqueeze(2).broadcast_to([128, 8, 64]), op=ALU.mult), reads=[don, dsm], writes=[don])
            c.op("pool", lambda e: e.tensor_tensor(out=on[:], in0=on[:], in1=lng[:], op=ALU.mult), reads=[don, dlng], writes=[don])
            c.op("pool", lambda e: e.tensor_tensor(out=on[:], in0=on[:], in1=lnb[:], op=ALU.add), reads=[don, dlnb], writes=[don])
            c.op("dve", lambda e: e.tensor_tensor(out=tmp[:].rearrange("p (h v) -> p h v", h=8), in0=v[:].rearrange("p (h v) -> p h v", h=8), in1=RK[:, tt, :].unsqueeze(2).broadcast_to([128, 8, 64]), op=ALU.mult),
                 reads=[dv, dRK, dtmp], writes=[dtmp])
            c.op("dve", lambda e: e.tensor_tensor(out=on[:], in0=on[:], in1=tmp[:], op=ALU.add), reads=[don, dtmp], writes=[don])
            pi = self.pb()
            c.op("pe", lambda e, pi=pi: e.matmul(self.ps[pi][:, 0:512], sgd[:, t0:t0 + 128], gup[:], start=True, stop=True), reads=[dsgd, dgup], writes=[self.dps[pi]])
            c.op("dve", lambda e, pi=pi: e.tensor_tensor(out=on[:], in0=on[:], in1=self.ps[pi][:, 0:512], op=ALU.mult), reads=[don, self.dps[pi]], writes=[don])
            c.dma("sp", self.OS[t0:t0 + 128, 0:512], on[:], reads=[don], writes=[self.dOS[tt]])


Prog.rwkv_scan = _rwkv_scan
Prog._rw_state_out = _rw_state_out
Prog.rwkv_post = _rwkv_post


def _ln_phase(self, l, gname, bname, final):
    c = self.c
    with self.scope():
        gt, dgt = self.bcast_row("lng", self.I[gname][l:l + 1, :], D)
        bt, dbt = self.bcast_row("lnb", self.I[bname][l:l + 1, :], D)
        xa_p = self.rot("lnxa", 2, [128, D], F32)
        xb_p = self.rot("lnxb", 2, [128, D], F32)
        xh_p = self.rot("lnxh", 2, [128, D], BF16)
        st = self.sbt("lnst", [128, 32], F32); dst_ = Dep()
        for tt in range(NT):
            rows = slice(tt * 128, (tt + 1) * 128)
            xa, dxa = xa_p.next()
            xb, dxb = xb_p.next()
            if self.res_from_input:
                src = self.I["xp"][rows, :] if tt < 16 else self.I["xs"]
                c.dma("sp", xa[:], src, writes=[dxa])
            else:
                c.dma("sp", xa[:], self.XRES[rows, :], reads=[self.dXRES[tt]], writes=[dxa])
            c.dma("sp", xb[:], self.MIX[rows, :], reads=[self.dMIX[tt]], writes=[dxb])
            c.op("dve", lambda e: e.scalar_tensor_tensor(out=xa[:], in0=xa[:], scalar=float(DN_ALPHA), in1=xb[:], op0=ALU.mult, op1=ALU.add), reads=[dxa, dxb], writes=[dxa])
            for i in range(4):
                c.op("dve", lambda e, i=i: e.bn_stats(out=st[:, i * 6:(i + 1) * 6], in_=xa[:, i * 512:(i + 1) * 512]), reads=[dxa], writes=[dst_])
            c.op("dve", lambda e: e.bn_aggr(out=st[:, 24:26], in_=st[:, 0:24]), reads=[dst_], writes=[dst_])
            c.op("act", lambda e: e.activation(out=st[:, 26:27], in_=st[:, 25:26], func=AF.Sqrt, bias=self.epsb(1e-5), scale=1.0), reads=[dst_, self.dcst], writes=[dst_])
            c.op("dve", lambda e: e.reciprocal(out=st[:, 27:28], in_=st[:, 26:27]), reads=[dst_], writes=[dst_])
            c.op("dve", lambda e: e.tensor_scalar(out=st[:, 28:29], in0=st[:, 24:25], scalar1=st[:, 27:28], scalar2=-1.0, op0=ALU.mult, op1=ALU.mult), reads=[dst_], writes=[dst_])
            c.op("act", lambda e: e.activation(out=xb[:], in_=xa[:], func=AF.Identity, scale=st[:, 27:28], bias=st[:, 28:29]), reads=[dxa, dst_, dxb], writes=[dxb])
            c.op("pool", lambda e: e.tensor_tensor(out=xb[:], in0=xb[:], in1=gt[:], op=ALU.mult), reads=[dxb, dgt], writes=[dxb])
            c.op("pool", lambda e: e.tensor_tensor(out=xb[:], in0=xb[:], in1=bt[:], op=ALU.add), reads=[dxb, dbt], writes=[dxb])
            if final:
                do = self.odep()
                dst = self.O["yp"][rows, :] if tt < 16 else self.O["ys"]
                c.dma("sp", dst, xb[:], reads=[dxb], writes=[do])
            else:
                c.dma("sp", self.XRES[rows, :], xb[:], reads=[dxb], writes=[self.dXRES[tt]])
                xh, dxh = xh_p.next()
                c.op("act", lambda e: e.copy(out=xh[:], in_=xb[:]), reads=[dxb], writes=[dxh])
                self.to_actT(xh, dxh, tt)
    self.res_from_input = False


def _proj_to_mix(self, w_ap):
    c = self.c
    with self.scope():
        wpool = self.rot("wpm", 3, [128, KC, 512], BF16)
        stg = self.rot("stgm", 4, [128, 512], F32)
        self.tog = 0

        def consume(bi, n0, ncols, tt, pi):
            buf, d = stg.next()
            self.evac(pi, 128, ncols, buf[:, 0:ncols], d, self.tog)
            self.tog += 1
            c.dma("sp", self.MIX[tt * 128:(tt + 1) * 128, n0:n0 + ncols], buf[:, 0:ncols], reads=[d], writes=[self.dMIX[tt]])
        blocks = [(i * 512, 512) for i in range(4)]
        self.proj_T(w_ap, blocks, KC, lambda bi: list(range(NT)), self.act_lhs, self.act_deps, consume, wpool)


def _out_ln1(self, l):
    c = self.c
    with self.scope():
        xb = self.rot("osb", 3, [128, D], BF16)
        for tt in range(NT):
            buf, d = xb.next()
            c.dma("pool", buf[:], self.OS[tt * 128:(tt + 1) * 128, :], reads=[self.dOS[tt]], writes=[d])
            self.to_actT(buf, d, tt)
    self.proj_to_mix(self.I["w_out"][l])
    self.ln_phase(l, "ln1_g", "ln1_b", False)


def _attn(self, l):
    c = self.c
    scale = 512 ** -0.5
    with self.scope():
        KT = self.sbt("xaKT", [128, KC, N_MEM], BF16); dKT = Dep()
        Vb = self.sbt("xaVb", [128, 2, D], BF16); dVb = Dep()
        with self.scope():
            mem = self.sbt("xamem", [128, 2, D], BF16); dmem = Dep()
            c.dma("pool", mem[:], self.I["memp"].rearrange("(c p) d -> p c d", p=128), writes=[dmem])
            memT = self.sbt("xamemT", [128, KC, N_MEM], BF16); dmemT = Dep()
            for g in range(4):
                pi = self.pb()
                psb = self.ps[pi][:].bitcast(BF16)
                for j in range(4):
                    kc = g * 4 + j
                    for mc in range(2):
                        c.op("pe", lambda e, j=j, mc=mc, kc=kc, psb=psb: e.transpose(psb[:, (j * 2 + mc) * 128:(j * 2 + mc + 1) * 128], mem[:, mc, kc * 128:(kc + 1) * 128], self.ident_bf[:]),
                             reads=[dmem, self.dcst], writes=[self.dps[pi]])
                c.op("dve" if g % 2 else "act", (lambda e, g=g, psb=psb: e.tensor_copy(out=memT[:, g * 4:(g + 1) * 4, :].rearrange("p k m -> p (k m)"), in_=psb[:, 0:1024])) if g % 2 else
                     (lambda e, g=g, psb=psb: e.copy(out=memT[:, g * 4:(g + 1) * 4, :].rearrange("p k m -> p (k m)"), in_=psb[:, 0:1024])), reads=[self.dps[pi]], writes=[dmemT])
            wpool = self.rot("xawkv", 3, [128, KC, 512], BF16)
            stg = self.rot("xastg", 4, [128, 512], F32)
            for (wname, oname) in (("xa_wk", "p_mk"), ("xa_wv", "p_mv")):
                w = self.I[wname][l]
                for nb in range(4):
                    wb, wd = self.load_w(wpool, w, nb * 512, 512, KC)
                    for mt in range(2):
                        pi = self.pb()
                        for kc in range(KC):
                            c.op("pe", lambda e, kc=kc, pi=pi, mt=mt, wb=wb: e.matmul(self.ps[pi][:, 0:512], memT[:, kc, mt * 128:(mt + 1) * 128], wb[:, kc, :], start=(kc == 0), stop=(kc == KC - 1)),
                                 reads=[dmemT, wd], writes=[self.dps[pi]])
                        buf, d = stg.next()
                        c.op("act", lambda e, buf=buf, pi=pi: e.copy(out=buf[:], in_=self.ps[pi][:, 0:512]), reads=[self.dps[pi]], writes=[d])
                        do = self.odep()
                        c.dma("sp", self.O[oname][l, mt * 128:(mt + 1) * 128, nb * 512:(nb + 1) * 512], buf[:], reads=[d], writes=[do])
                        if wname == "xa_wv":
                            c.op("dve", lambda e, buf=buf, mt=mt, nb=nb: e.tensor_copy(out=Vb[:, mt, nb * 512:(nb + 1) * 512], in_=buf[:]), reads=[d], writes=[dVb])
                    if wname == "xa_wk":
                        for j in range(4):
                            pi = self.pb()
                            for kc in range(KC):
                                c.op("pe", lambda e, kc=kc, pi=pi, j=j, wb=wb: e.matmul(self.ps[pi][:, 0:N_MEM], wb[:, kc, j * 128:(j + 1) * 128], memT[:, kc, :], start=(kc == 0), stop=(kc == KC - 1)),
                                     reads=[dmemT, wd], writes=[self.dps[pi]])
                            c.op("dve", lambda e, pi=pi, j=j, nb=nb: e.tensor_copy(out=KT[:, nb * 4 + j, :], in_=self.ps[pi][:, 0:N_MEM]), reads=[self.dps[pi]], writes=[dKT])
        if KATT < 2:
            return
        qT = self.sbt("xaqT", [128, KC, TOK], BF16)
        dqT = [[Dep() for _ in range(4)] for _ in range(NT)]
        with self.scope():
            wpool = self.rot("xawq", 2, [128, KC, 512], BF16)
            self.tog = 0

            def consume_q(col0, cw, t0, tw, pi):
                ch = col0 // 128
                deps = [dqT[tt][ch // 4] for tt in range(t0 // 128, (t0 + tw) // 128)]
                dst = qT[:, ch, t0:t0 + tw]
                src = self.ps[pi][:, 0:tw]
                if self.tog % 2 == 0:
                    c.op("act", lambda e: e.copy(out=dst, in_=src), reads=[self.dps[pi]], writes=deps)
                else:
                    c.op("dve", lambda e: e.tensor_copy(out=dst, in_=src), reads=[self.dps[pi]], writes=deps)
                self.tog += 1
            self.proj_F(self.I["xa_wq"][l], [(i * 512, 512) for i in range(4)], KC, consume_q, wpool)
        if KATT < 3:
            return
        with self.scope():
            ef = self.rot("xae", 2, [128, N_MEM], F32)
            scp = self.rot("xasc", 2, [128, N_MEM], F32)
            prp = self.rot("xapr", 2, [128, N_MEM], BF16)
            prTp = self.rot("xaprT", 2, [128, 2, 128], BF16)
            sm = self.sbt("xasm", [128, 16], F32); dsm = Dep()

            def softmax(pi, smc):
                sc_, dsc_ = scp.next()
                c.op("act", lambda e: e.copy(out=sc_[:], in_=self.ps[pi][:, 0:N_MEM]), reads=[self.dps[pi]], writes=[dsc_])
                c.op("dve", lambda e: e.tensor_reduce(out=sm[:, smc:smc + 1], in_=sc_[:], axis=AX.X, op=ALU.max), reads=[dsc_], writes=[dsm])
                c.op("dve", lambda e: e.tensor_scalar(out=sm[:, smc + 1:smc + 2], in0=sm[:, smc:smc + 1], scalar1=-scale, scalar2=None, op0=ALU.mult), reads=[dsm], writes=[dsm])
                e_, de_ = ef.next()
                c.op("act", lambda e: e.activation(out=e_[:], in_=sc_[:], func=AF.Exp, scale=scale, bias=sm[:, smc + 1:smc + 2]),
                     reads=[dsc_, dsm], writes=[de_])
                c.op("dve", lambda e: e.tensor_reduce(out=sm[:, smc + 2:smc + 3], in_=e_[:], axis=AX.X, op=ALU.add), reads=[de_], writes=[dsm])
                c.op("dve", lambda e: e.reciprocal(out=sm[:, smc + 3:smc + 4], in_=sm[:, smc + 2:smc + 3]), reads=[dsm], writes=[dsm])
                pr, dpr = prp.next()
                c.op("dve", lambda e: e.tensor_scalar(out=pr[:], in0=e_[:], scalar1=sm[:, smc + 3:smc + 4], scalar2=None, op0=ALU.mult), reads=[de_, dsm], writes=[dpr])
                return pr, dpr

            def transpose_pr(pr, dpr, prT, dprT):
                pi = self.pb()
                psb = self.ps[pi][:].bitcast(BF16)
                for mc in range(2):
                    c.op("pe", lambda e, mc=mc: e.transpose(psb[:, mc * 128:(mc + 1) * 128], pr[:, mc * 128:(mc + 1) * 128], self.ident_bf[:]), reads=[dpr, self.dcst], writes=[self.dps[pi]])
                c.op("act", lambda e: e.copy(out=prT[:].rearrange("p c t -> p (c t)"), in_=psb[:, 0:256]), reads=[self.dps[pi]], writes=[dprT])

            for tt in range(16):
                t0 = tt * 128
                for h in range(4):
                    pi = self.pb()
                    for dc in range(4):
                        c.op("pe", lambda e, dc=dc, pi=pi: e.matmul(self.ps[pi][:, 0:N_MEM], qT[:, 4 * h + dc, t0:t0 + 128], KT[:, 4 * h + dc, :], start=(dc == 0), stop=(dc == 3)),
                             reads=[dqT[tt][h], dKT], writes=[self.dps[pi]])
                    pr, dpr = softmax(pi, (h % 2) * 4)
                    prT, dprT = prTp.next()
                    transpose_pr(pr, dpr, prT, dprT)
                    p2 = self.pb()
                    for dc in range(4):
                        for mc in range(2):
                            c.op("pe", lambda e, dc=dc, mc=mc, p2=p2: e.matmul(self.ps[p2][:, dc * 128:(dc + 1) * 128], Vb[:, mc, (4 * h + dc) * 128:(4 * h + dc + 1) * 128], prT[:, mc, :], start=(mc == 0), stop=(mc == 1)),
                                 reads=[dVb, dprT], writes=[self.dps[p2]])
                    c.op("dve", lambda e, p2=p2: e.tensor_copy(out=self.actT[:, 4 * h:4 * h + 4, t0:t0 + 128], in_=self.ps[p2][:, 0:512].rearrange("p (k t) -> p k t", k=4)),
                         reads=[self.dps[p2]], writes=[self.dact[tt][h]])
            if KATT < 4:
                return
            t0 = 16 * 128
            old_pb = self.pb
            self.ps_i = 0

            def pb4():
                i = self.ps_i % 4
                self.ps_i += 1
                return i
            self.pb = pb4
            SB = [4, 5, 6, 7]
            kvb = self.rot("xakv", 1, [128, 2, D], BF16)
            KTb = self.rot("xaKTb", 1, [128, KC, N_MEM], BF16)
            qmb = self.rot("xaqm", 1, [128, KC, 128], BF16)
            prS = self.sbt("xaprS", [128, 4, N_MEM], BF16); dprS = Dep()
            prTS = self.sbt("xaprTS", [128, 4, 2, 128], BF16); dprTS = Dep()
            for b in range(NB_S):
                kb, dkb = kvb.next()
                c.dma("pool", kb[:], self.I["ck"][l, b].rearrange("(c p) d -> p c d", p=128), writes=[dkb])
                ktb, dktb = KTb.next()
                for g in range(4):
                    pi = self.pb()
                    psb = self.ps[pi][:].bitcast(BF16)
                    for j in range(4):
                        kc = g * 4 + j
                        for mc in range(2):
                            c.op("pe", lambda e, j=j, mc=mc, kc=kc, psb=psb: e.transpose(psb[:, (j * 2 + mc) * 128:(j * 2 + mc + 1) * 128], kb[:, mc, kc * 128:(kc + 1) * 128], self.ident_bf[:]),
                                 reads=[dkb, self.dcst], writes=[self.dps[pi]])
                    c.op("dve" if g % 2 else "act", (lambda e, g=g, psb=psb: e.tensor_copy(out=ktb[:, g * 4:(g + 1) * 4, :].rearrange("p k m -> p (k m)"), in_=psb[:, 0:1024])) if g % 2 else
                         (lambda e, g=g, psb=psb: e.copy(out=ktb[:, g * 4:(g + 1) * 4, :].rearrange("p k m -> p (k m)"), in_=psb[:, 0:1024])), reads=[self.dps[pi]], writes=[dktb])
                qm, dqm = qmb.next()
                mrb = self.maskrow[:, b, :].unsqueeze(1).broadcast_to([128, KC, 128])
                c.op("pool", lambda e: e.tensor_tensor(out=qm[:], in0=qT[:, :, t0:t0 + 128], in1=mrb, op=ALU.mult), reads=dqT[16] + [self.dcst], writes=[dqm])
                for h in range(4):
                    for dc in range(4):
                        c.op("pe", lambda e, h=h, dc=dc: e.matmul(self.ps[SB[h]][:, 0:N_MEM], qm[:, 4 * h + dc, :], ktb[:, 4 * h + dc, :], start=(b == 0 and dc == 0), stop=(b == NB_S - 1 and dc == 3), skip_group_check=True),
                             reads=[dqm, dktb], writes=[self.dps[SB[h]]])
            for h in range(4):
                pr, dpr = softmax(SB[h], (h % 2) * 4)
                c.op("pool", lambda e, h=h, pr=pr: e.tensor_copy(out=prS[:, h, :], in_=pr[:]), reads=[dpr], writes=[dprS])
            for h in range(4):
                pi = self.pb()
                psb = self.ps[pi][:].bitcast(BF16)
                for mc in range(2):
                    c.op("pe", lambda e, mc=mc, h=h: e.transpose(psb[:, mc * 128:(mc + 1) * 128], prS[:, h, mc * 128:(mc + 1) * 128], self.ident_bf[:]), reads=[dprS, self.dcst], writes=[self.dps[pi]])
                c.op("act", lambda e, h=h: e.copy(out=prTS[:, h, :, :].rearrange("p c t -> p (c t)"), in_=psb[:, 0:256]), reads=[self.dps[pi]], writes=[dprTS])
            zrow = self.sbt("xazrow", [1, 512], BF16); dzrow = Dep()
            c.op("dve", lambda e: e.memset(zrow[:], 0.0), writes=[dzrow])
            for h in range(4):
                c.op("pe", lambda e, h=h: e.matmul(self.ps[SB[h]][:, 0:512], zrow[0:1, 0:128], zrow[0:1, 0:512], start=True, stop=False, skip_group_check=True), reads=[dzrow], writes=[self.dps[SB[h]]])
            prm = self.rot("xaprm", 2, [128, 4, 2, 128], BF16)
            for b in range(NB_S):
                vb_, dvb_ = kvb.next()
                c.dma("pool", vb_[:], self.I["cv"][l, b].rearrange("(c p) d -> p c d", p=128), writes=[dvb_])
                pm, dpm = prm.next()
                mrb = self.maskrow[:, b, :].unsqueeze(1).broadcast_to([128, 8, 128])
                c.op("pool", lambda e: e.tensor_tensor(out=pm[:].rearrange("p h c t -> p (h c) t"), in0=prTS[:].rearrange("p h c t -> p (h c) t"), in1=mrb, op=ALU.mult), reads=[dprTS, self.dcst], writes=[dpm])
                for ch in range(KC):
                    h = ch // 4
                    for mc in range(2):
                        c.op("pe", lambda e, ch=ch, mc=mc, h=h: e.matmul(self.ps[SB[h]][:, (ch % 4) * 128:(ch % 4 + 1) * 128], vb_[:, mc, ch * 128:(ch + 1) * 128], pm[:, h, mc, :],
                                                                   start=False, stop=(b == NB_S - 1 and mc == 1), skip_group_check=True), reads=[dvb_, dpm], writes=[self.dps[SB[h]]])
            for h in range(4):
                c.op("dve" if h % 2 else "act", (lambda e, h=h: e.tensor_copy(out=self.actT[:, 4 * h:4 * h + 4, t0:t0 + 128], in_=self.ps[SB[h]][:, 0:512].rearrange("p (k t) -> p k t", k=4))) if h % 2 else
                     (lambda e, h=h: e.copy(out=self.actT[:, 4 * h:4 * h + 4, t0:t0 + 128], in_=self.ps[SB[h]][:, 0:512].rearrange("p (k t) -> p k t", k=4))),
                     reads=[self.dps[SB[h]]], writes=[self.dact[16][h]])
            self.pb = old_pb
    if KATT < 5:
        return
    self.proj_to_mix(self.I["xa_wo"][l])
    if KATT < 6:
        return
    self.ln_phase(l, "ln2_g", "ln2_b", False)


def _ffn(self, l):
    c = self.c
    with self.scope():
        wpool = self.rot("ffw", 4, [128, KC, 512], BF16)
        sgp = self.rot("ffsg", 2, [128, 512], F32)
        hbp = self.rot("ffhb", 3, [128, 512], BF16)
        wg = self.I["ffn_w_gate"][l]
        wu = self.I["ffn_w_up"][l]
        nxt = (self.load_w(wpool, wg, 0, 512, KC), self.load_w(wpool, wu, 0, 512, KC))
        for nb in range(D_FF // 512):
            (gb, gd), (ub, ud) = nxt
            if nb + 1 < D_FF // 512:
                nxt = (self.load_w(wpool, wg, (nb + 1) * 512, 512, KC), self.load_w(wpool, wu, (nb + 1) * 512, 512, KC))
            for j in range(4):
                for (t0, tw) in TBLK:
                    rd = [d for tt in range(t0 // 128, (t0 + tw) // 128) for d in self.dact[tt]]
                    pg = self.pb()
                    for kc in range(KC):
                        c.op("pe", lambda e, kc=kc, pg=pg: e.matmul(self.ps[pg][:, 0:tw], gb[:, kc, j * 128:(j + 1) * 128], self.actT[:, kc, t0:t0 + tw], start=(kc == 0), stop=(kc == KC - 1)),
                             reads=[gd] + rd, writes=[self.dps[pg]])
                    pu = self.pb()
                    for kc in range(KC):
                        c.op("pe", lambda e, kc=kc, pu=pu: e.matmul(self.ps[pu][:, 0:tw], ub[:, kc, j * 128:(j + 1) * 128], self.actT[:, kc, t0:t0 + tw], start=(kc == 0), stop=(kc == KC - 1)),
                             reads=[ud] + rd, writes=[self.dps[pu]])
                    sg, dsg = sgp.next()
                    c.op("act", lambda e: e.activation(out=sg[:, 0:tw], in_=self.ps[pg][:, 0:tw], func=AF.Silu), reads=[self.dps[pg]], writes=[dsg])
                    hb, dhb = hbp.next()
                    c.op("dve", lambda e: e.tensor_tensor(out=hb[:, 0:tw], in0=sg[:, 0:tw], in1=self.ps[pu][:, 0:tw], op=ALU.mult), reads=[dsg, self.dps[pu]], writes=[dhb])
                    c.dma("sp", self.HT[nb * 4 + j][:, t0:t0 + tw], hb[:, 0:tw], reads=[dhb], writes=[self.dHT])
    with self.scope():
        wpool = self.rot("ffwd", 2, [128, FC, 256], BF16)
        hTp = self.rot("ffhT", 2, [128, FC, 256], BF16)
        stg = self.rot("ffstg", 4, [128, 256], F32)
        wd_ = self.I["ffn_w_down"][l]
        groups = [(i * 256, 256) for i in range(8)] + [(2048, 128)]
        self.tog = 0
        nxt = self.load_w(wpool, wd_, 0, 256, FC)
        for nb in range(D // 256):
            wb, wdp = nxt
            if nb + 1 < D // 256:
                nxt = self.load_w(wpool, wd_, (nb + 1) * 256, 256, FC)
            for (g0, gw) in groups:
                hT, dhT = hTp.next()
                c.dma("sp", hT[:, :, 0:gw], self.HT[:, :, g0:g0 + gw].rearrange("c p t -> p c t"), reads=[self.dHT], writes=[dhT])
                for tl in range(gw // 128):
                    tt = g0 // 128 + tl
                    pi = self.pb()
                    for kc in range(FC):
                        c.op("pe", lambda e, kc=kc, pi=pi, tl=tl: e.matmul(self.ps[pi][:, 0:256], hT[:, kc, tl * 128:(tl + 1) * 128], wb[:, kc, :], start=(kc == 0), stop=(kc == FC - 1)),
                             reads=[dhT, wdp], writes=[self.dps[pi]])
                    buf, d = stg.next()
                    self.evac(pi, 128, 256, buf[:], d, self.tog)
                    self.tog += 1
                    c.dma("sp", self.MIX[tt * 128:(tt + 1) * 128, nb * 256:(nb + 1) * 256], buf[:], reads=[d], writes=[self.dMIX[tt]])
    self.ln_phase(l, "ln3_g", "ln3_b", l == DEPTH - 1)


Prog.ln_phase = _ln_phase
Prog.proj_to_mix = _proj_to_mix
Prog.out_ln1 = _out_ln1
Prog.attn = _attn
Prog.ffn = _ffn
```

```python
import os
import math
import numpy as np
from contextlib import ExitStack, contextmanager
import concourse.bass as bass
import concourse.mybir as mybir
from concourse.bass_utils import run_bass_kernel_spmd

F32 = mybir.dt.float32
BF16 = mybir.dt.bfloat16
AF = mybir.ActivationFunctionType
ALU = mybir.AluOpType
AX = mybir.AxisListType

D = 2048
SEQ = 2048
DEPTH = 2
NB_S = 16
TS = 8
NT = 17
TOK = NT * 128
KC = D // 128
GROUP = 512
RW_IN = 1792
MB_IN = 1544
GLA_IN = 1552
RET_IN = 2048
N_IN = 6936
C_RW = 0
C_MB = RW_IN
C_GLA = RW_IN + MB_IN
C_RET = C_GLA + GLA_IN
D_FF = 5632
FC = D_FF // 128
N_MEM = 256
DN_ALPHA = (2 * DEPTH) ** 0.25
PAST_LEN = 16384
RW_LN_EPS = 64e-5
LB = 32
NBLK = SEQ // LB + NB_S
TBLK = [(i * 512, 512) for i in range(4)] + [(2048, 128)]
PF_RW = 0
PF_XBC = 1792
PF_GQ = 2816
PF_GK = 3072
PF_GKD = 3328
PF_ROWS = 3344

STAGE = int(os.environ.get("KSTAGE", "99"))
NL = int(os.environ.get("KLAYERS", "2"))
KATT = int(os.environ.get("KATT", "9"))


class Dep:
    __slots__ = ("writer", "readers")

    def __init__(self):
        self.writer = None
        self.readers = {}


class Ctx:
    COMPUTE = ("pe", "act", "dve", "pool")

    def __init__(self, nc, stack):
        self.nc = nc
        self.eng = {"pe": nc.tensor, "act": nc.scalar, "dve": nc.vector, "pool": nc.gpsimd, "sp": nc.sync}
        self.sems = {}
        self.cnt = {}
        for e in self.COMPUTE:
            self.sems[e] = stack.enter_context(nc.semaphore("c_" + e))
            self.cnt[e] = 0
        self.ring = {}
        self.ring_i = {}
        for e, n in (("sp", 48), ("pool", 40), ("act", 8)):
            self.ring[e] = [stack.enter_context(nc.semaphore("d_%s_%d" % (e, i))) for i in range(n)]
            self.ring_i[e] = 0
        self.waited = {}
        self.semobj = {}
        for e in self.COMPUTE:
            self.semobj[("c", e)] = self.sems[e]
        for e in self.ring:
            for i, s in enumerate(self.ring[e]):
                self.semobj[("d", e, i)] = s
        self.n_inst = 0
        self.n_wait = 0

    def wait(self, eng, tok, force=False):
        key, val = tok
        if key[0] == "c" and key[1] == eng and not force:
            if eng == "pe":
                return
        w = self.waited.get((eng, key), 0)
        if w >= val:
            return
        self.eng[eng].wait_ge(self.semobj[key], val)
        self.waited[(eng, key)] = val
        self.n_wait += 1

    def _deps(self, eng, reads, writes, force=False):
        toks = {}

        def add(t):
            if t is None:
                return
            k, v = t
            if toks.get(k, 0) < v:
                toks[k] = v
        for d in reads:
            add(d.writer)
        for d in writes:
            add(d.writer)
            for k, v in d.readers.items():
                add((k, v))
        for k, v in toks.items():
            self.wait(eng, (k, v), force)

    def _update(self, tok, reads, writes):
        k, v = tok
        for d in reads:
            if d.readers.get(k, 0) < v:
                d.readers[k] = v
        for d in writes:
            d.writer = tok
            d.readers = {}

    def op(self, eng, fn, reads=(), writes=()):
        self._deps(eng, reads, writes)
        ins = fn(self.eng[eng])
        self.cnt[eng] += 1
        ins.then_inc(self.sems[eng], 1)
        tok = (("c", eng), self.cnt[eng])
        self._update(tok, reads, writes)
        self.n_inst += 1
        return tok

    def dma(self, issuer, out, in_, reads=(), writes=(), **kw):
        i = self.ring_i[issuer]
        n = len(self.ring[issuer])
        slot = i % n
        rnd = i // n
        key = ("d", issuer, slot)
        if rnd > 0:
            self.wait(issuer, (key, 16 * rnd))
        self._deps(issuer, reads, writes, force=True)
        ins = self.eng[issuer].dma_start(out=out, in_=in_, **kw)
        ins.then_inc(self.ring[issuer][slot], 16)
        self.ring_i[issuer] = i + 1
        tok = (key, 16 * (rnd + 1))
        self._update(tok, reads, writes)
        self.n_inst += 1
        return tok

    def barrier(self, engines=("pe", "act", "dve", "pool", "sp")):
        toks = []
        for e in self.COMPUTE:
            if self.cnt[e] > 0:
                toks.append((("c", e), self.cnt[e]))
        for e in self.ring:
            i = self.ring_i[e]
            n = len(self.ring[e])
            for slot in range(n):
                if i > slot:
                    rnd = (i - 1 - slot) // n
                    toks.append((("d", e, slot), 16 * (rnd + 1)))
        for e in engines:
            for t in toks:
                self.wait(e, t, force=True)


def _bf(x):
    return x


def build_consts():
    c = {}
    i = np.arange(128)
    c["ident"] = np.eye(128, dtype=np.float32)
    c["ones"] = np.ones((128, 128), np.float32)
    le = (i[:, None] <= i[None, :]).astype(np.float32)
    gt = (i[:, None] > i[None, :]).astype(np.float32)
    c["le_P"] = le
    c["gt_P"] = gt
    c["causal_P"] = le.copy()
    same = (i[:, None] // TS == i[None, :] // TS).astype(np.float32)
    c["le_S"] = le * same
    c["gt_S"] = gt * same
    c["causal_S"] = le * same
    c["seqmask"] = (i[:, None] // TS == np.arange(NB_S)[None, :]).astype(np.float32)
    mr = (np.arange(128)[None, :] // TS == np.arange(NB_S)[:, None]).astype(np.float32)
    maskrow = np.broadcast_to(mr.reshape(1, NB_S * 128), (128, NB_S * 128)).astype(np.float32).copy()
    bo = np.zeros((128, 128), np.float32)
    bo[:64, :64] = 1
    bo[64:, 64:] = 1
    c["blockones"] = bo
    hs = np.zeros((128, 2), np.float32)
    hs[:64, 0] = 1
    hs[64:, 1] = 1
    c["halfsel"] = hs
    lg = np.log1p(-np.exp2(-5.0 - np.arange(4, dtype=np.float64)))
    dP = np.zeros((128, 4, 128), np.float64)
    dS = np.zeros((128, 4, 128), np.float64)
    for h in range(4):
        diff = (i[None, :] - i[:, None]).astype(np.float64)
        dP[:, h, :] = np.where(diff >= 0, np.exp(lg[h] * diff), 0.0)
        dS[:, h, :] = np.where((diff >= 0) & (same > 0), np.exp(lg[h] * diff), 0.0)
    c["retdec_P"] = dP.reshape(128, 512).astype(np.float32)
    c["retdec_S"] = dS.reshape(128, 512).astype(np.float32)
    c["ret_expcum_P"] = np.exp(lg[None, :] * (i[:, None] + 1)).astype(np.float32)
    c["ret_decend_P"] = np.exp(lg[None, :] * (127 - i[:, None])).astype(np.float32)
    c["ret_expcum_S"] = np.exp(lg[None, :] * ((i[:, None] % TS) + 1)).astype(np.float32)
    c["ret_decend_S"] = np.exp(lg[None, :] * (TS - 1 - (i[:, None] % TS))).astype(np.float32)
    cdP = np.exp(lg * 128)
    cdS = np.exp(lg * TS)
    c["ret_cd_P"] = np.broadcast_to(np.repeat(cdP, 128)[None, :], (128, 512)).astype(np.float32).copy()
    c["ret_cd_S"] = np.broadcast_to(np.repeat(cdS, 128)[None, :], (128, 512)).astype(np.float32).copy()
    bm = np.zeros((128, 256), np.float32)
    for r in range(8):
        cc = r // 2
        bm[r, cc * 64:(cc + 1) * 64] = 1
        bm[32 + r, cc * 64:(cc + 1) * 64] = 1
    c["rw_blockmask"] = bm
    hm = np.zeros((128, 2), np.float32)
    hm[:64, 0] = 1
    hm[64:, 1] = 1
    c["rw_halfmask"] = hm
    offs = {}
    o = 0
    for k, v in c.items():
        offs[k] = (o, v.shape[1])
        o += v.shape[1]
    pack = np.concatenate([c[k] for k in c], axis=1).astype(np.float32)
    t = np.arange(TOK)
    reset = np.ones(TOK, np.float32)
    notlast = np.ones(TOK, np.float32)
    reset[:SEQ][t[:SEQ] % LB == 0] = 0
    notlast[:SEQ][t[:SEQ] % LB == LB - 1] = 0
    ts_ = t[SEQ:] - SEQ
    reset[SEQ:][ts_ % TS == 0] = 0
    notlast[SEQ:][ts_ % TS == TS - 1] = 0
    tokmask = np.stack([reset, notlast], 0).astype(np.float32)
    half = 64
    inv = (1.0 / (10000.0 ** np.linspace(0.0, 1.0, half, dtype=np.float32))).astype(np.float32)
    pos = np.concatenate([np.arange(SEQ, dtype=np.float32),
                          np.tile(PAST_LEN + np.arange(TS, dtype=np.float32), NB_S)])
    ang = (pos[:, None] * inv[None, :]).astype(np.float32)
    cs = np.cos(ang).astype(np.float32)
    sn = np.sin(ang).astype(np.float32)
    sc = np.float32(128 ** -0.5)
    rot = np.concatenate([cs, sn, cs * sc, sn * sc], axis=1).reshape(NT, 128, 256).astype(np.float32)
    return pack, offs, tokmask, rot, maskrow


_CONSTS = build_consts()

WEIGHT_NAMES = ["w_in", "w_out", "ln1_g", "ln1_b", "rw_mu", "rw_w0", "rw_w_up", "rw_a0", "rw_a_up", "rw_g_up",
                "rw_k_k", "rw_k_a", "rw_r_k", "rw_ln_g", "rw_ln_b", "mb_conv_w", "mb_conv_b", "mb_dt_bias",
                "mb_a_log", "mb_d", "mb_norm_g", "gla_gk_up", "gla_gk_b", "gla_norm_g", "ret_norm_g",
                "ln2_g", "ln2_b", "xa_wq", "xa_wk", "xa_wv", "xa_wo", "ln3_g", "ln3_b",
                "ffn_w_gate", "ffn_w_up", "ffn_w_down"]

IN_SHAPES = {
    "xp": [SEQ, D], "xs": [128, D], "memp": [N_MEM, D],
    "st_shift": [DEPTH, NB_S, RW_IN], "st_wkv": [DEPTH, NB_S, 8, 64, 64], "st_conv": [DEPTH, NB_S, 3, 1024],
    "st_ssm": [DEPTH, NB_S, 8, 128, 64], "st_gla": [DEPTH, NB_S, 4, 64, 128], "st_ret": [DEPTH, NB_S, 4, 128, 128],
    "ck": [DEPTH, NB_S, N_MEM, D], "cv": [DEPTH, NB_S, N_MEM, D],
    "w_in": [DEPTH, D, N_IN], "w_out": [DEPTH, D, D], "ln1_g": [DEPTH, D], "ln1_b": [DEPTH, D],
    "rw_mu": [DEPTH, RW_IN], "rw_w0": [DEPTH, 512], "rw_w_up": [DEPTH, 64, 512], "rw_a0": [DEPTH, 512],
    "rw_a_up": [DEPTH, 64, 512], "rw_g_up": [DEPTH, 128, 512], "rw_k_k": [DEPTH, 512], "rw_k_a": [DEPTH, 512],
    "rw_r_k": [DEPTH, 512], "rw_ln_g": [DEPTH, 512], "rw_ln_b": [DEPTH, 512],
    "mb_conv_w": [DEPTH, 4, 1024], "mb_conv_b": [DEPTH, 1024], "mb_dt_bias": [DEPTH, 8], "mb_a_log": [DEPTH, 8],
    "mb_d": [DEPTH, 8], "mb_norm_g": [DEPTH, 512], "gla_gk_up": [DEPTH, 16, 256], "gla_gk_b": [DEPTH, 256],
    "gla_norm_g": [DEPTH, 512], "ret_norm_g": [DEPTH, 512], "ln2_g": [DEPTH, D], "ln2_b": [DEPTH, D],
    "xa_wq": [DEPTH, D, D], "xa_wk": [DEPTH, D, D], "xa_wv": [DEPTH, D, D], "xa_wo": [DEPTH, D, D],
    "ln3_g": [DEPTH, D], "ln3_b": [DEPTH, D], "ffn_w_gate": [DEPTH, D, D_FF], "ffn_w_up": [DEPTH, D, D_FF],
    "ffn_w_down": [DEPTH, D_FF, D],
    "cpack": list(_CONSTS[0].shape), "tokmask": [2, TOK], "rot": [NT, 128, 256], "maskrow": [128, NB_S * 128],
}
OUT_SHAPES = {
    "yp": [SEQ, D], "ys": [128, D],
    "p_shift": [DEPTH, RW_IN], "p_wkv": [DEPTH, 8, 64, 64], "p_conv": [DEPTH, 3, 1024], "p_ssm": [DEPTH, 8, 128, 64],
    "p_gla": [DEPTH, 4, 64, 128], "p_ret": [DEPTH, 4, 128, 128], "p_mk": [DEPTH, N_MEM, D], "p_mv": [DEPTH, N_MEM, D],
    "s_shift": [DEPTH, NB_S, RW_IN], "s_wkv": [DEPTH, NB_S, 8, 64, 64], "s_conv": [DEPTH, NB_S, 3, 1024],
    "s_ssm": [DEPTH, NB_S, 8, 128, 64], "s_gla": [DEPTH, NB_S, 4, 64, 128], "s_ret": [DEPTH, NB_S, 4, 128, 128],
}


def in_shapes():
    out = {}
    for k, v in IN_SHAPES.items():
        if STAGE < 7 and (k.startswith("xa_") or k in ("ck", "cv", "memp", "ln2_g", "ln2_b")):
            continue
        if STAGE < 8 and (k.startswith("ffn_") or k in ("ln3_g", "ln3_b")):
            continue
        if STAGE < 6 and k in ("w_out", "ln1_g", "ln1_b"):
            continue
        v = list(v)
        if v[0] == DEPTH and k not in ("tokmask",) and len(v) >= 2 and k in WEIGHT_NAMES + ["st_shift", "st_wkv", "st_conv", "st_ssm", "st_gla", "st_ret", "ck", "cv"]:
            v[0] = NL
        out[k] = v
    return out


class Rot:
    def __init__(self, items):
        self.items = items
        self.i = 0

    def next(self):
        it = self.items[self.i % len(self.items)]
        self.i += 1
        return it


class Prog:
    def __init__(self):
        nc = bass.Bass("TRN2", target_bir_lowering=False)
        self.nc = nc
        self.I = {k: nc.dram_tensor(k, list(v), F32, kind="ExternalInput").ap() for k, v in in_shapes().items()}
        self.O = {k: nc.dram_tensor(k, list(v), F32, kind="ExternalOutput").ap() for k, v in OUT_SHAPES.items()}
        self.uid = 0
        self.out_deps = []

    def scr(self, name, shape, dt=F32):
        return self.nc.dram_tensor("scr_" + name, list(shape), dt).ap()

    def sbt(self, name, shape, dt=F32):
        self.uid += 1
        return self.scopes[-1].enter_context(self.nc.sbuf_tensor("%s_%d" % (name, self.uid), list(shape), dt))

    def rot(self, name, n, shape, dt=F32):
        return Rot([(self.sbt(name, shape, dt), Dep()) for _ in range(n)])

    @contextmanager
    def scope(self):
        st = ExitStack()
        self.scopes.append(st)
        try:
            yield
        finally:
            self.c.barrier()
            self.scopes.pop()
            st.close()

    def pb(self):
        i = self.ps_i % 8
        self.ps_i += 1
        return i

    def cst(self, name):
        o, n = _CONSTS[1][name]
        return self.cpk[:, o:o + n]

    def odep(self):
        d = Dep()
        self.out_deps.append(d)
        return d

    def build(self):
        nc = self.nc
        with ExitStack() as st:
            self.c = c = Ctx(nc, st)
            self.scopes = [st]
            st.enter_context(nc.Block())
            self.ps = [st.enter_context(nc.psum_tensor("psb%d" % i, [128, 512], F32)) for i in range(8)]
            self.dps = [Dep() for _ in range(8)]
            self.ps_i = 0
            self.actT = self.sbt("actT", [128, KC, TOK], BF16)
            self.dact = [[Dep() for _ in range(4)] for _ in range(NT)]
            ncst = _CONSTS[0].shape[1]
            self.cpk = self.sbt("cpk", [128, ncst], F32)
            self.dcst = Dep()
            c.dma("sp", self.cpk[:], self.I["cpack"], writes=[self.dcst])
            self.ident_bf = self.sbt("identbf", [128, 128], BF16)
            o, n = _CONSTS[1]["ident"]
            c.dma("pool", self.ident_bf[:], self.I["cpack"][:, o:o + n], writes=[self.dcst])
            self.maskrow = self.sbt("maskrow", [128, NB_S, 128], BF16)
            c.dma("pool", self.maskrow[:], self.I["maskrow"].rearrange("p (b i) -> p b i", b=NB_S), writes=[self.dcst])
            self.PT = self.scr("PT", [TOK, N_IN])
            self.dPT = [Dep() for _ in range(NT)]
            self.PF = self.scr("PF", [PF_ROWS, TOK])
            self.dPF = Dep()
            self.OS = self.scr("OS", [TOK, D])
            self.dOS = [Dep() for _ in range(NT)]
            self.MIX = self.scr("MIX", [TOK, D])
            self.dMIX = [Dep() for _ in range(NT)]
            self.XRES = self.scr("XRES", [TOK, D])
            self.dXRES = [Dep() for _ in range(NT)]
            self.HT = self.scr("HT", [FC, 128, TOK], BF16)
            self.dHT = Dep()
            self.ORW = self.scr("ORW", [TOK, 512], BF16)
            self.dORW = Dep()
            self.KAPT = self.scr("KAPT", [4, 128, TOK + 8], BF16)
            self.RHT = self.scr("RHT", [4, 128, TOK], BF16)
            self.BH = self.scr("BH", [TOK, 512], BF16)
            self.KH = self.scr("KH", [TOK, 512], BF16)
            self.VV = self.scr("VV", [TOK, 512], F32)
            self.dRWS = Dep()
            self.epsT = self.sbt("epsT", [128, 8], F32)
            c.op("dve", lambda e: e.memset(self.epsT[:, 0:1], 1e-5), writes=[self.dcst])
            c.op("dve", lambda e: e.memset(self.epsT[:, 1:2], RW_LN_EPS), writes=[self.dcst])
            c.op("dve", lambda e: e.memset(self.epsT[:, 2:3], 0.0), writes=[self.dcst])
            c.op("dve", lambda e: e.memset(self.epsT[:, 3:4], 1.0), writes=[self.dcst])
            self.epst = {1e-5: self.epsT[:, 0:1], float(RW_LN_EPS): self.epsT[:, 1:2], 0.0: self.epsT[:, 2:3], 1.0: self.epsT[:, 3:4]}
            c.barrier()
            self.res_from_input = True
            self.load_x0()
            for l in range(NL):
                if STAGE < 1:
                    break
                self.inproj(l)
                if STAGE < 2:
                    break
                self.mix_ret(l)
                if STAGE < 3:
                    break
                self.mix_gla(l)
                if STAGE < 4:
                    break
                self.mix_ssd(l)
                if STAGE < 5:
                    break
                self.mix_rwkv(l)
                if STAGE < 6:
                    break
                self.out_ln1(l)
                if STAGE < 7:
                    break
                self.attn(l)
                if STAGE < 8:
                    break
                self.ffn(l)
            for d in self.out_deps:
                if d.writer is not None:
                    c.wait("sp", d.writer, force=True)
            c.barrier(engines=("sp",))
        return nc

    def tile_rows(self, tt):
        return slice(tt * 128, (tt + 1) * 128)

    def resid_src(self, l, tt):
        if l == 0:
            if tt < 16:
                return self.I["xp"][tt * 128:(tt + 1) * 128, :], None
            return self.I["xs"], None
        return self.XRES[tt * 128:(tt + 1) * 128, :], self.dXRES[tt]

    def to_actT(self, xh, dxh, tt):
        c = self.c
        for g in range(2):
            pi = self.pb()
            psb = self.ps[pi][:].bitcast(BF16)
            for j in range(8):
                kc = g * 8 + j
                c.op("pe", lambda e, j=j, kc=kc: e.transpose(psb[:, j * 128:(j + 1) * 128], xh[:, kc * 128:(kc + 1) * 128], self.ident_bf[:]),
                     reads=[dxh, self.dcst], writes=[self.dps[pi]])
            dst = self.actT[:, g * 8:(g + 1) * 8, tt * 128:(tt + 1) * 128]
            src = psb.rearrange("p (j t) -> p j t", j=8)
            eng = "act" if g == 0 else "dve"
            if eng == "act":
                c.op("act", lambda e: e.copy(out=dst, in_=src), reads=[self.dps[pi]], writes=[self.dact[tt][2 * g], self.dact[tt][2 * g + 1]])
            else:
                c.op("dve", lambda e: e.tensor_copy(out=dst, in_=src), reads=[self.dps[pi]], writes=[self.dact[tt][2 * g], self.dact[tt][2 * g + 1]])

    def load_x0(self):
        c = self.c
        with self.scope():
            xb = self.rot("x0", 3, [128, D], BF16)
            for tt in range(NT):
                src, _ = self.resid_src(0, tt)
                buf, d = xb.next()
                c.dma("pool", buf[:], src, writes=[d])
                self.to_actT(buf, d, tt)

    def load_w(self, pool, w_ap, n0, ncols, kcn):
        buf, dep = pool.next()
        src = w_ap[:, n0:n0 + ncols].rearrange("(kc p) n -> p kc n", p=128)
        self.c.dma("pool", buf[:, 0:kcn, 0:ncols], src, writes=[dep])
        return buf, dep

    def evac(self, pi, rows, cols, dst, ddst, toggle):
        c = self.c
        src = self.ps[pi][0:rows, 0:cols]
        if toggle % 2 == 0:
            c.op("act", lambda e: e.copy(out=dst, in_=src), reads=[self.dps[pi]], writes=[ddst])
        else:
            c.op("dve", lambda e: e.tensor_copy(out=dst, in_=src), reads=[self.dps[pi]], writes=[ddst])

    def proj_T(self, w_ap, blocks, kcn, tiles_fn, lhs_fn, lhs_deps_fn, consume, wpool):
        c = self.c
        nxt = self.load_w(wpool, w_ap, blocks[0][0], blocks[0][1], kcn)
        for bi, (n0, ncols) in enumerate(blocks):
            wb, wd = nxt
            if bi + 1 < len(blocks):
                nxt = self.load_w(wpool, w_ap, blocks[bi + 1][0], blocks[bi + 1][1], kcn)
            for tt in tiles_fn(bi):
                pi = self.pb()
                ld = lhs_deps_fn(tt)
                for kc in range(kcn):
                    c.op("pe", lambda e, kc=kc, pi=pi, tt=tt: e.matmul(self.ps[pi][:, 0:ncols], lhs_fn(tt, kc), wb[:, kc, 0:ncols],
                                                                     start=(kc == 0), stop=(kc == kcn - 1)),
                         reads=[wd] + ld, writes=[self.dps[pi]])
                consume(bi, n0, ncols, tt, pi)

    def proj_F(self, w_ap, blocks, kcn, consume, wpool, rhs_fn=None, rhs_deps_fn=None, tblk=TBLK):
        c = self.c
        if rhs_fn is None:
            rhs_fn = lambda kc, t0, tw: self.actT[:, kc, t0:t0 + tw]
            rhs_deps_fn = lambda t0, tw: [d for tt in range(t0 // 128, (t0 + tw) // 128) for d in self.dact[tt]]
        nxt = self.load_w(wpool, w_ap, blocks[0][0], blocks[0][1], kcn)
        for bi, (n0, ncols) in enumerate(blocks):
            wb, wd = nxt
            if bi + 1 < len(blocks):
                nxt = self.load_w(wpool, w_ap, blocks[bi + 1][0], blocks[bi + 1][1], kcn)
            for j in range((ncols + 127) // 128):
                cw = min(128, ncols - 128 * j)
                for (t0, tw) in tblk:
                    pi = self.pb()
                    rd = rhs_deps_fn(t0, tw)
                    for kc in range(kcn):
                        c.op("pe", lambda e, kc=kc, pi=pi, j=j, cw=cw, t0=t0, tw=tw: e.matmul(
                            self.ps[pi][0:cw, 0:tw], wb[:, kc, 128 * j:128 * j + cw], rhs_fn(kc, t0, tw),
                            start=(kc == 0), stop=(kc == kcn - 1)), reads=[wd] + rd, writes=[self.dps[pi]])
                    consume(n0 + 128 * j, cw, t0, tw, pi)

    def act_lhs(self, tt, kc):
        return self.actT[:, kc, tt * 128:(tt + 1) * 128]

    def act_deps(self, tt):
        return list(self.dact[tt])

    def inproj(self, l):
        c = self.c
        w = self.I["w_in"][l]
        with self.scope():
            wpool = self.rot("win", 3, [128, KC, 512], BF16)
            stg = self.rot("stg", 4, [128, 512], F32)
            self.tog = 0
            def blocks_of(c0, n):
                out = []
                o = 0
                while o < n:
                    out.append((c0 + o, min(512, n - o)))
                    o += 512
                return out
            allt = list(range(NT))
            last2 = [15, 16]
            tb = []
            for b in blocks_of(C_RW, RW_IN):
                tb.append((b, last2))
            for b in blocks_of(C_MB, 512):
                tb.append((b, allt))
            for b in blocks_of(C_MB + 512, 1024):
                tb.append((b, last2))
            tb.append(((C_MB + 1536, 8), allt))
            for b in blocks_of(C_GLA + 256, 256):
                tb.append((b, allt))
            for b in blocks_of(C_GLA + 512, 512):
                tb.append((b, allt))
            for b in blocks_of(C_GLA + 1040, 512):
                tb.append((b, allt))
            for b in blocks_of(C_RET, RET_IN):
                tb.append((b, allt))
            blocks = [x[0] for x in tb]

            def consume_T(bi, n0, ncols, tt, pi):
                buf, d = stg.next()
                self.evac(pi, 128, ncols, buf[:, 0:ncols], d, self.tog)
                self.tog += 1
                c.dma("sp", self.PT[tt * 128:(tt + 1) * 128, n0:n0 + ncols], buf[:, 0:ncols], reads=[d], writes=[self.dPT[tt]])
            self.proj_T(w, blocks, KC, lambda bi: tb[bi][1], self.act_lhs, self.act_deps, consume_T, wpool)
            fsegs = [(C_RW, RW_IN, PF_RW), (C_MB + 512, 1024, PF_XBC), (C_GLA, 256, PF_GQ), (C_GLA + 256, 256, PF_GK),
                     (C_GLA + 1024, 16, PF_GKD)]
            for (c0, n, r0) in fsegs:
                def consume_F(col0, cw, t0, tw, pi, c0=c0, r0=r0):
                    buf, d = stg.next()
                    self.evac(pi, cw, tw, buf[0:cw, 0:tw], d, self.tog)
                    self.tog += 1
                    rr = r0 + (col0 - c0)
                    c.dma("sp", self.PF[rr:rr + cw, t0:t0 + tw], buf[0:cw, 0:tw], reads=[d], writes=[self.dPF])
                self.proj_F(w, blocks_of(c0, n), KC, consume_F, wpool)
            d1 = self.odep()
            c.dma("sp", self.O["p_shift"][l:l + 1, :], self.PT[2047:2048, C_RW:C_RW + RW_IN], reads=[self.dPT[15]], writes=[d1])
            d2 = self.odep()
            c.dma("sp", self.O["p_conv"][l], self.PT[2045:2048, C_MB + 512:C_MB + 1536], reads=[self.dPT[15]], writes=[d2])
            d3 = self.odep()
            src = self.PT[2048:2176, C_RW:C_RW + RW_IN].rearrange("(b t) n -> b t n", t=TS)[:, TS - 1, :]
            c.dma("sp", self.O["s_shift"][l], src, reads=[self.dPT[16]], writes=[d3])
            d4 = self.odep()
            src = self.PT[2048:2176, C_MB + 512:C_MB + 1536].rearrange("(b t) n -> b t n", t=TS)[:, TS - 3:TS, :]
            c.dma("sp", self.O["s_conv"][l], src, reads=[self.dPT[16]], writes=[d4])

    def bcast_row(self, name, row_ap, n):
        t = self.sbt(name, [128, n], F32)
        d = Dep()
        self.c.dma("sp", t[:], row_ap.partition_broadcast(128), writes=[d])
        return t, d

    def col_load(self, name, vec_ap, nchunks):
        t = self.sbt(name, [128, nchunks], F32)
        d = Dep()
        with self.nc.allow_non_contiguous_dma(reason="small per-feature vector"):
            self.c.dma("sp", t[:], vec_ap.rearrange("(c p) -> p c", p=128), writes=[d])
        return t, d

    def V(self, eng, fn, reads, writes):
        return self.c.op(eng, fn, reads=reads, writes=writes)

    def rms_gate(self, y, dy, ngroups, gsz, gn, dgn, gate_ap, dgate, eps, out_dram, dout, tmp, dtmp, small, dsmall):
        c = self.c
        y3 = y[:].rearrange("p (g v) -> p g v", g=ngroups)
        c.op("dve", lambda e: e.tensor_tensor(out=tmp[:], in0=y[:], in1=y[:], op=ALU.mult), reads=[dy], writes=[dtmp])
        c.op("dve", lambda e: e.tensor_reduce(out=small[:, 0:ngroups], in_=tmp[:].rearrange("p (g v) -> p g v", g=ngroups), axis=AX.X, op=ALU.add),
             reads=[dtmp], writes=[dsmall])
        c.op("act", lambda e: e.activation(out=small[:, 8:8 + ngroups], in_=small[:, 0:ngroups], func=AF.Sqrt, scale=1.0 / gsz, bias=self.epsb(eps)),
             reads=[dsmall, self.dcst], writes=[dsmall])
        c.op("dve", lambda e: e.reciprocal(out=small[:, 16:16 + ngroups], in_=small[:, 8:8 + ngroups]), reads=[dsmall], writes=[dsmall])
        rb = small[:, 16:16 + ngroups].unsqueeze(2).broadcast_to([128, ngroups, gsz])
        c.op("dve", lambda e: e.tensor_tensor(out=y3, in0=y3, in1=rb, op=ALU.mult), reads=[dy, dsmall], writes=[dy])
        c.op("pool", lambda e: e.tensor_tensor(out=y[:], in0=y[:], in1=gn[:], op=ALU.mult), reads=[dy, dgn], writes=[dy])
        c.op("act", lambda e: e.activation(out=tmp[:], in_=gate_ap, func=AF.Silu), reads=[dgate, dtmp], writes=[dtmp])
        c.op("dve", lambda e: e.tensor_tensor(out=y[:], in0=y[:], in1=tmp[:], op=ALU.mult), reads=[dy, dtmp], writes=[dy])
        c.dma("sp", out_dram, y[:], reads=[dy], writes=[dout])

    def epsb(self, eps):
        key = float(eps)
        if key not in self.epst:
            raise KeyError(key)
        return self.epst[key]

    def mix_ret(self, l):
        c = self.c
        with self.scope():
            gn, dgn = self.bcast_row("retgn", self.I["ret_norm_g"][l:l + 1, :], 512)
            S = self.sbt("retS", [128, 512], F32)
            Sbf = self.sbt("retSbf", [128, 512], BF16)
            dS = Dep()
            dSbf = Dep()
            c.op("dve", lambda e: e.memset(S[:], 0.0), writes=[dS])
            c.op("dve", lambda e: e.memset(Sbf[:], 0.0), writes=[dSbf])
            S0 = self.sbt("retS0", [128, NB_S, 512], F32)
            S0bf = self.sbt("retS0bf", [128, NB_S, 512], BF16)
            dS0 = Dep()
            dS0bf = Dep()
            c.dma("sp", S0[:].rearrange("d b (h v) -> d b h v", h=4), self.I["st_ret"][l].rearrange("b h d v -> d b h v"), writes=[dS0])
            c.op("act", lambda e: e.copy(out=S0bf[:], in_=S0[:]), reads=[dS0], writes=[dS0bf])
            inb = self.rot("retin", 2, [128, 2048], F32)
            rtb = self.rot("retrot", 2, [128, 256], F32)
            t1 = self.sbt("rt1", [128, 256], F32); t2 = self.sbt("rt2", [128, 256], F32)
            t3 = self.sbt("rt3", [128, 256], F32); t4 = self.sbt("rt4", [128, 256], F32)
            dt1 = Dep(); dt2 = Dep(); dt3 = Dep(); dt4 = Dep()
            qr = self.sbt("qr", [128, 512], BF16); kr = self.sbt("kr", [128, 512], BF16)
            dqr = Dep(); dkr = Dep()
            qT = self.sbt("qT", [128, 512], BF16); kT = self.sbt("kT", [128, 512], BF16)
            dqT = Dep(); dkT = Dep()
            W = self.sbt("retW", [128, 512], BF16); dW = Dep()
            vbf = self.sbt("retvbf", [128, 512], BF16); dvbf = Dep()
            kd = self.sbt("retkd", [128, 512], BF16); dkd = Dep()
            kdb = self.rot("retkdb", 2, [128, 512], BF16)
            qmall = self.sbt("retqmall", [128, NB_S, 512], BF16); dqmall = Dep()
            y = self.sbt("rety", [128, 512], F32); dy = Dep()
            tmp = self.sbt("rettmp", [128, 512], F32); dtmp = Dep()
            small = self.sbt("retsmall", [128, 32], F32); dsmall = Dep()
            stg = self.rot("retstg", 2, [128, 512], F32)
            for tt in range(NT):
                smp = tt == 16
                sfx = "_S" if smp else "_P"
                buf, dbuf = inb.next()
                c.dma("sp", buf[:], self.PT[tt * 128:(tt + 1) * 128, C_RET:C_RET + 2048], reads=[self.dPT[tt]], writes=[dbuf])
                rt, drt = rtb.next()
                c.dma("sp", rt[:], self.I["rot"][tt], writes=[drt])
                for (eng, x0, co, so, out, dout, ta, dta, tb_, dtb) in (("dve", 0, 0, 64, qr, dqr, t1, dt1, t2, dt2),
                                                                         ("pool", 512, 128, 192, kr, dkr, t3, dt3, t4, dt4)):
                    x4 = buf[:, x0:x0 + 512].rearrange("p (h i two) -> p h i two", h=4, two=2)
                    xe = x4[:, :, :, 0]
                    xo = x4[:, :, :, 1]
                    cs = rt[:, co:co + 64].unsqueeze(1).broadcast_to([128, 4, 64])
                    sn = rt[:, so:so + 64].unsqueeze(1).broadcast_to([128, 4, 64])
                    o4 = out[:].rearrange("p (h i two) -> p h i two", h=4, two=2)
                    a3 = ta[:].rearrange("p (h i) -> p h i", h=4)
                    b3 = tb_[:].rearrange("p (h i) -> p h i", h=4)
                    c.op(eng, lambda e, a3=a3, xe=xe, cs=cs: e.tensor_tensor(out=a3, in0=xe, in1=cs, op=ALU.mult), reads=[dbuf, drt], writes=[dta])
                    c.op(eng, lambda e, b3=b3, xo=xo, sn=sn: e.tensor_tensor(out=b3, in0=xo, in1=sn, op=ALU.mult), reads=[dbuf, drt], writes=[dtb])
                    c.op(eng, lambda e, o4=o4, a3=a3, b3=b3: e.tensor_tensor(out=o4[:, :, :, 0], in0=a3, in1=b3, op=ALU.subtract), reads=[dta, dtb], writes=[dout])
                    c.op(eng, lambda e, a3=a3, xe=xe, sn=sn: e.tensor_tensor(out=a3, in0=xe, in1=sn, op=ALU.mult), reads=[dbuf, drt, dout], writes=[dta])
                    c.op(eng, lambda e, b3=b3, xo=xo, cs=cs: e.tensor_tensor(out=b3, in0=xo, in1=cs, op=ALU.mult), reads=[dbuf, drt, dout], writes=[dtb])
                    c.op(eng, lambda e, o4=o4, a3=a3, b3=b3: e.tensor_tensor(out=o4[:, :, :, 1], in0=a3, in1=b3, op=ALU.add), reads=[dta, dtb], writes=[dout])
                for (src, dsrc, dst, ddst, eng) in ((qr, dqr, qT, dqT, "act"), (kr, dkr, kT, dkT, "dve")):
                    pi = self.pb()
                    psb = self.ps[pi][:].bitcast(BF16)
                    for h in range(4):
                        c.op("pe", lambda e, h=h, psb=psb, src=src: e.transpose(psb[:, h * 128:(h + 1) * 128], src[:, h * 128:(h + 1) * 128], self.ident_bf[:]),
                             reads=[dsrc, self.dcst], writes=[self.dps[pi]])
                    if eng == "act":
                        c.op("act", lambda e, psb=psb, dst=dst: e.copy(out=dst[:], in_=psb[:, 0:512]), reads=[self.dps[pi]], writes=[ddst])
                    else:
                        c.op("dve", lambda e, psb=psb, dst=dst: e.tensor_copy(out=dst[:], in_=psb[:, 0:512]), reads=[self.dps[pi]], writes=[ddst])
                p1 = self.pb()
                for h in range(4):
                    c.op("pe", lambda e, h=h: e.matmul(self.ps[p1][:, h * 128:(h + 1) * 128], kT[:, h * 128:(h + 1) * 128], qT[:, h * 128:(h + 1) * 128], start=True, stop=True),
                         reads=[dkT, dqT], writes=[self.dps[p1]])
                c.op("dve", lambda e: e.tensor_tensor(out=W[:], in0=self.ps[p1][:, 0:512], in1=self.cst("retdec" + sfx), op=ALU.mult),
                     reads=[self.dps[p1], self.dcst], writes=[dW])
                c.op("act", lambda e: e.copy(out=vbf[:], in_=buf[:, 1024:1536]), reads=[dbuf], writes=[dvbf])
                p2 = self.pb()
                for h in range(4):
                    c.op("pe", lambda e, h=h: e.matmul(self.ps[p2][:, h * 128:(h + 1) * 128], W[:, h * 128:(h + 1) * 128], vbf[:, h * 128:(h + 1) * 128], start=True, stop=True),
                         reads=[dW, dvbf], writes=[self.dps[p2]])
                p3 = self.pb()
                if not smp:
                    for h in range(4):
                        c.op("pe", lambda e, h=h: e.matmul(self.ps[p3][:, h * 128:(h + 1) * 128], qT[:, h * 128:(h + 1) * 128], Sbf[:, h * 128:(h + 1) * 128], start=True, stop=True),
                             reads=[dqT, dSbf], writes=[self.dps[p3]])
                else:
                    for b in range(NB_S):
                        mrb = self.maskrow[:, b, :].unsqueeze(1).broadcast_to([128, 4, 128])
                        c.op("pool", lambda e, b=b, mrb=mrb: e.tensor_tensor(out=qmall[:, b, :].rearrange("p (h i) -> p h i", h=4), in0=qT[:].rearrange("p (h i) -> p h i", h=4), in1=mrb, op=ALU.mult),
                             reads=[dqT, self.dcst], writes=[dqmall])
                    for h in range(4):
                        for b in range(NB_S):
                            c.op("pe", lambda e, h=h, b=b: e.matmul(self.ps[p3][:, h * 128:(h + 1) * 128], qmall[:, b, h * 128:(h + 1) * 128], S0bf[:, b, h * 128:(h + 1) * 128],
                                                                      start=(b == 0), stop=(b == NB_S - 1), skip_group_check=True),
                                 reads=[dqmall, dS0bf], writes=[self.dps[p3]])
                ecb = self.cst("ret_expcum" + sfx).unsqueeze(2).broadcast_to([128, 4, 128])
                c.op("dve", lambda e: e.tensor_tensor(out=y[:].rearrange("p (h v) -> p h v", h=4), in0=self.ps[p3][:, 0:512].rearrange("p (h v) -> p h v", h=4), in1=ecb, op=ALU.mult),
                     reads=[self.dps[p3], self.dcst, self.dOS[tt]], writes=[dy])
                c.op("dve", lambda e: e.tensor_tensor(out=y[:], in0=y[:], in1=self.ps[p2][:, 0:512], op=ALU.add), reads=[dy, self.dps[p2]], writes=[dy])
                self.rms_gate(y, dy, 4, 128, gn, dgn, buf[:, 1536:2048], dbuf, 1e-5, self.OS[tt * 128:(tt + 1) * 128, 1536:2048], self.dOS[tt], tmp, dtmp, small, dsmall)
                deb = self.cst("ret_decend" + sfx).unsqueeze(2).broadcast_to([128, 4, 128])
                c.op("pool", lambda e: e.tensor_tensor(out=kd[:].rearrange("p (h d) -> p h d", h=4), in0=kr[:].rearrange("p (h d) -> p h d", h=4), in1=deb, op=ALU.mult),
                     reads=[dkr, self.dcst], writes=[dkd])
                if not smp:
                    p4 = self.pb()
                    for h in range(4):
                        c.op("pe", lambda e, h=h: e.matmul(self.ps[p4][:, h * 128:(h + 1) * 128], kd[:, h * 128:(h + 1) * 128], vbf[:, h * 128:(h + 1) * 128], start=True, stop=True),
                             reads=[dkd, dvbf], writes=[self.dps[p4]])
                    c.op("pool", lambda e: e.tensor_tensor(out=S[:], in0=S[:], in1=self.cst("ret_cd_P"), op=ALU.mult), reads=[dS, self.dcst], writes=[dS])
                    c.op("dve", lambda e: e.tensor_tensor(out=S[:], in0=S[:], in1=self.ps[p4][:, 0:512], op=ALU.add), reads=[dS, self.dps[p4]], writes=[dS])
                    c.op("act", lambda e: e.copy(out=Sbf[:], in_=S[:]), reads=[dS], writes=[dSbf])
                    if tt == 15:
                        do = self.odep()
                        c.dma("sp", self.O["p_ret"][l].rearrange("h d v -> d h v"), S[:].rearrange("d (h v) -> d h v", h=4), reads=[dS], writes=[do])
                else:
                    for b in range(NB_S):
                        kb, dkb = kdb.next()
                        c.op("pool", lambda e, kb=kb, b=b: e.tensor_scalar(out=kb[:], in0=kd[:], scalar1=self.cst("seqmask")[:, b:b + 1], scalar2=None, op0=ALU.mult),
                             reads=[dkd, self.dcst], writes=[dkb])
                        p4 = self.pb()
                        for h in range(4):
                            c.op("pe", lambda e, h=h, kb=kb, p4=p4: e.matmul(self.ps[p4][:, h * 128:(h + 1) * 128], kb[:, h * 128:(h + 1) * 128], vbf[:, h * 128:(h + 1) * 128], start=True, stop=True),
                                 reads=[dkb, dvbf], writes=[self.dps[p4]])
                        sb_, dsb = stg.next()
                        c.op("pool", lambda e, sb_=sb_, b=b: e.tensor_tensor(out=sb_[:], in0=S0[:, b, :], in1=self.cst("ret_cd_S"), op=ALU.mult), reads=[dS0, self.dcst], writes=[dsb])
                        c.op("dve", lambda e, sb_=sb_, p4=p4: e.tensor_tensor(out=sb_[:], in0=sb_[:], in1=self.ps[p4][:, 0:512], op=ALU.add), reads=[dsb, self.dps[p4]], writes=[dsb])
                        do = self.odep()
                        c.dma("sp", self.O["s_ret"][l, b].rearrange("h d v -> d h v"), sb_[:].rearrange("d (h v) -> d h v", h=4), reads=[dsb], writes=[do])


_PROG_CACHE = {}


def _get_prog():
    if "p" not in _PROG_CACHE:
        p = Prog()
        p.build()
        _PROG_CACHE["p"] = p
    return _PROG_CACHE["p"]


def kernel(**inp):
    f = lambda a: np.ascontiguousarray(np.asarray(a, dtype=np.float32))
    prog = _get_prog()
    pack, offs, tokmask, rot, maskrow = _CONSTS
    shared = {k: f(inp[k]) for k in WEIGHT_NAMES}
    shared["rw_r_k"] = shared["rw_r_k"].reshape(DEPTH, 512)
    shared["cpack"] = pack
    shared["tokmask"] = tokmask
    shared["rot"] = rot
    shared["maskrow"] = maskrow
    xp = f(inp["x_prompt"]); xs = f(inp["x_sample"]); memp = f(inp["mem_prompt"])
    st = {"st_shift": f(inp["state_rwkv_shift"]), "st_wkv": f(inp["state_rwkv_wkv"]), "st_conv": f(inp["state_mamba_conv"]),
          "st_ssm": f(inp["state_mamba_ssm"]), "st_gla": f(inp["state_gla"]), "st_ret": f(inp["state_ret"]),
          "ck": f(inp["cache_mem_k"]), "cv": f(inp["cache_mem_v"])}
    in_maps = []
    for cid in range(8):
        b = cid % 4
        m = dict(shared)
        m["xp"] = xp[b]
        m["xs"] = np.ascontiguousarray(xs[cid * NB_S:(cid + 1) * NB_S].reshape(128, D))
        m["memp"] = memp[b]
        for k, v in st.items():
            sl = np.ascontiguousarray(v[:, cid * NB_S:(cid + 1) * NB_S])
            if k in ("ck", "cv"):
                sl = sl.reshape(DEPTH, NB_S, N_MEM, D)
            m[k] = sl
        ish = in_shapes()
        m = {k: (np.ascontiguousarray(v[:NL]) if (k in ish and ish[k][0] == NL and v.shape[0] == DEPTH and NL != DEPTH) else v) for k, v in m.items() if k in ish}
        for k in m:
            assert list(m[k].shape) == ish[k], (k, m[k].shape, ish[k])
        in_maps.append(m)
    res = run_bass_kernel_spmd(prog.nc, in_maps, core_ids=list(range(8)))
    R = res.results
    B = 4
    yp = np.stack([R[b]["yp"] for b in range(B)], 0)
    ys = np.concatenate([R[c]["ys"].reshape(NB_S, TS, D) for c in range(8)], 0)

    def pstack(k):
        return np.stack([R[b][k] for b in range(B)], 1)

    def sstack(k):
        return np.concatenate([R[c][k] for c in range(8)], 1)
    p_mk = pstack("p_mk").reshape(DEPTH, B, N_MEM, 4, 512)
    p_mv = pstack("p_mv").reshape(DEPTH, B, N_MEM, 4, 512)
    outs = (yp, ys, pstack("p_shift"), pstack("p_wkv"), pstack("p_conv"), pstack("p_ssm"), pstack("p_gla"), pstack("p_ret"),
            p_mk, p_mv, sstack("s_shift"), sstack("s_wkv"), sstack("s_conv"), sstack("s_ssm"), sstack("s_gla"), sstack("s_ret"))
    outs = tuple(np.ascontiguousarray(o.astype(np.float32)) for o in outs)
    if os.environ.get("KDUMP"):
        for i, o in enumerate(outs):
            np.save(os.environ["KDUMP"] + "_%d.npy" % i, o)
    return outs


def _gla_phase(self, l):
    c = self.c
    with self.scope():
        gn, dgn = self.bcast_row("glagn", self.I["gla_norm_g"][l:l + 1, :], 512)
        gb, dgb = self.bcast_row("glagb", self.I["gla_gk_b"][l:l + 1, :], 256)
        gkup = self.sbt("gkup", [16, 256], BF16); dgkup = Dep()
        c.dma("pool", gkup[:], self.I["gla_gk_up"][l], writes=[dgkup])
        St = self.sbt("glaS", [128, 256], F32); dSt = Dep()
        Stbf = self.sbt("glaSbf", [128, 256], BF16); dStbf = Dep()
        c.op("dve", lambda e: e.memset(St[:], 0.0), writes=[dSt])
        c.op("dve", lambda e: e.memset(Stbf[:], 0.0), writes=[dStbf])
        S0 = self.sbt("glaS0", [128, NB_S, 256], F32); dS0 = Dep()
        S0bf = self.sbt("glaS0bf", [128, NB_S, 256], BF16); dS0bf = Dep()
        c.dma("sp", S0[:].rearrange("p b (c v) -> p b c v", c=2), self.I["st_gla"][l].rearrange("b (c h2) k v -> (h2 k) b c v", h2=2), writes=[dS0])
        c.op("act", lambda e: e.copy(out=S0bf[:], in_=S0[:]), reads=[dS0], writes=[dS0bf])
        qkb = self.rot("glaqk", 2, [128, 4, 128], F32)
        tmb = self.rot("glatm", 2, [128, 1280], F32)
        gkd = self.rot("glagkd", 2, [16, 128], BF16)
        xg = self.sbt("glaxg", [128, 256], F32); dxg = Dep()
        sp = self.sbt("glasp", [128, 256], F32); dsp = Dep()
        E3 = self.sbt("glaE3", [128, 256], F32); dE3 = Dep()
        E1T = self.sbt("glaE1T", [128, 2, 128], F32); dE1T = Dep()
        E2T = self.sbt("glaE2T", [128, 2, 128], F32); dE2T = Dep()
        khat = self.sbt("glakhat", [128, 256], BF16); dkhat = Dep()
        qhT = self.sbt("glaqhT", [128, 2, 128], BF16); dqhT = Dep()
        khT = self.sbt("glakhT", [128, 2, 128], BF16); dkhT = Dep()
        W = self.sbt("glaW", [128, 512], BF16); dW = Dep()
        vbf = self.sbt("glavbf", [128, 512], BF16); dvbf = Dep()
        y = self.sbt("glay", [128, 512], F32); dy = Dep()
        tmp = self.sbt("glatmp", [128, 512], F32); dtmp = Dep()
        small = self.sbt("glasmall", [128, 32], F32); dsmall = Dep()
        qmall = self.sbt("glaqmall", [128, NB_S, 2, 128], BF16); dqmall = Dep()
        kbb = self.rot("glakb", 2, [128, 256], BF16)
        stg = self.rot("glastg", 2, [128, 256], F32)
        for tt in range(NT):
            smp = tt == 16
            sfx = "_S" if smp else "_P"
            t0 = tt * 128
            qk, dqk = qkb.next()
            c.dma("sp", qk[:, 0:2, :], self.PF[PF_GQ:PF_GQ + 256, t0:t0 + 128].rearrange("(c p) t -> p c t", p=128), reads=[self.dPF], writes=[dqk])
            c.dma("sp", qk[:, 2:4, :], self.PF[PF_GK:PF_GK + 256, t0:t0 + 128].rearrange("(c p) t -> p c t", p=128), reads=[self.dPF], writes=[dqk])
            tm, dtm = tmb.next()
            c.dma("sp", tm[:, 0:768], self.PT[t0:t0 + 128, C_GLA + 256:C_GLA + 1024], reads=[self.dPT[tt]], writes=[dtm])
            c.dma("sp", tm[:, 768:1280], self.PT[t0:t0 + 128, C_GLA + 1040:C_GLA + 1552], reads=[self.dPT[tt]], writes=[dtm])
            gk, dgk = gkd.next()
            c.dma("pool", gk[:], self.PF[PF_GKD:PF_GKD + 16, t0:t0 + 128], reads=[self.dPF], writes=[dgk])
            p0 = self.pb()
            c.op("pe", lambda e: e.matmul(self.ps[p0][:, 0:256], gk[:], gkup[:], start=True, stop=True), reads=[dgk, dgkup], writes=[self.dps[p0]])
            c.op("dve", lambda e: e.tensor_tensor(out=xg[:], in0=self.ps[p0][:, 0:256], in1=gb[:], op=ALU.add), reads=[self.dps[p0], dgb], writes=[dxg])
            c.op("act", lambda e: e.activation(out=xg[:], in_=xg[:], func=AF.Exp, scale=-1.0), reads=[dxg], writes=[dxg])
            c.op("act", lambda e: e.activation(out=sp[:], in_=xg[:], func=AF.Ln, bias=self.epsb(1.0), scale=1.0), reads=[dxg, self.dcst], writes=[dsp])
            p1 = self.pb()
            c.op("pe", lambda e: e.matmul(self.ps[p1][:, 0:256], self.cst("le" + sfx), sp[:], start=True, stop=True), reads=[dsp, self.dcst], writes=[self.dps[p1]])
            for cc in range(2):
                c.op("pe", lambda e, cc=cc: e.matmul(self.ps[p1][:, 256 + cc * 128:256 + (cc + 1) * 128], sp[:, cc * 128:(cc + 1) * 128], self.cst("le" + sfx), start=True, stop=True),
                     reads=[dsp, self.dcst], writes=[self.dps[p1]])
            c.op("act", lambda e: e.activation(out=E3[:], in_=self.ps[p1][:, 0:256], func=AF.Exp, scale=1.0 / 16.0), reads=[self.dps[p1]], writes=[dE3])
            c.op("act", lambda e: e.activation(out=E1T[:].rearrange("p c t -> p (c t)"), in_=self.ps[p1][:, 256:512], func=AF.Exp, scale=-1.0 / 16.0), reads=[self.dps[p1]], writes=[dE1T])
            c.op("act", lambda e: e.activation(out=E2T[:].rearrange("p c t -> p (c t)"), in_=self.ps[p1][:, 256:512], func=AF.Exp, scale=1.0 / 16.0), reads=[self.dps[p1]], writes=[dE2T])
            c.op("dve", lambda e: e.tensor_tensor(out=khat[:], in0=tm[:, 0:256], in1=E3[:], op=ALU.mult), reads=[dtm, dE3], writes=[dkhat])
            c.op("dve", lambda e: e.scalar_tensor_tensor(out=qhT[:].rearrange("p c t -> p (c t)"), in0=qk[:, 0:2, :].rearrange("p c t -> p (c t)"), scalar=0.125,
                                                        in1=E1T[:].rearrange("p c t -> p (c t)"), op0=ALU.mult, op1=ALU.mult), reads=[dqk, dE1T], writes=[dqhT])
            c.op("pool", lambda e: e.tensor_tensor(out=khT[:], in0=qk[:, 2:4, :], in1=E2T[:], op=ALU.mult), reads=[dqk, dE2T], writes=[dkhT])
            c.op("act", lambda e: e.copy(out=vbf[:], in_=tm[:, 256:768]), reads=[dtm], writes=[dvbf])
            p2 = [self.pb(), self.pb()]
            for h2 in range(2):
                for cc in range(2):
                    c.op("pe", lambda e, cc=cc, h2=h2: e.matmul(self.ps[p2[h2]][:, cc * 128:(cc + 1) * 128], khT[h2 * 64:(h2 + 1) * 64, cc, :], qhT[h2 * 64:(h2 + 1) * 64, cc, :], start=True, stop=True),
                         reads=[dkhT, dqhT], writes=[self.dps[p2[h2]]])
            cau = self.cst("causal" + sfx).unsqueeze(1).broadcast_to([128, 2, 128])
            for h2 in range(2):
                c.op("dve", lambda e, h2=h2: e.tensor_tensor(out=W[:, h2 * 256:(h2 + 1) * 256].rearrange("p (c i) -> p c i", c=2), in0=self.ps[p2[h2]][:, 0:256].rearrange("p (c i) -> p c i", c=2), in1=cau, op=ALU.mult),
                     reads=[self.dps[p2[h2]], self.dcst], writes=[dW])
            if smp:
                for b in range(NB_S):
                    mrb = self.maskrow[:, b, :].unsqueeze(1).broadcast_to([128, 2, 128])
                    c.op("pool", lambda e, b=b, mrb=mrb: e.tensor_tensor(out=qmall[:, b, :, :], in0=qhT[:], in1=mrb, op=ALU.mult), reads=[dqhT, self.dcst], writes=[dqmall])
            p3 = [self.pb(), self.pb()]
            for h2 in range(2):
                for cc in range(2):
                    h = 2 * cc + h2
                    c.op("pe", lambda e, h=h, cc=cc, h2=h2: e.matmul(self.ps[p3[h2]][:, cc * 128:(cc + 1) * 128], W[:, h2 * 256 + cc * 128:h2 * 256 + (cc + 1) * 128], vbf[:, h * 128:(h + 1) * 128],
                                                                 start=True, stop=False, skip_group_check=True), reads=[dW, dvbf], writes=[self.dps[p3[h2]]])
                    if not smp:
                        c.op("pe", lambda e, cc=cc, h2=h2: e.matmul(self.ps[p3[h2]][:, cc * 128:(cc + 1) * 128], qhT[h2 * 64:(h2 + 1) * 64, cc, :], Stbf[h2 * 64:(h2 + 1) * 64, cc * 128:(cc + 1) * 128],
                                                                start=False, stop=True, skip_group_check=True), reads=[dqhT, dStbf], writes=[self.dps[p3[h2]]])
                    else:
                        for b in range(NB_S):
                            c.op("pe", lambda e, cc=cc, h2=h2, b=b: e.matmul(self.ps[p3[h2]][:, cc * 128:(cc + 1) * 128], qmall[h2 * 64:(h2 + 1) * 64, b, cc, :],
                                                                         S0bf[h2 * 64:(h2 + 1) * 64, b, cc * 128:(cc + 1) * 128],
                                                                         start=False, stop=(b == NB_S - 1), skip_group_check=True),
                                 reads=[dqmall, dS0bf], writes=[self.dps[p3[h2]]])
            y4 = y[:].rearrange("p (c h v) -> p c h v", c=2, h=2)
            for h2 in range(2):
                c.op("act", lambda e, h2=h2: e.copy(out=y4[:, :, h2, :], in_=self.ps[p3[h2]][:, 0:256].rearrange("p (c v) -> p c v", c=2)), reads=[self.dps[p3[h2]], self.dOS[tt]], writes=[dy])
            self.rms_gate(y, dy, 4, 128, gn, dgn, tm[:, 768:1280], dtm, 1e-5, self.OS[t0:t0 + 128, 1024:1536], self.dOS[tt], tmp, dtmp, small, dsmall)
            if not smp:
                p4 = self.pb()
                for h in range(4):
                    cc, h2 = h // 2, h % 2
                    c.op("pe", lambda e, h=h, cc=cc, h2=h2: e.matmul(self.ps[p4][h2 * 64:(h2 + 1) * 64, cc * 128:(cc + 1) * 128], khat[:, h * 64:(h + 1) * 64], vbf[:, h * 128:(h + 1) * 128],
                                                                 start=True, stop=True, skip_group_check=True), reads=[dkhat, dvbf], writes=[self.dps[p4]])
                c.op("dve", lambda e: e.tensor_tensor(out=St[:], in0=St[:], in1=self.ps[p4][:, 0:256], op=ALU.add), reads=[dSt, self.dps[p4]], writes=[dSt])
                eb = E1T[:, :, 127:128].broadcast_to([128, 2, 128])
                c.op("dve", lambda e: e.tensor_tensor(out=St[:].rearrange("p (c v) -> p c v", c=2), in0=St[:].rearrange("p (c v) -> p c v", c=2), in1=eb, op=ALU.mult),
                     reads=[dSt, dE1T], writes=[dSt])
                c.op("act", lambda e: e.copy(out=Stbf[:], in_=St[:]), reads=[dSt], writes=[dStbf])
                if tt == 15:
                    do = self.odep()
                    c.dma("sp", self.O["p_gla"][l].rearrange("(c h2) k v -> (h2 k) c v", h2=2), St[:].rearrange("p (c v) -> p c v", c=2), reads=[dSt], writes=[do])
            else:
                for b in range(NB_S):
                    kb, dkb = kbb.next()
                    c.op("pool", lambda e, kb=kb, b=b: e.tensor_scalar(out=kb[:], in0=khat[:], scalar1=self.cst("seqmask")[:, b:b + 1], scalar2=None, op0=ALU.mult),
                         reads=[dkhat, self.dcst], writes=[dkb])
                    p4 = self.pb()
                    for h in range(4):
                        cc, h2 = h // 2, h % 2
                        c.op("pe", lambda e, h=h, cc=cc, h2=h2, kb=kb, p4=p4: e.matmul(self.ps[p4][h2 * 64:(h2 + 1) * 64, cc * 128:(cc + 1) * 128], kb[:, h * 64:(h + 1) * 64], vbf[:, h * 128:(h + 1) * 128],
                                                                                  start=True, stop=True, skip_group_check=True), reads=[dkb, dvbf], writes=[self.dps[p4]])
                    sb_, dsb = stg.next()
                    c.op("dve", lambda e, sb_=sb_, b=b, p4=p4: e.tensor_tensor(out=sb_[:], in0=S0[:, b, :], in1=self.ps[p4][:, 0:256], op=ALU.add), reads=[dS0, self.dps[p4]], writes=[dsb])
                    col = TS * b + TS - 1
                    eb = E1T[:, :, col:col + 1].broadcast_to([128, 2, 128])
                    c.op("dve", lambda e, sb_=sb_, eb=eb: e.tensor_tensor(out=sb_[:].rearrange("p (c v) -> p c v", c=2), in0=sb_[:].rearrange("p (c v) -> p c v", c=2), in1=eb, op=ALU.mult),
                         reads=[dsb, dE1T], writes=[dsb])
                    do = self.odep()
                    c.dma("sp", self.O["s_gla"][l, b].rearrange("(c h2) k v -> (h2 k) c v", h2=2), sb_[:].rearrange("p (c v) -> p c v", c=2), reads=[dsb], writes=[do])


Prog.mix_gla = _gla_phase


def _ssd_phase(self, l):
    c = self.c
    with self.scope():
        gn, dgn = self.bcast_row("mbgn", self.I["mb_norm_g"][l:l + 1, :], 512)
        dtb, ddtb = self.bcast_row("mbdtb", self.I["mb_dt_bias"][l:l + 1, :], 8)
        aneg, daneg = self.bcast_row("mbaneg", self.I["mb_a_log"][l:l + 1, :], 8)
        c.op("act", lambda e: e.activation(out=aneg[:], in_=aneg[:], func=AF.Exp), reads=[daneg], writes=[daneg])
        c.op("dve", lambda e: e.tensor_scalar(out=aneg[:], in0=aneg[:], scalar1=-1.0, scalar2=None, op0=ALU.mult), reads=[daneg], writes=[daneg])
        Dt, dDt = self.bcast_row("mbD", self.I["mb_d"][l:l + 1, :], 8)
        cw = self.sbt("mbcw", [128, 4, 8], F32); dcw = Dep()
        with self.nc.allow_non_contiguous_dma(reason="tiny conv weights"):
            for wi in range(4):
                c.dma("sp", cw[:, wi, :], self.I["mb_conv_w"][l, wi].rearrange("(c p) -> p c", p=128), writes=[dcw])
        cb, dcb = self.col_load("mbcb", self.I["mb_conv_b"][l], 8)
        hT = self.sbt("mbhT", [48, 1024], F32); dhT = Dep()
        c.dma("sp", hT[:], self.I["st_conv"][l].rearrange("b r n -> (b r) n"), writes=[dhT])
        hist = self.sbt("mbhist", [128, 8, 48], F32); dhist = Dep()
        ph = self.pb()
        for j in range(8):
            c.op("pe", lambda e, j=j: e.transpose(self.ps[ph][:, j * 48:(j + 1) * 48], hT[:, j * 128:(j + 1) * 128], self.cst("ident")[0:48, 0:48]),
                 reads=[dhT, self.dcst], writes=[self.dps[ph]])
        c.op("dve", lambda e: e.tensor_copy(out=hist[:].rearrange("p c r -> p (c r)"), in_=self.ps[ph][:, 0:384]), reads=[self.dps[ph]], writes=[dhist])
        XH = self.sbt("mbXH", [128, 4, TOK], F32); dXH = Dep()
        BT = self.sbt("mbBT", [128, 2, TOK], BF16); dBT = Dep()
        CT = self.sbt("mbCT", [128, 2, TOK], BF16); dCT = Dep()
        with self.scope():
            xbp = self.rot("mbxb", 2, [128, 3 + SEQ], F32)
            xsp = self.rot("mbxs", 2, [128, NB_S, 3 + TS], F32)
            cv = self.sbt("mbcv", [128, TOK], F32); dcv = Dep()
            for j in range(8):
                xb, dxb = xbp.next()
                xs, dxs = xsp.next()
                r0 = PF_XBC + j * 128
                c.op("pool", lambda e, xb=xb: e.memset(xb[:, 0:3], 0.0), writes=[dxb])
                c.dma("sp", xb[:, 3:3 + SEQ], self.PF[r0:r0 + 128, 0:SEQ], reads=[self.dPF], writes=[dxb])
                c.dma("sp", xs[:, :, 3:3 + TS], self.PF[r0:r0 + 128, SEQ:TOK].rearrange("p (b t) -> p b t", t=TS), reads=[self.dPF], writes=[dxs])
                c.op("pool", lambda e, xs=xs, j=j: e.tensor_copy(out=xs[:, :, 0:3], in_=hist[:, j, :].rearrange("p (b r) -> p b r", r=3)), reads=[dhist, dxs], writes=[dxs])
                cvp = cv[:, 0:SEQ]
                cvs = cv[:, SEQ:TOK].rearrange("p (b t) -> p b t", t=TS)
                c.op("dve", lambda e, xb=xb, j=j: e.tensor_scalar(out=cvp, in0=xb[:, 3:3 + SEQ], scalar1=cw[:, 3, j:j + 1], scalar2=cb[:, j:j + 1], op0=ALU.mult, op1=ALU.add),
                     reads=[dxb, dcw, dcb], writes=[dcv])
                c.op("pool", lambda e, xs=xs, j=j: e.tensor_scalar(out=cvs, in0=xs[:, :, 3:3 + TS], scalar1=cw[:, 3, j:j + 1], scalar2=cb[:, j:j + 1], op0=ALU.mult, op1=ALU.add),
                     reads=[dxs, dcw, dcb], writes=[dcv])
                for i in range(3):
                    c.op("dve", lambda e, xb=xb, j=j, i=i: e.scalar_tensor_tensor(out=cvp, in0=xb[:, i:i + SEQ], scalar=cw[:, i, j:j + 1], in1=cvp, op0=ALU.mult, op1=ALU.add),
                         reads=[dxb, dcw, dcv], writes=[dcv])
                    c.op("dve", lambda e, xs=xs, j=j, i=i: e.scalar_tensor_tensor(out=cvs, in0=xs[:, :, i:i + TS], scalar=cw[:, i, j:j + 1], in1=cvs, op0=ALU.mult, op1=ALU.add),
                         reads=[dxs, dcw, dcv], writes=[dcv])
                if j < 4:
                    c.op("act", lambda e, j=j: e.activation(out=XH[:, j, :], in_=cv[:], func=AF.Silu), reads=[dcv], writes=[dXH])
                elif j < 6:
                    c.op("act", lambda e, j=j: e.activation(out=BT[:, j - 4, :], in_=cv[:], func=AF.Silu), reads=[dcv], writes=[dBT])
                else:
                    c.op("act", lambda e, j=j: e.activation(out=CT[:, j - 6, :], in_=cv[:], func=AF.Silu), reads=[dcv], writes=[dCT])
        H = self.sbt("mbH", [128, 512], F32); dH = Dep()
        Hbf = self.sbt("mbHbf", [128, 512], BF16); dHbf = Dep()
        c.op("dve", lambda e: e.memset(H[:], 0.0), writes=[dH])
        c.op("dve", lambda e: e.memset(Hbf[:], 0.0), writes=[dHbf])
        S0bf = self.sbt("mbS0bf", [128, NB_S, 512], BF16); dS0bf = Dep()
        for bq in range(4):
            c.dma("pool", S0bf[:, bq * 4:(bq + 1) * 4, :].rearrange("n b (h d) -> n b h d", h=8), self.I["st_ssm"][l, bq * 4:(bq + 1) * 4].rearrange("b h n d -> n b h d"), writes=[dS0bf])
        s0p = self.rot("mbs0", 2, [128, 512], F32)
        tmb = self.rot("mbtm", 2, [128, 520], F32)
        xh = self.sbt("mbxh", [128, 512], F32); dxh = Dep()
        Bt = self.sbt("mbBt", [128, 256], BF16); dBt = Dep()
        la = self.sbt("mbla", [128, 8], F32); dla = Dep()
        dtt = self.sbt("mbdt", [128, 8], F32); ddt = Dep()
        lam = self.sbt("mblam", [128, NB_S, 8], F32); dlam = Dep()
        e3 = self.sbt("mbe3", [128, 24], F32); de3 = Dep()
        cdS = self.sbt("mbcdS", [128, NB_S, 8], F32); dcdS = Dep()
        Ah = self.rot("mbAh", 2, [128, 128], F32)
        Edec = self.sbt("mbEdec", [128, 1024], F32); dEdec = Dep()
        msc = self.sbt("mbmsc", [128, 256], F32); dmsc = Dep()
        W = self.sbt("mbW", [128, 1024], BF16); dW = Dep()
        xdt = self.sbt("mbxdt", [128, 512], BF16); dxdt = Dep()
        xdd = self.sbt("mbxdd", [128, 512], BF16); dxdd = Dep()
        xddb = self.rot("mbxddb", 2, [128, 512], BF16)
        ctmall = self.sbt("mbctmall", [128, NB_S, 2, 128], BF16); dctmall = Dep()
        y = self.sbt("mby", [128, 512], F32); dy = Dep()
        tmp = self.sbt("mbtmp", [128, 512], F32); dtmp = Dep()
        small = self.sbt("mbsmall", [128, 32], F32); dsmall = Dep()
        for tt in range(NT):
            smp = tt == 16
            sfx = "_S" if smp else "_P"
            t0 = tt * 128
            tm, dtm = tmb.next()
            c.dma("sp", tm[:, 0:512], self.PT[t0:t0 + 128, C_MB:C_MB + 512], reads=[self.dPT[tt]], writes=[dtm])
            c.dma("sp", tm[:, 512:520], self.PT[t0:t0 + 128, C_MB + 1536:C_MB + 1544], reads=[self.dPT[tt]], writes=[dtm])
            p0 = self.pb()
            for j in range(4):
                c.op("pe", lambda e, j=j: e.transpose(self.ps[p0][:, j * 128:(j + 1) * 128], XH[:, j, t0:t0 + 128], self.cst("ident")), reads=[dXH, self.dcst], writes=[self.dps[p0]])
            c.op("act", lambda e: e.copy(out=xh[:], in_=self.ps[p0][:, 0:512]), reads=[self.dps[p0]], writes=[dxh])
            p1 = self.pb()
            psb = self.ps[p1][:].bitcast(BF16)
            for g in range(2):
                c.op("pe", lambda e, g=g: e.transpose(psb[:, g * 128:(g + 1) * 128], BT[:, g, t0:t0 + 128], self.ident_bf[:]), reads=[dBT, self.dcst], writes=[self.dps[p1]])
            c.op("dve", lambda e: e.tensor_copy(out=Bt[:], in_=psb[:, 0:256]), reads=[self.dps[p1]], writes=[dBt])
            c.op("dve", lambda e: e.tensor_tensor(out=dtt[:], in0=tm[:, 512:520], in1=dtb[:], op=ALU.add), reads=[dtm, ddtb], writes=[ddt])
            c.op("act", lambda e: e.activation(out=dtt[:], in_=dtt[:], func=AF.Exp), reads=[ddt], writes=[ddt])
            c.op("act", lambda e: e.activation(out=dtt[:], in_=dtt[:], func=AF.Ln, bias=self.epsb(1.0), scale=1.0), reads=[ddt, self.dcst], writes=[ddt])
            c.op("dve", lambda e: e.tensor_tensor(out=la[:], in0=dtt[:], in1=aneg[:], op=ALU.mult), reads=[ddt, daneg], writes=[dla])
            p2 = self.pb()
            c.op("pe", lambda e: e.matmul(self.ps[p2][:, 0:8], self.cst("le" + sfx), la[:], start=True, stop=True), reads=[dla, self.dcst], writes=[self.dps[p2]])
            c.op("pe", lambda e: e.matmul(self.ps[p2][:, 8:16], self.cst("gt" + sfx), la[:], start=True, stop=True), reads=[dla, self.dcst], writes=[self.dps[p2]])
            c.op("pe", lambda e: e.matmul(self.ps[p2][:, 16:24], self.cst("ones"), la[:], start=True, stop=True), reads=[dla, self.dcst], writes=[self.dps[p2]])
            if smp:
                c.op("dve", lambda e: e.tensor_tensor(out=lam[:], in0=la[:].unsqueeze(1).broadcast_to([128, NB_S, 8]), in1=self.cst("seqmask").unsqueeze(2).broadcast_to([128, NB_S, 8]), op=ALU.mult),
                     reads=[dla, self.dcst], writes=[dlam])
                c.op("pe", lambda e: e.matmul(self.ps[p2][:, 128:256], self.cst("ones"), lam[:].rearrange("p b h -> p (b h)"), start=True, stop=True), reads=[dlam, self.dcst], writes=[self.dps[p2]])
                c.op("act", lambda e: e.activation(out=cdS[:].rearrange("p b h -> p (b h)"), in_=self.ps[p2][:, 128:256], func=AF.Exp), reads=[self.dps[p2]], writes=[dcdS])
            c.op("act", lambda e: e.activation(out=e3[:], in_=self.ps[p2][:, 0:24], func=AF.Exp), reads=[self.dps[p2]], writes=[de3])
            for hg in range(2):
                p3 = self.pb()
                for hh in range(4):
                    h = hg * 4 + hh
                    ah, dah = Ah.next()
                    c.op("dve", lambda e, ah=ah, h=h: e.tensor_scalar(out=ah[:], in0=self.cst("gt" + sfx), scalar1=la[:, h:h + 1], scalar2=None, op0=ALU.mult), reads=[dla, self.dcst], writes=[dah])
                    c.op("pe", lambda e, ah=ah, hh=hh, p3=p3: e.matmul(self.ps[p3][:, hh * 128:(hh + 1) * 128], ah[:], self.cst("le" + sfx), start=True, stop=True), reads=[dah, self.dcst], writes=[self.dps[p3]])
                c.op("act", lambda e, hg=hg, p3=p3: e.activation(out=Edec[:, hg * 512:(hg + 1) * 512], in_=self.ps[p3][:, 0:512], func=AF.Exp), reads=[self.dps[p3]], writes=[dEdec])
            p4 = self.pb()
            for g in range(2):
                c.op("pe", lambda e, g=g: e.matmul(self.ps[p4][:, g * 128:(g + 1) * 128], BT[:, g, t0:t0 + 128], CT[:, g, t0:t0 + 128], start=True, stop=True), reads=[dBT, dCT], writes=[self.dps[p4]])
            cau = self.cst("causal" + sfx).unsqueeze(1).broadcast_to([128, 2, 128])
            c.op("dve", lambda e: e.tensor_tensor(out=msc[:].rearrange("p (g i) -> p g i", g=2), in0=self.ps[p4][:, 0:256].rearrange("p (g i) -> p g i", g=2), in1=cau, op=ALU.mult),
                 reads=[self.dps[p4], self.dcst], writes=[dmsc])
            for g in range(2):
                mb_ = msc[:, g * 128:(g + 1) * 128].unsqueeze(1).broadcast_to([128, 4, 128])
                eng = "dve" if g == 0 else "pool"
                c.op(eng, lambda e, g=g, mb_=mb_: e.tensor_tensor(out=W[:, g * 512:(g + 1) * 512].rearrange("p (h i) -> p h i", h=4), in0=Edec[:, g * 512:(g + 1) * 512].rearrange("p (h i) -> p h i", h=4), in1=mb_, op=ALU.mult),
                     reads=[dEdec, dmsc], writes=[dW])
            dtbc = dtt[:].unsqueeze(2).broadcast_to([128, 8, 64])
            c.op("dve", lambda e: e.tensor_tensor(out=xdt[:].rearrange("p (h d) -> p h d", h=8), in0=xh[:].rearrange("p (h d) -> p h d", h=8), in1=dtbc, op=ALU.mult), reads=[dxh, ddt], writes=[dxdt])
            debc = e3[:, 8:16].unsqueeze(2).broadcast_to([128, 8, 64])
            c.op("pool", lambda e: e.tensor_tensor(out=xdd[:].rearrange("p (h d) -> p h d", h=8), in0=xdt[:].rearrange("p (h d) -> p h d", h=8), in1=debc, op=ALU.mult), reads=[dxdt, de3], writes=[dxdd])
            p5 = self.pb()
            for h in range(8):
                c.op("pe", lambda e, h=h: e.matmul(self.ps[p5][:, h * 64:(h + 1) * 64], W[:, h * 128:(h + 1) * 128], xdt[:, h * 64:(h + 1) * 64], start=True, stop=True), reads=[dW, dxdt], writes=[self.dps[p5]])
            p6 = self.pb()
            if not smp:
                for g in range(2):
                    c.op("pe", lambda e, g=g: e.matmul(self.ps[p6][:, g * 256:(g + 1) * 256], CT[:, g, t0:t0 + 128], Hbf[:, g * 256:(g + 1) * 256], start=True, stop=True), reads=[dCT, dHbf], writes=[self.dps[p6]])
            else:
                for b in range(NB_S):
                    mrb = self.maskrow[:, b, :].unsqueeze(1).broadcast_to([128, 2, 128])
                    c.op("pool", lambda e, b=b, mrb=mrb: e.tensor_tensor(out=ctmall[:, b, :, :], in0=CT[:, :, t0:t0 + 128], in1=mrb, op=ALU.mult), reads=[dCT, self.dcst], writes=[dctmall])
                for g in range(2):
                    for b in range(NB_S):
                        c.op("pe", lambda e, g=g, b=b: e.matmul(self.ps[p6][:, g * 256:(g + 1) * 256], ctmall[:, b, g, :], S0bf[:, b, g * 256:(g + 1) * 256], start=(b == 0), stop=(b == NB_S - 1), skip_group_check=True),
                             reads=[dctmall, dS0bf], writes=[self.dps[p6]])
            ecb = e3[:, 0:8].unsqueeze(2).broadcast_to([128, 8, 64])
            c.op("dve", lambda e: e.tensor_tensor(out=y[:].rearrange("p (h d) -> p h d", h=8), in0=self.ps[p6][:, 0:512].rearrange("p (h d) -> p h d", h=8), in1=ecb, op=ALU.mult),
                 reads=[self.dps[p6], de3, self.dOS[tt]], writes=[dy])
            c.op("dve", lambda e: e.tensor_tensor(out=y[:], in0=y[:], in1=self.ps[p5][:, 0:512], op=ALU.add), reads=[dy, self.dps[p5]], writes=[dy])
            Dbc = Dt[:].unsqueeze(2).broadcast_to([128, 8, 64])
            c.op("pool", lambda e: e.tensor_tensor(out=tmp[:].rearrange("p (h d) -> p h d", h=8), in0=xh[:].rearrange("p (h d) -> p h d", h=8), in1=Dbc, op=ALU.mult), reads=[dxh, dDt], writes=[dtmp])
            c.op("dve", lambda e: e.tensor_tensor(out=y[:], in0=y[:], in1=tmp[:], op=ALU.add), reads=[dy, dtmp], writes=[dy])
            c.op("act", lambda e: e.activation(out=tmp[:], in_=tm[:, 0:512], func=AF.Silu), reads=[dtm, dtmp], writes=[dtmp])
            c.op("dve", lambda e: e.tensor_tensor(out=y[:], in0=y[:], in1=tmp[:], op=ALU.mult), reads=[dy, dtmp], writes=[dy])
            c.op("dve", lambda e: e.tensor_tensor(out=tmp[:], in0=y[:], in1=y[:], op=ALU.mult), reads=[dy], writes=[dtmp])
            c.op("dve", lambda e: e.tensor_reduce(out=small[:, 0:2], in_=tmp[:].rearrange("p (g v) -> p g v", g=2), axis=AX.X, op=ALU.add), reads=[dtmp], writes=[dsmall])
            c.op("act", lambda e: e.activation(out=small[:, 8:10], in_=small[:, 0:2], func=AF.Sqrt, scale=1.0 / 256, bias=self.epsb(1e-5)), reads=[dsmall, self.dcst], writes=[dsmall])
            c.op("dve", lambda e: e.reciprocal(out=small[:, 16:18], in_=small[:, 8:10]), reads=[dsmall], writes=[dsmall])
            rb = small[:, 16:18].unsqueeze(2).broadcast_to([128, 2, 256])
            c.op("dve", lambda e: e.tensor_tensor(out=y[:].rearrange("p (g v) -> p g v", g=2), in0=y[:].rearrange("p (g v) -> p g v", g=2), in1=rb, op=ALU.mult), reads=[dy, dsmall], writes=[dy])
            c.op("pool", lambda e: e.tensor_tensor(out=y[:], in0=y[:], in1=gn[:], op=ALU.mult), reads=[dy, dgn], writes=[dy])
            c.dma("sp", self.OS[t0:t0 + 128, 512:1024], y[:], reads=[dy], writes=[self.dOS[tt]])
            if not smp:
                p7 = self.pb()
                for g in range(2):
                    c.op("pe", lambda e, g=g: e.matmul(self.ps[p7][:, g * 256:(g + 1) * 256], Bt[:, g * 128:(g + 1) * 128], xdd[:, g * 256:(g + 1) * 256], start=True, stop=True), reads=[dBt, dxdd], writes=[self.dps[p7]])
                cdb = e3[:, 16:24].unsqueeze(2).broadcast_to([128, 8, 64])
                c.op("pool", lambda e: e.tensor_tensor(out=H[:].rearrange("p (h d) -> p h d", h=8), in0=H[:].rearrange("p (h d) -> p h d", h=8), in1=cdb, op=ALU.mult), reads=[dH, de3], writes=[dH])
                c.op("dve", lambda e: e.tensor_tensor(out=H[:], in0=H[:], in1=self.ps[p7][:, 0:512], op=ALU.add), reads=[dH, self.dps[p7]], writes=[dH])
                c.op("act", lambda e: e.copy(out=Hbf[:], in_=H[:]), reads=[dH], writes=[dHbf])
                if tt == 15:
                    do = self.odep()
                    c.dma("sp", self.O["p_ssm"][l].rearrange("h n d -> n h d"), H[:].rearrange("n (h d) -> n h d", h=8), reads=[dH], writes=[do])
            else:
                for b in range(NB_S):
                    xb_, dxb_ = xddb.next()
                    c.op("pool", lambda e, xb_=xb_, b=b: e.tensor_scalar(out=xb_[:], in0=xdd[:], scalar1=self.cst("seqmask")[:, b:b + 1], scalar2=None, op0=ALU.mult), reads=[dxdd, self.dcst], writes=[dxb_])
                    p7 = self.pb()
                    for g in range(2):
                        c.op("pe", lambda e, g=g, xb_=xb_, p7=p7: e.matmul(self.ps[p7][:, g * 256:(g + 1) * 256], Bt[:, g * 128:(g + 1) * 128], xb_[:, g * 256:(g + 1) * 256], start=True, stop=True), reads=[dBt, dxb_], writes=[self.dps[p7]])
                    s0, ds0 = s0p.next()
                    c.dma("sp", s0[:].rearrange("n (h d) -> n h d", h=8), self.I["st_ssm"][l, b].rearrange("h n d -> n h d"), writes=[ds0])
                    cdb = cdS[:, b, :].unsqueeze(2).broadcast_to([128, 8, 64])
                    c.op("pool", lambda e, s0=s0, cdb=cdb: e.tensor_tensor(out=s0[:].rearrange("p (h d) -> p h d", h=8), in0=s0[:].rearrange("p (h d) -> p h d", h=8), in1=cdb, op=ALU.mult), reads=[ds0, dcdS], writes=[ds0])
                    c.op("dve", lambda e, s0=s0, p7=p7: e.tensor_tensor(out=s0[:], in0=s0[:], in1=self.ps[p7][:, 0:512], op=ALU.add), reads=[ds0, self.dps[p7]], writes=[ds0])
                    do = self.odep()
                    c.dma("sp", self.O["s_ssm"][l, b].rearrange("h n d -> n h d"), s0[:].rearrange("n (h d) -> n h d", h=8), reads=[ds0], writes=[do])


Prog.mix_ssd = _ssd_phase


SDEC = 0.6065306597126334


def _rwkv_phase(self, l):
    c = self.c
    with self.scope():
        mu, dmu = self.col_load("rwmu", self.I["rw_mu"][l], 14)
        w0, dw0 = self.col_load("rww0", self.I["rw_w0"][l], 4)
        a0, da0 = self.col_load("rwa0", self.I["rw_a0"][l], 4)
        kk_, dkk_ = self.col_load("rwkk", self.I["rw_k_k"][l], 4)
        ka, dka = self.col_load("rwka", self.I["rw_k_a"][l], 4)
        rk_, drk_ = self.col_load("rwrk", self.I["rw_r_k"][l], 4)
        omka = self.sbt("rwomka", [128, 4], F32); domka = Dep()
        c.op("dve", lambda e: e.tensor_scalar(out=omka[:], in0=ka[:], scalar1=-1.0, scalar2=1.0, op0=ALU.mult, op1=ALU.add), reads=[dka], writes=[domka])
        lora = self.sbt("rwlora", [128, 512], BF16); dlora = Dep()
        c.dma("pool", lora[0:64, :], self.I["rw_w_up"][l], writes=[dlora])
        c.dma("pool", lora[64:128, :], self.I["rw_a_up"][l], writes=[dlora])
        gup = self.sbt("rwgup", [128, 512], BF16); dgup = Dep()
        c.dma("pool", gup[:], self.I["rw_g_up"][l], writes=[dgup])
        lng, dlng = self.bcast_row("rwlng", self.I["rw_ln_g"][l:l + 1, :], 512)
        lnb, dlnb = self.bcast_row("rwlnb", self.I["rw_ln_b"][l:l + 1, :], 512)
        shT = self.sbt("rwshT", [128, 14, 16], F32); dshT = Dep()
        with self.scope():
            shraw = self.sbt("rwshraw", [16, RW_IN], F32); dshraw = Dep()
            c.dma("sp", shraw[:], self.I["st_shift"][l], writes=[dshraw])
            ph = self.pb()
            for j in range(14):
                c.op("pe", lambda e, j=j: e.transpose(self.ps[ph][:, j * 16:(j + 1) * 16], shraw[:, j * 128:(j + 1) * 128], self.cst("ident")[0:16, 0:16]), reads=[dshraw, self.dcst], writes=[self.dps[ph]])
            c.op("dve", lambda e: e.tensor_copy(out=shT[:].rearrange("p j b -> p (j b)"), in_=self.ps[ph][:, 0:224]), reads=[self.dps[ph]], writes=[dshT])
        EB = self.sbt("rwEB", [128, 4, NBLK], F32); dEB = Dep()
        RK = self.sbt("rwRK", [128, NT, 8], F32); dRK = Dep()
        sgd = self.sbt("rwsgd", [128, TOK], BF16); dsgd = Dep()
        dscr = self.dRWS
        with self.scope():
            reset = self.sbt("rwreset", [128, TOK], F32); dreset = Dep()
            c.dma("sp", reset[:], self.I["tokmask"][0:1, :].partition_broadcast(128), writes=[dreset])
            X = self.sbt("rwX", [128, 1 + TOK], F32); dX = Dep()
            bufs = {n: (self.sbt("rw" + n, [128, TOK], F32), Dep()) for n in "ABCDEF"}
            twd = self.sbt("rwtwd", [128, TOK], BF16); dtwd = Dep()
            adb = self.sbt("rwadb", [128, TOK], BF16); dadb = Dep()
            bT = self.sbt("rwbT", [128, TOK], BF16); dbT = Dep()
            kT = self.sbt("rwkT", [128, TOK], BF16); dkT = Dep()
            stb = self.rot("rwstb", 2, [128, TOK], BF16)
            tst = self.rot("rwtst", 3, [128, 128], BF16)
            tsf = self.rot("rwtsf", 2, [128, 128], F32)
            c.op("pool", lambda e: e.memset(X[:, 0:1], 0.0), writes=[dX])

            def xs_chunk(j, dst, ddst):
                r0 = PF_RW + j * 128
                c.dma("sp", X[:, 1:1 + TOK], self.PF[r0:r0 + 128, :], reads=[self.dPF], writes=[dX])
                c.op("dve", lambda e: e.tensor_tensor(out=dst[:], in0=X[:, 0:TOK], in1=X[:, 1:1 + TOK], op=ALU.subtract), reads=[dX], writes=[ddst])
                c.op("dve", lambda e: e.scalar_tensor_tensor(out=dst[:], in0=dst[:], scalar=mu[:, j:j + 1], in1=X[:, 1:1 + TOK], op0=ALU.mult, op1=ALU.add), reads=[dX, dmu, ddst], writes=[ddst])
                d0 = dst[:, SEQ:TOK].rearrange("p (b t) -> p b t", t=TS)[:, :, 0]
                p0 = X[:, 1 + SEQ:1 + TOK].rearrange("p (b t) -> p b t", t=TS)[:, :, 0]
                c.op("dve", lambda e: e.tensor_tensor(out=d0, in0=shT[:, j, :], in1=p0, op=ALU.subtract), reads=[dX, dshT, ddst], writes=[ddst])
                c.op("dve", lambda e: e.scalar_tensor_tensor(out=d0, in0=d0, scalar=mu[:, j:j + 1], in1=p0, op0=ALU.mult, op1=ALU.add), reads=[dX, dmu, ddst], writes=[ddst])

            A, dA = bufs["A"]; B, dB = bufs["B"]; C, dC = bufs["C"]; Dd, dDd = bufs["D"]; E, dE = bufs["E"]; Fb, dFb = bufs["F"]
            xs_chunk(12, A, dA)
            c.op("act", lambda e: e.activation(out=twd[0:64, :], in_=A[0:64, :], func=AF.Tanh), reads=[dA], writes=[dtwd])
            c.op("act", lambda e: e.copy(out=adb[64:128, :], in_=A[64:128, :]), reads=[dA], writes=[dadb])
            xs_chunk(13, A, dA)
            c.op("act", lambda e: e.activation(out=sgd[:], in_=A[:], func=AF.Sigmoid), reads=[dA], writes=[dsgd])
            for cc in range(4):
                xs_chunk(8 + cc, A, dA)
                for tt in range(NT):
                    pi = self.pb()
                    c.op("pe", lambda e, tt=tt, pi=pi: e.transpose(self.ps[pi][:, 0:128], A[:, tt * 128:(tt + 1) * 128], self.cst("ident")), reads=[dA, self.dcst], writes=[self.dps[pi]])
                    sf, dsf = tsf.next()
                    self.evac(pi, 128, 128, sf[:], dsf, tt)
                    c.dma("sp", self.VV[tt * 128:(tt + 1) * 128, cc * 128:(cc + 1) * 128], sf[:], reads=[dsf], writes=[dscr])
                for (t0, tw) in TBLK:
                    pi = self.pb()
                    c.op("pe", lambda e, pi=pi, t0=t0, tw=tw: e.matmul(self.ps[pi][:, 0:tw], lora[0:64, cc * 128:(cc + 1) * 128], twd[0:64, t0:t0 + tw], start=True, stop=True), reads=[dlora, dtwd], writes=[self.dps[pi]])
                    c.op("act", lambda e, pi=pi, t0=t0, tw=tw: e.activation(out=B[:, t0:t0 + tw], in_=self.ps[pi][:, 0:tw], func=AF.Sigmoid, bias=w0[:, cc:cc + 1], scale=1.0), reads=[self.dps[pi], dw0], writes=[dB])
                    pi = self.pb()
                    c.op("pe", lambda e, pi=pi, t0=t0, tw=tw: e.matmul(self.ps[pi][:, 0:tw], lora[64:128, cc * 128:(cc + 1) * 128], adb[64:128, t0:t0 + tw], start=True, stop=True), reads=[dlora, dadb], writes=[self.dps[pi]])
                    c.op("act", lambda e, pi=pi, t0=t0, tw=tw: e.activation(out=C[:, t0:t0 + tw], in_=self.ps[pi][:, 0:tw], func=AF.Sigmoid, bias=a0[:, cc:cc + 1], scale=1.0), reads=[self.dps[pi], da0], writes=[dC])
                c.op("dve", lambda e: e.tensor_tensor_scan(out=Dd[:], data0=reset[:], data1=B[:], initial=0.0, op0=ALU.mult, op1=ALU.add), reads=[dreset, dB], writes=[dDd])
                xs_chunk(4 + cc, A, dA)
                c.op("dve", lambda e: e.tensor_scalar(out=E[:], in0=A[:], scalar1=kk_[:, cc:cc + 1], scalar2=None, op0=ALU.mult), reads=[dA, dkk_], writes=[dE])
                c.op("pool", lambda e: e.tensor_tensor(out=Fb[:], in0=E[:], in1=E[:], op=ALU.mult), reads=[dE], writes=[dFb])
                for (t0, tw) in TBLK:
                    pi = self.pb()
                    c.op("pe", lambda e, pi=pi, t0=t0, tw=tw: e.matmul(self.ps[pi][:, 0:tw], self.cst("blockones"), Fb[:, t0:t0 + tw], start=True, stop=True), reads=[dFb, self.dcst], writes=[self.dps[pi]])
                    c.op("dve", lambda e, pi=pi, t0=t0, tw=tw: e.tensor_scalar(out=Fb[:, t0:t0 + tw], in0=self.ps[pi][:, 0:tw], scalar1=1e-24, scalar2=None, op0=ALU.max), reads=[self.dps[pi], dFb], writes=[dFb])
                c.op("act", lambda e: e.activation(out=Fb[:], in_=Fb[:], func=AF.Sqrt), reads=[dFb], writes=[dFb])
                c.op("dve", lambda e: e.reciprocal(out=Fb[:], in_=Fb[:]), reads=[dFb], writes=[dFb])
                c.op("dve", lambda e: e.tensor_tensor(out=E[:], in0=E[:], in1=Fb[:], op=ALU.mult), reads=[dE, dFb], writes=[dE])
                c.op("dve", lambda e: e.tensor_scalar(out=Fb[:], in0=C[:], scalar1=ka[:, cc:cc + 1], scalar2=omka[:, cc:cc + 1], op0=ALU.mult, op1=ALU.add), reads=[dC, dka, domka, dFb], writes=[dFb])
                c.op("pool", lambda e: e.tensor_tensor(out=Fb[:], in0=Fb[:], in1=A[:], op=ALU.mult), reads=[dFb, dA], writes=[dFb])
                c.op("dve", lambda e: e.tensor_tensor(out=C[:], in0=C[:], in1=E[:], op=ALU.mult), reads=[dC, dE], writes=[dC])
                c.op("dve", lambda e: e.tensor_tensor(out=A[:], in0=Dd[:], in1=B[:], op=ALU.subtract), reads=[dDd, dB, dA], writes=[dA])
                c.op("act", lambda e: e.activation(out=A[:], in_=A[:], func=AF.Exp, scale=-SDEC), reads=[dA], writes=[dA])
                st1, dst1 = stb.next()
                c.op("dve", lambda e, st1=st1: e.tensor_tensor(out=st1[:], in0=E[:], in1=A[:], op=ALU.mult), reads=[dE, dA], writes=[dst1])
                c.dma("sp", self.KAPT[cc][:, 0:TOK], st1[:], reads=[dst1], writes=[dscr])
                c.op("act", lambda e: e.activation(out=A[:], in_=Dd[:], func=AF.Exp, scale=SDEC), reads=[dDd, dA], writes=[dA])
                c.op("dve", lambda e: e.scalar_tensor_tensor(out=bT[:], in0=C[:], scalar=-1.0, in1=A[:], op0=ALU.mult, op1=ALU.mult), reads=[dC, dA], writes=[dbT])
                c.op("pool", lambda e: e.tensor_tensor(out=kT[:], in0=Fb[:], in1=A[:], op=ALU.mult), reads=[dFb, dA], writes=[dkT])
                for tt in range(NT):
                    for (srcT, dsrcT, dstS) in ((bT, dbT, self.BH), (kT, dkT, self.KH)):
                        pi = self.pb()
                        psb = self.ps[pi][:].bitcast(BF16)
                        c.op("pe", lambda e, tt=tt, psb=psb, srcT=srcT: e.transpose(psb[:, 0:128], srcT[:, tt * 128:(tt + 1) * 128], self.ident_bf[:]), reads=[dsrcT, self.dcst], writes=[self.dps[pi]])
                        sb_, dsb = tst.next()
                        c.op("act" if tt % 2 == 0 else "dve", (lambda e, sb_=sb_, psb=psb: e.copy(out=sb_[:], in_=psb[:, 0:128])) if tt % 2 == 0 else (lambda e, sb_=sb_, psb=psb: e.tensor_copy(out=sb_[:], in_=psb[:, 0:128])),
                             reads=[self.dps[pi]], writes=[dsb])
                        c.dma("sp", dstS[tt * 128:(tt + 1) * 128, cc * 128:(cc + 1) * 128], sb_[:], reads=[dsb], writes=[dscr])
                xs_chunk(cc, E, dE)
                c.op("dve", lambda e: e.scalar_tensor_tensor(out=C[:], in0=E[:], scalar=rk_[:, cc:cc + 1], in1=Fb[:], op0=ALU.mult, op1=ALU.mult), reads=[dE, drk_, dFb, dC], writes=[dC])
                for tt in range(NT):
                    pi = self.pb()
                    c.op("pe", lambda e, tt=tt, pi=pi: e.matmul(self.ps[pi][:, 0:2], C[:, tt * 128:(tt + 1) * 128], self.cst("halfsel"), start=True, stop=True), reads=[dC, self.dcst], writes=[self.dps[pi]])
                    c.op("dve", lambda e, tt=tt, pi=pi: e.tensor_copy(out=RK[:, tt, 2 * cc:2 * cc + 2], in_=self.ps[pi][:, 0:2]), reads=[self.dps[pi]], writes=[dRK])
                c.op("act", lambda e: e.activation(out=A[:], in_=Dd[:], func=AF.Exp, scale=-SDEC), reads=[dDd, dA], writes=[dA])
                c.op("dve", lambda e: e.tensor_copy(out=EB[:, cc, 0:SEQ // LB], in_=A[:, 0:SEQ].rearrange("p (n t) -> p n t", t=LB)[:, :, LB - 1]), reads=[dA], writes=[dEB])
                c.op("dve", lambda e: e.tensor_copy(out=EB[:, cc, SEQ // LB:NBLK], in_=A[:, SEQ:TOK].rearrange("p (n t) -> p n t", t=TS)[:, :, TS - 1]), reads=[dA], writes=[dEB])
                st2, dst2 = stb.next()
                c.op("dve", lambda e, st2=st2: e.tensor_tensor(out=st2[:], in0=E[:], in1=A[:], op=ALU.mult), reads=[dE, dA], writes=[dst2])
                c.op("pool", lambda e, st2=st2: e.tensor_copy(out=st2[:, 0:SEQ].rearrange("p (n t) -> p n t", t=LB)[:, :, LB - 1], in_=E[:, 0:SEQ].rearrange("p (n t) -> p n t", t=LB)[:, :, LB - 1]), reads=[dE, dst2], writes=[dst2])
                c.op("pool", lambda e, st2=st2: e.tensor_copy(out=st2[:, SEQ:TOK].rearrange("p (n t) -> p n t", t=TS)[:, :, TS - 1], in_=E[:, SEQ:TOK].rearrange("p (n t) -> p n t", t=TS)[:, :, TS - 1]), reads=[dE, dst2], writes=[dst2])
                c.dma("sp", self.RHT[cc], st2[:], reads=[dst2], writes=[dscr])
        self.rwkv_scan(l, EB, dEB)
        self.rwkv_post(l, RK, dRK, sgd, dsgd, gup, dgup, lng, dlng, lnb, dlnb)


Prog.mix_rwkv = _rwkv_phase


def _rwkv_scan(self, l, EB, dEB):
    c = self.c
    dscr = self.dRWS
    NR = 32
    with self.scope():
        self.ps_i = 0
        old_pb = self.pb

        def pb4():
            i = self.ps_i % 4
            self.ps_i += 1
            return i
        self.pb = pb4
        ACC = [4, 5]
        P1 = [6, 7]
        zt = self.sbt("rwz", [128, 8], BF16); dzt = Dep()
        c.op("dve", lambda e: e.memset(zt[:], 0.0), writes=[dzt])
        for cc in range(4):
            c.dma("sp", self.KAPT[cc][:, TOK:TOK + 8], zt[:], reads=[dzt], writes=[dscr])
        Sbf = self.sbt("rwSbf", [128, 256], BF16)
        dSbf = [Dep(), Dep()]
        L1p = self.rot("rwL1", 2, [128, 129, 48], BF16)
        L2p = self.rot("rwL2", 1, [72, 128, 128], BF16)
        Rp = [self.rot("rwRa", 2, [72, NR, 128], BF16), self.rot("rwRb", 2, [72, NR, 128], BF16)]
        for (t_, d_) in L1p.items + L2p.items + Rp[0].items + Rp[1].items:
            c.op("pool", lambda e, t_=t_: e.memset(t_[:], 0.0), writes=[d_])
        kapb = self.rot("rwkap", 2, [128, 4, 129], BF16)
        rhb = self.rot("rwrh", 2, [128, 4, 128], BF16)
        s0raw = self.rot("rws0raw", 2, [64, 512], F32)
        s0st = self.rot("rws0st", 2, [128, 256], F32)
        sfin = self.rot("rwsfin", 2, [128, 256], F32)
        sout = self.rot("rwsout", 2, [64, 512], F32)
        bm = self.cst("rw_blockmask")
        hm = self.cst("rw_halfmask")

        def build_tile(tt):
            t0 = tt * 128
            kap, dkap = kapb.next()
            rh, drh = rhb.next()
            for cc in range(4):
                c.dma("sp", kap[:, cc, :], self.KAPT[cc][:, t0:t0 + 129], reads=[dscr], writes=[dkap])
                c.dma("sp", rh[:, cc, :], self.RHT[cc][:, t0:t0 + 128], reads=[dscr], writes=[drh])
            L1, dL1 = L1p.next()
            hmb = hm.unsqueeze(1).unsqueeze(1).broadcast_to([128, 129, 4, 2])
            c.op("pool", lambda e: e.tensor_tensor(out=L1[:, :, 0:8].rearrange("p t (c h) -> p t c h", c=4), in0=kap[:].rearrange("p c t -> p t c").unsqueeze(3).broadcast_to([128, 129, 4, 2]),
                                                   in1=hmb, op=ALU.mult), reads=[dkap, self.dcst], writes=[dL1])
            hmb2 = hm.unsqueeze(1).unsqueeze(1).broadcast_to([128, 128, 4, 2])
            c.op("pool", lambda e: e.tensor_tensor(out=L1[:, 1:129, 32:40].rearrange("p t (c h) -> p t c h", c=4), in0=rh[:].rearrange("p c t -> p t c").unsqueeze(3).broadcast_to([128, 128, 4, 2]),
                                                   in1=hmb2, op=ALU.mult), reads=[drh, self.dcst], writes=[dL1])
            L2, dL2 = L2p.next()
            for h2 in range(2):
                srcb = self.BH[t0:t0 + 128, :].rearrange("t (c h k) -> c h t k", c=4, h=2)[:, h2]
                c.dma("sp", L2[h2:8:2, :, h2 * 64:(h2 + 1) * 64], srcb, reads=[dscr], writes=[dL2])
                srck = self.KH[t0:t0 + 128, :].rearrange("t (c h k) -> c h t k", c=4, h=2)[:, h2]
                c.dma("sp", L2[64 + h2:72:2, :, h2 * 64:(h2 + 1) * 64], srck, reads=[dscr], writes=[dL2])
            return L1, dL1, L2, dL2

        def fill_R(X, tok0, n):
            R, dR = Rp[X].next()
            for cl in range(2):
                cc = 2 * X + cl
                src = self.VV[tok0:tok0 + n, cc * 128:(cc + 1) * 128].rearrange("t (h v) -> h t v", h=2)
                c.dma("pool", R[64 + 2 * cc:64 + 2 * cc + 2, 0:n, cl * 64:(cl + 1) * 64], src, reads=[dscr], writes=[dR])
            return R, dR

        def extract_o(X, R, dR, tok_first, s_first, n):
            for cl in range(2):
                cc = 2 * X + cl
                dst = self.ORW[tok_first:tok_first + n, cc * 128:(cc + 1) * 128].rearrange("t (h v) -> h t v", h=2)
                c.dma("sp", dst, R[32 + 2 * cc:32 + 2 * cc + 2, s_first:s_first + n, cl * 64:(cl + 1) * 64], reads=[dR], writes=[self.dORW])

        def mask_op(X, R, dR, slot):
            c.op("dve", lambda e: e.tensor_tensor(out=R[0:40, slot, :], in0=self.ps[P1[X]][0:40, 0:128], in1=bm[0:40, X * 128:(X + 1) * 128], op=ALU.mult),
                 reads=[self.dps[P1[X]], self.dcst], writes=[dR])

        def stage1(X, L1, dL1, entry):
            c.op("pe", lambda e: e.matmul(self.ps[P1[X]][0:40, 0:128], L1[:, entry, 0:40], Sbf[:, X * 128:(X + 1) * 128], start=True, stop=True),
                 reads=[dL1, dSbf[X]], writes=[self.dps[P1[X]]])

        def run_chain(t_first, n, first_start, blk_of):
            Rcur = [None, None]

            def ring(X, idx):
                t = t_first + idx
                slot = idx % NR
                q = idx // NR
                if slot == 0:
                    if Rcur[X] is not None:
                        pb_ = t_first + (q - 1) * NR
                        s0_ = 1 if q - 1 == 0 else 0
                        extract_o(X, Rcur[X][0], Rcur[X][1], pb_ + s0_ - 1, s0_, NR - s0_)
                    if idx < n:
                        Rcur[X] = fill_R(X, t, min(NR, n - idx))
                    else:
                        Rcur[X] = Rp[X].next()
                R, dR = Rcur[X]
                mask_op(X, R, dR, slot)
                if idx == n:
                    base = t_first + q * NR
                    s0_ = 1 if q == 0 else 0
                    if slot + 1 - s0_ > 0:
                        extract_o(X, R, dR, base + s0_ - 1, s0_, slot + 1 - s0_)

            def s2(X, idx, L2, dL2):
                t = t_first + idx
                R, dR = Rcur[X]
                acc = self.ps[ACC[X]]
                c.op("pe", lambda e: e.matmul(acc[:, 0:128], L2[0:72, t % 128, :], R[0:72, idx % NR, :], start=(first_start and idx == 0), stop=True, skip_group_check=True),
                     reads=[dL2, dR], writes=[self.dps[ACC[X]]])

            def cp(X, idx):
                t = t_first + idx
                acc = self.ps[ACC[X]]
                bend = blk_of(t)
                if bend is not None:
                    eb = EB[:, 2 * X:2 * X + 2, bend:bend + 1].broadcast_to([128, 2, 64])
                    c.op("dve", lambda e: e.tensor_tensor(out=acc[:, 0:128].rearrange("p (c v) -> p c v", c=2), in0=acc[:, 0:128].rearrange("p (c v) -> p c v", c=2), in1=eb, op=ALU.mult),
                         reads=[self.dps[ACC[X]], dEB], writes=[self.dps[ACC[X]]])
                c.op("act", lambda e: e.copy(out=Sbf[:, X * 128:(X + 1) * 128], in_=acc[:, 0:128]), reads=[self.dps[ACC[X]]], writes=[dSbf[X]])

            Lprev = None
            for idx in range(n):
                t = t_first + idx
                tl = t % 128
                if tl == 0 and self._cur_tile != t // 128:
                    self._cur_L = build_tile(t // 128)
                    self._cur_tile = t // 128
                L1, dL1, L2, dL2 = self._cur_L
                ring(0, idx)
                s2(0, idx, L2, dL2)
                if idx > 0:
                    pL1, pdL1, ptl = Lprev
                    stage1(1, pL1, pdL1, ptl + 1)
                cp(0, idx)
                stage1(0, L1, dL1, tl + 1)
                ring(1, idx)
                s2(1, idx, L2, dL2)
                cp(1, idx)
                Lprev = (L1, dL1, tl)
            pL1, pdL1, ptl = Lprev
            stage1(1, pL1, pdL1, ptl + 1)
            ring(0, n)
            ring(1, n)

        self._cur_L = None
        self._cur_tile = -1
        c.op("dve", lambda e: e.memset(Sbf[:], 0.0), writes=dSbf)
        self._cur_L = build_tile(0)
        self._cur_tile = 0
        for X in range(2):
            stage1(X, self._cur_L[0], self._cur_L[1], 0)
        run_chain(0, SEQ, True, lambda t: (t // LB) if (t % LB == LB - 1) else None)
        self._rw_state_out(ACC, sfin, sout, self.O["p_wkv"][l])
        for b in range(NB_S):
            raw, draw = s0raw.next()
            c.dma("sp", raw[:].rearrange("v (h k) -> v h k", h=8), self.I["st_wkv"][l, b].rearrange("h v k -> v h k"), writes=[draw])
            pi = self.pb()
            for cc in range(4):
                c.op("pe", lambda e, cc=cc, pi=pi: e.transpose(self.ps[pi][:, cc * 64:(cc + 1) * 64], raw[:, cc * 128:(cc + 1) * 128], self.cst("ident")[0:64, 0:64]), reads=[draw, self.dcst], writes=[self.dps[pi]])
            s0, ds0 = s0st.next()
            c.op("dve", lambda e, s0=s0, pi=pi: e.tensor_copy(out=s0[:], in_=self.ps[pi][:, 0:256]), reads=[self.dps[pi]], writes=[ds0])
            for X in range(2):
                c.op("pe", lambda e, s0=s0, X=X: e.matmul(self.ps[ACC[X]][:, 0:128], self.cst("ident"), s0[:, X * 128:(X + 1) * 128], start=True, stop=True, skip_group_check=True),
                     reads=[ds0, self.dcst], writes=[self.dps[ACC[X]]])
                c.op("act", lambda e, s0=s0, X=X: e.copy(out=Sbf[:, X * 128:(X + 1) * 128], in_=s0[:, X * 128:(X + 1) * 128]), reads=[ds0], writes=[dSbf[X]])
            t0 = SEQ + b * TS
            if self._cur_tile != 16:
                self._cur_L = build_tile(16)
                self._cur_tile = 16
            for X in range(2):
                stage1(X, self._cur_L[0], self._cur_L[1], (t0 % 128))
            run_chain(t0, TS, False, lambda t: (SEQ // LB + (t - SEQ) // TS) if ((t - SEQ) % TS == TS - 1) else None)
            self._rw_state_out(ACC, sfin, sout, self.O["s_wkv"][l, b])
        self.pb = old_pb


def _rw_state_out(self, ACC, sfin, sout, out_ap):
    c = self.c
    sf, dsf = sfin.next()
    for X in range(2):
        c.op("dve", lambda e, X=X: e.tensor_copy(out=sf[:, X * 128:(X + 1) * 128], in_=self.ps[ACC[X]][:, 0:128]), reads=[self.dps[ACC[X]]], writes=[dsf])
    pi = self.pb()
    for cc in range(4):
        c.op("pe", lambda e, cc=cc: e.transpose(self.ps[pi][0:64, cc * 128:(cc + 1) * 128], sf[:, cc * 64:(cc + 1) * 64], self.cst("ident")), reads=[dsf, self.dcst], writes=[self.dps[pi]])
    so, dso = sout.next()
    c.op("act", lambda e: e.copy(out=so[:], in_=self.ps[pi][0:64, 0:512]), reads=[self.dps[pi]], writes=[dso])
    do = self.odep()
    c.dma("sp", out_ap.rearrange("h v k -> v h k"), so[:].rearrange("v (h k) -> v h k", h=8), reads=[dso], writes=[do])


def _rwkv_post(self, l, RK, dRK, sgd, dsgd, gup, dgup, lng, dlng, lnb, dlnb):
    c = self.c
    with self.scope():
        ob = self.rot("rwo", 2, [128, 512], BF16)
        vb = self.rot("rwv", 2, [128, 512], F32)
        on = self.sbt("rwon", [128, 512], F32); don = Dep()
        tmp = self.sbt("rwtmp", [128, 512], F32); dtmp = Dep()
        sm = self.sbt("rwsm", [128, 48], F32); dsm = Dep()
        for tt in range(NT):
            t0 = tt * 128
            o, do_ = ob.next()
            c.dma("sp", o[:], self.ORW[t0:t0 + 128, :], reads=[self.dORW], writes=[do_])
            v, dv = vb.next()
            c.dma("sp", v[:], self.VV[t0:t0 + 128, :], reads=[self.dRWS], writes=[dv])
            o3 = o[:].rearrange("p (h v) -> p h v", h=8)
            c.op("dve", lambda e: e.tensor_reduce(out=sm[:, 0:8], in_=o3, axis=AX.X, op=ALU.add), reads=[do_], writes=[dsm])
            c.op("dve", lambda e: e.tensor_tensor(out=tmp[:], in0=o[:], in1=o[:], op=ALU.mult), reads=[do_], writes=[dtmp])
            c.op("dve", lambda e: e.tensor_reduce(out=sm[:, 8:16], in_=tmp[:].rearrange("p (h v) -> p h v", h=8), axis=AX.X, op=ALU.add), reads=[dtmp], writes=[dsm])
            c.op("dve", lambda e: e.tensor_scalar(out=sm[:, 0:16], in0=sm[:, 0:16], scalar1=1.0 / 64, scalar2=None, op0=ALU.mult), reads=[dsm], writes=[dsm])
            c.op("dve", lambda e: e.tensor_tensor(out=sm[:, 16:24], in0=sm[:, 0:8], in1=sm[:, 0:8], op=ALU.mult), reads=[dsm], writes=[dsm])
            c.op("dve", lambda e: e.tensor_tensor(out=sm[:, 24:32], in0=sm[:, 8:16], in1=sm[:, 16:24], op=ALU.subtract), reads=[dsm], writes=[dsm])
            c.op("act", lambda e: e.activation(out=sm[:, 32:40], in_=sm[:, 24:32], func=AF.Sqrt, bias=self.epsb(float(RW_LN_EPS)), scale=1.0), reads=[dsm, self.dcst], writes=[dsm])
            c.op("dve", lambda e: e.reciprocal(out=sm[:, 40:48], in_=sm[:, 32:40]), reads=[dsm], writes=[dsm])
            on3 = on[:].rearrange("p (h v) -> p h v", h=8)
            c.op("dve", lambda e: e.tensor_tensor(out=on3, in0=o3, in1=sm[:, 0:8].unsqueeze(2).broadcast_to([128, 8, 64]), op=ALU.subtract), reads=[do_, dsm, self.dOS[tt]], writes=[don])
            c.op("dve", lambda e: e.tensor_tensor(out=on3, in0=on3, in1=sm[:, 40:48].unsqueeze(2).broadcast_to([128, 8, 64]), op=ALU.mult), reads=[don, dsm], writes=[don])
            c.op("pool", lambda e: e.tensor_tensor(out=on[:], in0=on[:], in1=lng[:], op=ALU.mult), reads=[don, dlng], writes=[don])
            c.op("pool", lambda e: e.tensor_tensor(out=on[:], in0=on[:], in1=lnb[:], op=ALU.add), reads=[don, dlnb], writes=[don])
            c.op("dve", lambda e: e.tensor_tensor(out=tmp[:].rearrange("p (h v) -> p h v", h=8), in0=v[:].rearrange("p (h v) -> p h v", h=8), in1=RK[:, tt, :].unsqueeze(2).broadcast_to([128, 8, 64]), op=ALU.mult),
                 reads=[dv, dRK, dtmp], writes=[dtmp])
            c.op("dve", lambda e: e.tensor_tensor(out=on[:], in0=on[:], in1=tmp[:], op=ALU.add), reads=[don, dtmp], writes=[don])
            pi = self.pb()
            c.op("pe", lambda e, pi=pi: e.matmul(self.ps[pi][:, 0:512], sgd[:, t0:t0 + 128], gup[:], start=True, stop=True), reads=[dsgd, dgup], writes=[self.dps[pi]])
            c.op("dve", lambda e, pi=pi: e.tensor_tensor(out=on[:], in0=on[:], in1=self.ps[pi][:, 0:512], op=ALU.mult), reads=[don, self.dps[pi]], writes=[don])
            c.dma("sp", self.OS[t0:t0 + 128, 0:512], on[:], reads=[don], writes=[self.dOS[tt]])


Prog.rwkv_scan = _rwkv_scan
Prog._rw_state_out = _rw_state_out
Prog.rwkv_post = _rwkv_post


def _ln_phase(self, l, gname, bname, final):
    c = self.c
    with self.scope():
        gt, dgt = self.bcast_row("lng", self.I[gname][l:l + 1, :], D)
        bt, dbt = self.bcast_row("lnb", self.I[bname][l:l + 1, :], D)
        xa_p = self.rot("lnxa", 2, [128, D], F32)
        xb_p = self.rot("lnxb", 2, [128, D], F32)
        xh_p = self.rot("lnxh", 2, [128, D], BF16)
        st = self.sbt("lnst", [128, 32], F32); dst_ = Dep()
        def issue_loads(tt):
            rows = slice(tt * 128, (tt + 1) * 128)
            xa, dxa = xa_p.next()
            xb, dxb = xb_p.next()
            if self.res_from_input:
                src = self.I["xp"][rows, :] if tt < 16 else self.I["xs"]
                c.dma("sp", xa[:], src, writes=[dxa])
            else:
                c.dma("sp", xa[:], self.XRES[rows, :], reads=[self.dXRES[tt]], writes=[dxa])
            c.dma("sp", xb[:], self.MIX[rows, :], reads=[self.dMIX[tt]], writes=[dxb])
            return xa, dxa, xb, dxb
        nxt_ld = issue_loads(0)
        for tt in range(NT):
            rows = slice(tt * 128, (tt + 1) * 128)
            xa, dxa, xb, dxb = nxt_ld
            if tt + 1 < NT:
                nxt_ld = issue_loads(tt + 1)
            c.op("dve", lambda e: e.scalar_tensor_tensor(out=xa[:], in0=xa[:], scalar=float(DN_ALPHA), in1=xb[:], op0=ALU.mult, op1=ALU.add), reads=[dxa, dxb], writes=[dxa])
            for i in range(4):
                c.op("dve", lambda e, i=i: e.bn_stats(out=st[:, i * 6:(i + 1) * 6], in_=xa[:, i * 512:(i + 1) * 512]), reads=[dxa], writes=[dst_])
            c.op("dve", lambda e: e.bn_aggr(out=st[:, 24:26], in_=st[:, 0:24]), reads=[dst_], writes=[dst_])
            c.op("act", lambda e: e.activation(out=st[:, 26:27], in_=st[:, 25:26], func=AF.Sqrt, bias=self.epsb(1e-5), scale=1.0), reads=[dst_, self.dcst], writes=[dst_])
            c.op("dve", lambda e: e.reciprocal(out=st[:, 27:28], in_=st[:, 26:27]), reads=[dst_], writes=[dst_])
            c.op("dve", lambda e: e.tensor_scalar(out=st[:, 28:29], in0=st[:, 24:25], scalar1=st[:, 27:28], scalar2=-1.0, op0=ALU.mult, op1=ALU.mult), reads=[dst_], writes=[dst_])
            c.op("act", lambda e: e.activation(out=xb[:], in_=xa[:], func=AF.Identity, scale=st[:, 27:28], bias=st[:, 28:29]), reads=[dxa, dst_, dxb], writes=[dxb])
            c.op("pool", lambda e: e.tensor_tensor(out=xb[:], in0=xb[:], in1=gt[:], op=ALU.mult), reads=[dxb, dgt], writes=[dxb])
            c.op("pool", lambda e: e.tensor_tensor(out=xb[:], in0=xb[:], in1=bt[:], op=ALU.add), reads=[dxb, dbt], writes=[dxb])
            if final:
                do = self.odep()
                dst = self.O["yp"][rows, :] if tt < 16 else self.O["ys"]
                c.dma("sp", dst, xb[:], reads=[dxb], writes=[do])
            else:
                c.dma("sp", self.XRES[rows, :], xb[:], reads=[dxb], writes=[self.dXRES[tt]])
                xh, dxh = xh_p.next()
                c.op("act", lambda e: e.copy(out=xh[:], in_=xb[:]), reads=[dxb], writes=[dxh])
                self.to_actT(xh, dxh, tt)
    self.res_from_input = False


def _proj_to_mix(self, w_ap):
    c = self.c
    with self.scope():
        wpool = self.rot("wpm", 3, [128, KC, 512], BF16)
        stg = self.rot("stgm", 4, [128, 512], F32)
        self.tog = 0

        def consume(bi, n0, ncols, tt, pi):
            buf, d = stg.next()
            self.evac(pi, 128, ncols, buf[:, 0:ncols], d, self.tog)
            self.tog += 1
            c.dma("sp", self.MIX[tt * 128:(tt + 1) * 128, n0:n0 + ncols], buf[:, 0:ncols], reads=[d], writes=[self.dMIX[tt]])
        blocks = [(i * 512, 512) for i in range(4)]
        self.proj_T(w_ap, blocks, KC, lambda bi: list(range(NT)), self.act_lhs, self.act_deps, consume, wpool)


def _out_ln1(self, l):
    c = self.c
    with self.scope():
        xb = self.rot("osb", 3, [128, D], BF16)
        for tt in range(NT):
            buf, d = xb.next()
            c.dma("pool", buf[:], self.OS[tt * 128:(tt + 1) * 128, :], reads=[self.dOS[tt]], writes=[d])
            self.to_actT(buf, d, tt)
    self.proj_to_mix(self.I["w_out"][l])
    self.ln_phase(l, "ln1_g", "ln1_b", False)


def _attn(self, l):
    c = self.c
    scale = 512 ** -0.5
    with self.scope():
        KT = self.sbt("xaKT", [128, KC, N_MEM], BF16); dKT = Dep()
        Vb = self.sbt("xaVb", [128, 2, D], BF16); dVb = Dep()
        with self.scope():
            mem = self.sbt("xamem", [128, 2, D], BF16); dmem = Dep()
            c.dma("pool", mem[:], self.I["memp"].rearrange("(c p) d -> p c d", p=128), writes=[dmem])
            memT = self.sbt("xamemT", [128, KC, N_MEM], BF16); dmemT = Dep()
            for g in range(4):
                pi = self.pb()
                psb = self.ps[pi][:].bitcast(BF16)
                for j in range(4):
                    kc = g * 4 + j
                    for mc in range(2):
                        c.op("pe", lambda e, j=j, mc=mc, kc=kc, psb=psb: e.transpose(psb[:, (j * 2 + mc) * 128:(j * 2 + mc + 1) * 128], mem[:, mc, kc * 128:(kc + 1) * 128], self.ident_bf[:]),
                             reads=[dmem, self.dcst], writes=[self.dps[pi]])
                c.op("dve" if g % 2 else "act", (lambda e, g=g, psb=psb: e.tensor_copy(out=memT[:, g * 4:(g + 1) * 4, :].rearrange("p k m -> p (k m)"), in_=psb[:, 0:1024])) if g % 2 else
                     (lambda e, g=g, psb=psb: e.copy(out=memT[:, g * 4:(g + 1) * 4, :].rearrange("p k m -> p (k m)"), in_=psb[:, 0:1024])), reads=[self.dps[pi]], writes=[dmemT])
            wpool = self.rot("xawkv", 3, [128, KC, 512], BF16)
            stg = self.rot("xastg", 4, [128, 512], F32)
            for (wname, oname) in (("xa_wk", "p_mk"), ("xa_wv", "p_mv")):
                w = self.I[wname][l]
                for nb in range(4):
                    wb, wd = self.load_w(wpool, w, nb * 512, 512, KC)
                    for mt in range(2):
                        pi = self.pb()
                        for kc in range(KC):
                            c.op("pe", lambda e, kc=kc, pi=pi, mt=mt, wb=wb: e.matmul(self.ps[pi][:, 0:512], memT[:, kc, mt * 128:(mt + 1) * 128], wb[:, kc, :], start=(kc == 0), stop=(kc == KC - 1)),
                                 reads=[dmemT, wd], writes=[self.dps[pi]])
                        buf, d = stg.next()
                        c.op("act", lambda e, buf=buf, pi=pi: e.copy(out=buf[:], in_=self.ps[pi][:, 0:512]), reads=[self.dps[pi]], writes=[d])
                        do = self.odep()
                        c.dma("sp", self.O[oname][l, mt * 128:(mt + 1) * 128, nb * 512:(nb + 1) * 512], buf[:], reads=[d], writes=[do])
                        if wname == "xa_wv":
                            c.op("dve", lambda e, buf=buf, mt=mt, nb=nb: e.tensor_copy(out=Vb[:, mt, nb * 512:(nb + 1) * 512], in_=buf[:]), reads=[d], writes=[dVb])
                    if wname == "xa_wk":
                        for j in range(4):
                            pi = self.pb()
                            for kc in range(KC):
                                c.op("pe", lambda e, kc=kc, pi=pi, j=j, wb=wb: e.matmul(self.ps[pi][:, 0:N_MEM], wb[:, kc, j * 128:(j + 1) * 128], memT[:, kc, :], start=(kc == 0), stop=(kc == KC - 1)),
                                     reads=[dmemT, wd], writes=[self.dps[pi]])
                            c.op("dve", lambda e, pi=pi, j=j, nb=nb: e.tensor_copy(out=KT[:, nb * 4 + j, :], in_=self.ps[pi][:, 0:N_MEM]), reads=[self.dps[pi]], writes=[dKT])
        if KATT < 2:
            return
        qT = self.sbt("xaqT", [128, KC, TOK], BF16)
        dqT = [[Dep() for _ in range(4)] for _ in range(NT)]
        with self.scope():
            wpool = self.rot("xawq", 2, [128, KC, 512], BF16)
            self.tog = 0

            def consume_q(col0, cw, t0, tw, pi):
                ch = col0 // 128
                deps = [dqT[tt][ch // 4] for tt in range(t0 // 128, (t0 + tw) // 128)]
                dst = qT[:, ch, t0:t0 + tw]
                src = self.ps[pi][:, 0:tw]
                if self.tog % 2 == 0:
                    c.op("act", lambda e: e.copy(out=dst, in_=src), reads=[self.dps[pi]], writes=deps)
                else:
                    c.op("dve", lambda e: e.tensor_copy(out=dst, in_=src), reads=[self.dps[pi]], writes=deps)
                self.tog += 1
            self.proj_F(self.I["xa_wq"][l], [(i * 512, 512) for i in range(4)], KC, consume_q, wpool)
        if KATT < 3:
            return
        with self.scope():
            ef = self.rot("xae", 2, [128, N_MEM], F32)
            scp = self.rot("xasc", 2, [128, N_MEM], F32)
            prp = self.rot("xapr", 2, [128, N_MEM], BF16)
            prTp = self.rot("xaprT", 2, [128, 2, 128], BF16)
            sm = self.sbt("xasm", [128, 16], F32); dsm = Dep()

            def softmax(pi, smc):
                sc_, dsc_ = scp.next()
                c.op("act", lambda e: e.copy(out=sc_[:], in_=self.ps[pi][:, 0:N_MEM]), reads=[self.dps[pi]], writes=[dsc_])
                c.op("dve", lambda e: e.tensor_reduce(out=sm[:, smc:smc + 1], in_=sc_[:], axis=AX.X, op=ALU.max), reads=[dsc_], writes=[dsm])
                c.op("dve", lambda e: e.tensor_scalar(out=sm[:, smc + 1:smc + 2], in0=sm[:, smc:smc + 1], scalar1=-scale, scalar2=None, op0=ALU.mult), reads=[dsm], writes=[dsm])
                e_, de_ = ef.next()
                c.op("act", lambda e: e.activation(out=e_[:], in_=sc_[:], func=AF.Exp, scale=scale, bias=sm[:, smc + 1:smc + 2]),
                     reads=[dsc_, dsm], writes=[de_])
                c.op("dve", lambda e: e.tensor_reduce(out=sm[:, smc + 2:smc + 3], in_=e_[:], axis=AX.X, op=ALU.add), reads=[de_], writes=[dsm])
                c.op("dve", lambda e: e.reciprocal(out=sm[:, smc + 3:smc + 4], in_=sm[:, smc + 2:smc + 3]), reads=[dsm], writes=[dsm])
                pr, dpr = prp.next()
                c.op("dve", lambda e: e.tensor_scalar(out=pr[:], in0=e_[:], scalar1=sm[:, smc + 3:smc + 4], scalar2=None, op0=ALU.mult), reads=[de_, dsm], writes=[dpr])
                return pr, dpr

            def transpose_pr(pr, dpr, prT, dprT):
                pi = self.pb()
                psb = self.ps[pi][:].bitcast(BF16)
                for mc in range(2):
                    c.op("pe", lambda e, mc=mc: e.transpose(psb[:, mc * 128:(mc + 1) * 128], pr[:, mc * 128:(mc + 1) * 128], self.ident_bf[:]), reads=[dpr, self.dcst], writes=[self.dps[pi]])
                c.op("act", lambda e: e.copy(out=prT[:].rearrange("p c t -> p (c t)"), in_=psb[:, 0:256]), reads=[self.dps[pi]], writes=[dprT])

            for tt in range(16):
                t0 = tt * 128
                for h in range(4):
                    pi = self.pb()
                    for dc in range(4):
                        c.op("pe", lambda e, dc=dc, pi=pi: e.matmul(self.ps[pi][:, 0:N_MEM], qT[:, 4 * h + dc, t0:t0 + 128], KT[:, 4 * h + dc, :], start=(dc == 0), stop=(dc == 3)),
                             reads=[dqT[tt][h], dKT], writes=[self.dps[pi]])
                    pr, dpr = softmax(pi, (h % 2) * 4)
                    prT, dprT = prTp.next()
                    transpose_pr(pr, dpr, prT, dprT)
                    p2 = self.pb()
                    for dc in range(4):
                        for mc in range(2):
                            c.op("pe", lambda e, dc=dc, mc=mc, p2=p2: e.matmul(self.ps[p2][:, dc * 128:(dc + 1) * 128], Vb[:, mc, (4 * h + dc) * 128:(4 * h + dc + 1) * 128], prT[:, mc, :], start=(mc == 0), stop=(mc == 1)),
                                 reads=[dVb, dprT], writes=[self.dps[p2]])
                    c.op("dve", lambda e, p2=p2: e.tensor_copy(out=self.actT[:, 4 * h:4 * h + 4, t0:t0 + 128], in_=self.ps[p2][:, 0:512].rearrange("p (k t) -> p k t", k=4)),
                         reads=[self.dps[p2]], writes=[self.dact[tt][h]])
            if KATT < 4:
                return
            t0 = 16 * 128
            old_pb = self.pb
            self.ps_i = 0

            def pb4():
                i = self.ps_i % 4
                self.ps_i += 1
                return i
            self.pb = pb4
            SB = [4, 5, 6, 7]
            kvb = self.rot("xakv", 1, [128, 2, D], BF16)
            KTb = self.rot("xaKTb", 1, [128, KC, N_MEM], BF16)
            qmb = self.rot("xaqm", 1, [128, KC, 128], BF16)
            prS = self.sbt("xaprS", [128, 4, N_MEM], BF16); dprS = Dep()
            prTS = self.sbt("xaprTS", [128, 4, 2, 128], BF16); dprTS = Dep()
            for b in range(NB_S):
                kb, dkb = kvb.next()
                c.dma("pool", kb[:], self.I["ck"][l, b].rearrange("(c p) d -> p c d", p=128), writes=[dkb])
                ktb, dktb = KTb.next()
                for g in range(4):
                    pi = self.pb()
                    psb = self.ps[pi][:].bitcast(BF16)
                    for j in range(4):
                        kc = g * 4 + j
                        for mc in range(2):
                            c.op("pe", lambda e, j=j, mc=mc, kc=kc, psb=psb: e.transpose(psb[:, (j * 2 + mc) * 128:(j * 2 + mc + 1) * 128], kb[:, mc, kc * 128:(kc + 1) * 128], self.ident_bf[:]),
                                 reads=[dkb, self.dcst], writes=[self.dps[pi]])
                    c.op("dve" if g % 2 else "act", (lambda e, g=g, psb=psb: e.tensor_copy(out=ktb[:, g * 4:(g + 1) * 4, :].rearrange("p k m -> p (k m)"), in_=psb[:, 0:1024])) if g % 2 else
                         (lambda e, g=g, psb=psb: e.copy(out=ktb[:, g * 4:(g + 1) * 4, :].rearrange("p k m -> p (k m)"), in_=psb[:, 0:1024])), reads=[self.dps[pi]], writes=[dktb])
                qm, dqm = qmb.next()
                mrb = self.maskrow[:, b, :].unsqueeze(1).broadcast_to([128, KC, 128])
                c.op("pool", lambda e: e.tensor_tensor(out=qm[:], in0=qT[:, :, t0:t0 + 128], in1=mrb, op=ALU.mult), reads=dqT[16] + [self.dcst], writes=[dqm])
                for h in range(4):
                    for dc in range(4):
                        c.op("pe", lambda e, h=h, dc=dc: e.matmul(self.ps[SB[h]][:, 0:N_MEM], qm[:, 4 * h + dc, :], ktb[:, 4 * h + dc, :], start=(b == 0 and dc == 0), stop=(b == NB_S - 1 and dc == 3), skip_group_check=True),
                             reads=[dqm, dktb], writes=[self.dps[SB[h]]])
            for h in range(4):
                pr, dpr = softmax(SB[h], (h % 2) * 4)
                c.op("pool", lambda e, h=h, pr=pr: e.tensor_copy(out=prS[:, h, :], in_=pr[:]), reads=[dpr], writes=[dprS])
            for h in range(4):
                pi = self.pb()
                psb = self.ps[pi][:].bitcast(BF16)
                for mc in range(2):
                    c.op("pe", lambda e, mc=mc, h=h: e.transpose(psb[:, mc * 128:(mc + 1) * 128], prS[:, h, mc * 128:(mc + 1) * 128], self.ident_bf[:]), reads=[dprS, self.dcst], writes=[self.dps[pi]])
                c.op("act", lambda e, h=h: e.copy(out=prTS[:, h, :, :].rearrange("p c t -> p (c t)"), in_=psb[:, 0:256]), reads=[self.dps[pi]], writes=[dprTS])
            zrow = self.sbt("xazrow", [1, 512], BF16); dzrow = Dep()
            c.op("dve", lambda e: e.memset(zrow[:], 0.0), writes=[dzrow])
            for h in range(4):
                c.op("pe", lambda e, h=h: e.matmul(self.ps[SB[h]][:, 0:512], zrow[0:1, 0:128], zrow[0:1, 0:512], start=True, stop=False, skip_group_check=True), reads=[dzrow], writes=[self.dps[SB[h]]])
            prm = self.rot("xaprm", 2, [128, 4, 2, 128], BF16)
            for b in range(NB_S):
                vb_, dvb_ = kvb.next()
                c.dma("pool", vb_[:], self.I["cv"][l, b].rearrange("(c p) d -> p c d", p=128), writes=[dvb_])
                pm, dpm = prm.next()
                mrb = self.maskrow[:, b, :].unsqueeze(1).broadcast_to([128, 8, 128])
                c.op("pool", lambda e: e.tensor_tensor(out=pm[:].rearrange("p h c t -> p (h c) t"), in0=prTS[:].rearrange("p h c t -> p (h c) t"), in1=mrb, op=ALU.mult), reads=[dprTS, self.dcst], writes=[dpm])
                for ch in range(KC):
                    h = ch // 4
                    for mc in range(2):
                        c.op("pe", lambda e, ch=ch, mc=mc, h=h: e.matmul(self.ps[SB[h]][:, (ch % 4) * 128:(ch % 4 + 1) * 128], vb_[:, mc, ch * 128:(ch + 1) * 128], pm[:, h, mc, :],
                                                                   start=False, stop=(b == NB_S - 1 and mc == 1), skip_group_check=True), reads=[dvb_, dpm], writes=[self.dps[SB[h]]])
            for h in range(4):
                c.op("dve" if h % 2 else "act", (lambda e, h=h: e.tensor_copy(out=self.actT[:, 4 * h:4 * h + 4, t0:t0 + 128], in_=self.ps[SB[h]][:, 0:512].rearrange("p (k t) -> p k t", k=4))) if h % 2 else
                     (lambda e, h=h: e.copy(out=self.actT[:, 4 * h:4 * h + 4, t0:t0 + 128], in_=self.ps[SB[h]][:, 0:512].rearrange("p (k t) -> p k t", k=4))),
                     reads=[self.dps[SB[h]]], writes=[self.dact[16][h]])
            self.pb = old_pb
    if KATT < 5:
        return
    self.proj_to_mix(self.I["xa_wo"][l])
    if KATT < 6:
        return
    self.ln_phase(l, "ln2_g", "ln2_b", False)


def _ffn(self, l):
    c = self.c
    with self.scope():
        wpool = self.rot("ffw", 4, [128, KC, 512], BF16)
        sgp = self.rot("ffsg", 2, [128, 512], F32)
        hbp = self.rot("ffhb", 3, [128, 512], BF16)
        wg = self.I["ffn_w_gate"][l]
        wu = self.I["ffn_w_up"][l]
        nxt = (self.load_w(wpool, wg, 0, 512, KC), self.load_w(wpool, wu, 0, 512, KC))
        for nb in range(D_FF // 512):
            (gb, gd), (ub, ud) = nxt
            if nb + 1 < D_FF // 512:
                nxt = (self.load_w(wpool, wg, (nb + 1) * 512, 512, KC), self.load_w(wpool, wu, (nb + 1) * 512, 512, KC))
            for j in range(4):
                for (t0, tw) in TBLK:
                    rd = [d for tt in range(t0 // 128, (t0 + tw) // 128) for d in self.dact[tt]]
                    pg = self.pb()
                    for kc in range(KC):
                        c.op("pe", lambda e, kc=kc, pg=pg: e.matmul(self.ps[pg][:, 0:tw], gb[:, kc, j * 128:(j + 1) * 128], self.actT[:, kc, t0:t0 + tw], start=(kc == 0), stop=(kc == KC - 1)),
                             reads=[gd] + rd, writes=[self.dps[pg]])
                    pu = self.pb()
                    for kc in range(KC):
                        c.op("pe", lambda e, kc=kc, pu=pu: e.matmul(self.ps[pu][:, 0:tw], ub[:, kc, j * 128:(j + 1) * 128], self.actT[:, kc, t0:t0 + tw], start=(kc == 0), stop=(kc == KC - 1)),
                             reads=[ud] + rd, writes=[self.dps[pu]])
                    sg, dsg = sgp.next()
                    c.op("act", lambda e: e.activation(out=sg[:, 0:tw], in_=self.ps[pg][:, 0:tw], func=AF.Silu), reads=[self.dps[pg]], writes=[dsg])
                    hb, dhb = hbp.next()
                    c.op("dve", lambda e: e.tensor_tensor(out=hb[:, 0:tw], in0=sg[:, 0:tw], in1=self.ps[pu][:, 0:tw], op=ALU.mult), reads=[dsg, self.dps[pu]], writes=[dhb])
                    c.dma("sp", self.HT[nb * 4 + j][:, t0:t0 + tw], hb[:, 0:tw], reads=[dhb], writes=[self.dHT])
    with self.scope():
        wpool = self.rot("ffwd", 2, [128, FC, 256], BF16)
        hTp = self.rot("ffhT", 2, [128, FC, 256], BF16)
        stg = self.rot("ffstg", 4, [128, 256], F32)
        wd_ = self.I["ffn_w_down"][l]
        groups = [(i * 256, 256) for i in range(8)] + [(2048, 128)]
        self.tog = 0
        nxt = self.load_w(wpool, wd_, 0, 256, FC)
        for nb in range(D // 256):
            wb, wdp = nxt
            if nb + 1 < D // 256:
                nxt = self.load_w(wpool, wd_, (nb + 1) * 256, 256, FC)
            for (g0, gw) in groups:
                hT, dhT = hTp.next()
                c.dma("sp", hT[:, :, 0:gw], self.HT[:, :, g0:g0 + gw].rearrange("c p t -> p c t"), reads=[self.dHT], writes=[dhT])
                for tl in range(gw // 128):
                    tt = g0 // 128 + tl
                    pi = self.pb()
                    for kc in range(FC):
                        c.op("pe", lambda e, kc=kc, pi=pi, tl=tl: e.matmul(self.ps[pi][:, 0:256], hT[:, kc, tl * 128:(tl + 1) * 128], wb[:, kc, :], start=(kc == 0), stop=(kc == FC - 1)),
                             reads=[dhT, wdp], writes=[self.dps[pi]])
                    buf, d = stg.next()
                    self.evac(pi, 128, 256, buf[:], d, self.tog)
                    self.tog += 1
                    c.dma("sp", self.MIX[tt * 128:(tt + 1) * 128, nb * 256:(nb + 1) * 256], buf[:], reads=[d], writes=[self.dMIX[tt]])
    self.ln_phase(l, "ln3_g", "ln3_b", l == DEPTH - 1)


Prog.ln_phase = _ln_phase
Prog.proj_to_mix = _proj_to_mix
Prog.out_ln1 = _out_ln1
Prog.attn = _attn
Prog.ffn = _ffn
```
